# Optimizing a Trainium2 kernel written in Bass

```python
import math
import jax, jax.numpy as jnp
from jax import lax
import numpy as np

D_MODEL = 1024
BATCH = 8
SEQ = 4096
DEPTH = 1
DEC_BATCH = 128
DEC_SEQ = 8
PAST_LEN = 8192
PAGE_SIZE = 128

H_RET = 4
DK_RET = D_MODEL // 8
DV_RET = D_MODEL // 8
RET_CHUNK = 128
ROPE_BASE = 10000.0
RET_QK_W = H_RET * DK_RET
RET_V_W = H_RET * DV_RET
ATT_GROUPS = ((128, 1), (512, 4), (2048, 16))
N_GROUPS = 3
H_ATT = 8
DH_ATT = D_MODEL // 16
ATT_SPAN = 128
ATT_QKV_W = N_GROUPS * H_ATT * DH_ATT
ATT_OUT_W = H_ATT * DH_ATT
NEG = -1e30
N_BUCKETS = 32
MAX_DISTANCE = 2048
N_KEYS = 128
N_EXPERTS = N_KEYS * N_KEYS
H_PEER = 8
DK_PEER = 256
TOPK_PEER = 16
PEER_BLOCK = 256
ALPHA = (2 * DEPTH) ** 0.25
BETA = (8 * DEPTH) ** -0.25
LN_EPS = 1e-5
HN_EPS = 1e-6
IN_SPLITS = (RET_QK_W, RET_QK_W, RET_V_W, RET_V_W, ATT_QKV_W, ATT_QKV_W, ATT_QKV_W, D_MODEL, D_MODEL)
IN_COLS = RET_QK_W * 2 + RET_V_W * 2 + ATT_QKV_W * 3 + D_MODEL * 2

kernel_name = "hybrid_retention_dilated_attn_peer_decode_step"


def _layer_norm(x, g, b):
    xf = x.astype(jnp.float32)
    mu = xf.mean(-1, keepdims=True)
    var = jnp.mean(jnp.square(xf - mu), -1, keepdims=True)
    return ((xf - mu) * lax.rsqrt(var + LN_EPS) * g + b).astype(x.dtype)


def _head_norm(o):
    mu = o.mean(-1, keepdims=True)
    var = jnp.mean(jnp.square(o - mu), -1, keepdims=True)
    return (o - mu) * lax.rsqrt(var + HN_EPS)


def _ada(c, w_ada, b_ada):
    mod = jax.nn.silu(c) @ w_ada + b_ada
    return jnp.split(mod[:, None, :], 6, axis=-1)


def _project(h, w_in):
    p = h @ w_in
    offs = np.cumsum(IN_SPLITS)[:-1].tolist()
    return jnp.split(p, offs, axis=-1)


def _rotary(x, pos):
    half = x.shape[-1] // 2
    inv = ROPE_BASE ** (-jnp.arange(half, dtype=jnp.float32) / half)
    ang = pos.astype(jnp.float32)[:, None] * inv[None, :]
    cos = jnp.cos(ang)[None, :, None, :]
    sin = jnp.sin(ang)[None, :, None, :]
    xf = x.astype(jnp.float32)
    x1, x2 = xf[..., :half], xf[..., half:]
    return jnp.concatenate([x1 * cos - x2 * sin, x1 * sin + x2 * cos], axis=-1)


def _retention_chunkwise(q, k, v, s0):
    n, L, H, dk = q.shape
    dv = v.shape[-1]
    C = math.gcd(L, RET_CHUNK)
    nc = L // C
    lg = jnp.log1p(-jnp.exp2(-5.0 - jnp.arange(H, dtype=jnp.float32)))
    idx = jnp.arange(C, dtype=jnp.float32)
    rel = idx[:, None] - idx[None, :]
    intra = jnp.where(rel >= 0, jnp.exp(lg[:, None, None] * jnp.maximum(rel, 0.0)), 0.0)
    q_dec = jnp.exp(lg[:, None] * (idx[None, :] + 1.0)).T[None, :, :, None]
    k_dec = jnp.exp(lg[:, None] * (C - 1.0 - idx[None, :])).T[None, :, :, None]
    chunk_dec = jnp.exp(lg * C)[None, :, None, None]

    def to_chunks(a):
        a = a.astype(jnp.float32)
        return a.reshape(n, nc, C, H, a.shape[-1]).transpose(1, 0, 2, 3, 4)

    def step(s, inp):
        qc, kc, vc = inp
        scores = jnp.einsum('bihd,bjhd->bhij', qc, kc) * intra
        o = jnp.einsum('bhij,bjhe->bihe', scores, vc)
        o = o + jnp.einsum('bihd,bhde->bihe', qc, s) * q_dec
        s = s * chunk_dec + jnp.einsum('bjhd,bjhe->bhde', kc * k_dec, vc)
        return s, o

    s, o = lax.scan(step, s0.astype(jnp.float32), (to_chunks(q), to_chunks(k), to_chunks(v)))
    return o.transpose(1, 0, 2, 3, 4).reshape(n, L, H, dv), s


def _t5_bucket(dist):
    max_exact = N_BUCKETS // 2
    d = dist.astype(jnp.float32)
    large = max_exact + (jnp.log(jnp.maximum(d, 1.0) / max_exact)
                         / math.log(MAX_DISTANCE / max_exact) * (N_BUCKETS - max_exact))
    large = jnp.minimum(large.astype(jnp.int32), N_BUCKETS - 1)
    return jnp.where(dist < max_exact, dist, large)


def _dilated_attn_prompt(q, k, v, dil, bias_tab):
    B, L, H, Dh = q.shape
    n = L // dil
    blk = ATT_SPAN
    nb = -(-n // blk)
    npad = nb * blk
    bd = B * dil

    def by_residue(a):
        return a.reshape(B, n, dil, H, Dh).transpose(0, 2, 1, 3, 4).reshape(bd, n, H, Dh)

    qb = jnp.pad(by_residue(q), ((0, 0), (0, npad - n), (0, 0), (0, 0))).reshape(bd, nb, blk, H, Dh)

    def band(a):
        ap = jnp.pad(by_residue(a), ((0, 0), (blk, npad - n), (0, 0), (0, 0))).reshape(bd, nb + 1, blk, H, Dh)
        return jnp.concatenate([ap[:, :-1], ap[:, 1:]], axis=2)

    kb, vb = band(k), band(v)
    a_ = jnp.arange(blk)[:, None]
    b_ = jnp.arange(2 * blk)[None, :]
    rel = a_ + blk - b_
    kpos = jnp.arange(nb)[:, None, None] * blk + b_[None] - blk
    valid = (rel >= 0) & (rel <= ATT_SPAN) & (kpos >= 0)
    bias = bias_tab[_t5_bucket(jnp.clip(rel, 0, None) * dil)].transpose(2, 0, 1)
    logits = jnp.einsum('znqhd,znkhd->znhqk', qb, kb).astype(jnp.float32) * (Dh ** -0.5) + bias[None, None]
    logits = jnp.where(valid[None, :, None], logits, NEG)
    m = logits.max(-1, keepdims=True)
    p = jnp.exp(logits - m)
    l = p.sum(-1, keepdims=True)
    o = jnp.einsum('znhqk,znkhd->znqhd', (p / l).astype(v.dtype), vb)
    lse = (m + jnp.log(l))[..., 0].transpose(0, 1, 3, 2)
    o = o.reshape(bd, npad, H, Dh)[:, :n].reshape(B, dil, n, H, Dh).transpose(0, 2, 1, 3, 4).reshape(B, L, H, Dh)
    lse = lse.reshape(bd, npad, H)[:, :n].reshape(B, dil, n, H).transpose(0, 2, 1, 3).reshape(B, L, H)
    return o, lse


def _dilated_attn_sample(q, k_all, v_all, n_buf, dil, bias_tab):
    T = q.shape[1]
    j = jnp.arange(ATT_SPAN + 1)
    idx = n_buf + jnp.arange(T)[:, None] - j[None, :] * dil
    valid = idx >= 0
    idx_c = jnp.maximum(idx, 0)
    kg = k_all[:, idx_c]
    vg = v_all[:, idx_c]
    bias = bias_tab[_t5_bucket(j * dil)].T
    logits = jnp.einsum('bthd,btjhd->bthj', q, kg).astype(jnp.float32) * (q.shape[-1] ** -0.5) + bias[None, None]
    logits = jnp.where(valid[None, :, None, :], logits, NEG)
    m = logits.max(-1, keepdims=True)
    p = jnp.exp(logits - m)
    l = p.sum(-1, keepdims=True)
    o = jnp.einsum('bthj,btjhd->bthd', (p / l).astype(vg.dtype), vg)
    return o, (m + jnp.log(l))[..., 0]


def _peer(h, w_q, sub_keys, u_tab, v_tab):
    shp = h.shape
    t = h.reshape(-1, D_MODEL)
    n = t.shape[0]
    nb = -(-n // PEER_BLOCK)
    tp = jnp.pad(t, ((0, nb * PEER_BLOCK - n), (0, 0))).reshape(nb, PEER_BLOCK, D_MODEL)

    def block(xb):
        q = (xb @ w_q).reshape(PEER_BLOCK, H_PEER, 2, DK_PEER // 2)
        s = jnp.einsum('thsc,hskc->thsk', q, sub_keys).astype(jnp.float32)
        sv, si = lax.top_k(s, TOPK_PEER)
        cand = (sv[:, :, 0, :, None] + sv[:, :, 1, None, :]).reshape(PEER_BLOCK, H_PEER, TOPK_PEER * TOPK_PEER)
        cv, ci = lax.top_k(cand, TOPK_PEER)
        i0 = jnp.take_along_axis(si[:, :, 0], ci // TOPK_PEER, axis=-1)
        i1 = jnp.take_along_axis(si[:, :, 1], ci % TOPK_PEER, axis=-1)
        e = i0 * N_KEYS + i1
        g = jax.nn.softmax(cv, axis=-1)
        a = jax.nn.gelu(jnp.einsum('td,thkd->thk', xb, u_tab[e]), approximate=False)
        return jnp.einsum('thk,thkd->td', (g * a).astype(xb.dtype), v_tab[e])

    y = lax.map(block, tp).reshape(-1, D_MODEL)[:n]
    return y.reshape(shp)


def _layer(x, c, pos0, ret_s0, att_bufs, w_ada, b_ada, w_in, w_ret_o, w_att_o, w_out, b_out,
           ln1_g, ln1_b, peer_wq, peer_keys, peer_u, peer_v, ln2_g, ln2_b, rel_bias):
    nb, L, _ = x.shape
    sh1, sc1, gt1, sh2, sc2, gt2 = _ada(c, w_ada, b_ada)
    h = x * (1 + sc1) + sh1
    rq, rk, rv, rg, aq, ak, av, g_ret, g_att = _project(h, w_in)
    pos = pos0 + jnp.arange(L, dtype=jnp.int32)
    rq = _rotary(rq.reshape(nb, L, H_RET, DK_RET), pos)
    rk = _rotary(rk.reshape(nb, L, H_RET, DK_RET), pos) * (DK_RET ** -0.5)
    rv = rv.reshape(nb, L, H_RET, DV_RET)
    if ret_s0 is None:
        ret_s0 = jnp.zeros((nb, H_RET, DK_RET, DV_RET), jnp.float32)
    ret_o, ret_s = _retention_chunkwise(rq, rk, rv, ret_s0)
    ret_y = (jax.nn.silu(rg) * _head_norm(ret_o).reshape(nb, L, RET_V_W).astype(x.dtype)) @ w_ret_o
    aq = aq.reshape(nb, L, N_GROUPS, H_ATT, DH_ATT)
    ak = ak.reshape(nb, L, N_GROUPS, H_ATT, DH_ATT)
    av = av.reshape(nb, L, N_GROUPS, H_ATT, DH_ATT)
    outs, lses, new_bufs = [], [], []
    for g, (win, dil) in enumerate(ATT_GROUPS):
        q_g, k_g, v_g = aq[:, :, g], ak[:, :, g], av[:, :, g]
        tab = rel_bias[:, g * H_ATT:(g + 1) * H_ATT]
        if att_bufs is None:
            o, lse = _dilated_attn_prompt(q_g, k_g, v_g, dil, tab)
            nw = min(win, L)
            new_bufs.append(jnp.stack([k_g[:, L - nw:], v_g[:, L - nw:]], axis=2))
        else:
            buf = att_bufs[g]
            n_buf = buf.shape[1]
            k_all = jnp.concatenate([buf[:, :, 0].astype(k_g.dtype), k_g], axis=1)
            v_all = jnp.concatenate([buf[:, :, 1].astype(v_g.dtype), v_g], axis=1)
            o, lse = _dilated_attn_sample(q_g, k_all, v_all, n_buf, dil, tab)
            new_bufs.append(jnp.stack([k_all[:, L:], v_all[:, L:]], axis=2))
        outs.append(o)
        lses.append(lse)
    w_grp = jax.nn.softmax(jnp.stack(lses, 0), axis=0)
    att_o = jnp.einsum('gblh,gblhd->blhd', w_grp.astype(x.dtype), jnp.stack(outs, 0))
    att_y = att_o.reshape(nb, L, ATT_OUT_W) @ w_att_o
    mix = (jax.nn.sigmoid(g_ret) * ret_y + jax.nn.sigmoid(g_att) * att_y) @ w_out + b_out
    x = _layer_norm(ALPHA * x + gt1 * mix, ln1_g, ln1_b)
    h2 = x * (1 + sc2) + sh2
    x = _layer_norm(ALPHA * x + gt2 * _peer(h2, peer_wq, peer_keys, peer_u, peer_v), ln2_g, ln2_b)
    return x, ret_s, new_bufs


def setup_inputs(seed: int = 0) -> dict:
    key = jax.random.key(seed)
    ks = jax.random.split(key, 24)
    f32 = jnp.float32

    def nrm(k, shape, s):
        return jax.random.normal(k, shape, f32) * s

    nbuf = [min(w, PAST_LEN) for (w, _) in ATT_GROUPS]
    return {
        "x_prompt": nrm(ks[0], (BATCH, SEQ, D_MODEL), 1.0),
        "x_sample": nrm(ks[1], (DEC_BATCH, DEC_SEQ, D_MODEL), 1.0),
        "state_ret": nrm(ks[2], (DEPTH, DEC_BATCH, H_RET, DK_RET, DV_RET), 0.5),
        "cache_att_w128": nrm(ks[3], (DEPTH, DEC_BATCH, nbuf[0], 2, H_ATT, DH_ATT), 1.0),
        "cache_att_w512": nrm(ks[4], (DEPTH, DEC_BATCH, nbuf[1], 2, H_ATT, DH_ATT), 1.0),
        "cache_att_w2048": nrm(ks[5], (DEPTH, DEC_BATCH, nbuf[2], 2, H_ATT, DH_ATT), 1.0),
        "c_prompt": nrm(ks[6], (BATCH, D_MODEL), 1.0),
        "c_sample": nrm(ks[7], (DEC_BATCH, D_MODEL), 1.0),
        "w_ada": nrm(ks[8], (DEPTH, D_MODEL, 6 * D_MODEL), 0.5 * D_MODEL ** -0.5),
        "b_ada": nrm(ks[9], (DEPTH, 6 * D_MODEL), 0.02),
        "w_in": nrm(ks[10], (DEPTH, D_MODEL, IN_COLS), D_MODEL ** -0.5),
        "w_ret_o": nrm(ks[11], (DEPTH, RET_V_W, D_MODEL), BETA * RET_V_W ** -0.5),
        "w_att_o": nrm(ks[12], (DEPTH, ATT_OUT_W, D_MODEL), BETA * ATT_OUT_W ** -0.5),
        "w_out": nrm(ks[13], (DEPTH, D_MODEL, D_MODEL), BETA * D_MODEL ** -0.5),
        "b_out": nrm(ks[14], (DEPTH, D_MODEL), 0.02),
        "ln1_g": 1.0 + nrm(ks[15], (DEPTH, D_MODEL), 0.02),
        "ln1_b": nrm(ks[16], (DEPTH, D_MODEL), 0.02),
        "peer_wq": nrm(ks[17], (DEPTH, D_MODEL, H_PEER * DK_PEER), D_MODEL ** -0.5),
        "peer_keys": nrm(ks[18], (DEPTH, H_PEER, 2, N_KEYS, DK_PEER // 2), (DK_PEER // 2) ** -0.5),
        "peer_u": nrm(ks[19], (DEPTH, N_EXPERTS, D_MODEL), D_MODEL ** -0.5),
        "peer_v": nrm(ks[20], (DEPTH, N_EXPERTS, D_MODEL), BETA),
        "ln2_g": 1.0 + nrm(ks[21], (DEPTH, D_MODEL), 0.02),
        "ln2_b": nrm(ks[22], (DEPTH, D_MODEL), 0.02),
        "rel_bias": nrm(ks[23], (N_BUCKETS, N_GROUPS * H_ATT), 0.5),
    }


def reference(x_prompt, x_sample, state_ret, cache_att_w128, cache_att_w512, cache_att_w2048,
              c_prompt, c_sample, w_ada, b_ada, w_in, w_ret_o, w_att_o, w_out, b_out,
              ln1_g, ln1_b, peer_wq, peer_keys, peer_u, peer_v, ln2_g, ln2_b, rel_bias):
    yp, ys = x_prompt, x_sample
    rp, rs = [], []
    bp = [[] for _ in range(N_GROUPS)]
    bs = [[] for _ in range(N_GROUPS)]
    for l in range(DEPTH):
        w = (w_ada[l], b_ada[l], w_in[l], w_ret_o[l], w_att_o[l], w_out[l], b_out[l],
             ln1_g[l], ln1_b[l], peer_wq[l], peer_keys[l], peer_u[l], peer_v[l],
             ln2_g[l], ln2_b[l], rel_bias)
        yp, r_p, bufs_p = _layer(yp, c_prompt, 0, None, None, *w)
        ys, r_s, bufs_s = _layer(ys, c_sample, PAST_LEN, state_ret[l],
                                 (cache_att_w128[l], cache_att_w512[l], cache_att_w2048[l]), *w)
        rp.append(r_p)
        rs.append(r_s)
        for g in range(N_GROUPS):
            bp[g].append(bufs_p[g])
            bs[g].append(bufs_s[g])
    new_state_ret_prompt = jnp.stack(rp, 0)
    new_cache_w128_prompt = jnp.stack(bp[0], 0)
    new_cache_w512_prompt = jnp.stack(bp[1], 0)
    new_cache_w2048_prompt = jnp.stack(bp[2], 0)
    new_state_ret_sample = jnp.stack(rs, 0)
    new_cache_w128_sample = jnp.stack(bs[0], 0)
    new_cache_w512_sample = jnp.stack(bs[1], 0)
    new_cache_w2048_sample = jnp.stack(bs[2], 0)
    return (yp, ys, new_state_ret_prompt, new_cache_w128_prompt, new_cache_w512_prompt, new_cache_w2048_prompt,
            new_state_ret_sample, new_cache_w128_sample, new_cache_w512_sample, new_cache_w2048_sample)
```

```python
import math
from contextlib import ExitStack

import numpy as np
import concourse.bass as bass
import concourse.mybir as mybir
from concourse.bass_utils import run_bass_kernel_spmd

F32 = mybir.dt.float32
BF16 = mybir.dt.bfloat16
U32 = mybir.dt.uint32
I32 = mybir.dt.int32
AF = mybir.ActivationFunctionType
ALU = mybir.AluOpType
AX = mybir.AxisListType

NCORES = 8
D = 1024
SEQ = 4096
NPT = 32
NT = 33
NTOK = NT * 128
NSEQ_S = 16
TS = 8
PAST = 8192
IN_COLS = 8704
GROUPS = ((128, 1), (512, 4), (2048, 16))
ALPHA = 2.0 ** 0.25
LN_EPS = 1e-5
HN_EPS = 1e-6
NEGB = -30000.0
SEM_LIMIT = 30000


class Sched:
    def __init__(self, nc, es):
        self.nc = nc
        self.es = es
        self.eng = {"pe": nc.tensor, "act": nc.scalar, "dve": nc.vector, "pool": nc.gpsimd, "sp": nc.sync}
        self.sem = {}
        self.cnt = {}
        self.nsem = 0
        for e in ("pe", "act", "dve", "pool"):
            self._new_engine_sem(e)
        self.known = {e: {} for e in self.eng}
        self.bufs = {}
        self.dpool = {}
        self.drr = {}
        for q, n in (("sp", 24), ("pool", 16), ("act", 8)):
            self.dpool[q] = [self._new_dma_slot() for _ in range(n)]
            self.drr[q] = 0
        self.ninstr = 0

    def _mksem(self, name):
        self.nsem += 1
        return self.es.enter_context(self.nc.semaphore(f"{name}_{self.nsem}"))

    def _new_engine_sem(self, e):
        self.sem[e] = self._mksem("c" + e)
        self.cnt[e] = 0

    def _new_dma_slot(self):
        return {"sem": self._mksem("d"), "val": 0}

    def _wait(self, e, ev):
        sem, val = ev
        k = self.known[e]
        if k.get(id(sem), 0) >= val:
            return
        self.eng[e].wait_ge(sem, val)
        self.ninstr += 1
        k[id(sem)] = val

    def _deps(self, r, w):
        deps = []
        for key in r:
            b = self.bufs.get(key)
            if b is not None and b["w"] is not None:
                deps.append(b["w"])
        for key in w:
            b = self.bufs.get(key)
            if b is not None:
                if b["w"] is not None:
                    deps.append(b["w"])
                deps.extend(b["r"].values())
        return deps

    def _record(self, ev, r, w):
        sem, val = ev
        for key in r:
            b = self.bufs.setdefault(key, {"w": None, "r": {}})
            old = b["r"].get(id(sem))
            if old is None or old[1] < val:
                b["r"][id(sem)] = ev
        for key in w:
            self.bufs[key] = {"w": ev, "r": {}}

    def op(self, e, fn, r=(), w=()):
        deps = self._deps(r, w)
        own = self.sem[e]
        for ev in deps:
            if e == "pe" and ev[0] is own:
                continue
            self._wait(e, ev)
        ins = fn(self.eng[e])
        if self.cnt[e] >= SEM_LIMIT:
            self._new_engine_sem(e)
        self.cnt[e] += 1
        ins.then_inc(self.sem[e], 1)
        self.ninstr += 1
        ev = (self.sem[e], self.cnt[e])
        self._record(ev, r, w)
        return ev

    def dma(self, q, fn, r=(), w=()):
        deps = self._deps(r, w)
        pool = self.dpool[q]
        i = self.drr[q]
        self.drr[q] = (i + 1) % len(pool)
        slot = pool[i]
        if slot["val"] >= SEM_LIMIT:
            slot = pool[i] = self._new_dma_slot()
        if slot["val"] > 0:
            self._wait(q, (slot["sem"], slot["val"]))
        for ev in deps:
            self._wait(q, ev)
        ins = fn(self.eng[q])
        slot["val"] += 16
        ins.then_inc(slot["sem"], 16)
        self.ninstr += 1
        ev = (slot["sem"], slot["val"])
        self._record(ev, r, w)
        return ev

    def barrier(self, e, keys):
        for ev in self._deps((), keys):
            self._wait(e, ev)

    def full_barrier(self):
        evs = [(self.sem[x], self.cnt[x]) for x in ("pe", "act", "dve", "pool") if self.cnt[x] > 0]
        for pool in self.dpool.values():
            for slot in pool:
                if slot["val"] > 0:
                    evs.append((slot["sem"], slot["val"]))
        for e in ("pe", "act", "dve", "pool", "sp"):
            for ev in evs:
                if ev[0] is self.sem.get(e):
                    continue
                self._wait(e, ev)

    def finish(self):
        for q, pool in self.dpool.items():
            for slot in pool:
                if slot["val"] > 0:
                    self._wait("sp", (slot["sem"], slot["val"]))


def _t5_bucket(dist):
    d = dist.astype(np.float32)
    large = 16 + (np.log(np.maximum(d, 1.0) / 16) / math.log(2048 / 16) * 16)
    large = np.minimum(large.astype(np.int32), 31)
    return np.where(dist < 16, dist, large)


_CONST_CACHE = {}


def _consts():
    if _CONST_CACHE:
        return _CONST_CACHE
    c = _CONST_CACHE
    c["ident"] = np.eye(128, dtype=np.float32)
    pos = np.concatenate([np.arange(SEQ), PAST + (np.arange(128) % TS)]).astype(np.float32)
    inv = (10000.0 ** (-np.arange(64, dtype=np.float32) / 64)).astype(np.float32)
    ang = (pos[:, None] * inv[None, :]).astype(np.float32)
    cos = np.cos(ang).astype(np.float32); sin = np.sin(ang).astype(np.float32)
    c["rotc"] = np.concatenate([cos, cos], 1)
    c["rots"] = np.concatenate([-sin, sin], 1)
    lg = np.log1p(-np.exp2(-5.0 - np.arange(4, dtype=np.float64)))
    p = np.arange(128)
    for name, C in (("p", 128), ("s", TS)):
        tpos = p % C
        seq = p // C
        rel = tpos[None, :] - tpos[:, None]
        ok = (rel >= 0) & (seq[None, :] == seq[:, None])
        intra = np.where(ok[:, None, :], np.exp(lg[None, :, None] * np.maximum(rel, 0)[:, None, :]), 0.0)
        c["intraT_" + name] = intra.reshape(128, 512).astype(np.float32)
        qd = np.exp(lg[:, None] * (tpos[None, :] + 1.0))
        c["qdec_" + name] = np.broadcast_to(qd.reshape(1, 512), (128, 512)).astype(np.float32).copy()
        c["kdec_" + name] = np.exp(lg[None, :] * (C - 1.0 - tpos[:, None])).astype(np.float32)
        c["cdec_" + name] = [float(v) for v in np.exp(lg * C)]
    c["rowmask"] = (p[:, None] // TS == np.arange(NSEQ_S)[None, :]).astype(np.float32)
    c["iota16"] = np.broadcast_to(np.arange(16, dtype=np.float32)[None, :], (128, 16)).copy()
    for g, (win, dil) in enumerate(GROUPS):
        oh = np.zeros((33, 385), np.float32)
        for m in range(385):
            rel = m - 128
            if 0 <= rel <= 128:
                oh[int(_t5_bucket(np.array([rel * dil]))[0]), m] = 1.0
            else:
                oh[32, m] = NEGB
        c[f"oh{g}"] = oh
    return c


def build_program(stop=99, nocopy=False, cbs=None, nocso=False, tiles=None):
    nc = bass.Bass("TRN2", target_bir_lowering=False)
    es = ExitStack()
    S = Sched(nc, es)
    CST = _consts()

    def din(name, shape, dt=F32):
        return nc.dram_tensor(name, list(shape), dt, kind="ExternalInput").ap()

    def dout(name, shape, dt=F32):
        return nc.dram_tensor(name, list(shape), dt, kind="ExternalOutput").ap()

    def dscr(name, shape, dt):
        return nc.dram_tensor(name, list(shape), dt).ap()

    def sb(name, shape, dt):
        return es.enter_context(nc.sbuf_tensor("s_" + name, list(shape), dt))

    xp = din("xp", [SEQ, D]); xs = din("xs", [128, D])
    cp_rep = din("cp_rep", [128, D]); cs_rep = din("cs_rep", [128, D])
    st_in = din("st_in", [NSEQ_S, 4, 128, 128])
    c_in = [din(f"c_in{g}", [NSEQ_S, GROUPS[g][0], 2, 512]) for g in range(3)]
    w_ada = din("w_ada", [D, 6 * D]); b_ada = din("b_ada", [1, 6 * D])
    w_in = din("w_in", [D, IN_COLS])
    ident_d = din("ident", [128, 128])
    rotc_d = din("rotc", [NTOK, 128]); rots_d = din("rots", [NTOK, 128])
    intra_d = [din("intraT_p", [128, 512]), din("intraT_s", [128, 512])]
    qdec_d = [din("qdec_p", [128, 512]), din("qdec_s", [128, 512])]
    kdec_d = [din("kdec_p", [128, 4]), din("kdec_s", [128, 4])]
    rowmask_d = din("rowmask", [128, NSEQ_S])
    iota16_d = din("iota16", [128, 16])
    oh_d = [din(f"oh{g}", [33, 385]) for g in range(3)]
    rel_bias_d = din("rel_bias", [32, 24])
    w_ret_o = din("w_ret_o", [512, D]); w_att_o = din("w_att_o", [512, D]); w_out = din("w_out", [D, D])
    b_out_rep = din("b_out_rep", [128, D])
    ln_rep = {k: din(k + "_rep", [128, D]) for k in ("ln1_g", "ln1_b", "ln2_g", "ln2_b")}
    peer_wq = din("peer_wq", [D, 2048]); peer_keys = din("peer_keys", [2048, 128])
    peer_u = din("peer_u", [16384, D]); peer_v = din("peer_v", [16384, D])

    yp = dout("yp", [SEQ, D]); ys = dout("ys", [128, D])
    srp = dout("srp", [4, 128, 128]); srs = dout("srs", [NSEQ_S, 4, 128, 128])
    cpo = [dout(f"cpo{g}", [GROUPS[g][0], 2, 512]) for g in range(3)]
    cso = [dout(f"cso{g}", [NSEQ_S, GROUPS[g][0], 2, 512]) for g in range(3)]

    P = dscr("proj", [NTOK, IN_COLS], BF16)
    RG = dscr("retg", [NTOK, 512], BF16)
    ATT = dscr("attacc", [NTOK, 520], F32)
    EXT = dscr("biasext", [24, 385], F32)
    EXT2 = dscr("biasext2", [24, 128 * 385], F32)

    banks = [es.enter_context(nc.psum_tensor(f"bank{i}", [128, 512], F32)) for i in range(8)]

    def bk(i):
        return f"bank{i}"

    ident_f = sb("ident_f", [128, 128], F32)
    ident_b = sb("ident_b", [128, 128], BF16)
    ones1 = sb("ones1", [1, 128], F32)
    modD = sb("modD", [128, 2, 4, D], F32)
    mod_stack = ExitStack()
    modp = mod_stack.enter_context(nc.sbuf_tensor("s_modp", [128, 6 * D], F32))
    mods = mod_stack.enter_context(nc.sbuf_tensor("s_mods", [128, 6 * D], F32))

    S.dma("sp", lambda e: e.dma_start(out=ident_f[:], in_=ident_d), w=["ident_f"])
    S.op("dve", lambda e: e.tensor_copy(out=ident_b[:], in_=ident_f[:]), r=["ident_f"], w=["ident_b"])
    S.op("dve", lambda e: e.memset(ones1[:], 1.0), w=["ones1"])

    for g in range(0 if not nocopy else 3, 3):
        nb = GROUPS[g][0]
        for b in range(NSEQ_S):
            src = c_in[g][b, TS:nb].rearrange("(a r) k c -> a (r k c)", r=8)
            dst = cso[g][b, 0:nb - TS].rearrange("(a r) k c -> a (r k c)", r=8)
            S.dma("act", lambda e, s=src, d=dst: e.dma_start(out=d, in_=s), w=[("cso_copy", g, b)])

    with ExitStack() as ph:
        def psb(name, shape, dt):
            return ph.enter_context(nc.sbuf_tensor("s_" + name, list(shape), dt))
        c_tok = psb("c_tok", [128, D], F32)
        c_act = psb("c_act", [128, D], F32)
        cT = [psb(f"cT{i}", [128, 8, 128], F32) for i in range(2)]
        wada_t = [psb(f"wada{i}", [128, 8, 512], F32) for i in range(2)]
        bada_t = psb("bada", [1, 6 * D], F32)
        S.dma("sp", lambda e: e.dma_start(out=bada_t[:], in_=b_ada), w=["bada"])
        for i, src in enumerate((cp_rep, cs_rep)):
            S.dma("sp", lambda e, s=src: e.dma_start(out=c_tok[:], in_=s), w=["c_tok"])
            S.op("act", lambda e: e.activation(out=c_act[:], in_=c_tok[:], func=AF.Silu), r=["c_tok"], w=["c_act"])
            for half in range(2):
                def tr4(e, half=half):
                    for j in range(4):
                        kc = half * 4 + j
                        ins = e.transpose(out=banks[half][:, j * 128:(j + 1) * 128],
                                          in_=c_act[:, kc * 128:(kc + 1) * 128], identity=ident_f[:])
                    return ins
                S.op("pe", tr4, r=["c_act", "ident_f"], w=[bk(half)])
                S.op("dve", lambda e, half=half, i=i: e.tensor_copy(
                    out=cT[i][:, half * 4:(half + 1) * 4, :],
                    in_=banks[half][:].rearrange("p (a b) -> p a b", a=4)), r=[bk(half)], w=[f"cT{i}"])
        wv = w_ada.rearrange("(kc p) n -> p kc n", p=128)
        for n in range(12):
            wt = wada_t[n % 2]
            wk = f"wada{n % 2}"
            S.dma("sp", lambda e, wt=wt, n=n: e.dma_start(out=wt[:], in_=wv[:, :, n * 512:(n + 1) * 512]), w=[wk])
            for i, mod in enumerate((modp, mods)):
                b_ = 2 + i
                def mm(e, b_=b_, n=n, wt=wt, i=i):
                    e.matmul(banks[b_][:], lhsT=ones1[0:1, :], rhs=bada_t[0:1, n * 512:(n + 1) * 512],
                             start=True, stop=False)
                    for kc in range(8):
                        ins = e.matmul(banks[b_][:], lhsT=cT[i][:, kc, :], rhs=wt[:, kc, :],
                                       start=False, stop=(kc == 7))
                    return ins
                S.op("pe", mm, r=["ones1", "bada", f"cT{i}", wk], w=[bk(b_)])
                S.op("act", lambda e, b_=b_, mod=mod, n=n: e.activation(
                    out=mod[:, n * 512:(n + 1) * 512], in_=banks[b_][:], func=AF.Copy),
                    r=[bk(b_)], w=[("mod", i, n)])
        for i, mod in enumerate((modp, mods)):
            for j in (1, 4):
                S.op("dve", lambda e, mod=mod, j=j: e.tensor_scalar_add(
                    out=mod[:, j * D:(j + 1) * D], in0=mod[:, j * D:(j + 1) * D], scalar1=1.0),
                    r=[], w=[("mod", i, 2 * j), ("mod", i, 2 * j + 1)])

    for i, mod in enumerate((modp, mods)):
        for jj, j in enumerate((2, 3, 4, 5)):
            S.op("dve" if jj % 2 == 0 else "act",
                 (lambda e, i=i, jj=jj, j=j, mod=mod: e.tensor_copy(out=modD[:, i, jj, :], in_=mod[:, j * D:(j + 1) * D]))
                 if jj % 2 == 0 else
                 (lambda e, i=i, jj=jj, j=j, mod=mod: e.activation(out=modD[:, i, jj, :], in_=mod[:, j * D:(j + 1) * D],
                                                                  func=AF.Copy)),
                 r=[("mod", i, 2 * j), ("mod", i, 2 * j + 1)], w=[("modD", i, jj)])
    S.full_barrier()
    if stop <= 0:
        S.finish()
        return nc, es, S

    def modkeys(i, j):
        return [("mod", i, 2 * j), ("mod", i, 2 * j + 1)]

    hT_stack = ExitStack()
    hT = hT_stack.enter_context(nc.sbuf_tensor("hT", [128, 8, NTOK], BF16))
    with ExitStack() as ph:
        def psb(name, shape, dt):
            return ph.enter_context(nc.sbuf_tensor("s_" + name, list(shape), dt))
        xt = [psb(f"xt{i}", [128, D], F32) for i in range(2)]
        ht = [psb(f"ht{i}", [128, D], F32) for i in range(2)]
        for t in range(NT):
            i = 0 if t < NPT else 1
            mod = modp if t < NPT else mods
            src = xp[t * 128:(t + 1) * 128, :] if t < NPT else xs
            x_ = xt[t % 2]; h_ = ht[t % 2]
            xk = f"xt{t % 2}"; hk = f"ht{t % 2}"
            S.dma("sp", lambda e, x_=x_, src=src: e.dma_start(out=x_[:], in_=src), w=[xk])
            S.op("dve", lambda e, x_=x_, h_=h_, mod=mod: e.tensor_tensor(
                out=h_[:], in0=x_[:], in1=mod[:, D:2 * D], op=ALU.mult), r=[xk] + modkeys(i, 1), w=[hk])
            S.op("dve", lambda e, h_=h_, mod=mod: e.tensor_tensor(
                out=h_[:], in0=h_[:], in1=mod[:, 0:D], op=ALU.add), r=[hk] + modkeys(i, 0), w=[hk])
            for half in range(2):
                b_ = (t % 2) * 2 + half
                def tr4(e, b_=b_, half=half, h_=h_):
                    for j in range(4):
                        kc = half * 4 + j
                        ins = e.transpose(out=banks[b_][:, j * 128:(j + 1) * 128],
                                          in_=h_[:, kc * 128:(kc + 1) * 128], identity=ident_f[:])
                    return ins
                S.op("pe", tr4, r=[hk, "ident_f"], w=[bk(b_)])
                eng = "act" if half == 0 else "dve"
                if eng == "act":
                    S.op("act", lambda e, b_=b_, half=half, t=t: e.activation(
                        out=hT[:, half * 4:(half + 1) * 4, t * 128:(t + 1) * 128],
                        in_=banks[b_][:].rearrange("p (a b) -> p a b", a=4), func=AF.Copy),
                        r=[bk(b_)], w=[("hT", t, half)])
                else:
                    S.op("dve", lambda e, b_=b_, half=half, t=t: e.tensor_copy(
                        out=hT[:, half * 4:(half + 1) * 4, t * 128:(t + 1) * 128],
                        in_=banks[b_][:].rearrange("p (a b) -> p a b", a=4)),
                        r=[bk(b_)], w=[("hT", t, half)])

    S.full_barrier()
    if stop <= 1:
        S.finish()
        return nc, es, S
    with ExitStack() as ph:
        def psb(name, shape, dt):
            return ph.enter_context(nc.sbuf_tensor("s_" + name, list(shape), dt))
        wf = [psb(f"wf{i}", [128, 8, 512], F32) for i in range(2)]
        wb = [psb(f"wb{i}", [128, 8, 512], BF16) for i in range(2)]
        stg = [psb(f"stg{i}", [128, 512], BF16) for i in range(4)]
        stg32 = [psb(f"stg32_{i}", [128, 512], F32) for i in range(2)]
        wv = w_in.rearrange("(kc p) n -> p kc n", p=128)
        it = 0
        i32 = 0
        for cb in (range(17) if cbs is None else cbs):
            wf_ = wf[cb % 2]; wb_ = wb[cb % 2]
            wfk = f"wf{cb % 2}"; wbk = f"wb{cb % 2}"
            S.dma("sp", lambda e, wf_=wf_, cb=cb: e.dma_start(out=wf_[:], in_=wv[:, :, cb * 512:(cb + 1) * 512]),
                  w=[wfk])
            S.op("dve", lambda e, wf_=wf_, wb_=wb_: e.tensor_copy(out=wb_[:, 0:4, :], in_=wf_[:, 0:4, :]),
                 r=[wfk], w=[(wbk, 0)])
            S.op("act", lambda e, wf_=wf_, wb_=wb_: e.activation(out=wb_[:, 4:8, :], in_=wf_[:, 4:8, :], func=AF.Copy),
                 r=[wfk], w=[(wbk, 1)])
            scale = 1.0
            if cb == 1:
                scale = 128.0 ** -0.5
            if 4 <= cb <= 6:
                scale = 0.125
            for t in range(NT):
                b_ = it % 4
                sg = stg[it % 4]; sgk = f"stg{it % 4}"
                it += 1
                def mm(e, b_=b_, t=t, wb_=wb_):
                    for kc in range(8):
                        ins = e.matmul(banks[b_][:], lhsT=hT[:, kc, t * 128:(t + 1) * 128], rhs=wb_[:, kc, :],
                                       start=(kc == 0), stop=(kc == 7))
                    return ins
                S.op("pe", mm, r=[("hT", t, 0), ("hT", t, 1), (wbk, 0), (wbk, 1)], w=[bk(b_)])
                S.op("act", lambda e, b_=b_, sg=sg, scale=scale: e.activation(
                    out=sg[:], in_=banks[b_][:], func=AF.Copy, scale=scale), r=[bk(b_)], w=[sgk])
                S.dma("sp", lambda e, sg=sg, t=t, cb=cb: e.dma_start(
                    out=P[t * 128:(t + 1) * 128, cb * 512:(cb + 1) * 512], in_=sg[:]),
                    r=[sgk], w=[("P", t, cb)])
                if 7 <= cb <= 12:
                    g = (cb - 7) % 3
                    kv = (cb - 7) // 3
                    win = GROUPS[g][0]
                    if t < NPT and t * 128 >= SEQ - win:
                        s32 = stg32[i32 % 2]; s32k = f"stg32_{i32 % 2}"; i32 += 1
                        S.op("act", lambda e, b_=b_, s32=s32: e.activation(out=s32[:], in_=banks[b_][:], func=AF.Copy),
                             r=[bk(b_)], w=[s32k])
                        r0 = t * 128 - (SEQ - win)
                        S.dma("sp", lambda e, s32=s32, g=g, kv=kv, r0=r0: e.dma_start(
                            out=cpo[g][r0:r0 + 128, kv, :], in_=s32[:]), r=[s32k], w=[("cpo", g, kv, t)])
                    if t == NPT and not nocso:
                        s32 = stg32[i32 % 2]; s32k = f"stg32_{i32 % 2}"; i32 += 1
                        S.op("act", lambda e, b_=b_, s32=s32: e.activation(out=s32[:], in_=banks[b_][:], func=AF.Copy),
                             r=[bk(b_)], w=[s32k])
                        for b in range(NSEQ_S):
                            S.dma("sp", lambda e, s32=s32, g=g, kv=kv, b=b, win=win: e.dma_start(
                                out=cso[g][b, win - TS:win, kv, :], in_=s32[b * TS:(b + 1) * TS, :]),
                                r=[s32k], w=[("cso_new", g, kv, b)])

    S.full_barrier()
    hT_stack.close()
    mod_stack.close()
    if stop <= 2:
        S.finish()
        return nc, es, S

    def bcast_mid(ap2d, n):
        return ap2d.unsqueeze(1).to_broadcast([128, n, ap2d.shape[1]])

    def bcast_last(ap2d, n):
        return ap2d.unsqueeze(2).to_broadcast([128, ap2d.shape[1], n])

    def v4(ap2d):
        return ap2d.rearrange("p (h d) -> p h d", h=4)

    with ExitStack() as ph:
        def psb(name, shape, dt):
            return ph.enter_context(nc.sbuf_tensor("s_" + name, list(shape), dt))
        intra_t = [psb(f"intra{i}", [128, 512], F32) for i in range(2)]
        qdec_t = [psb(f"qdec{i}", [128, 512], F32) for i in range(2)]
        kdec_t = [psb(f"kdec{i}", [128, 4], F32) for i in range(2)]
        rowmask_t = psb("rowmask", [128, NSEQ_S], F32)
        for i in range(2):
            S.dma("sp", lambda e, i=i: e.dma_start(out=intra_t[i][:], in_=intra_d[i]), w=[f"intra{i}"])
            S.dma("sp", lambda e, i=i: e.dma_start(out=qdec_t[i][:], in_=qdec_d[i]), w=[f"qdec{i}"])
            S.dma("sp", lambda e, i=i: e.dma_start(out=kdec_t[i][:], in_=kdec_d[i]), w=[f"kdec{i}"])
        S.dma("sp", lambda e: e.dma_start(out=rowmask_t[:], in_=rowmask_d), w=["rowmask"])
        qin = [psb(f"qin{i}", [128, 512], BF16) for i in range(2)]
        kin = [psb(f"kin{i}", [128, 512], BF16) for i in range(2)]
        vin = [psb(f"vin{i}", [128, 512], BF16) for i in range(2)]
        gin = [psb(f"gin{i}", [128, 512], BF16) for i in range(2)]
        rc = [psb(f"rc{i}", [128, 128], F32) for i in range(2)]
        rs = [psb(f"rs{i}", [128, 128], F32) for i in range(2)]
        At = psb("rotA", [128, 512], F32)
        Bt = psb("rotB", [128, 512], F32)
        qr = psb("qr", [128, 512], BF16)
        kr = psb("kr", [128, 512], BF16)
        kd = psb("kd", [128, 512], BF16)
        qT = psb("qT", [128, 4, 128], BF16)
        qdT = psb("qdT", [128, 4, 128], BF16)
        kT = psb("kT", [128, 4, 128], BF16)
        PTr = psb("PTr", [128, 4, 128], BF16)
        Sst = psb("Sst", [128, 4, 128], F32)
        Sb = psb("Sb", [128, 4, 128], BF16)
        o_sb = psb("o_sb", [128, 512], F32)
        osq = psb("osq", [128, 512], F32)
        sil = psb("sil", [128, 512], F32)
        retg = [psb(f"retg{i}", [128, 512], BF16) for i in range(2)]
        ssum = psb("ssum", [128, 4], F32); ssq = psb("ssq", [128, 4], F32)
        mean = psb("mean", [128, 4], F32); msq = psb("msq", [128, 4], F32)
        var = psb("var", [128, 4], F32); rstd = psb("rstd", [128, 4], F32)
        S0f = psb("S0f", [128, NSEQ_S, 4, 128], F32)
        S0b = psb("S0b", [128, NSEQ_S, 4, 128], BF16)
        qdTm = psb("qdTm", [128, NSEQ_S, 4, 128], BF16)
        kdm = psb("kdm", [128, NSEQ_S, 512], BF16)
        Snew = [psb(f"Snew{i}", [128, 4, 128], F32) for i in range(2)]
        S.dma("sp", lambda e: e.dma_start(out=S0f[:], in_=st_in.rearrange("b h k v -> k b h v")), w=["S0f"])
        S.op("act", lambda e: e.activation(out=S0b[:], in_=S0f[:], func=AF.Copy), r=["S0f"], w=["S0b"])
        S.op("dve", lambda e: e.memset(qdTm[:], 0.0), w=["qdTm"])
        bq = banks[0][:].bitcast(BF16)[:, 0:512]
        bkk = banks[1][:].bitcast(BF16)[:, 0:512]

        for t in range(NT):
            i = 0 if t < NPT else 1
            par = t % 2
            cdec = CST["cdec_p"] if i == 0 else CST["cdec_s"]
            rows = slice(t * 128, (t + 1) * 128)
            for name, tl, c0 in (("qin", qin, 0), ("kin", kin, 512), ("vin", vin, 1024), ("gin", gin, 1536)):
                S.dma("sp", lambda e, tl=tl, c0=c0: e.dma_start(out=tl[par][:], in_=P[rows, c0:c0 + 512]),
                      r=[("P", t, c0 // 512)], w=[f"{name}{par}"])
            S.dma("sp", lambda e: e.dma_start(out=rc[par][:], in_=rotc_d[rows, :]), w=[f"rc{par}"])
            S.dma("sp", lambda e: e.dma_start(out=rs[par][:], in_=rots_d[rows, :]), w=[f"rs{par}"])

            def rotary(src, srck, dst, dstk):
                s4 = v4(src[:]); a4 = v4(At[:]); b4 = v4(Bt[:])
                S.op("dve", lambda e: e.tensor_tensor(out=a4, in0=s4, in1=bcast_mid(rc[par][:, :], 4), op=ALU.mult),
                     r=[srck, f"rc{par}"], w=["rotA"])
                S.op("dve", lambda e: e.tensor_tensor(out=b4[:, :, 0:64], in0=s4[:, :, 64:128],
                                                      in1=bcast_mid(rs[par][:, 0:64], 4), op=ALU.mult),
                     r=[srck, f"rs{par}"], w=["rotB0"])
                S.op("dve", lambda e: e.tensor_tensor(out=b4[:, :, 64:128], in0=s4[:, :, 0:64],
                                                      in1=bcast_mid(rs[par][:, 64:128], 4), op=ALU.mult),
                     r=[srck, f"rs{par}"], w=["rotB1"])
                S.op("dve", lambda e: e.tensor_tensor(out=dst[:], in0=At[:], in1=Bt[:], op=ALU.add),
                     r=["rotA", "rotB0", "rotB1"], w=[dstk])
            rotary(qin[par], f"qin{par}", qr, "qr")
            rotary(kin[par], f"kin{par}", kr, "kr")
            S.op("dve", lambda e: e.tensor_tensor(out=v4(kd[:]), in0=v4(kr[:]), in1=bcast_last(kdec_t[i][:, :], 128),
                                                  op=ALU.mult), r=["kr", f"kdec{i}"], w=["kd"])

            def tr4(e, src, dstb):
                for h in range(4):
                    ins = e.transpose(out=dstb[:, h * 128:(h + 1) * 128], in_=src[:, h * 128:(h + 1) * 128],
                                      identity=ident_b[:])
                return ins
            S.op("pe", lambda e: tr4(e, qr, bq), r=["qr", "ident_b"], w=[bk(0)])
            S.op("act", lambda e: e.activation(out=qT[:].rearrange("p h d -> p (h d)"), in_=bq, func=AF.Copy),
                 r=[], w=[bk(0), "qT"])
            S.op("dve", lambda e: e.tensor_tensor(out=qdT[:].rearrange("p h d -> p (h d)"), in0=bq,
                                                  in1=qdec_t[i][:], op=ALU.mult),
                 r=[f"qdec{i}"], w=[bk(0), "qdT"])
            S.op("pe", lambda e: tr4(e, kr, bkk), r=["kr", "ident_b"], w=[bk(1)])
            S.op("act", lambda e: e.activation(out=kT[:].rearrange("p h d -> p (h d)"), in_=bkk, func=AF.Copy),
                 r=[], w=[bk(1), "kT"])

            def sc4(e):
                for h in range(4):
                    ins = e.matmul(banks[2][:, h * 128:(h + 1) * 128], lhsT=kT[:, h, :], rhs=qT[:, h, :],
                                   start=True, stop=True)
                return ins
            S.op("pe", sc4, r=["kT", "qT"], w=[bk(2)])
            S.op("dve", lambda e: e.tensor_tensor(out=PTr[:].rearrange("p h d -> p (h d)"), in0=banks[2][:],
                                                  in1=intra_t[i][:], op=ALU.mult),
                 r=[f"intra{i}"], w=[bk(2), "PTr"])

            if i == 1:
                for b in range(NSEQ_S):
                    S.op("dve", lambda e, b=b: e.tensor_copy(out=qdTm[:, b, :, b * TS:(b + 1) * TS],
                                                             in_=qdT[:, :, b * TS:(b + 1) * TS]),
                         r=["qdT"], w=["qdTm"])
                    S.op("dve", lambda e, b=b: e.tensor_scalar(out=kdm[:, b, :], in0=kd[:], scalar1=rowmask_t[:, b:b + 1],
                                                               scalar2=None, op0=ALU.mult),
                         r=["kd", "rowmask"], w=[("kdm", b)])

            def o4(e):
                for h in range(4):
                    hs = slice(h * 128, (h + 1) * 128)
                    first_only = (i == 0 and t == 0)
                    ins = e.matmul(banks[3][:, hs], lhsT=PTr[:, h, :], rhs=vin[par][:, hs], start=True, stop=first_only)
                    if i == 0 and t > 0:
                        ins = e.matmul(banks[3][:, hs], lhsT=qdT[:, h, :], rhs=Sb[:, h, :], start=False, stop=True)
                    if i == 1:
                        for b in range(NSEQ_S):
                            ins = e.matmul(banks[3][:, hs], lhsT=qdTm[:, b, h, :], rhs=S0b[:, b, h, :],
                                           start=False, stop=(b == NSEQ_S - 1))
                return ins
            S.op("pe", o4, r=["PTr", f"vin{par}", "qdT", "Sb", "qdTm", "S0b"], w=[bk(3)])

            if i == 0:
                def ds4(e):
                    for h in range(4):
                        hs = slice(h * 128, (h + 1) * 128)
                        ins = e.matmul(banks[4][:, hs], lhsT=kd[:, hs], rhs=vin[par][:, hs], start=True, stop=True)
                    return ins
                S.op("pe", ds4, r=["kd", f"vin{par}"], w=[bk(4)])
                if t == 0:
                    S.op("dve", lambda e: e.tensor_copy(out=Sst[:].rearrange("p h d -> p (h d)"), in_=banks[4][:]),
                         r=[], w=[bk(4), "Sst"])
                else:
                    def upd(e):
                        for h in range(4):
                            ins = e.scalar_tensor_tensor(out=Sst[:, h, :], in0=Sst[:, h, :], scalar=cdec[h],
                                                         in1=banks[4][:, h * 128:(h + 1) * 128],
                                                         op0=ALU.mult, op1=ALU.add)
                        return ins
                    S.op("dve", upd, r=[], w=[bk(4), "Sst"])
                S.op("act", lambda e: e.activation(out=Sb[:], in_=Sst[:], func=AF.Copy), r=["Sst"], w=["Sb"])
                if t == NPT - 1:
                    S.dma("sp", lambda e: e.dma_start(out=srp.rearrange("h k v -> k h v"), in_=Sst[:]),
                          r=["Sst"], w=["srp"])
            else:
                for b in range(NSEQ_S):
                    bb = 4 + (b % 2)
                    sn = Snew[b % 2]; snk = f"Snew{b % 2}"
                    def ds4(e, b=b, bb=bb):
                        for h in range(4):
                            hs = slice(h * 128, (h + 1) * 128)
                            ins = e.matmul(banks[bb][:, hs], lhsT=kdm[:, b, hs], rhs=vin[par][:, hs],
                                           start=True, stop=True)
                        return ins
                    S.op("pe", ds4, r=[("kdm", b), f"vin{par}"], w=[bk(bb)])
                    def upd(e, b=b, bb=bb, sn=sn):
                        for h in range(4):
                            ins = e.scalar_tensor_tensor(out=sn[:, h, :], in0=S0f[:, b, h, :], scalar=cdec[h],
                                                         in1=banks[bb][:, h * 128:(h + 1) * 128],
                                                         op0=ALU.mult, op1=ALU.add)
                        return ins
                    S.op("dve", upd, r=["S0f"], w=[bk(bb), snk])
                    S.dma("sp", lambda e, b=b, sn=sn: e.dma_start(out=srs[b].rearrange("h k v -> k h v"), in_=sn[:]),
                          r=[snk], w=[("srs", b)])

            S.op("act", lambda e: e.activation(out=o_sb[:], in_=banks[3][:], func=AF.Copy), r=[], w=[bk(3), "o_sb"])
            S.op("dve", lambda e: e.tensor_reduce(out=ssum[:], in_=v4(o_sb[:]), axis=AX.X, op=ALU.add),
                 r=["o_sb"], w=["ssum"])
            S.op("act", lambda e: e.activation(out=osq[:], in_=o_sb[:], func=AF.Square), r=["o_sb"], w=["osq"])
            S.op("dve", lambda e: e.tensor_reduce(out=ssq[:], in_=v4(osq[:]), axis=AX.X, op=ALU.add),
                 r=["osq"], w=["ssq"])
            S.op("dve", lambda e: e.tensor_scalar_mul(out=mean[:], in0=ssum[:], scalar1=1.0 / 128), r=["ssum"], w=["mean"])
            S.op("dve", lambda e: e.tensor_tensor(out=msq[:], in0=mean[:], in1=mean[:], op=ALU.mult), r=["mean"], w=["msq"])
            S.op("dve", lambda e: e.scalar_tensor_tensor(out=var[:], in0=ssq[:], scalar=1.0 / 128, in1=msq[:],
                                                         op0=ALU.mult, op1=ALU.subtract), r=["ssq", "msq"], w=["var"])
            S.op("dve", lambda e: e.tensor_scalar_add(out=var[:], in0=var[:], scalar1=HN_EPS), r=["var"], w=["var"])
            S.op("act", lambda e: e.activation(out=var[:], in_=var[:], func=AF.Sqrt), r=["var"], w=["var"])
            S.op("dve", lambda e: e.reciprocal(out=rstd[:], in_=var[:]), r=["var"], w=["rstd"])
            S.op("dve", lambda e: e.tensor_tensor(out=v4(o_sb[:]), in0=v4(o_sb[:]), in1=bcast_last(mean[:, :], 128),
                                                  op=ALU.subtract), r=["o_sb", "mean", "osq"], w=["o_sb"])
            S.op("dve", lambda e: e.tensor_tensor(out=v4(o_sb[:]), in0=v4(o_sb[:]), in1=bcast_last(rstd[:, :], 128),
                                                  op=ALU.mult), r=["o_sb", "rstd"], w=["o_sb"])
            S.op("act", lambda e: e.activation(out=sil[:], in_=gin[par][:], func=AF.Silu), r=[f"gin{par}"], w=["sil"])
            S.op("dve", lambda e: e.tensor_tensor(out=retg[par][:], in0=o_sb[:], in1=sil[:], op=ALU.mult),
                 r=["o_sb", "sil"], w=[f"retg{par}"])
            S.dma("sp", lambda e: e.dma_start(out=RG[rows, :], in_=retg[par][:]), r=[f"retg{par}"], w=[("RG", t)])

    S.full_barrier()
    if stop <= 3:
        S.finish()
        return nc, es, S

    with ExitStack() as ph:
        def psb(name, shape, dt):
            return ph.enter_context(nc.sbuf_tensor("s_" + name, list(shape), dt))
        rb_aug = psb("rb_aug", [33, 24], F32)
        oh_t = [psb(f"oh{g}", [33, 385], F32) for g in range(3)]
        ext_sb = psb("ext_sb", [8, 385], F32)
        btmp = [psb(f"btmp{i}", [128, 256], F32) for i in range(2)]
        biasT = psb("biasT", [128, 24, 256], BF16)
        S.op("dve", lambda e: e.memset(rb_aug[:], 1.0), w=["rb_aug"])
        S.dma("sp", lambda e: e.dma_start(out=rb_aug[0:32, :], in_=rel_bias_d), w=["rb_aug"])
        for g in range(3):
            S.dma("sp", lambda e, g=g: e.dma_start(out=oh_t[g][:], in_=oh_d[g]), w=[f"oh{g}"])
            S.op("pe", lambda e, g=g: e.matmul(banks[0][0:8, 0:385], lhsT=rb_aug[:, g * 8:(g + 1) * 8], rhs=oh_t[g][:],
                                               start=True, stop=True), r=["rb_aug", f"oh{g}"], w=[bk(0)])
            S.op("act", lambda e: e.activation(out=ext_sb[:], in_=banks[0][0:8, 0:385], func=AF.Copy),
                 r=[], w=[bk(0), "ext_sb"])
            S.dma("sp", lambda e, g=g: e.dma_start(out=EXT[g * 8:(g + 1) * 8, :], in_=ext_sb[:]),
                  r=["ext_sb"], w=[("EXT", g)])
        for gh in range(24):
            bt = btmp[gh % 2]; btk = f"btmp{gh % 2}"
            srcb = bass.AP(tensor=EXT.tensor, offset=gh * 385, ap=[[0, 128], [1, 385]])
            dstb = bass.AP(tensor=EXT2.tensor, offset=gh * 128 * 385, ap=[[385, 128], [1, 385]])
            S.dma("sp", lambda e, srcb=srcb, dstb=dstb: e.dma_start(out=dstb, in_=srcb),
                  r=[("EXT", gh // 8)], w=[("EXT2", gh)])
            for kc, c0 in ((0, 256), (1, 128)):
                src = bass.AP(tensor=EXT2.tensor, offset=gh * 128 * 385 + c0, ap=[[384, 128], [1, 128]])
                S.dma("sp", lambda e, bt=bt, kc=kc, src=src: e.dma_start(out=bt[:, kc * 128:(kc + 1) * 128], in_=src),
                      r=[("EXT2", gh)], w=[(btk, kc)])
            S.op("dve", lambda e, bt=bt, gh=gh: e.tensor_copy(out=biasT[:, gh, :], in_=bt[:]),
                 r=[(btk, 0), (btk, 1)], w=[("biasT", gh)])

        Qt = [psb(f"Qt{i}", [128, 512], BF16) for i in range(2)]
        Kt = [psb(f"Kt{i}", [128, 512], BF16) for i in range(2)]
        QT = [psb(f"QT{i}", [128, 4, 128], BF16) for i in range(2)]
        KT = [psb(f"KT{i}", [128, 4, 128], BF16) for i in range(3)]
        Va = [psb(f"Va{i}", [128, 8, 65], BF16) for i in range(3)]
        PT = [psb(f"PT{i}", [128, 2, 2, 128], BF16) for i in range(2)]
        OLs = [psb(f"OLs{i}", [128, 520], F32) for i in range(2)]
        kvf = [psb(f"kvf{i}", [128, 2, 512], F32) for i in range(2)]
        for i in range(3):
            S.op("dve", lambda e, i=i: e.memset(Va[i][:], 1.0), w=[f"Va{i}"])
        for i in range(2):
            S.op("dve", lambda e, i=i: e.memset(Qt[i][:], 0.0), w=[f"Qt{i}"])
            S.op("dve", lambda e, i=i: e.memset(Kt[i][:], 0.0), w=[f"Kt{i}"])
        bq = banks[0][:].bitcast(BF16)[:, 0:512]
        bkk = banks[1][:].bitcast(BF16)[:, 0:512]
        state = {"blk": 0}

        def tr4(e, src, dstb):
            for j in range(4):
                ins = e.transpose(out=dstb[:, j * 128:(j + 1) * 128], in_=src[:, j * 128:(j + 1) * 128],
                                  identity=ident_b[:])
            return ins

        def attn_core(g, NQ, qT, qTk, kTc, kTck, kTp, kTpk, vc, vck, vp, vpk):
            blk = state["blk"]; state["blk"] += 1
            has_prev = kTp is not None
            for hp in range(4):
                bnk = banks[2 + hp]
                pt = PT[hp % 2]; ptk = f"PT{hp % 2}"
                def sc(e, hp=hp, bnk=bnk):
                    for hh in range(2):
                        h = 2 * hp + hh
                        gh = g * 8 + h
                        ps_ = slice((h % 2) * 64, (h % 2) * 64 + 64)
                        base = hh * 256
                        if has_prev:
                            e.matmul(bnk[:, base:base + NQ], lhsT=ident_b[:], rhs=biasT[:, gh, 0:NQ],
                                     start=True, stop=False)
                            e.matmul(bnk[:, base:base + NQ], lhsT=kTp[ps_, h // 2, :], rhs=qT[ps_, h // 2, 0:NQ],
                                     start=False, stop=True)
                        e.matmul(bnk[:, base + 128:base + 128 + NQ], lhsT=ident_b[:], rhs=biasT[:, gh, 128:128 + NQ],
                                 start=True, stop=False)
                        ins = e.matmul(bnk[:, base + 128:base + 128 + NQ], lhsT=kTc[ps_, h // 2, :],
                                       rhs=qT[ps_, h // 2, 0:NQ], start=False, stop=True)
                    return ins
                S.op("pe", sc, r=[qTk, kTck, "ident_b"] + ([kTpk] if has_prev else []) +
                     [("biasT", g * 8 + 2 * hp), ("biasT", g * 8 + 2 * hp + 1)], w=[bk(2 + hp)])
                bv = bnk[:].rearrange("p (a b c) -> p a b c", a=2, b=2)
                if has_prev:
                    S.op("act", lambda e, pt=pt, bv=bv: e.activation(out=pt[:, :, :, 0:NQ], in_=bv[:, :, :, 0:NQ],
                                                                     func=AF.Exp), r=[], w=[bk(2 + hp), ptk])
                else:
                    S.op("act", lambda e, pt=pt, bv=bv: e.activation(out=pt[:, :, 1, 0:NQ], in_=bv[:, :, 1, 0:NQ],
                                                                     func=AF.Exp), r=[], w=[bk(2 + hp), ptk])
                def pv(e, hp=hp, pt=pt):
                    for hh in range(2):
                        h = 2 * hp + hh
                        ob = banks[6 + h // 4]
                        oreg = ob[0:NQ, (h % 4) * 65:(h % 4) * 65 + 65]
                        if has_prev:
                            e.matmul(oreg, lhsT=pt[:, hh, 0, 0:NQ], rhs=vp[:, h, :], start=True, stop=False)
                        ins = e.matmul(oreg, lhsT=pt[:, hh, 1, 0:NQ], rhs=vc[:, h, :], start=(not has_prev), stop=True)
                    return ins
                S.op("pe", pv, r=[ptk, vck] + ([vpk] if has_prev else []), w=[bk(6 + hp // 2)])
            ol = OLs[blk % 2]; olk = f"OLs{blk % 2}"
            S.op("act", lambda e: e.activation(out=ol[0:NQ, 0:260], in_=banks[6][0:NQ, 0:260], func=AF.Copy),
                 r=[], w=[bk(6), (olk, 0)])
            S.op("dve", lambda e: e.tensor_copy(out=ol[0:NQ, 260:520], in_=banks[7][0:NQ, 0:260]),
                 r=[], w=[bk(7), (olk, 1)])
            return ol, olk

        att_events = []
        for g, (win, dil) in enumerate(GROUPS):
            prev_events = att_events
            att_events = []
            Pv = P[0:SEQ].rearrange("(u d) c -> d u c", d=dil)
            Av = ATT[0:SEQ].rearrange("(u d) c -> d u c", d=dil)
            nb = NPT // dil
            cnt = 0
            for r in range(dil):
                for ub in range(nb):
                    par = cnt % 2; p3 = cnt % 3; pp3 = (cnt - 1) % 3
                    cnt += 1
                    us = slice(ub * 128, (ub + 1) * 128)
                    rkeys = [("P", (r + dil * (ub * 128 + i)) // 128, 0) for i in (0, 127)]
                    S.dma("sp", lambda e: e.dma_start(out=Qt[par][:], in_=Pv[r, us, 2048 + g * 512:2048 + (g + 1) * 512]),
                          w=[f"Qt{par}"])
                    S.dma("sp", lambda e: e.dma_start(out=Kt[par][:], in_=Pv[r, us, 3584 + g * 512:3584 + (g + 1) * 512]),
                          w=[f"Kt{par}"])
                    S.dma("sp", lambda e: e.dma_start(
                        out=Va[p3][:, :, 0:64],
                        in_=Pv[r, us, 5120 + g * 512:5120 + (g + 1) * 512].rearrange("p (h d) -> p h d", h=8)),
                        w=[f"Va{p3}"])
                    S.op("pe", lambda e: tr4(e, Qt[par], bq), r=[f"Qt{par}", "ident_b"], w=[bk(0)])
                    S.op("act", lambda e: e.activation(out=QT[par][:].rearrange("p h d -> p (h d)"), in_=bq, func=AF.Copy),
                         r=[], w=[bk(0), f"QT{par}"])
                    S.op("pe", lambda e: tr4(e, Kt[par], bkk), r=[f"Kt{par}", "ident_b"], w=[bk(1)])
                    S.op("dve", lambda e: e.tensor_copy(out=KT[p3][:].rearrange("p h d -> p (h d)"), in_=bkk),
                         r=[], w=[bk(1), f"KT{p3}"])
                    hp_ = ub > 0
                    ol, olk = attn_core(g, 128, QT[par], f"QT{par}", KT[p3], f"KT{p3}",
                                        KT[pp3] if hp_ else None, f"KT{pp3}", Va[p3], f"Va{p3}",
                                        Va[pp3] if hp_ else None, f"Va{pp3}")
                    aq_ = "sp" if g == 0 else "pool"
                    if g > 0 and r == 0 and ub == 0:
                        for ev in prev_events:
                            S._wait(aq_, ev)
                    kw = {} if g == 0 else {"accum_op": ALU.add}
                    ev = S.dma(aq_, lambda e: e.dma_start(out=Av[r, us, :], in_=ol[:], **kw),
                               r=[(olk, 0), (olk, 1)], w=[("ATT", g, r, ub)])
                    att_events.append(ev)
        if stop > 4:
            for g, (win, dil) in enumerate(GROUPS):
                nq = TS // dil if dil <= TS else 1
                classes = list(range(min(dil, TS)))
                prev_events = att_events
                att_events = []
                cnt = 0
                for b in range(NSEQ_S):
                    cv = c_in[g][b].rearrange("(u d) k c -> d u k c", d=dil)
                    for r in classes:
                        par = cnt % 2; p3 = cnt % 3
                        cnt += 1
                        tok0 = SEQ + b * TS + r
                        rows = slice(tok0, tok0 + dil * (nq - 1) + 1, dil)
                        S.dma("sp", lambda e: e.dma_start(out=kvf[par][:], in_=cv[r]), w=[f"kvf{par}"])
                        S.dma("sp", lambda e: e.dma_start(out=Qt[par][0:nq, :],
                                                          in_=P[rows, 2048 + g * 512:2048 + (g + 1) * 512]),
                              r=[("P", NPT, 4 + g)], w=[f"Qt{par}"])
                        S.dma("sp", lambda e: e.dma_start(out=Kt[par][0:nq, :],
                                                          in_=P[rows, 3584 + g * 512:3584 + (g + 1) * 512]),
                              r=[("P", NPT, 7 + g)], w=[f"Kt{par}"])
                        vcur = Va[(2 * cnt) % 3]; vck = f"Va{(2 * cnt) % 3}"
                        vprv = Va[(2 * cnt + 1) % 3]; vpk = f"Va{(2 * cnt + 1) % 3}"
                        S.dma("sp", lambda e: e.dma_start(
                            out=vcur[0:nq, :, 0:64],
                            in_=P[rows, 5120 + g * 512:5120 + (g + 1) * 512].rearrange("p (h d) -> p h d", h=8)),
                            r=[("P", NPT, 10 + g)], w=[vck])
                        S.op("act", lambda e: e.activation(out=Kt[1 - par][:], in_=kvf[par][:, 0, :], func=AF.Copy),
                             r=[f"kvf{par}"], w=[f"Kt{1 - par}"])
                        S.op("dve", lambda e: e.tensor_copy(out=vprv[:, :, 0:64],
                                                            in_=kvf[par][:, 1, :].rearrange("p (h d) -> p h d", h=8)),
                             r=[f"kvf{par}"], w=[vpk])
                        S.op("pe", lambda e: tr4(e, Qt[par], bq), r=[f"Qt{par}", "ident_b"], w=[bk(0)])
                        S.op("act", lambda e: e.activation(out=QT[par][:].rearrange("p h d -> p (h d)"), in_=bq,
                                                           func=AF.Copy), r=[], w=[bk(0), f"QT{par}"])
                        S.op("pe", lambda e: tr4(e, Kt[par], bkk), r=[f"Kt{par}", "ident_b"], w=[bk(1)])
                        S.op("dve", lambda e: e.tensor_copy(out=KT[0][:].rearrange("p h d -> p (h d)"), in_=bkk),
                             r=[], w=[bk(1), "KT0"])
                        S.op("pe", lambda e: tr4(e, Kt[1 - par], bq), r=[f"Kt{1 - par}", "ident_b"], w=[bk(0)])
                        S.op("act", lambda e: e.activation(out=KT[1][:].rearrange("p h d -> p (h d)"), in_=bq,
                                                           func=AF.Copy), r=[], w=[bk(0), "KT1"])
                        ol, olk = attn_core(g, 8, QT[par], f"QT{par}", KT[0], "KT0", KT[1], "KT1",
                                            vcur, vck, vprv, vpk)
                        aq_ = "sp" if g == 0 else "pool"
                        if g > 0 and cnt == 1:
                            for ev in prev_events:
                                S._wait(aq_, ev)
                        kw = {} if g == 0 else {"accum_op": ALU.add}
                        ev = S.dma(aq_, lambda e: e.dma_start(out=ATT[rows, :], in_=ol[0:nq, :], **kw),
                                   r=[(olk, 0), (olk, 1)], w=[("ATTs", g, b, r)])
                        att_events.append(ev)

    S.full_barrier()
    if stop <= 5:
        S.finish()
        return nc, es, S

    with ExitStack() as ph:
        def psb(name, shape, dt):
            return ph.enter_context(nc.sbuf_tensor("s_" + name, list(shape), dt))
        wro_b = psb("wro_b", [128, 4, D], BF16)
        wao_b = psb("wao_b", [128, 4, D], BF16)
        wout_b = psb("wout_b", [128, 8, D], BF16)
        wq_b = psb("wq_b", [128, 8, 2048], BF16)
        keysT = psb("keysT", [128, 16, 128], BF16)
        bout_t = psb("bout_t", [128, D], F32)
        lnt = {k: psb(k + "_t", [128, D], F32) for k in ("ln1_g", "ln1_b", "ln2_g", "ln2_b")}
        iota16 = psb("iota16", [128, 16], F32)
        S.dma("sp", lambda e: e.dma_start(out=bout_t[:], in_=b_out_rep), w=["bout_t"])
        for k in lnt:
            S.dma("sp", lambda e, k=k: e.dma_start(out=lnt[k][:], in_=ln_rep[k]), w=[k + "_t"])
        S.dma("sp", lambda e: e.dma_start(out=iota16[:], in_=iota16_d), w=["iota16"])
        with ExitStack() as wl:
            wst = [wl.enter_context(nc.sbuf_tensor(f"s_wst{i}", [128, 4, 1024], F32)) for i in range(2)]
            jobs = []
            for kc0 in (0,):
                jobs.append((w_ret_o.rearrange("(kc p) n -> p kc n", p=128), wro_b[:, :, :], "wro_b"))
                jobs.append((w_att_o.rearrange("(kc p) n -> p kc n", p=128), wao_b[:, :, :], "wao_b"))
            wov = w_out.rearrange("(kc p) n -> p kc n", p=128)
            jobs.append((wov[:, 0:4, :], wout_b[:, 0:4, :], ("wout_b", 0)))
            jobs.append((wov[:, 4:8, :], wout_b[:, 4:8, :], ("wout_b", 1)))
            wqv = peer_wq.rearrange("(kc p) n -> p kc n", p=128)
            for a in range(2):
                for b in range(2):
                    jobs.append((wqv[:, a * 4:(a + 1) * 4, b * 1024:(b + 1) * 1024],
                                 wq_b[:, a * 4:(a + 1) * 4, b * 1024:(b + 1) * 1024], ("wq_b", a, b)))
            for j, (src, dst, key) in enumerate(jobs):
                st_ = wst[j % 2]; stk = f"wst{j % 2}"
                S.dma("sp", lambda e, st_=st_, src=src: e.dma_start(out=st_[:], in_=src), w=[stk])
                if j % 2 == 0:
                    S.op("dve", lambda e, st_=st_, dst=dst: e.tensor_copy(out=dst, in_=st_[:]), r=[stk], w=[key])
                else:
                    S.op("act", lambda e, st_=st_, dst=dst: e.activation(out=dst, in_=st_[:], func=AF.Copy),
                         r=[stk], w=[key])
            kst = wst[0]
            for q4 in range(4):
                S.dma("sp", lambda e, q4=q4: e.dma_start(
                    out=kst[:, q4, 0:512].rearrange("p (a c) -> p a c", a=4),
                    in_=peer_keys[q4 * 512:(q4 + 1) * 512, :].rearrange("(a p) c -> p a c", p=128)), w=["wst0"])
            for q4 in range(4):
                def trk(e, q4=q4):
                    for a in range(4):
                        ins = e.transpose(out=banks[q4][:, a * 128:(a + 1) * 128], in_=kst[:, q4, a * 128:(a + 1) * 128],
                                          identity=ident_f[:])
                    return ins
                S.op("pe", trk, r=["wst0", "ident_f"], w=[bk(q4)])
                S.op("act", lambda e, q4=q4: e.activation(
                    out=keysT[:, q4 * 4:(q4 + 1) * 4, :].rearrange("p a k -> p (a k)"), in_=banks[q4][:], func=AF.Copy),
                    r=[], w=[bk(q4), ("keysT", q4)])
            S.full_barrier()
        wkeys = ["wro_b", "wao_b", ("wout_b", 0), ("wout_b", 1)]
        wqkeys = [("wq_b", a, b) for a in range(2) for b in range(2)]

        x_t = psb("x_t", [128, D], F32)
        att_t = psb("att_t", [128, 520], F32)
        rg_t = psb("rg_t", [128, 512], BF16)
        attn_t = psb("attn_t", [128, 512], BF16)
        rl = psb("rl", [128, 8], F32)
        aT = psb("aT", [128, 4, 128], BF16)
        rT = psb("rT", [128, 4, 128], BF16)
        bufA = psb("bufA", [128, 2048], F32)
        bufB = psb("bufB", [128, 2048], F32)
        tmp1 = psb("tmp1", [128, D], F32)
        x1 = psb("x1", [128, D], F32)
        h2 = psb("h2", [128, D], F32)
        h2b = psb("h2b", [128, D], BF16)
        h2T = psb("h2T", [128, 8, 128], BF16)
        junk = psb("junk", [128, D], BF16)
        st1 = psb("st1", [128, 8], F32)
        sv = psb("sv", [128, 16, 16], F32)
        si = psb("si", [128, 16, 16], U32)
        sif = psb("sif", [128, 16, 16], F32)
        cv = psb("cv", [128, 8, 16], F32)
        ci = psb("ci", [128, 8, 16], U32)
        hi_u = psb("hi_u", [128, 8, 16], U32); lo_u = psb("lo_u", [128, 8, 16], U32)
        hi_f = psb("hi_f", [128, 8, 16], F32); lo_f = psb("lo_f", [128, 8, 16], F32)
        i0 = psb("i0", [128, 8, 16], F32); i1 = psb("i1", [128, 8, 16], F32)
        e_f = psb("e_f", [128, 128], F32); e_i = psb("e_i", [128, 128], I32)
        gsm = psb("gsm", [128, 8, 16], F32); ssm = psb("ssm", [128, 8], F32)
        a_t = psb("a_t", [128, 128], F32); ga = psb("ga", [128, 128], BF16)
        NSL = 4
        Ug = [psb(f"Ug{i}", [128, D], BF16) for i in range(NSL)]
        Vg = [psb(f"Vg{i}", [128, D], BF16) for i in range(NSL)]
        Dg = [psb(f"Dg{i}", [128, 16, 128], BF16) for i in range(2)]
        Gb = bufA[:].bitcast(BF16)
        G_t = Gb[:, 0:2048]; sg_t = Gb[:, 2048:4096]
        Bb = bufB[:].bitcast(BF16)
        mixin = Bb[:, 0:1024]; mixT = Bb[:, 1024:2048].rearrange("p (k t) -> p k t", k=8)
        qpe = Bb[:, 0:2048]; qpT = Bb[:, 2048:4096].rearrange("p (k t) -> p k t", k=16)
        s_sc = bufA[:].rearrange("p (a k) -> p a k", a=16)
        oh4 = bufA[:].rearrange("p (h k i) -> p h k i", h=8, k=16)
        cand = bufB[:].rearrange("p (h c) -> p h c", h=8)
        cand4 = bufB[:].rearrange("p (h i j) -> p h i j", h=8, i=16)
        prod4 = bufB[:].rearrange("p (h k i) -> p h k i", h=8, k=16)
        bf_banks = [banks[i][:].bitcast(BF16) for i in range(8)]

        def layer_norm(src, srck, gk, bk_, dst, dstk):
            S.op("dve", lambda e: e.tensor_reduce(out=st1[:, 0:1], in_=src[:], axis=AX.X, op=ALU.add), r=[srck], w=["st_sum"])
            S.op("act", lambda e: e.activation(out=tmp1[:], in_=src[:], func=AF.Square), r=[srck], w=["tmp1"])
            S.op("dve", lambda e: e.tensor_reduce(out=st1[:, 1:2], in_=tmp1[:], axis=AX.X, op=ALU.add), r=["tmp1"], w=["st_sq"])
            S.op("dve", lambda e: e.tensor_scalar_mul(out=st1[:, 2:3], in0=st1[:, 0:1], scalar1=1.0 / D), r=["st_sum"], w=["st_mean"])
            S.op("dve", lambda e: e.tensor_tensor(out=st1[:, 3:4], in0=st1[:, 2:3], in1=st1[:, 2:3], op=ALU.mult),
                 r=["st_mean"], w=["st_msq"])
            S.op("dve", lambda e: e.scalar_tensor_tensor(out=st1[:, 4:5], in0=st1[:, 1:2], scalar=1.0 / D, in1=st1[:, 3:4],
                                                         op0=ALU.mult, op1=ALU.subtract), r=["st_sq", "st_msq"], w=["st_var"])
            S.op("dve", lambda e: e.tensor_scalar_add(out=st1[:, 4:5], in0=st1[:, 4:5], scalar1=LN_EPS), r=["st_var"], w=["st_var"])
            S.op("act", lambda e: e.activation(out=st1[:, 5:6], in_=st1[:, 4:5], func=AF.Sqrt), r=["st_var"], w=["st_std"])
            S.op("dve", lambda e: e.reciprocal(out=st1[:, 6:7], in_=st1[:, 5:6]), r=["st_std"], w=["st_rstd"])
            S.op("dve", lambda e: e.tensor_scalar(out=src[:], in0=src[:], scalar1=st1[:, 2:3], scalar2=st1[:, 6:7],
                                                  op0=ALU.subtract, op1=ALU.mult), r=[srck, "st_mean", "st_rstd"], w=[srck])
            S.op("dve", lambda e: e.tensor_tensor(out=src[:], in0=src[:], in1=lnt[gk][:], op=ALU.mult), r=[srck, gk + "_t"], w=[srck])
            S.op("dve", lambda e: e.tensor_tensor(out=dst[:], in0=src[:], in1=lnt[bk_][:], op=ALU.add), r=[srck, bk_ + "_t"], w=[dstk])

        def tr_n(e, src, dstb, n):
            for j in range(n):
                ins = e.transpose(out=dstb[:, j * 128:(j + 1) * 128], in_=src[:, j * 128:(j + 1) * 128], identity=ident_b[:])
            return ins

        gslot = {"u": 0, "v": 0}
        for t in (range(NT) if tiles is None else tiles):
            i = 0 if t < NPT else 1
            rows = slice(t * 128, (t + 1) * 128)
            xsrc = xp[rows, :] if t < NPT else xs
            ydst = yp[rows, :] if t < NPT else ys
            S.dma("sp", lambda e: e.dma_start(out=x_t[:], in_=xsrc), w=["x_t"])
            S.dma("sp", lambda e: e.dma_start(out=att_t[:], in_=ATT[rows, :]), w=["att_t"])
            S.dma("sp", lambda e: e.dma_start(out=rg_t[:], in_=RG[rows, :]), r=[("RG", t)], w=["rg_t"])
            S.dma("sp", lambda e: e.dma_start(out=G_t, in_=P[rows, 6656:8704]), w=["bufA"])
            a3 = att_t[:].rearrange("p (h c) -> p h c", h=8)
            S.op("dve", lambda e: e.reciprocal(out=rl[:], in_=a3[:, :, 64]), r=["att_t"], w=["rl"])
            S.op("dve", lambda e: e.tensor_tensor(out=attn_t[:].rearrange("p (h d) -> p h d", h=8), in0=a3[:, :, 0:64],
                                                  in1=bcast_last(rl[:, :], 64), op=ALU.mult), r=["att_t", "rl"], w=["attn_t"])
            S.op("pe", lambda e: tr_n(e, attn_t, bf_banks[0], 4), r=["attn_t", "ident_b"], w=[bk(0)])
            S.op("act", lambda e: e.activation(out=aT[:].rearrange("p k t -> p (k t)"), in_=bf_banks[0][:, 0:512], func=AF.Copy),
                 r=[], w=[bk(0), "aT"])
            S.op("pe", lambda e: tr_n(e, rg_t, bf_banks[1], 4), r=["rg_t", "ident_b"], w=[bk(1)])
            S.op("dve", lambda e: e.tensor_copy(out=rT[:].rearrange("p k t -> p (k t)"), in_=bf_banks[1][:, 0:512]),
                 r=[], w=[bk(1), "rT"])
            S.op("act", lambda e: e.activation(out=sg_t, in_=G_t, func=AF.Sigmoid), r=["bufA"], w=["sg_t"])

            def proj4(e, srcT, w_b, b0):
                for half in range(2):
                    for kc in range(4):
                        ins = e.matmul(banks[b0 + half][:], lhsT=srcT[:, kc, :], rhs=w_b[:, kc, half * 512:(half + 1) * 512],
                                       start=(kc == 0), stop=(kc == 3))
                return ins
            S.op("pe", lambda e: proj4(e, rT, wro_b, 2), r=["rT", "wro_b"], w=[bk(2), bk(3)])
            S.op("pe", lambda e: proj4(e, aT, wao_b, 4), r=["aT", "wao_b"], w=[bk(4), bk(5)])
            for half in range(2):
                hs_ = slice(half * 512, (half + 1) * 512)
                S.op("dve", lambda e, half=half, hs_=hs_: e.tensor_tensor(out=tmp1[:, hs_], in0=banks[2 + half][:],
                                                                          in1=sg_t[:, hs_], op=ALU.mult),
                     r=["sg_t"], w=[bk(2 + half), ("tmp1h", half)])
                S.op("dve", lambda e, half=half, hs_=hs_: e.tensor_tensor(
                    out=h2[:, hs_], in0=banks[4 + half][:], in1=sg_t[:, 1024 + half * 512:1024 + (half + 1) * 512],
                    op=ALU.mult), r=["sg_t"], w=[bk(4 + half), ("h2h", half)])
            S.op("dve", lambda e: e.tensor_tensor(out=mixin, in0=tmp1[:], in1=h2[:], op=ALU.add),
                 r=[("tmp1h", 0), ("tmp1h", 1), ("h2h", 0), ("h2h", 1)], w=["bufB", "tmp1", "h2"])
            S.op("pe", lambda e: tr_n(e, mixin, bf_banks[0], 8), r=["bufB", "ident_b"], w=[bk(0)])
            S.op("act", lambda e: e.activation(out=mixT.rearrange("p k t -> p (k t)"), in_=bf_banks[0][:, 0:1024], func=AF.Copy),
                 r=[], w=[bk(0), "mixT"])

            def proj8(e, srcT, w_b, b0, nb_):
                for nb in range(nb_):
                    for kc in range(8):
                        ins = e.matmul(banks[b0 + nb][:], lhsT=srcT[:, kc, :], rhs=w_b[:, kc, nb * 512:(nb + 1) * 512],
                                       start=(kc == 0), stop=(kc == 7))
                return ins
            S.op("pe", lambda e: proj8(e, mixT, wout_b, 6, 2), r=["mixT", ("wout_b", 0), ("wout_b", 1)], w=[bk(6), bk(7)])
            for half in range(2):
                hs_ = slice(half * 512, (half + 1) * 512)
                S.op("dve", lambda e, half=half, hs_=hs_: e.tensor_tensor(out=h2[:, hs_], in0=banks[6 + half][:],
                                                                          in1=bout_t[:, hs_], op=ALU.add),
                     r=["bout_t", "h2"], w=[bk(6 + half), ("h2h", half)])
            S.op("dve", lambda e: e.tensor_tensor(out=h2[:], in0=h2[:], in1=modD[:, i, 0, :], op=ALU.mult),
                 r=[("h2h", 0), ("h2h", 1), ("modD", i, 0)], w=["h2"])
            S.op("dve", lambda e: e.scalar_tensor_tensor(out=x_t[:], in0=x_t[:], scalar=ALPHA, in1=h2[:],
                                                         op0=ALU.mult, op1=ALU.add), r=["x_t", "h2"], w=["x_t"])
            layer_norm(x_t, "x_t", "ln1_g", "ln1_b", x1, "x1")
            S.op("dve", lambda e: e.tensor_tensor(out=h2[:], in0=x1[:], in1=modD[:, i, 2, :], op=ALU.mult),
                 r=["x1", ("modD", i, 2)], w=["h2"])
            S.op("dve", lambda e: e.tensor_tensor(out=h2b[:], in0=h2[:], in1=modD[:, i, 1, :], op=ALU.add),
                 r=["h2", ("modD", i, 1)], w=["h2b"])
            S.op("pe", lambda e: tr_n(e, h2b, bf_banks[1], 8), r=["h2b", "ident_b"], w=[bk(1)])
            S.op("act", lambda e: e.activation(out=h2T[:].rearrange("p k t -> p (k t)"), in_=bf_banks[1][:, 0:1024], func=AF.Copy),
                 r=[], w=[bk(1), "h2T"])
            S.op("pe", lambda e: proj8(e, h2T, wq_b, 2, 4), r=["h2T"] + wqkeys, w=[bk(2), bk(3), bk(4), bk(5)])
            for nb in range(4):
                if nb % 2 == 0:
                    S.op("act", lambda e, nb=nb: e.activation(out=qpe[:, nb * 512:(nb + 1) * 512], in_=banks[2 + nb][:], func=AF.Copy),
                         r=["mixT"], w=[bk(2 + nb), ("qpe", nb)])
                else:
                    S.op("dve", lambda e, nb=nb: e.tensor_copy(out=qpe[:, nb * 512:(nb + 1) * 512], in_=banks[2 + nb][:]),
                         r=["mixT"], w=[bk(2 + nb), ("qpe", nb)])
            for half in range(2):
                def trq(e, half=half):
                    for j in range(8):
                        hs = half * 8 + j
                        ins = e.transpose(out=bf_banks[half][:, j * 128:(j + 1) * 128], in_=qpe[:, hs * 128:(hs + 1) * 128],
                                          identity=ident_b[:])
                    return ins
                S.op("pe", trq, r=[("qpe", 2 * half), ("qpe", 2 * half + 1), "ident_b"], w=[bk(half)])
                if half == 0:
                    S.op("act", lambda e: e.activation(out=qpT[:, 0:8, :].rearrange("p k t -> p (k t)"), in_=bf_banks[0][:, 0:1024],
                                                       func=AF.Copy), r=["mixT"], w=[bk(0), ("qpT", 0)])
                else:
                    S.op("dve", lambda e: e.tensor_copy(out=qpT[:, 8:16, :].rearrange("p k t -> p (k t)"), in_=bf_banks[1][:, 0:1024]),
                         r=["mixT"], w=[bk(1), ("qpT", 1)])
            for q4 in range(4):
                def scq(e, q4=q4):
                    for a in range(4):
                        hs = q4 * 4 + a
                        ins = e.matmul(banks[2 + q4][:, a * 128:(a + 1) * 128], lhsT=qpT[:, hs, :], rhs=keysT[:, hs, :],
                                       start=True, stop=True)
                    return ins
                S.op("pe", scq, r=[("qpT", q4 // 2), ("keysT", q4)], w=[bk(2 + q4)])
                if q4 % 2 == 0:
                    S.op("act", lambda e, q4=q4: e.activation(out=bufA[:, q4 * 512:(q4 + 1) * 512], in_=banks[2 + q4][:], func=AF.Copy),
                         r=["sg_t"], w=[bk(2 + q4), ("s_sc", q4)])
                else:
                    S.op("dve", lambda e, q4=q4: e.tensor_copy(out=bufA[:, q4 * 512:(q4 + 1) * 512], in_=banks[2 + q4][:]),
                         r=["sg_t"], w=[bk(2 + q4), ("s_sc", q4)])
            ssk = [("s_sc", q4) for q4 in range(4)]

            def topk_rounds(n, vals, vk, outv, outvk, outi, outik):
                for rnd in range(2):
                    sl = slice(rnd * 8, rnd * 8 + 8)
                    def mx(e, sl=sl):
                        for j in range(n):
                            ins = e.max(out=outv[:, j, sl], in_=vals[:, j, :])
                        return ins
                    S.op("dve", mx, r=vk, w=[(outvk, rnd)])
                    def mi(e, sl=sl):
                        for j in range(n):
                            ins = e.max_index(out=outi[:, j, sl], in_max=outv[:, j, sl], in_values=vals[:, j, :])
                        return ins
                    S.op("dve", mi, r=vk + [(outvk, rnd)], w=[(outik, rnd)])
                    if rnd == 0:
                        def mr(e, sl=sl):
                            for j in range(n):
                                ins = e.match_replace(out=vals[:, j, :], in_to_replace=outv[:, j, sl], in_values=vals[:, j, :],
                                                      imm_value=-1e30)
                            return ins
                        S.op("dve", mr, r=[(outvk, rnd), (outik, rnd)], w=vk)
            topk_rounds(16, s_sc, ssk, sv, "sv", si, "si")
            sv4 = sv[:].rearrange("p (h s) k -> p h s k", s=2)
            S.op("dve", lambda e: e.tensor_tensor(
                out=cand4, in0=sv4[:, :, 0, :].unsqueeze(3).to_broadcast([128, 8, 16, 16]),
                in1=sv4[:, :, 1, :].unsqueeze(2).to_broadcast([128, 8, 16, 16]), op=ALU.add),
                r=[("sv", 0), ("sv", 1), ("qpe", 0), ("qpe", 1), ("qpe", 2), ("qpe", 3), ("qpT", 0), ("qpT", 1)], w=["cand"])
            topk_rounds(8, cand, ["cand"], cv, "cv", ci, "ci")
            cvk = [("cv", 0), ("cv", 1)]; cik = [("ci", 0), ("ci", 1)]
            S.op("dve", lambda e: e.tensor_tensor(out=gsm[:], in0=cv[:], in1=bcast_last(cv[:, :, 0], 16), op=ALU.subtract),
                 r=cvk, w=["gsm"])
            S.op("act", lambda e: e.activation(out=gsm[:], in_=gsm[:], func=AF.Exp), r=["gsm"], w=["gsm"])
            S.op("dve", lambda e: e.tensor_reduce(out=ssm[:], in_=gsm[:], axis=AX.X, op=ALU.add), r=["gsm"], w=["ssm"])
            S.op("dve", lambda e: e.reciprocal(out=ssm[:], in_=ssm[:]), r=["ssm"], w=["ssm"])
            S.op("dve", lambda e: e.tensor_tensor(out=gsm[:], in0=gsm[:], in1=bcast_last(ssm[:, :], 16), op=ALU.mult),
                 r=["gsm", "ssm"], w=["gsm"])
            S.op("dve", lambda e: e.tensor_single_scalar(out=hi_u[:], in_=ci[:], scalar=4, op=ALU.logical_shift_right),
                 r=cik, w=["hi_u"])
            S.op("dve", lambda e: e.tensor_single_scalar(out=lo_u[:], in_=ci[:], scalar=15, op=ALU.bitwise_and),
                 r=cik, w=["lo_u"])
            S.op("dve", lambda e: e.tensor_copy(out=hi_f[:], in_=hi_u[:]), r=["hi_u"], w=["hi_f"])
            S.op("dve", lambda e: e.tensor_copy(out=lo_f[:], in_=lo_u[:]), r=["lo_u"], w=["lo_f"])
            S.op("dve", lambda e: e.tensor_copy(out=sif[:], in_=si[:]), r=[("si", 0), ("si", 1)], w=["sif"])
            sif4 = sif[:].rearrange("p (h s) k -> p h s k", s=2)
            iot4 = iota16[:, :].unsqueeze(1).unsqueeze(1).to_broadcast([128, 8, 16, 16])
            for side, (xf, xk, dsti, dstk) in enumerate(((hi_f, "hi_f", i0, "i0"), (lo_f, "lo_f", i1, "i1"))):
                S.op("dve", lambda e, xf=xf: e.tensor_tensor(
                    out=oh4, in0=iot4, in1=xf[:].unsqueeze(3).to_broadcast([128, 8, 16, 16]), op=ALU.is_equal),
                    r=[xk, "iota16"] + ssk, w=["oh4"])
                S.op("dve", lambda e, side=side: e.tensor_tensor(
                    out=prod4, in0=oh4, in1=sif4[:, :, side, :].unsqueeze(2).to_broadcast([128, 8, 16, 16]), op=ALU.mult),
                    r=["oh4", "sif", "cand"], w=["prod4"])
                S.op("dve", lambda e, dsti=dsti: e.tensor_reduce(out=dsti[:], in_=prod4, axis=AX.X, op=ALU.add),
                     r=["prod4"], w=[dstk])
            S.op("dve", lambda e: e.scalar_tensor_tensor(out=e_f[:].rearrange("p (h k) -> p h k", h=8), in0=i0[:], scalar=128.0,
                                                         in1=i1[:], op0=ALU.mult, op1=ALU.add), r=["i0", "i1"], w=["e_f"])
            S.op("dve", lambda e: e.tensor_copy(out=e_i[:], in_=e_f[:]), r=["e_f"], w=["e_i"])
            for hk in range(128):
                sl_ = gslot["u"] % NSL; gslot["u"] += 1
                S.dma("pool", lambda e, sl_=sl_, hk=hk: e.indirect_dma_start(
                    out=Ug[sl_][:], out_offset=None, in_=peer_u,
                    in_offset=bass.IndirectOffsetOnAxis(ap=e_i[:, hk:hk + 1], axis=0)), r=["e_i"], w=[f"Ug{sl_}"])
                S.op("dve", lambda e, sl_=sl_, hk=hk: e.scalar_tensor_tensor(
                    out=junk[:], in0=Ug[sl_][:], scalar=1.0, in1=h2b[:], op0=ALU.mult, op1=ALU.mult,
                    accum_out=a_t[:, hk:hk + 1]), r=[f"Ug{sl_}", "h2b"], w=["junk", ("a_t", hk)])
            S.op("act", lambda e: e.activation(out=a_t[:], in_=a_t[:], func=AF.Gelu), r=[("a_t", hk) for hk in range(128)], w=["a_g"])
            S.op("dve", lambda e: e.tensor_tensor(out=ga[:], in0=a_t[:], in1=gsm[:].rearrange("p h k -> p (h k)"), op=ALU.mult),
                 r=["a_g", "gsm"], w=["ga"])
            for h in range(8):
                dg = Dg[h % 2]; dgk = f"Dg{h % 2}"
                S.op("dve", lambda e, dg=dg, h=h: e.tensor_tensor(
                    out=dg[:], in0=ident_b[:, :].unsqueeze(1).to_broadcast([128, 16, 128]),
                    in1=ga[:, h * 16:(h + 1) * 16].unsqueeze(2).to_broadcast([128, 16, 128]), op=ALU.mult),
                    r=["ga", "ident_b"], w=[dgk])
                for k in range(16):
                    hk = h * 16 + k
                    sl_ = gslot["v"] % NSL; gslot["v"] += 1
                    S.dma("pool", lambda e, sl_=sl_, hk=hk: e.indirect_dma_start(
                        out=Vg[sl_][:], out_offset=None, in_=peer_v,
                        in_offset=bass.IndirectOffsetOnAxis(ap=e_i[:, hk:hk + 1], axis=0)), r=["e_i"], w=[f"Vg{sl_}"])
                    def vmm(e, sl_=sl_, hk=hk, dg=dg, k=k):
                        for half in range(2):
                            ins = e.matmul(banks[6 + half][:], lhsT=dg[:, k, :], rhs=Vg[sl_][:, half * 512:(half + 1) * 512],
                                           start=(hk == 0), stop=(hk == 127))
                        return ins
                    S.op("pe", vmm, r=[f"Vg{sl_}", dgk], w=[bk(6), bk(7)])
            for half in range(2):
                hs_ = slice(half * 512, (half + 1) * 512)
                S.op("dve", lambda e, half=half, hs_=hs_: e.tensor_tensor(out=h2[:, hs_], in0=banks[6 + half][:],
                                                                          in1=modD[:, i, 3, hs_], op=ALU.mult),
                     r=[("modD", i, 3), "h2"], w=[bk(6 + half), ("h2h", half)])
            S.op("dve", lambda e: e.scalar_tensor_tensor(out=x1[:], in0=x1[:], scalar=ALPHA, in1=h2[:],
                                                         op0=ALU.mult, op1=ALU.add), r=["x1", ("h2h", 0), ("h2h", 1)], w=["x1", "h2"])
            layer_norm(x1, "x1", "ln2_g", "ln2_b", x_t, "x_t")
            S.dma("sp", lambda e: e.dma_start(out=ydst, in_=x_t[:]), r=["x_t"], w=[("y", t)])

    S.finish()
    return nc, es, S


def _shard_inputs(inp):
    c = _consts()
    maps = []
    f = np.ascontiguousarray
    for i in range(NCORES):
        sl = slice(i * NSEQ_S, (i + 1) * NSEQ_S)
        m = {
            "xp": f(inp["x_prompt"][i]),
            "xs": f(inp["x_sample"][sl].reshape(128, D)),
            "cp_rep": f(np.broadcast_to(inp["c_prompt"][i:i + 1], (128, D))),
            "cs_rep": f(np.repeat(inp["c_sample"][sl], TS, axis=0)),
            "st_in": f(inp["state_ret"][0, sl]),
            "c_in0": f(inp["cache_att_w128"][0, sl].reshape(NSEQ_S, 128, 2, 512)),
            "c_in1": f(inp["cache_att_w512"][0, sl].reshape(NSEQ_S, 512, 2, 512)),
            "c_in2": f(inp["cache_att_w2048"][0, sl].reshape(NSEQ_S, 2048, 2, 512)),
            "w_ada": f(inp["w_ada"][0]),
            "b_ada": f(inp["b_ada"][0:1]),
            "w_in": f(inp["w_in"][0]),
            "ident": c["ident"],
            "rotc": c["rotc"], "rots": c["rots"],
            "intraT_p": c["intraT_p"], "intraT_s": c["intraT_s"],
            "qdec_p": c["qdec_p"], "qdec_s": c["qdec_s"],
            "kdec_p": c["kdec_p"], "kdec_s": c["kdec_s"],
            "rowmask": c["rowmask"],
            "iota16": c["iota16"],
            "oh0": c["oh0"], "oh1": c["oh1"], "oh2": c["oh2"],
            "rel_bias": f(inp["rel_bias"]),
            "w_ret_o": f(inp["w_ret_o"][0]), "w_att_o": f(inp["w_att_o"][0]), "w_out": f(inp["w_out"][0]),
            "b_out_rep": f(np.broadcast_to(inp["b_out"][0:1], (128, D))),
            "ln1_g_rep": f(np.broadcast_to(inp["ln1_g"][0:1], (128, D))),
            "ln1_b_rep": f(np.broadcast_to(inp["ln1_b"][0:1], (128, D))),
            "ln2_g_rep": f(np.broadcast_to(inp["ln2_g"][0:1], (128, D))),
            "ln2_b_rep": f(np.broadcast_to(inp["ln2_b"][0:1], (128, D))),
            "peer_wq": f(inp["peer_wq"][0]), "peer_keys": f(inp["peer_keys"][0].reshape(2048, 128)),
            "peer_u": f(inp["peer_u"][0]), "peer_v": f(inp["peer_v"][0]),
        }
        maps.append(m)
    return maps


def kernel(**inputs):
    inp = {k: np.asarray(v) for k, v in inputs.items()}
    nc, es, S = build_program()
    with es:
        maps = _shard_inputs(inp)
        res = run_bass_kernel_spmd(nc, maps, core_ids=list(range(NCORES)))
    R = res.results
    yp = np.stack([R[i]["yp"] for i in range(NCORES)], 0)
    ys = np.concatenate([R[i]["ys"].reshape(NSEQ_S, TS, D) for i in range(NCORES)], 0)
    srp = np.stack([R[i]["srp"] for i in range(NCORES)], 0)[None]
    srs = np.concatenate([R[i]["srs"] for i in range(NCORES)], 0)[None]
    cpo = [np.stack([R[i][f"cpo{g}"] for i in range(NCORES)], 0).reshape(1, NCORES, GROUPS[g][0], 2, 8, 64)
           for g in range(3)]
    cso = [np.concatenate([R[i][f"cso{g}"] for i in range(NCORES)], 0).reshape(
        1, NCORES * NSEQ_S, GROUPS[g][0], 2, 8, 64) for g in range(3)]
    return (yp.astype(np.float32), ys.astype(np.float32), srp, cpo[0], cpo[1], cpo[2], srs, cso[0], cso[1], cso[2])
```

```python
import math
from contextlib import ExitStack

import numpy as np
import concourse.bass as bass
import concourse.mybir as mybir
from concourse.bass_utils import run_bass_kernel_spmd

F32 = mybir.dt.float32
BF16 = mybir.dt.bfloat16
U32 = mybir.dt.uint32
I32 = mybir.dt.int32
AF = mybir.ActivationFunctionType
ALU = mybir.AluOpType
AX = mybir.AxisListType

NCORES = 8
D = 1024
SEQ = 4096
NPT = 32
NT = 33
NTOK = NT * 128
NSEQ_S = 16
TS = 8
PAST = 8192
IN_COLS = 8704
GROUPS = ((128, 1), (512, 4), (2048, 16))
ALPHA = 2.0 ** 0.25
LN_EPS = 1e-5
HN_EPS = 1e-6
NEGB = -30000.0
SEM_LIMIT = 30000


class Sched:
    def __init__(self, nc, es):
        self.nc = nc
        self.es = es
        self.eng = {"pe": nc.tensor, "act": nc.scalar, "dve": nc.vector, "pool": nc.gpsimd, "sp": nc.sync}
        self.sem = {}
        self.cnt = {}
        self.nsem = 0
        for e in ("pe", "act", "dve", "pool"):
            self._new_engine_sem(e)
        self.known = {e: {} for e in self.eng}
        self.bufs = {}
        self.dpool = {}
        self.drr = {}
        for q, n in (("sp", 24), ("pool", 16), ("act", 8)):
            self.dpool[q] = [self._new_dma_slot() for _ in range(n)]
            self.drr[q] = 0
        self.ninstr = 0

    def _mksem(self, name):
        self.nsem += 1
        return self.es.enter_context(self.nc.semaphore(f"{name}_{self.nsem}"))

    def _new_engine_sem(self, e):
        self.sem[e] = self._mksem("c" + e)
        self.cnt[e] = 0

    def _new_dma_slot(self):
        return {"sem": self._mksem("d"), "val": 0}

    def _wait(self, e, ev):
        sem, val = ev
        k = self.known[e]
        if k.get(id(sem), 0) >= val:
            return
        self.eng[e].wait_ge(sem, val)
        self.ninstr += 1
        k[id(sem)] = val

    def _deps(self, r, w):
        deps = []
        for key in r:
            b = self.bufs.get(key)
            if b is not None and b["w"] is not None:
                deps.append(b["w"])
        for key in w:
            b = self.bufs.get(key)
            if b is not None:
                if b["w"] is not None:
                    deps.append(b["w"])
                deps.extend(b["r"].values())
        return deps

    def _record(self, ev, r, w):
        sem, val = ev
        for key in r:
            b = self.bufs.setdefault(key, {"w": None, "r": {}})
            old = b["r"].get(id(sem))
            if old is None or old[1] < val:
                b["r"][id(sem)] = ev
        for key in w:
            self.bufs[key] = {"w": ev, "r": {}}

    def op(self, e, fn, r=(), w=()):
        deps = self._deps(r, w)
        own = self.sem[e]
        for ev in deps:
            if e == "pe" and ev[0] is own:
                continue
            self._wait(e, ev)
        ins = fn(self.eng[e])
        if self.cnt[e] >= SEM_LIMIT:
            self._new_engine_sem(e)
        self.cnt[e] += 1
        ins.then_inc(self.sem[e], 1)
        self.ninstr += 1
        ev = (self.sem[e], self.cnt[e])
        self._record(ev, r, w)
        return ev

    def dma(self, q, fn, r=(), w=()):
        deps = self._deps(r, w)
        pool = self.dpool[q]
        i = self.drr[q]
        self.drr[q] = (i + 1) % len(pool)
        slot = pool[i]
        if slot["val"] >= SEM_LIMIT:
            slot = pool[i] = self._new_dma_slot()
        if slot["val"] > 0:
            self._wait(q, (slot["sem"], slot["val"]))
        for ev in deps:
            self._wait(q, ev)
        ins = fn(self.eng[q])
        slot["val"] += 16
        ins.then_inc(slot["sem"], 16)
        self.ninstr += 1
        ev = (slot["sem"], slot["val"])
        self._record(ev, r, w)
        return ev

    def barrier(self, e, keys):
        for ev in self._deps((), keys):
            self._wait(e, ev)

    def full_barrier(self):
        evs = [(self.sem[x], self.cnt[x]) for x in ("pe", "act", "dve", "pool") if self.cnt[x] > 0]
        for pool in self.dpool.values():
            for slot in pool:
                if slot["val"] > 0:
                    evs.append((slot["sem"], slot["val"]))
        for e in ("pe", "act", "dve", "pool", "sp"):
            for ev in evs:
                if ev[0] is self.sem.get(e):
                    continue
                self._wait(e, ev)

    def finish(self):
        for q, pool in self.dpool.items():
            for slot in pool:
                if slot["val"] > 0:
                    self._wait("sp", (slot["sem"], slot["val"]))


def _t5_bucket(dist):
    d = dist.astype(np.float32)
    large = 16 + (np.log(np.maximum(d, 1.0) / 16) / math.log(2048 / 16) * 16)
    large = np.minimum(large.astype(np.int32), 31)
    return np.where(dist < 16, dist, large)


_CONST_CACHE = {}


def _consts():
    if _CONST_CACHE:
        return _CONST_CACHE
    c = _CONST_CACHE
    c["ident"] = np.eye(128, dtype=np.float32)
    pos = np.concatenate([np.arange(SEQ), PAST + (np.arange(128) % TS)]).astype(np.float32)
    inv = (10000.0 ** (-np.arange(64, dtype=np.float32) / 64)).astype(np.float32)
    ang = (pos[:, None] * inv[None, :]).astype(np.float32)
    cos = np.cos(ang).astype(np.float32); sin = np.sin(ang).astype(np.float32)
    c["rotc"] = np.concatenate([cos, cos], 1)
    c["rots"] = np.concatenate([-sin, sin], 1)
    lg = np.log1p(-np.exp2(-5.0 - np.arange(4, dtype=np.float64)))
    p = np.arange(128)
    for name, C in (("p", 128), ("s", TS)):
        tpos = p % C
        seq = p // C
        rel = tpos[None, :] - tpos[:, None]
        ok = (rel >= 0) & (seq[None, :] == seq[:, None])
        intra = np.where(ok[:, None, :], np.exp(lg[None, :, None] * np.maximum(rel, 0)[:, None, :]), 0.0)
        c["intraT_" + name] = intra.reshape(128, 512).astype(np.float32)
        qd = np.exp(lg[:, None] * (tpos[None, :] + 1.0))
        c["qdec_" + name] = np.broadcast_to(qd.reshape(1, 512), (128, 512)).astype(np.float32).copy()
        c["kdec_" + name] = np.exp(lg[None, :] * (C - 1.0 - tpos[:, None])).astype(np.float32)
        c["cdec_" + name] = [float(v) for v in np.exp(lg * C)]
    c["rowmask"] = (p[:, None] // TS == np.arange(NSEQ_S)[None, :]).astype(np.float32)
    c["iota16"] = np.broadcast_to(np.arange(16, dtype=np.float32)[None, :], (128, 16)).copy()
    for g, (win, dil) in enumerate(GROUPS):
        oh = np.zeros((33, 385), np.float32)
        for m in range(385):
            rel = m - 128
            if 0 <= rel <= 128:
                oh[int(_t5_bucket(np.array([rel * dil]))[0]), m] = 1.0
            else:
                oh[32, m] = NEGB
        c[f"oh{g}"] = oh
    return c


def build_program(stop=99, nocopy=False, cbs=None, nocso=False, tiles=None):
    nc = bass.Bass("TRN2", target_bir_lowering=False)
    es = ExitStack()
    S = Sched(nc, es)
    CST = _consts()

    def din(name, shape, dt=F32):
        return nc.dram_tensor(name, list(shape), dt, kind="ExternalInput").ap()

    def dout(name, shape, dt=F32):
        return nc.dram_tensor(name, list(shape), dt, kind="ExternalOutput").ap()

    def dscr(name, shape, dt):
        return nc.dram_tensor(name, list(shape), dt).ap()

    def sb(name, shape, dt):
        return es.enter_context(nc.sbuf_tensor("s_" + name, list(shape), dt))

    xp = din("xp", [SEQ, D]); xs = din("xs", [128, D])
    cp_rep = din("cp_rep", [128, D]); cs_rep = din("cs_rep", [128, D])
    st_in = din("st_in", [NSEQ_S, 4, 128, 128])
    c_in = [din(f"c_in{g}", [NSEQ_S, GROUPS[g][0], 2, 512]) for g in range(3)]
    w_ada = din("w_ada", [D, 6 * D]); b_ada = din("b_ada", [1, 6 * D])
    w_in = din("w_in", [D, IN_COLS])
    ident_d = din("ident", [128, 128])
    rotc_d = din("rotc", [NTOK, 128]); rots_d = din("rots", [NTOK, 128])
    intra_d = [din("intraT_p", [128, 512]), din("intraT_s", [128, 512])]
    qdec_d = [din("qdec_p", [128, 512]), din("qdec_s", [128, 512])]
    kdec_d = [din("kdec_p", [128, 4]), din("kdec_s", [128, 4])]
    rowmask_d = din("rowmask", [128, NSEQ_S])
    iota16_d = din("iota16", [128, 16])
    oh_d = [din(f"oh{g}", [33, 385]) for g in range(3)]
    rel_bias_d = din("rel_bias", [32, 24])
    w_ret_o = din("w_ret_o", [512, D]); w_att_o = din("w_att_o", [512, D]); w_out = din("w_out", [D, D])
    b_out_rep = din("b_out_rep", [128, D])
    ln_rep = {k: din(k + "_rep", [128, D]) for k in ("ln1_g", "ln1_b", "ln2_g", "ln2_b")}
    peer_wq = din("peer_wq", [D, 2048]); peer_keys = din("peer_keys", [2048, 128])
    peer_u = din("peer_u", [16384, D]); peer_v = din("peer_v", [16384, D])

    yp = dout("yp", [SEQ, D]); ys = dout("ys", [128, D])
    srp = dout("srp", [4, 128, 128]); srs = dout("srs", [NSEQ_S, 4, 128, 128])
    cpo = [dout(f"cpo{g}", [GROUPS[g][0], 2, 512]) for g in range(3)]
    cso = [dout(f"cso{g}", [NSEQ_S, GROUPS[g][0], 2, 512]) for g in range(3)]

    P = dscr("proj", [NTOK, IN_COLS], BF16)
    RG = dscr("retg", [NTOK, 512], BF16)
    ATT = dscr("attacc", [NTOK, 520], F32)
    EXT = dscr("biasext", [24, 385], F32)
    UB = dscr("peer_u_bf", [16384, D], BF16)
    VB = dscr("peer_v_bf", [16384, D], BF16)
    EXT2 = dscr("biasext2", [24, 128 * 385], F32)

    banks = [es.enter_context(nc.psum_tensor(f"bank{i}", [128, 512], F32)) for i in range(8)]

    def bk(i):
        return f"bank{i}"

    ident_f = sb("ident_f", [128, 128], F32)
    ident_b = sb("ident_b", [128, 128], BF16)
    ones1 = sb("ones1", [1, 128], F32)
    modD = sb("modD", [128, 2, 4, D], F32)
    mod_stack = ExitStack()
    modp = mod_stack.enter_context(nc.sbuf_tensor("s_modp", [128, 6 * D], F32))
    mods = mod_stack.enter_context(nc.sbuf_tensor("s_mods", [128, 6 * D], F32))

    S.dma("sp", lambda e: e.dma_start(out=ident_f[:], in_=ident_d), w=["ident_f"])
    S.op("dve", lambda e: e.tensor_copy(out=ident_b[:], in_=ident_f[:]), r=["ident_f"], w=["ident_b"])
    S.op("dve", lambda e: e.memset(ones1[:], 1.0), w=["ones1"])

    tabkeys = []
    for name, src_t, dst_t in (("UB", peer_u, UB), ("VB", peer_v, VB)):
        for c in range(8):
            rs_ = slice(c * 2048, (c + 1) * 2048)
            S.dma("pool", lambda e, src_t=src_t, dst_t=dst_t, rs_=rs_: e.dma_start(out=dst_t[rs_, :], in_=src_t[rs_, :]),
                  w=[(name, c)])
            tabkeys.append((name, c))
    ubkeys = [k for k in tabkeys if k[0] == "UB"]
    vbkeys = [k for k in tabkeys if k[0] == "VB"]
    for g in range(0 if not nocopy else 3, 3):
        nb = GROUPS[g][0]
        for b in range(NSEQ_S):
            src = c_in[g][b, TS:nb].rearrange("(a r) k c -> a (r k c)", r=8)
            dst = cso[g][b, 0:nb - TS].rearrange("(a r) k c -> a (r k c)", r=8)
            S.dma("act", lambda e, s=src, d=dst: e.dma_start(out=d, in_=s), w=[("cso_copy", g, b)])

    with ExitStack() as ph:
        def psb(name, shape, dt):
            return ph.enter_context(nc.sbuf_tensor("s_" + name, list(shape), dt))
        c_tok = psb("c_tok", [128, D], F32)
        c_act = psb("c_act", [128, D], F32)
        cT = [psb(f"cT{i}", [128, 8, 128], F32) for i in range(2)]
        wada_t = [psb(f"wada{i}", [128, 8, 512], F32) for i in range(2)]
        bada_t = psb("bada", [1, 6 * D], F32)
        S.dma("sp", lambda e: e.dma_start(out=bada_t[:], in_=b_ada), w=["bada"])
        for i, src in enumerate((cp_rep, cs_rep)):
            S.dma("sp", lambda e, s=src: e.dma_start(out=c_tok[:], in_=s), w=["c_tok"])
            S.op("act", lambda e: e.activation(out=c_act[:], in_=c_tok[:], func=AF.Silu), r=["c_tok"], w=["c_act"])
            for half in range(2):
                def tr4(e, half=half):
                    for j in range(4):
                        kc = half * 4 + j
                        ins = e.transpose(out=banks[half][:, j * 128:(j + 1) * 128],
                                          in_=c_act[:, kc * 128:(kc + 1) * 128], identity=ident_f[:])
                    return ins
                S.op("pe", tr4, r=["c_act", "ident_f"], w=[bk(half)])
                S.op("dve", lambda e, half=half, i=i: e.tensor_copy(
                    out=cT[i][:, half * 4:(half + 1) * 4, :],
                    in_=banks[half][:].rearrange("p (a b) -> p a b", a=4)), r=[bk(half)], w=[f"cT{i}"])
        wv = w_ada.rearrange("(kc p) n -> p kc n", p=128)
        for n in range(12):
            wt = wada_t[n % 2]
            wk = f"wada{n % 2}"
            S.dma("sp", lambda e, wt=wt, n=n: e.dma_start(out=wt[:], in_=wv[:, :, n * 512:(n + 1) * 512]), w=[wk])
            for i, mod in enumerate((modp, mods)):
                b_ = 2 + i
                def mm(e, b_=b_, n=n, wt=wt, i=i):
                    e.matmul(banks[b_][:], lhsT=ones1[0:1, :], rhs=bada_t[0:1, n * 512:(n + 1) * 512],
                             start=True, stop=False)
                    for kc in range(8):
                        ins = e.matmul(banks[b_][:], lhsT=cT[i][:, kc, :], rhs=wt[:, kc, :],
                                       start=False, stop=(kc == 7))
                    return ins
                S.op("pe", mm, r=["ones1", "bada", f"cT{i}", wk], w=[bk(b_)])
                S.op("act", lambda e, b_=b_, mod=mod, n=n: e.activation(
                    out=mod[:, n * 512:(n + 1) * 512], in_=banks[b_][:], func=AF.Copy),
                    r=[bk(b_)], w=[("mod", i, n)])
        for i, mod in enumerate((modp, mods)):
            for j in (1, 4):
                S.op("dve", lambda e, mod=mod, j=j: e.tensor_scalar_add(
                    out=mod[:, j * D:(j + 1) * D], in0=mod[:, j * D:(j + 1) * D], scalar1=1.0),
                    r=[], w=[("mod", i, 2 * j), ("mod", i, 2 * j + 1)])

    for i, mod in enumerate((modp, mods)):
        for jj, j in enumerate((2, 3, 4, 5)):
            S.op("dve" if jj % 2 == 0 else "act",
                 (lambda e, i=i, jj=jj, j=j, mod=mod: e.tensor_copy(out=modD[:, i, jj, :], in_=mod[:, j * D:(j + 1) * D]))
                 if jj % 2 == 0 else
                 (lambda e, i=i, jj=jj, j=j, mod=mod: e.activation(out=modD[:, i, jj, :], in_=mod[:, j * D:(j + 1) * D],
                                                                  func=AF.Copy)),
                 r=[("mod", i, 2 * j), ("mod", i, 2 * j + 1)], w=[("modD", i, jj)])
    S.full_barrier()
    if stop <= 0:
        S.finish()
        return nc, es, S

    def modkeys(i, j):
        return [("mod", i, 2 * j), ("mod", i, 2 * j + 1)]

    hT_stack = ExitStack()
    hT = hT_stack.enter_context(nc.sbuf_tensor("hT", [128, 8, NTOK], BF16))
    with ExitStack() as ph:
        def psb(name, shape, dt):
            return ph.enter_context(nc.sbuf_tensor("s_" + name, list(shape), dt))
        xt = [psb(f"xt{i}", [128, D], F32) for i in range(2)]
        ht = [psb(f"ht{i}", [128, D], F32) for i in range(2)]
        for t in range(NT):
            i = 0 if t < NPT else 1
            mod = modp if t < NPT else mods
            src = xp[t * 128:(t + 1) * 128, :] if t < NPT else xs
            x_ = xt[t % 2]; h_ = ht[t % 2]
            xk = f"xt{t % 2}"; hk = f"ht{t % 2}"
            S.dma("sp", lambda e, x_=x_, src=src: e.dma_start(out=x_[:], in_=src), w=[xk])
            S.op("dve", lambda e, x_=x_, h_=h_, mod=mod: e.tensor_tensor(
                out=h_[:], in0=x_[:], in1=mod[:, D:2 * D], op=ALU.mult), r=[xk] + modkeys(i, 1), w=[hk])
            S.op("dve", lambda e, h_=h_, mod=mod: e.tensor_tensor(
                out=h_[:], in0=h_[:], in1=mod[:, 0:D], op=ALU.add), r=[hk] + modkeys(i, 0), w=[hk])
            for half in range(2):
                b_ = (t % 2) * 2 + half
                def tr4(e, b_=b_, half=half, h_=h_):
                    for j in range(4):
                        kc = half * 4 + j
                        ins = e.transpose(out=banks[b_][:, j * 128:(j + 1) * 128],
                                          in_=h_[:, kc * 128:(kc + 1) * 128], identity=ident_f[:])
                    return ins
                S.op("pe", tr4, r=[hk, "ident_f"], w=[bk(b_)])
                eng = "act" if half == 0 else "dve"
                if eng == "act":
                    S.op("act", lambda e, b_=b_, half=half, t=t: e.activation(
                        out=hT[:, half * 4:(half + 1) * 4, t * 128:(t + 1) * 128],
                        in_=banks[b_][:].rearrange("p (a b) -> p a b", a=4), func=AF.Copy),
                        r=[bk(b_)], w=[("hT", t, half)])
                else:
                    S.op("dve", lambda e, b_=b_, half=half, t=t: e.tensor_copy(
                        out=hT[:, half * 4:(half + 1) * 4, t * 128:(t + 1) * 128],
                        in_=banks[b_][:].rearrange("p (a b) -> p a b", a=4)),
                        r=[bk(b_)], w=[("hT", t, half)])

    S.full_barrier()
    if stop <= 1:
        S.finish()
        return nc, es, S
    with ExitStack() as ph:
        def psb(name, shape, dt):
            return ph.enter_context(nc.sbuf_tensor("s_" + name, list(shape), dt))
        wf = [psb(f"wf{i}", [128, 8, 512], F32) for i in range(2)]
        wb = [psb(f"wb{i}", [128, 8, 512], BF16) for i in range(2)]
        stg = [psb(f"stg{i}", [128, 512], BF16) for i in range(4)]
        stg32 = [psb(f"stg32_{i}", [128, 512], F32) for i in range(2)]
        wv = w_in.rearrange("(kc p) n -> p kc n", p=128)
        it = 0
        i32 = 0
        for cb in (range(17) if cbs is None else cbs):
            wf_ = wf[cb % 2]; wb_ = wb[cb % 2]
            wfk = f"wf{cb % 2}"; wbk = f"wb{cb % 2}"
            S.dma("sp", lambda e, wf_=wf_, cb=cb: e.dma_start(out=wf_[:], in_=wv[:, :, cb * 512:(cb + 1) * 512]),
                  w=[wfk])
            S.op("dve", lambda e, wf_=wf_, wb_=wb_: e.tensor_copy(out=wb_[:, 0:4, :], in_=wf_[:, 0:4, :]),
                 r=[wfk], w=[(wbk, 0)])
            S.op("act", lambda e, wf_=wf_, wb_=wb_: e.activation(out=wb_[:, 4:8, :], in_=wf_[:, 4:8, :], func=AF.Copy),
                 r=[wfk], w=[(wbk, 1)])
            scale = 1.0
            if cb == 1:
                scale = 128.0 ** -0.5
            if 4 <= cb <= 6:
                scale = 0.125
            for t in range(NT):
                b_ = it % 4
                sg = stg[it % 4]; sgk = f"stg{it % 4}"
                it += 1
                def mm(e, b_=b_, t=t, wb_=wb_):
                    for kc in range(8):
                        ins = e.matmul(banks[b_][:], lhsT=hT[:, kc, t * 128:(t + 1) * 128], rhs=wb_[:, kc, :],
                                       start=(kc == 0), stop=(kc == 7))
                    return ins
                S.op("pe", mm, r=[("hT", t, 0), ("hT", t, 1), (wbk, 0), (wbk, 1)], w=[bk(b_)])
                S.op("act", lambda e, b_=b_, sg=sg, scale=scale: e.activation(
                    out=sg[:], in_=banks[b_][:], func=AF.Copy, scale=scale), r=[bk(b_)], w=[sgk])
                S.dma("sp", lambda e, sg=sg, t=t, cb=cb: e.dma_start(
                    out=P[t * 128:(t + 1) * 128, cb * 512:(cb + 1) * 512], in_=sg[:]),
                    r=[sgk], w=[("P", t, cb)])
                if 7 <= cb <= 12:
                    g = (cb - 7) % 3
                    kv = (cb - 7) // 3
                    win = GROUPS[g][0]
                    if t < NPT and t * 128 >= SEQ - win:
                        s32 = stg32[i32 % 2]; s32k = f"stg32_{i32 % 2}"; i32 += 1
                        S.op("act", lambda e, b_=b_, s32=s32: e.activation(out=s32[:], in_=banks[b_][:], func=AF.Copy),
                             r=[bk(b_)], w=[s32k])
                        r0 = t * 128 - (SEQ - win)
                        S.dma("sp", lambda e, s32=s32, g=g, kv=kv, r0=r0: e.dma_start(
                            out=cpo[g][r0:r0 + 128, kv, :], in_=s32[:]), r=[s32k], w=[("cpo", g, kv, t)])
                    if t == NPT and not nocso:
                        s32 = stg32[i32 % 2]; s32k = f"stg32_{i32 % 2}"; i32 += 1
                        S.op("act", lambda e, b_=b_, s32=s32: e.activation(out=s32[:], in_=banks[b_][:], func=AF.Copy),
                             r=[bk(b_)], w=[s32k])
                        for b in range(NSEQ_S):
                            S.dma("sp", lambda e, s32=s32, g=g, kv=kv, b=b, win=win: e.dma_start(
                                out=cso[g][b, win - TS:win, kv, :], in_=s32[b * TS:(b + 1) * TS, :]),
                                r=[s32k], w=[("cso_new", g, kv, b)])

    S.full_barrier()
    hT_stack.close()
    mod_stack.close()
    if stop <= 2:
        S.finish()
        return nc, es, S

    def bcast_mid(ap2d, n):
        return ap2d.unsqueeze(1).to_broadcast([128, n, ap2d.shape[1]])

    def bcast_last(ap2d, n):
        return ap2d.unsqueeze(2).to_broadcast([128, ap2d.shape[1], n])

    def v4(ap2d):
        return ap2d.rearrange("p (h d) -> p h d", h=4)

    with ExitStack() as ph:
        def psb(name, shape, dt):
            return ph.enter_context(nc.sbuf_tensor("s_" + name, list(shape), dt))
        intra_t = [psb(f"intra{i}", [128, 512], F32) for i in range(2)]
        qdec_t = [psb(f"qdec{i}", [128, 512], F32) for i in range(2)]
        kdec_t = [psb(f"kdec{i}", [128, 4], F32) for i in range(2)]
        rowmask_t = psb("rowmask", [128, NSEQ_S], F32)
        for i in range(2):
            S.dma("sp", lambda e, i=i: e.dma_start(out=intra_t[i][:], in_=intra_d[i]), w=[f"intra{i}"])
            S.dma("sp", lambda e, i=i: e.dma_start(out=qdec_t[i][:], in_=qdec_d[i]), w=[f"qdec{i}"])
            S.dma("sp", lambda e, i=i: e.dma_start(out=kdec_t[i][:], in_=kdec_d[i]), w=[f"kdec{i}"])
        S.dma("sp", lambda e: e.dma_start(out=rowmask_t[:], in_=rowmask_d), w=["rowmask"])
        qin = [psb(f"qin{i}", [128, 512], BF16) for i in range(2)]
        kin = [psb(f"kin{i}", [128, 512], BF16) for i in range(2)]
        vin = [psb(f"vin{i}", [128, 512], BF16) for i in range(2)]
        gin = [psb(f"gin{i}", [128, 512], BF16) for i in range(2)]
        rc = [psb(f"rc{i}", [128, 128], F32) for i in range(2)]
        rs = [psb(f"rs{i}", [128, 128], F32) for i in range(2)]
        At = psb("rotA", [128, 512], F32)
        Bt = psb("rotB", [128, 512], F32)
        qr = psb("qr", [128, 512], BF16)
        kr = psb("kr", [128, 512], BF16)
        kd = psb("kd", [128, 512], BF16)
        qT = psb("qT", [128, 4, 128], BF16)
        qdT = psb("qdT", [128, 4, 128], BF16)
        kT = psb("kT", [128, 4, 128], BF16)
        PTr = psb("PTr", [128, 4, 128], BF16)
        Sst = psb("Sst", [128, 4, 128], F32)
        Sb = psb("Sb", [128, 4, 128], BF16)
        o_sb = psb("o_sb", [128, 512], F32)
        osq = psb("osq", [128, 512], F32)
        sil = psb("sil", [128, 512], F32)
        retg = [psb(f"retg{i}", [128, 512], BF16) for i in range(2)]
        ssum = psb("ssum", [128, 4], F32); ssq = psb("ssq", [128, 4], F32)
        mean = psb("mean", [128, 4], F32); msq = psb("msq", [128, 4], F32)
        var = psb("var", [128, 4], F32); rstd = psb("rstd", [128, 4], F32)
        S0f = psb("S0f", [128, NSEQ_S, 4, 128], F32)
        S0b = psb("S0b", [128, NSEQ_S, 4, 128], BF16)
        qdTm = psb("qdTm", [128, NSEQ_S, 4, 128], BF16)
        kdm = psb("kdm", [128, NSEQ_S, 512], BF16)
        Snew = [psb(f"Snew{i}", [128, 4, 128], F32) for i in range(2)]
        S.dma("sp", lambda e: e.dma_start(out=S0f[:], in_=st_in.rearrange("b h k v -> k b h v")), w=["S0f"])
        S.op("act", lambda e: e.activation(out=S0b[:], in_=S0f[:], func=AF.Copy), r=["S0f"], w=["S0b"])
        S.op("dve", lambda e: e.memset(qdTm[:], 0.0), w=["qdTm"])
        bq = banks[0][:].bitcast(BF16)[:, 0:512]
        bkk = banks[1][:].bitcast(BF16)[:, 0:512]

        for t in range(NT):
            i = 0 if t < NPT else 1
            par = t % 2
            cdec = CST["cdec_p"] if i == 0 else CST["cdec_s"]
            rows = slice(t * 128, (t + 1) * 128)
            for name, tl, c0 in (("qin", qin, 0), ("kin", kin, 512), ("vin", vin, 1024), ("gin", gin, 1536)):
                S.dma("sp", lambda e, tl=tl, c0=c0: e.dma_start(out=tl[par][:], in_=P[rows, c0:c0 + 512]),
                      r=[("P", t, c0 // 512)], w=[f"{name}{par}"])
            S.dma("sp", lambda e: e.dma_start(out=rc[par][:], in_=rotc_d[rows, :]), w=[f"rc{par}"])
            S.dma("sp", lambda e: e.dma_start(out=rs[par][:], in_=rots_d[rows, :]), w=[f"rs{par}"])

            def rotary(src, srck, dst, dstk):
                s4 = v4(src[:]); a4 = v4(At[:]); b4 = v4(Bt[:])
                S.op("dve", lambda e: e.tensor_tensor(out=a4, in0=s4, in1=bcast_mid(rc[par][:, :], 4), op=ALU.mult),
                     r=[srck, f"rc{par}"], w=["rotA"])
                S.op("dve", lambda e: e.tensor_tensor(out=b4[:, :, 0:64], in0=s4[:, :, 64:128],
                                                      in1=bcast_mid(rs[par][:, 0:64], 4), op=ALU.mult),
                     r=[srck, f"rs{par}"], w=["rotB0"])
                S.op("dve", lambda e: e.tensor_tensor(out=b4[:, :, 64:128], in0=s4[:, :, 0:64],
                                                      in1=bcast_mid(rs[par][:, 64:128], 4), op=ALU.mult),
                     r=[srck, f"rs{par}"], w=["rotB1"])
                S.op("dve", lambda e: e.tensor_tensor(out=dst[:], in0=At[:], in1=Bt[:], op=ALU.add),
                     r=["rotA", "rotB0", "rotB1"], w=[dstk])
            rotary(qin[par], f"qin{par}", qr, "qr")
            rotary(kin[par], f"kin{par}", kr, "kr")
            S.op("dve", lambda e: e.tensor_tensor(out=v4(kd[:]), in0=v4(kr[:]), in1=bcast_last(kdec_t[i][:, :], 128),
                                                  op=ALU.mult), r=["kr", f"kdec{i}"], w=["kd"])

            def tr4(e, src, dstb):
                for h in range(4):
                    ins = e.transpose(out=dstb[:, h * 128:(h + 1) * 128], in_=src[:, h * 128:(h + 1) * 128],
                                      identity=ident_b[:])
                return ins
            S.op("pe", lambda e: tr4(e, qr, bq), r=["qr", "ident_b"], w=[bk(0)])
            S.op("act", lambda e: e.activation(out=qT[:].rearrange("p h d -> p (h d)"), in_=bq, func=AF.Copy),
                 r=[], w=[bk(0), "qT"])
            S.op("dve", lambda e: e.tensor_tensor(out=qdT[:].rearrange("p h d -> p (h d)"), in0=bq,
                                                  in1=qdec_t[i][:], op=ALU.mult),
                 r=[f"qdec{i}"], w=[bk(0), "qdT"])
            S.op("pe", lambda e: tr4(e, kr, bkk), r=["kr", "ident_b"], w=[bk(1)])
            S.op("act", lambda e: e.activation(out=kT[:].rearrange("p h d -> p (h d)"), in_=bkk, func=AF.Copy),
                 r=[], w=[bk(1), "kT"])

            def sc4(e):
                for h in range(4):
                    ins = e.matmul(banks[2][:, h * 128:(h + 1) * 128], lhsT=kT[:, h, :], rhs=qT[:, h, :],
                                   start=True, stop=True)
                return ins
            S.op("pe", sc4, r=["kT", "qT"], w=[bk(2)])
            S.op("dve", lambda e: e.tensor_tensor(out=PTr[:].rearrange("p h d -> p (h d)"), in0=banks[2][:],
                                                  in1=intra_t[i][:], op=ALU.mult),
                 r=[f"intra{i}"], w=[bk(2), "PTr"])

            if i == 1:
                for b in range(NSEQ_S):
                    S.op("dve", lambda e, b=b: e.tensor_copy(out=qdTm[:, b, :, b * TS:(b + 1) * TS],
                                                             in_=qdT[:, :, b * TS:(b + 1) * TS]),
                         r=["qdT"], w=["qdTm"])
                    S.op("dve", lambda e, b=b: e.tensor_scalar(out=kdm[:, b, :], in0=kd[:], scalar1=rowmask_t[:, b:b + 1],
                                                               scalar2=None, op0=ALU.mult),
                         r=["kd", "rowmask"], w=[("kdm", b)])

            def o4(e):
                for h in range(4):
                    hs = slice(h * 128, (h + 1) * 128)
                    first_only = (i == 0 and t == 0)
                    ins = e.matmul(banks[3][:, hs], lhsT=PTr[:, h, :], rhs=vin[par][:, hs], start=True, stop=first_only)
                    if i == 0 and t > 0:
                        ins = e.matmul(banks[3][:, hs], lhsT=qdT[:, h, :], rhs=Sb[:, h, :], start=False, stop=True)
                    if i == 1:
                        for b in range(NSEQ_S):
                            ins = e.matmul(banks[3][:, hs], lhsT=qdTm[:, b, h, :], rhs=S0b[:, b, h, :],
                                           start=False, stop=(b == NSEQ_S - 1))
                return ins
            S.op("pe", o4, r=["PTr", f"vin{par}", "qdT", "Sb", "qdTm", "S0b"], w=[bk(3)])

            if i == 0:
                def ds4(e):
                    for h in range(4):
                        hs = slice(h * 128, (h + 1) * 128)
                        ins = e.matmul(banks[4][:, hs], lhsT=kd[:, hs], rhs=vin[par][:, hs], start=True, stop=True)
                    return ins
                S.op("pe", ds4, r=["kd", f"vin{par}"], w=[bk(4)])
                if t == 0:
                    S.op("dve", lambda e: e.tensor_copy(out=Sst[:].rearrange("p h d -> p (h d)"), in_=banks[4][:]),
                         r=[], w=[bk(4), "Sst"])
                else:
                    def upd(e):
                        for h in range(4):
                            ins = e.scalar_tensor_tensor(out=Sst[:, h, :], in0=Sst[:, h, :], scalar=cdec[h],
                                                         in1=banks[4][:, h * 128:(h + 1) * 128],
                                                         op0=ALU.mult, op1=ALU.add)
                        return ins
                    S.op("dve", upd, r=[], w=[bk(4), "Sst"])
                S.op("act", lambda e: e.activation(out=Sb[:], in_=Sst[:], func=AF.Copy), r=["Sst"], w=["Sb"])
                if t == NPT - 1:
                    S.dma("sp", lambda e: e.dma_start(out=srp.rearrange("h k v -> k h v"), in_=Sst[:]),
                          r=["Sst"], w=["srp"])
            else:
                for b in range(NSEQ_S):
                    bb = 4 + (b % 2)
                    sn = Snew[b % 2]; snk = f"Snew{b % 2}"
                    def ds4(e, b=b, bb=bb):
                        for h in range(4):
                            hs = slice(h * 128, (h + 1) * 128)
                            ins = e.matmul(banks[bb][:, hs], lhsT=kdm[:, b, hs], rhs=vin[par][:, hs],
                                           start=True, stop=True)
                        return ins
                    S.op("pe", ds4, r=[("kdm", b), f"vin{par}"], w=[bk(bb)])
                    def upd(e, b=b, bb=bb, sn=sn):
                        for h in range(4):
                            ins = e.scalar_tensor_tensor(out=sn[:, h, :], in0=S0f[:, b, h, :], scalar=cdec[h],
                                                         in1=banks[bb][:, h * 128:(h + 1) * 128],
                                                         op0=ALU.mult, op1=ALU.add)
                        return ins
                    S.op("dve", upd, r=["S0f"], w=[bk(bb), snk])
                    S.dma("sp", lambda e, b=b, sn=sn: e.dma_start(out=srs[b].rearrange("h k v -> k h v"), in_=sn[:]),
                          r=[snk], w=[("srs", b)])

            S.op("act", lambda e: e.activation(out=o_sb[:], in_=banks[3][:], func=AF.Copy), r=[], w=[bk(3), "o_sb"])
            S.op("dve", lambda e: e.tensor_reduce(out=ssum[:], in_=v4(o_sb[:]), axis=AX.X, op=ALU.add),
                 r=["o_sb"], w=["ssum"])
            S.op("act", lambda e: e.activation(out=osq[:], in_=o_sb[:], func=AF.Square), r=["o_sb"], w=["osq"])
            S.op("dve", lambda e: e.tensor_reduce(out=ssq[:], in_=v4(osq[:]), axis=AX.X, op=ALU.add),
                 r=["osq"], w=["ssq"])
            S.op("dve", lambda e: e.tensor_scalar_mul(out=mean[:], in0=ssum[:], scalar1=1.0 / 128), r=["ssum"], w=["mean"])
            S.op("dve", lambda e: e.tensor_tensor(out=msq[:], in0=mean[:], in1=mean[:], op=ALU.mult), r=["mean"], w=["msq"])
            S.op("dve", lambda e: e.scalar_tensor_tensor(out=var[:], in0=ssq[:], scalar=1.0 / 128, in1=msq[:],
                                                         op0=ALU.mult, op1=ALU.subtract), r=["ssq", "msq"], w=["var"])
            S.op("dve", lambda e: e.tensor_scalar_add(out=var[:], in0=var[:], scalar1=HN_EPS), r=["var"], w=["var"])
            S.op("act", lambda e: e.activation(out=var[:], in_=var[:], func=AF.Sqrt), r=["var"], w=["var"])
            S.op("dve", lambda e: e.reciprocal(out=rstd[:], in_=var[:]), r=["var"], w=["rstd"])
            S.op("dve", lambda e: e.tensor_tensor(out=v4(o_sb[:]), in0=v4(o_sb[:]), in1=bcast_last(mean[:, :], 128),
                                                  op=ALU.subtract), r=["o_sb", "mean", "osq"], w=["o_sb"])
            S.op("dve", lambda e: e.tensor_tensor(out=v4(o_sb[:]), in0=v4(o_sb[:]), in1=bcast_last(rstd[:, :], 128),
                                                  op=ALU.mult), r=["o_sb", "rstd"], w=["o_sb"])
            S.op("act", lambda e: e.activation(out=sil[:], in_=gin[par][:], func=AF.Silu), r=[f"gin{par}"], w=["sil"])
            S.op("dve", lambda e: e.tensor_tensor(out=retg[par][:], in0=o_sb[:], in1=sil[:], op=ALU.mult),
                 r=["o_sb", "sil"], w=[f"retg{par}"])
            S.dma("sp", lambda e: e.dma_start(out=RG[rows, :], in_=retg[par][:]), r=[f"retg{par}"], w=[("RG", t)])

    S.full_barrier()
    if stop <= 3:
        S.finish()
        return nc, es, S

    with ExitStack() as ph:
        def psb(name, shape, dt):
            return ph.enter_context(nc.sbuf_tensor("s_" + name, list(shape), dt))
        rb_aug = psb("rb_aug", [33, 24], F32)
        oh_t = [psb(f"oh{g}", [33, 385], F32) for g in range(3)]
        ext_sb = psb("ext_sb", [8, 385], F32)
        btmp = [psb(f"btmp{i}", [128, 256], F32) for i in range(2)]
        biasT = psb("biasT", [128, 24, 256], BF16)
        S.op("dve", lambda e: e.memset(rb_aug[:], 1.0), w=["rb_aug"])
        S.dma("sp", lambda e: e.dma_start(out=rb_aug[0:32, :], in_=rel_bias_d), w=["rb_aug"])
        for g in range(3):
            S.dma("sp", lambda e, g=g: e.dma_start(out=oh_t[g][:], in_=oh_d[g]), w=[f"oh{g}"])
            S.op("pe", lambda e, g=g: e.matmul(banks[0][0:8, 0:385], lhsT=rb_aug[:, g * 8:(g + 1) * 8], rhs=oh_t[g][:],
                                               start=True, stop=True), r=["rb_aug", f"oh{g}"], w=[bk(0)])
            S.op("act", lambda e: e.activation(out=ext_sb[:], in_=banks[0][0:8, 0:385], func=AF.Copy),
                 r=[], w=[bk(0), "ext_sb"])
            S.dma("sp", lambda e, g=g: e.dma_start(out=EXT[g * 8:(g + 1) * 8, :], in_=ext_sb[:]),
                  r=["ext_sb"], w=[("EXT", g)])
        for gh in range(24):
            bt = btmp[gh % 2]; btk = f"btmp{gh % 2}"
            srcb = bass.AP(tensor=EXT.tensor, offset=gh * 385, ap=[[0, 128], [1, 385]])
            dstb = bass.AP(tensor=EXT2.tensor, offset=gh * 128 * 385, ap=[[385, 128], [1, 385]])
            S.dma("sp", lambda e, srcb=srcb, dstb=dstb: e.dma_start(out=dstb, in_=srcb),
                  r=[("EXT", gh // 8)], w=[("EXT2", gh)])
            for kc, c0 in ((0, 256), (1, 128)):
                src = bass.AP(tensor=EXT2.tensor, offset=gh * 128 * 385 + c0, ap=[[384, 128], [1, 128]])
                S.dma("sp", lambda e, bt=bt, kc=kc, src=src: e.dma_start(out=bt[:, kc * 128:(kc + 1) * 128], in_=src),
                      r=[("EXT2", gh)], w=[(btk, kc)])
            S.op("dve", lambda e, bt=bt, gh=gh: e.tensor_copy(out=biasT[:, gh, :], in_=bt[:]),
                 r=[(btk, 0), (btk, 1)], w=[("biasT", gh)])

        Qt = [psb(f"Qt{i}", [128, 512], BF16) for i in range(2)]
        Kt = [psb(f"Kt{i}", [128, 512], BF16) for i in range(2)]
        QT = [psb(f"QT{i}", [128, 4, 128], BF16) for i in range(2)]
        KT = [psb(f"KT{i}", [128, 4, 128], BF16) for i in range(3)]
        Va = [psb(f"Va{i}", [128, 8, 65], BF16) for i in range(3)]
        PT = [psb(f"PT{i}", [128, 2, 2, 128], BF16) for i in range(2)]
        OLs = [psb(f"OLs{i}", [128, 520], F32) for i in range(2)]
        kvf = [psb(f"kvf{i}", [128, 2, 512], F32) for i in range(2)]
        for i in range(3):
            S.op("dve", lambda e, i=i: e.memset(Va[i][:], 1.0), w=[f"Va{i}"])
        for i in range(2):
            S.op("dve", lambda e, i=i: e.memset(Qt[i][:], 0.0), w=[f"Qt{i}"])
            S.op("dve", lambda e, i=i: e.memset(Kt[i][:], 0.0), w=[f"Kt{i}"])
        bq = banks[0][:].bitcast(BF16)[:, 0:512]
        bkk = banks[1][:].bitcast(BF16)[:, 0:512]
        state = {"blk": 0}

        def tr4(e, src, dstb):
            for j in range(4):
                ins = e.transpose(out=dstb[:, j * 128:(j + 1) * 128], in_=src[:, j * 128:(j + 1) * 128],
                                  identity=ident_b[:])
            return ins

        def attn_core(g, NQ, qT, qTk, kTc, kTck, kTp, kTpk, vc, vck, vp, vpk):
            blk = state["blk"]; state["blk"] += 1
            has_prev = kTp is not None
            for hp in range(4):
                bnk = banks[2 + hp]
                pt = PT[hp % 2]; ptk = f"PT{hp % 2}"
                def sc(e, hp=hp, bnk=bnk):
                    for hh in range(2):
                        h = 2 * hp + hh
                        gh = g * 8 + h
                        ps_ = slice((h % 2) * 64, (h % 2) * 64 + 64)
                        base = hh * 256
                        if has_prev:
                            e.matmul(bnk[:, base:base + NQ], lhsT=ident_b[:], rhs=biasT[:, gh, 0:NQ],
                                     start=True, stop=False)
                            e.matmul(bnk[:, base:base + NQ], lhsT=kTp[ps_, h // 2, :], rhs=qT[ps_, h // 2, 0:NQ],
                                     start=False, stop=True)
                        e.matmul(bnk[:, base + 128:base + 128 + NQ], lhsT=ident_b[:], rhs=biasT[:, gh, 128:128 + NQ],
                                 start=True, stop=False)
                        ins = e.matmul(bnk[:, base + 128:base + 128 + NQ], lhsT=kTc[ps_, h // 2, :],
                                       rhs=qT[ps_, h // 2, 0:NQ], start=False, stop=True)
                    return ins
                S.op("pe", sc, r=[qTk, kTck, "ident_b"] + ([kTpk] if has_prev else []) +
                     [("biasT", g * 8 + 2 * hp), ("biasT", g * 8 + 2 * hp + 1)], w=[bk(2 + hp)])
                bv = bnk[:].rearrange("p (a b c) -> p a b c", a=2, b=2)
                if has_prev:
                    S.op("act", lambda e, pt=pt, bv=bv: e.activation(out=pt[:, :, :, 0:NQ], in_=bv[:, :, :, 0:NQ],
                                                                     func=AF.Exp), r=[], w=[bk(2 + hp), ptk])
                else:
                    S.op("act", lambda e, pt=pt, bv=bv: e.activation(out=pt[:, :, 1, 0:NQ], in_=bv[:, :, 1, 0:NQ],
                                                                     func=AF.Exp), r=[], w=[bk(2 + hp), ptk])
                def pv(e, hp=hp, pt=pt):
                    for hh in range(2):
                        h = 2 * hp + hh
                        ob = banks[6 + h // 4]
                        oreg = ob[0:NQ, (h % 4) * 65:(h % 4) * 65 + 65]
                        if has_prev:
                            e.matmul(oreg, lhsT=pt[:, hh, 0, 0:NQ], rhs=vp[:, h, :], start=True, stop=False)
                        ins = e.matmul(oreg, lhsT=pt[:, hh, 1, 0:NQ], rhs=vc[:, h, :], start=(not has_prev), stop=True)
                    return ins
                S.op("pe", pv, r=[ptk, vck] + ([vpk] if has_prev else []), w=[bk(6 + hp // 2)])
            ol = OLs[blk % 2]; olk = f"OLs{blk % 2}"
            S.op("act", lambda e: e.activation(out=ol[0:NQ, 0:260], in_=banks[6][0:NQ, 0:260], func=AF.Copy),
                 r=[], w=[bk(6), (olk, 0)])
            S.op("dve", lambda e: e.tensor_copy(out=ol[0:NQ, 260:520], in_=banks[7][0:NQ, 0:260]),
                 r=[], w=[bk(7), (olk, 1)])
            return ol, olk

        att_events = []
        for g, (win, dil) in enumerate(GROUPS):
            prev_events = att_events
            att_events = []
            Pv = P[0:SEQ].rearrange("(u d) c -> d u c", d=dil)
            Av = ATT[0:SEQ].rearrange("(u d) c -> d u c", d=dil)
            nb = NPT // dil
            cnt = 0
            for r in range(dil):
                for ub in range(nb):
                    par = cnt % 2; p3 = cnt % 3; pp3 = (cnt - 1) % 3
                    cnt += 1
                    us = slice(ub * 128, (ub + 1) * 128)
                    rkeys = [("P", (r + dil * (ub * 128 + i)) // 128, 0) for i in (0, 127)]
                    S.dma("sp", lambda e: e.dma_start(out=Qt[par][:], in_=Pv[r, us, 2048 + g * 512:2048 + (g + 1) * 512]),
                          w=[f"Qt{par}"])
                    S.dma("sp", lambda e: e.dma_start(out=Kt[par][:], in_=Pv[r, us, 3584 + g * 512:3584 + (g + 1) * 512]),
                          w=[f"Kt{par}"])
                    S.dma("sp", lambda e: e.dma_start(
                        out=Va[p3][:, :, 0:64],
                        in_=Pv[r, us, 5120 + g * 512:5120 + (g + 1) * 512].rearrange("p (h d) -> p h d", h=8)),
                        w=[f"Va{p3}"])
                    S.op("pe", lambda e: tr4(e, Qt[par], bq), r=[f"Qt{par}", "ident_b"], w=[bk(0)])
                    S.op("act", lambda e: e.activation(out=QT[par][:].rearrange("p h d -> p (h d)"), in_=bq, func=AF.Copy),
                         r=[], w=[bk(0), f"QT{par}"])
                    S.op("pe", lambda e: tr4(e, Kt[par], bkk), r=[f"Kt{par}", "ident_b"], w=[bk(1)])
                    S.op("dve", lambda e: e.tensor_copy(out=KT[p3][:].rearrange("p h d -> p (h d)"), in_=bkk),
                         r=[], w=[bk(1), f"KT{p3}"])
                    hp_ = ub > 0
                    ol, olk = attn_core(g, 128, QT[par], f"QT{par}", KT[p3], f"KT{p3}",
                                        KT[pp3] if hp_ else None, f"KT{pp3}", Va[p3], f"Va{p3}",
                                        Va[pp3] if hp_ else None, f"Va{pp3}")
                    aq_ = "sp" if g == 0 else "pool"
                    if g > 0 and r == 0 and ub == 0:
                        for ev in prev_events:
                            S._wait(aq_, ev)
                    kw = {} if g == 0 else {"accum_op": ALU.add}
                    ev = S.dma(aq_, lambda e: e.dma_start(out=Av[r, us, :], in_=ol[:], **kw),
                               r=[(olk, 0), (olk, 1)], w=[("ATT", g, r, ub)])
                    att_events.append(ev)
        if stop > 4:
            for g, (win, dil) in enumerate(GROUPS):
                nq = TS // dil if dil <= TS else 1
                classes = list(range(min(dil, TS)))
                prev_events = att_events
                att_events = []
                cnt = 0
                for b in range(NSEQ_S):
                    cv = c_in[g][b].rearrange("(u d) k c -> d u k c", d=dil)
                    for r in classes:
                        par = cnt % 2; p3 = cnt % 3
                        cnt += 1
                        tok0 = SEQ + b * TS + r
                        rows = slice(tok0, tok0 + dil * (nq - 1) + 1, dil)
                        S.dma("sp", lambda e: e.dma_start(out=kvf[par][:], in_=cv[r]), w=[f"kvf{par}"])
                        S.dma("sp", lambda e: e.dma_start(out=Qt[par][0:nq, :],
                                                          in_=P[rows, 2048 + g * 512:2048 + (g + 1) * 512]),
                              r=[("P", NPT, 4 + g)], w=[f"Qt{par}"])
                        S.dma("sp", lambda e: e.dma_start(out=Kt[par][0:nq, :],
                                                          in_=P[rows, 3584 + g * 512:3584 + (g + 1) * 512]),
                              r=[("P", NPT, 7 + g)], w=[f"Kt{par}"])
                        vcur = Va[(2 * cnt) % 3]; vck = f"Va{(2 * cnt) % 3}"
                        vprv = Va[(2 * cnt + 1) % 3]; vpk = f"Va{(2 * cnt + 1) % 3}"
                        S.dma("sp", lambda e: e.dma_start(
                            out=vcur[0:nq, :, 0:64],
                            in_=P[rows, 5120 + g * 512:5120 + (g + 1) * 512].rearrange("p (h d) -> p h d", h=8)),
                            r=[("P", NPT, 10 + g)], w=[vck])
                        S.op("act", lambda e: e.activation(out=Kt[1 - par][:], in_=kvf[par][:, 0, :], func=AF.Copy),
                             r=[f"kvf{par}"], w=[f"Kt{1 - par}"])
                        S.op("dve", lambda e: e.tensor_copy(out=vprv[:, :, 0:64],
                                                            in_=kvf[par][:, 1, :].rearrange("p (h d) -> p h d", h=8)),
                             r=[f"kvf{par}"], w=[vpk])
                        S.op("pe", lambda e: tr4(e, Qt[par], bq), r=[f"Qt{par}", "ident_b"], w=[bk(0)])
                        S.op("act", lambda e: e.activation(out=QT[par][:].rearrange("p h d -> p (h d)"), in_=bq,
                                                           func=AF.Copy), r=[], w=[bk(0), f"QT{par}"])
                        S.op("pe", lambda e: tr4(e, Kt[par], bkk), r=[f"Kt{par}", "ident_b"], w=[bk(1)])
                        S.op("dve", lambda e: e.tensor_copy(out=KT[0][:].rearrange("p h d -> p (h d)"), in_=bkk),
                             r=[], w=[bk(1), "KT0"])
                        S.op("pe", lambda e: tr4(e, Kt[1 - par], bq), r=[f"Kt{1 - par}", "ident_b"], w=[bk(0)])
                        S.op("act", lambda e: e.activation(out=KT[1][:].rearrange("p h d -> p (h d)"), in_=bq,
                                                           func=AF.Copy), r=[], w=[bk(0), "KT1"])
                        ol, olk = attn_core(g, 8, QT[par], f"QT{par}", KT[0], "KT0", KT[1], "KT1",
                                            vcur, vck, vprv, vpk)
                        aq_ = "sp" if g == 0 else "pool"
                        if g > 0 and cnt == 1:
                            for ev in prev_events:
                                S._wait(aq_, ev)
                        kw = {} if g == 0 else {"accum_op": ALU.add}
                        ev = S.dma(aq_, lambda e: e.dma_start(out=ATT[rows, :], in_=ol[0:nq, :], **kw),
                                   r=[(olk, 0), (olk, 1)], w=[("ATTs", g, b, r)])
                        att_events.append(ev)

    S.full_barrier()
    if stop <= 5:
        S.finish()
        return nc, es, S

    with ExitStack() as ph:
        def psb(name, shape, dt):
            return ph.enter_context(nc.sbuf_tensor("s_" + name, list(shape), dt))
        wro_b = psb("wro_b", [128, 4, D], BF16)
        wao_b = psb("wao_b", [128, 4, D], BF16)
        wout_b = psb("wout_b", [128, 8, D], BF16)
        wq_b = psb("wq_b", [128, 8, 2048], BF16)
        keysT = psb("keysT", [128, 16, 128], BF16)
        bout_t = psb("bout_t", [128, D], F32)
        lnt = {k: psb(k + "_t", [128, D], F32) for k in ("ln1_g", "ln1_b", "ln2_g", "ln2_b")}
        iota16 = psb("iota16", [128, 16], F32)
        S.dma("sp", lambda e: e.dma_start(out=bout_t[:], in_=b_out_rep), w=["bout_t"])
        for k in lnt:
            S.dma("sp", lambda e, k=k: e.dma_start(out=lnt[k][:], in_=ln_rep[k]), w=[k + "_t"])
        S.dma("sp", lambda e: e.dma_start(out=iota16[:], in_=iota16_d), w=["iota16"])
        with ExitStack() as wl:
            wst = [wl.enter_context(nc.sbuf_tensor(f"s_wst{i}", [128, 4, 1024], F32)) for i in range(2)]
            jobs = []
            for kc0 in (0,):
                jobs.append((w_ret_o.rearrange("(kc p) n -> p kc n", p=128), wro_b[:, :, :], "wro_b"))
                jobs.append((w_att_o.rearrange("(kc p) n -> p kc n", p=128), wao_b[:, :, :], "wao_b"))
            wov = w_out.rearrange("(kc p) n -> p kc n", p=128)
            jobs.append((wov[:, 0:4, :], wout_b[:, 0:4, :], ("wout_b", 0)))
            jobs.append((wov[:, 4:8, :], wout_b[:, 4:8, :], ("wout_b", 1)))
            wqv = peer_wq.rearrange("(kc p) n -> p kc n", p=128)
            for a in range(2):
                for b in range(2):
                    jobs.append((wqv[:, a * 4:(a + 1) * 4, b * 1024:(b + 1) * 1024],
                                 wq_b[:, a * 4:(a + 1) * 4, b * 1024:(b + 1) * 1024], ("wq_b", a, b)))
            for j, (src, dst, key) in enumerate(jobs):
                st_ = wst[j % 2]; stk = f"wst{j % 2}"
                S.dma("sp", lambda e, st_=st_, src=src: e.dma_start(out=st_[:], in_=src), w=[stk])
                if j % 2 == 0:
                    S.op("dve", lambda e, st_=st_, dst=dst: e.tensor_copy(out=dst, in_=st_[:]), r=[stk], w=[key])
                else:
                    S.op("act", lambda e, st_=st_, dst=dst: e.activation(out=dst, in_=st_[:], func=AF.Copy),
                         r=[stk], w=[key])
            kst = wst[0]
            for q4 in range(4):
                S.dma("sp", lambda e, q4=q4: e.dma_start(
                    out=kst[:, q4, 0:512].rearrange("p (a c) -> p a c", a=4),
                    in_=peer_keys[q4 * 512:(q4 + 1) * 512, :].rearrange("(a p) c -> p a c", p=128)), w=["wst0"])
            for q4 in range(4):
                def trk(e, q4=q4):
                    for a in range(4):
                        ins = e.transpose(out=banks[q4][:, a * 128:(a + 1) * 128], in_=kst[:, q4, a * 128:(a + 1) * 128],
                                          identity=ident_f[:])
                    return ins
                S.op("pe", trk, r=["wst0", "ident_f"], w=[bk(q4)])
                S.op("act", lambda e, q4=q4: e.activation(
                    out=keysT[:, q4 * 4:(q4 + 1) * 4, :].rearrange("p a k -> p (a k)"), in_=banks[q4][:], func=AF.Copy),
                    r=[], w=[bk(q4), ("keysT", q4)])
            S.full_barrier()
        wkeys = ["wro_b", "wao_b", ("wout_b", 0), ("wout_b", 1)]
        wqkeys = [("wq_b", a, b) for a in range(2) for b in range(2)]

        x_t = psb("x_t", [128, D], F32)
        att_t = psb("att_t", [128, 520], F32)
        rg_t = psb("rg_t", [128, 512], BF16)
        attn_t = psb("attn_t", [128, 512], BF16)
        rl = psb("rl", [128, 8], F32)
        aT = psb("aT", [128, 4, 128], BF16)
        rT = psb("rT", [128, 4, 128], BF16)
        bufA = psb("bufA", [128, 2048], F32)
        bufB = psb("bufB", [128, 2048], F32)
        tmp1 = psb("tmp1", [128, D], F32)
        x1 = psb("x1", [128, D], F32)
        h2 = psb("h2", [128, D], F32)
        h2b = psb("h2b", [128, D], BF16)
        h2T = psb("h2T", [128, 8, 128], BF16)
        junk = psb("junk", [128, D], BF16)
        st1 = psb("st1", [128, 8], F32)
        sv = psb("sv", [128, 16, 16], F32)
        si = psb("si", [128, 16, 16], U32)
        sif = psb("sif", [128, 16, 16], F32)
        cv = psb("cv", [128, 8, 16], F32)
        ci = psb("ci", [128, 8, 16], U32)
        hi_u = psb("hi_u", [128, 8, 16], U32); lo_u = psb("lo_u", [128, 8, 16], U32)
        hi_f = psb("hi_f", [128, 8, 16], F32); lo_f = psb("lo_f", [128, 8, 16], F32)
        i0 = psb("i0", [128, 8, 16], F32); i1 = psb("i1", [128, 8, 16], F32)
        e_f = psb("e_f", [128, 128], F32); e_i = psb("e_i", [128, 128], I32)
        gsm = psb("gsm", [128, 8, 16], F32); ssm = psb("ssm", [128, 8], F32)
        a_t = psb("a_t", [128, 128], F32); ga = psb("ga", [128, 128], BF16)
        NSL = 4
        Ug = [psb(f"Ug{i}", [128, D], BF16) for i in range(NSL)]
        Vg = [psb(f"Vg{i}", [128, D], BF16) for i in range(NSL)]
        Dg = [psb(f"Dg{i}", [128, 16, 128], BF16) for i in range(2)]
        Gb = bufA[:].bitcast(BF16)
        G_t = Gb[:, 0:2048]; sg_t = Gb[:, 2048:4096]
        Bb = bufB[:].bitcast(BF16)
        mixin = Bb[:, 0:1024]; mixT = Bb[:, 1024:2048].rearrange("p (k t) -> p k t", k=8)
        qpe = Bb[:, 0:2048]; qpT = Bb[:, 2048:4096].rearrange("p (k t) -> p k t", k=16)
        s_sc = bufA[:].rearrange("p (a k) -> p a k", a=16)
        oh4 = bufA[:].rearrange("p (h k i) -> p h k i", h=8, k=16)
        cand = bufB[:].rearrange("p (h c) -> p h c", h=8)
        cand4 = bufB[:].rearrange("p (h i j) -> p h i j", h=8, i=16)
        prod4 = bufB[:].rearrange("p (h k i) -> p h k i", h=8, k=16)
        bf_banks = [banks[i][:].bitcast(BF16) for i in range(8)]

        def layer_norm(src, srck, gk, bk_, dst, dstk):
            S.op("dve", lambda e: e.tensor_reduce(out=st1[:, 0:1], in_=src[:], axis=AX.X, op=ALU.add), r=[srck], w=["st_sum"])
            S.op("act", lambda e: e.activation(out=tmp1[:], in_=src[:], func=AF.Square), r=[srck], w=["tmp1"])
            S.op("dve", lambda e: e.tensor_reduce(out=st1[:, 1:2], in_=tmp1[:], axis=AX.X, op=ALU.add), r=["tmp1"], w=["st_sq"])
            S.op("dve", lambda e: e.tensor_scalar_mul(out=st1[:, 2:3], in0=st1[:, 0:1], scalar1=1.0 / D), r=["st_sum"], w=["st_mean"])
            S.op("dve", lambda e: e.tensor_tensor(out=st1[:, 3:4], in0=st1[:, 2:3], in1=st1[:, 2:3], op=ALU.mult),
                 r=["st_mean"], w=["st_msq"])
            S.op("dve", lambda e: e.scalar_tensor_tensor(out=st1[:, 4:5], in0=st1[:, 1:2], scalar=1.0 / D, in1=st1[:, 3:4],
                                                         op0=ALU.mult, op1=ALU.subtract), r=["st_sq", "st_msq"], w=["st_var"])
            S.op("dve", lambda e: e.tensor_scalar_add(out=st1[:, 4:5], in0=st1[:, 4:5], scalar1=LN_EPS), r=["st_var"], w=["st_var"])
            S.op("act", lambda e: e.activation(out=st1[:, 5:6], in_=st1[:, 4:5], func=AF.Sqrt), r=["st_var"], w=["st_std"])
            S.op("dve", lambda e: e.reciprocal(out=st1[:, 6:7], in_=st1[:, 5:6]), r=["st_std"], w=["st_rstd"])
            S.op("dve", lambda e: e.tensor_scalar(out=src[:], in0=src[:], scalar1=st1[:, 2:3], scalar2=st1[:, 6:7],
                                                  op0=ALU.subtract, op1=ALU.mult), r=[srck, "st_mean", "st_rstd"], w=[srck])
            S.op("dve", lambda e: e.tensor_tensor(out=src[:], in0=src[:], in1=lnt[gk][:], op=ALU.mult), r=[srck, gk + "_t"], w=[srck])
            S.op("dve", lambda e: e.tensor_tensor(out=dst[:], in0=src[:], in1=lnt[bk_][:], op=ALU.add), r=[srck, bk_ + "_t"], w=[dstk])

        def tr_n(e, src, dstb, n):
            for j in range(n):
                ins = e.transpose(out=dstb[:, j * 128:(j + 1) * 128], in_=src[:, j * 128:(j + 1) * 128], identity=ident_b[:])
            return ins

        gslot = {"u": 0, "v": 0}
        x1_2 = [x1, psb("x1b", [128, D], F32)]
        e_i2 = [e_i, psb("e_ib", [128, 128], I32)]
        tlist = list(range(NT) if tiles is None else tiles)

        def ctx(n):
            t = tlist[n]
            return dict(t=t, i=0 if t < NPT else 1, rows=slice(t * 128, (t + 1) * 128),
                        xsrc=xp[t * 128:(t + 1) * 128, :] if t < NPT else xs,
                        ydst=yp[t * 128:(t + 1) * 128, :] if t < NPT else ys,
                        x1c=x1_2[n % 2], x1k=f"x1_{n % 2}", e_ic=e_i2[n % 2], e_ik=f"e_i_{n % 2}")

        def stage1(t, i, rows, xsrc, ydst, x1c, x1k, e_ic, e_ik):
                S.dma("sp", lambda e: e.dma_start(out=x_t[:], in_=xsrc), w=["x_t"])
                S.dma("sp", lambda e: e.dma_start(out=att_t[:], in_=ATT[rows, :]), w=["att_t"])
                S.dma("sp", lambda e: e.dma_start(out=rg_t[:], in_=RG[rows, :]), r=[("RG", t)], w=["rg_t"])
                S.dma("sp", lambda e: e.dma_start(out=G_t, in_=P[rows, 6656:8704]), w=["bufA"])
                a3 = att_t[:].rearrange("p (h c) -> p h c", h=8)
                S.op("dve", lambda e: e.reciprocal(out=rl[:], in_=a3[:, :, 64]), r=["att_t"], w=["rl"])
                S.op("dve", lambda e: e.tensor_tensor(out=attn_t[:].rearrange("p (h d) -> p h d", h=8), in0=a3[:, :, 0:64],
                                                      in1=bcast_last(rl[:, :], 64), op=ALU.mult), r=["att_t", "rl"], w=["attn_t"])
                S.op("pe", lambda e: tr_n(e, attn_t, bf_banks[0], 4), r=["attn_t", "ident_b"], w=[bk(0)])
                S.op("act", lambda e: e.activation(out=aT[:].rearrange("p k t -> p (k t)"), in_=bf_banks[0][:, 0:512], func=AF.Copy),
                     r=[], w=[bk(0), "aT"])
                S.op("pe", lambda e: tr_n(e, rg_t, bf_banks[1], 4), r=["rg_t", "ident_b"], w=[bk(1)])
                S.op("dve", lambda e: e.tensor_copy(out=rT[:].rearrange("p k t -> p (k t)"), in_=bf_banks[1][:, 0:512]),
                     r=[], w=[bk(1), "rT"])
                S.op("act", lambda e: e.activation(out=sg_t, in_=G_t, func=AF.Sigmoid), r=["bufA"], w=["bufA"])

                def proj4(e, srcT, w_b, b0):
                    for half in range(2):
                        for kc in range(4):
                            ins = e.matmul(banks[b0 + half][:], lhsT=srcT[:, kc, :], rhs=w_b[:, kc, half * 512:(half + 1) * 512],
                                           start=(kc == 0), stop=(kc == 3))
                    return ins
                S.op("pe", lambda e: proj4(e, rT, wro_b, 2), r=["rT", "wro_b"], w=[bk(2), bk(3)])
                S.op("pe", lambda e: proj4(e, aT, wao_b, 4), r=["aT", "wao_b"], w=[bk(4), bk(5)])
                for half in range(2):
                    hs_ = slice(half * 512, (half + 1) * 512)
                    S.op("dve", lambda e, half=half, hs_=hs_: e.tensor_tensor(out=tmp1[:, hs_], in0=banks[2 + half][:],
                                                                              in1=sg_t[:, hs_], op=ALU.mult),
                         r=["bufA"], w=[bk(2 + half), ("tmp1h", half)])
                    S.op("dve", lambda e, half=half, hs_=hs_: e.tensor_tensor(
                        out=h2[:, hs_], in0=banks[4 + half][:], in1=sg_t[:, 1024 + half * 512:1024 + (half + 1) * 512],
                        op=ALU.mult), r=["bufA"], w=[bk(4 + half), ("h2h", half)])
                S.op("dve", lambda e: e.tensor_tensor(out=mixin, in0=tmp1[:], in1=h2[:], op=ALU.add),
                     r=[("tmp1h", 0), ("tmp1h", 1), ("h2h", 0), ("h2h", 1)], w=["bufB", "tmp1", "h2"])
                S.op("pe", lambda e: tr_n(e, mixin, bf_banks[0], 8), r=["bufB", "ident_b"], w=[bk(0)])
                S.op("act", lambda e: e.activation(out=mixT.rearrange("p k t -> p (k t)"), in_=bf_banks[0][:, 0:1024], func=AF.Copy),
                     r=[], w=[bk(0), "bufB"])

                def proj8(e, srcT, w_b, b0, nb_):
                    for nb in range(nb_):
                        for kc in range(8):
                            ins = e.matmul(banks[b0 + nb][:], lhsT=srcT[:, kc, :], rhs=w_b[:, kc, nb * 512:(nb + 1) * 512],
                                           start=(kc == 0), stop=(kc == 7))
                    return ins
                S.op("pe", lambda e: proj8(e, mixT, wout_b, 2, 2), r=["bufB", ("wout_b", 0), ("wout_b", 1)], w=[bk(2), bk(3)])
                for half in range(2):
                    hs_ = slice(half * 512, (half + 1) * 512)
                    S.op("dve", lambda e, half=half, hs_=hs_: e.tensor_tensor(out=h2[:, hs_], in0=banks[2 + half][:],
                                                                              in1=bout_t[:, hs_], op=ALU.add),
                         r=["bout_t", "h2"], w=[bk(2 + half), ("h2h", half)])
                S.op("dve", lambda e: e.tensor_tensor(out=h2[:], in0=h2[:], in1=modD[:, i, 0, :], op=ALU.mult),
                     r=[("h2h", 0), ("h2h", 1), ("modD", i, 0)], w=["h2"])
                S.op("dve", lambda e: e.scalar_tensor_tensor(out=x_t[:], in0=x_t[:], scalar=ALPHA, in1=h2[:],
                                                             op0=ALU.mult, op1=ALU.add), r=["x_t", "h2"], w=["x_t"])
                layer_norm(x_t, "x_t", "ln1_g", "ln1_b", x1c, x1k)
                S.op("dve", lambda e: e.tensor_tensor(out=h2[:], in0=x1c[:], in1=modD[:, i, 2, :], op=ALU.mult),
                     r=[x1k, ("modD", i, 2)], w=["h2"])
                S.op("dve", lambda e: e.tensor_tensor(out=h2b[:], in0=h2[:], in1=modD[:, i, 1, :], op=ALU.add),
                     r=["h2", ("modD", i, 1)], w=["h2b"])
                S.op("pe", lambda e: tr_n(e, h2b, bf_banks[1], 8), r=["h2b", "ident_b"], w=[bk(1)])
                S.op("act", lambda e: e.activation(out=h2T[:].rearrange("p k t -> p (k t)"), in_=bf_banks[1][:, 0:1024], func=AF.Copy),
                     r=[], w=[bk(1), "h2T"])
                S.op("pe", lambda e: proj8(e, h2T, wq_b, 2, 4), r=["h2T"] + wqkeys, w=[bk(2), bk(3), bk(4), bk(5)])
                for nb in range(4):
                    if nb % 2 == 0:
                        S.op("act", lambda e, nb=nb: e.activation(out=qpe[:, nb * 512:(nb + 1) * 512], in_=banks[2 + nb][:], func=AF.Copy),
                             r=["bufB"], w=[bk(2 + nb), "bufB"])
                    else:
                        S.op("dve", lambda e, nb=nb: e.tensor_copy(out=qpe[:, nb * 512:(nb + 1) * 512], in_=banks[2 + nb][:]),
                             r=["bufB"], w=[bk(2 + nb), "bufB"])
                for half in range(2):
                    def trq(e, half=half):
                        for j in range(8):
                            hs = half * 8 + j
                            ins = e.transpose(out=bf_banks[half][:, j * 128:(j + 1) * 128], in_=qpe[:, hs * 128:(hs + 1) * 128],
                                              identity=ident_b[:])
                        return ins
                    S.op("pe", trq, r=["bufB", "ident_b"], w=[bk(half)])
                    if half == 0:
                        S.op("act", lambda e: e.activation(out=qpT[:, 0:8, :].rearrange("p k t -> p (k t)"), in_=bf_banks[0][:, 0:1024],
                                                           func=AF.Copy), r=["bufB"], w=[bk(0), "bufB"])
                    else:
                        S.op("dve", lambda e: e.tensor_copy(out=qpT[:, 8:16, :].rearrange("p k t -> p (k t)"), in_=bf_banks[1][:, 0:1024]),
                             r=["bufB"], w=[bk(1), "bufB"])
                for q4 in range(4):
                    def scq(e, q4=q4):
                        for a in range(4):
                            hs = q4 * 4 + a
                            ins = e.matmul(banks[2 + q4][:, a * 128:(a + 1) * 128], lhsT=qpT[:, hs, :], rhs=keysT[:, hs, :],
                                           start=True, stop=True)
                        return ins
                    S.op("pe", scq, r=["bufB", ("keysT", q4)], w=[bk(2 + q4)])
                    if q4 % 2 == 0:
                        S.op("act", lambda e, q4=q4: e.activation(out=bufA[:, q4 * 512:(q4 + 1) * 512], in_=banks[2 + q4][:], func=AF.Copy),
                             r=["bufA"], w=[bk(2 + q4), "bufA"])
                    else:
                        S.op("dve", lambda e, q4=q4: e.tensor_copy(out=bufA[:, q4 * 512:(q4 + 1) * 512], in_=banks[2 + q4][:]),
                             r=["bufA"], w=[bk(2 + q4), "bufA"])
                ssk = ["bufA"]

                def topk_rounds(n, vals, vk, outv, outvk, outi, outik):
                    for rnd in range(2):
                        sl = slice(rnd * 8, rnd * 8 + 8)
                        def mx(e, sl=sl):
                            for j in range(n):
                                ins = e.max(out=outv[:, j, sl], in_=vals[:, j, :])
                            return ins
                        S.op("dve", mx, r=vk, w=[(outvk, rnd)])
                        def mi(e, sl=sl):
                            for j in range(n):
                                ins = e.max_index(out=outi[:, j, sl], in_max=outv[:, j, sl], in_values=vals[:, j, :])
                            return ins
                        S.op("dve", mi, r=vk + [(outvk, rnd)], w=[(outik, rnd)])
                        if rnd == 0:
                            def mr(e, sl=sl):
                                for j in range(n):
                                    ins = e.match_replace(out=vals[:, j, :], in_to_replace=outv[:, j, sl], in_values=vals[:, j, :],
                                                          imm_value=-1e30)
                                return ins
                            S.op("dve", mr, r=[(outvk, rnd), (outik, rnd)], w=vk)
                topk_rounds(16, s_sc, ssk, sv, "sv", si, "si")
                sv4 = sv[:].rearrange("p (h s) k -> p h s k", s=2)
                S.op("dve", lambda e: e.tensor_tensor(
                    out=cand4, in0=sv4[:, :, 0, :].unsqueeze(3).to_broadcast([128, 8, 16, 16]),
                    in1=sv4[:, :, 1, :].unsqueeze(2).to_broadcast([128, 8, 16, 16]), op=ALU.add),
                    r=[("sv", 0), ("sv", 1), "bufB"], w=["bufB"])
                topk_rounds(8, cand, ["bufB"], cv, "cv", ci, "ci")
                cvk = [("cv", 0), ("cv", 1)]; cik = [("ci", 0), ("ci", 1)]
                S.op("dve", lambda e: e.tensor_tensor(out=gsm[:], in0=cv[:], in1=bcast_last(cv[:, :, 0], 16), op=ALU.subtract),
                     r=cvk, w=["gsm"])
                S.op("act", lambda e: e.activation(out=gsm[:], in_=gsm[:], func=AF.Exp), r=["gsm"], w=["gsm"])
                S.op("dve", lambda e: e.tensor_reduce(out=ssm[:], in_=gsm[:], axis=AX.X, op=ALU.add), r=["gsm"], w=["ssm"])
                S.op("dve", lambda e: e.reciprocal(out=ssm[:], in_=ssm[:]), r=["ssm"], w=["ssm"])
                S.op("dve", lambda e: e.tensor_tensor(out=gsm[:], in0=gsm[:], in1=bcast_last(ssm[:, :], 16), op=ALU.mult),
                     r=["gsm", "ssm"], w=["gsm"])
                S.op("dve", lambda e: e.tensor_single_scalar(out=hi_u[:], in_=ci[:], scalar=4, op=ALU.logical_shift_right),
                     r=cik, w=["hi_u"])
                S.op("dve", lambda e: e.tensor_single_scalar(out=lo_u[:], in_=ci[:], scalar=15, op=ALU.bitwise_and),
                     r=cik, w=["lo_u"])
                S.op("dve", lambda e: e.tensor_copy(out=hi_f[:], in_=hi_u[:]), r=["hi_u"], w=["hi_f"])
                S.op("dve", lambda e: e.tensor_copy(out=lo_f[:], in_=lo_u[:]), r=["lo_u"], w=["lo_f"])
                S.op("dve", lambda e: e.tensor_copy(out=sif[:], in_=si[:]), r=[("si", 0), ("si", 1)], w=["sif"])
                sif4 = sif[:].rearrange("p (h s) k -> p h s k", s=2)
                iot4 = iota16[:, :].unsqueeze(1).unsqueeze(1).to_broadcast([128, 8, 16, 16])
                for side, (xf, xk, dsti, dstk) in enumerate(((hi_f, "hi_f", i0, "i0"), (lo_f, "lo_f", i1, "i1"))):
                    S.op("dve", lambda e, xf=xf: e.tensor_tensor(
                        out=oh4, in0=iot4, in1=xf[:].unsqueeze(3).to_broadcast([128, 8, 16, 16]), op=ALU.is_equal),
                        r=[xk, "iota16"] + ssk, w=["bufA"])
                    S.op("dve", lambda e, side=side: e.tensor_tensor(
                        out=prod4, in0=oh4, in1=sif4[:, :, side, :].unsqueeze(2).to_broadcast([128, 8, 16, 16]), op=ALU.mult),
                        r=["bufA", "sif", "bufB"], w=["bufB"])
                    S.op("dve", lambda e, dsti=dsti: e.tensor_reduce(out=dsti[:], in_=prod4, axis=AX.X, op=ALU.add),
                         r=["bufB"], w=[dstk])
                S.op("dve", lambda e: e.scalar_tensor_tensor(out=e_f[:].rearrange("p (h k) -> p h k", h=8), in0=i0[:], scalar=128.0,
                                                             in1=i1[:], op0=ALU.mult, op1=ALU.add), r=["i0", "i1"], w=["e_f"])
                S.op("dve", lambda e: e.tensor_copy(out=e_ic[:], in_=e_f[:]), r=["e_f"], w=[e_ik])

        def stage2(t, i, rows, xsrc, ydst, x1c, x1k, e_ic, e_ik):
                for hk in range(128):
                    sl_ = gslot["u"] % NSL; gslot["u"] += 1
                    S.dma("pool", lambda e, sl_=sl_, hk=hk: e.indirect_dma_start(
                        out=Ug[sl_][:], out_offset=None, in_=UB,
                        in_offset=bass.IndirectOffsetOnAxis(ap=e_ic[:, hk:hk + 1], axis=0)), r=[e_ik] + ubkeys, w=[f"Ug{sl_}"])
                    S.op("dve", lambda e, sl_=sl_, hk=hk: e.scalar_tensor_tensor(
                        out=junk[:], in0=Ug[sl_][:], scalar=1.0, in1=h2b[:], op0=ALU.mult, op1=ALU.mult,
                        accum_out=a_t[:, hk:hk + 1]), r=[f"Ug{sl_}", "h2b"], w=["junk", ("a_t", hk)])
                S.op("act", lambda e: e.activation(out=a_t[:], in_=a_t[:], func=AF.Gelu), r=[("a_t", hk) for hk in range(128)], w=["a_g"])
                S.op("dve", lambda e: e.tensor_tensor(out=ga[:], in0=a_t[:], in1=gsm[:].rearrange("p h k -> p (h k)"), op=ALU.mult),
                     r=["a_g", "gsm"], w=["ga"])

        def stage3a(t, i, rows, xsrc, ydst, x1c, x1k, e_ic, e_ik):
                for h in range(8):
                    dg = Dg[h % 2]; dgk = f"Dg{h % 2}"
                    S.op("dve", lambda e, dg=dg, h=h: e.tensor_tensor(
                        out=dg[:], in0=ident_b[:, :].unsqueeze(1).to_broadcast([128, 16, 128]),
                        in1=ga[:, h * 16:(h + 1) * 16].unsqueeze(2).to_broadcast([128, 16, 128]), op=ALU.mult),
                        r=["ga", "ident_b"], w=[dgk])
                    for k in range(16):
                        hk = h * 16 + k
                        sl_ = gslot["v"] % NSL; gslot["v"] += 1
                        S.dma("pool", lambda e, sl_=sl_, hk=hk: e.indirect_dma_start(
                            out=Vg[sl_][:], out_offset=None, in_=VB,
                            in_offset=bass.IndirectOffsetOnAxis(ap=e_ic[:, hk:hk + 1], axis=0)), r=[e_ik] + vbkeys, w=[f"Vg{sl_}"])
                        def vmm(e, sl_=sl_, hk=hk, dg=dg, k=k):
                            for half in range(2):
                                ins = e.matmul(banks[6 + half][:], lhsT=dg[:, k, :], rhs=Vg[sl_][:, half * 512:(half + 1) * 512],
                                               start=(hk == 0), stop=(hk == 127))
                            return ins
                        S.op("pe", vmm, r=[f"Vg{sl_}", dgk], w=[bk(6), bk(7)])

        def stage3b(t, i, rows, xsrc, ydst, x1c, x1k, e_ic, e_ik):
                for half in range(2):
                    hs_ = slice(half * 512, (half + 1) * 512)
                    S.op("dve", lambda e, half=half, hs_=hs_: e.tensor_tensor(out=h2[:, hs_], in0=banks[6 + half][:],
                                                                              in1=modD[:, i, 3, hs_], op=ALU.mult),
                         r=[("modD", i, 3), "h2"], w=[bk(6 + half), ("h2h", half)])
                S.op("dve", lambda e: e.scalar_tensor_tensor(out=x1c[:], in0=x1c[:], scalar=ALPHA, in1=h2[:],
                                                             op0=ALU.mult, op1=ALU.add), r=[x1k, ("h2h", 0), ("h2h", 1)], w=[x1k, "h2"])
                layer_norm(x1c, x1k, "ln2_g", "ln2_b", x_t, "x_t")
                S.dma("sp", lambda e: e.dma_start(out=ydst, in_=x_t[:]), r=["x_t"], w=[("y", t)])


        stage1(**ctx(0))
        for n in range(len(tlist)):
            stage2(**ctx(n))
            stage3a(**ctx(n))
            if n + 1 < len(tlist):
                stage1(**ctx(n + 1))
            stage3b(**ctx(n))

    S.finish()
    return nc, es, S


def _shard_inputs(inp):
    c = _consts()
    maps = []
    f = np.ascontiguousarray
    for i in range(NCORES):
        sl = slice(i * NSEQ_S, (i + 1) * NSEQ_S)
        m = {
            "xp": f(inp["x_prompt"][i]),
            "xs": f(inp["x_sample"][sl].reshape(128, D)),
            "cp_rep": f(np.broadcast_to(inp["c_prompt"][i:i + 1], (128, D))),
            "cs_rep": f(np.repeat(inp["c_sample"][sl], TS, axis=0)),
            "st_in": f(inp["state_ret"][0, sl]),
            "c_in0": f(inp["cache_att_w128"][0, sl].reshape(NSEQ_S, 128, 2, 512)),
            "c_in1": f(inp["cache_att_w512"][0, sl].reshape(NSEQ_S, 512, 2, 512)),
            "c_in2": f(inp["cache_att_w2048"][0, sl].reshape(NSEQ_S, 2048, 2, 512)),
            "w_ada": f(inp["w_ada"][0]),
            "b_ada": f(inp["b_ada"][0:1]),
            "w_in": f(inp["w_in"][0]),
            "ident": c["ident"],
            "rotc": c["rotc"], "rots": c["rots"],
            "intraT_p": c["intraT_p"], "intraT_s": c["intraT_s"],
            "qdec_p": c["qdec_p"], "qdec_s": c["qdec_s"],
            "kdec_p": c["kdec_p"], "kdec_s": c["kdec_s"],
            "rowmask": c["rowmask"],
            "iota16": c["iota16"],
            "oh0": c["oh0"], "oh1": c["oh1"], "oh2": c["oh2"],
            "rel_bias": f(inp["rel_bias"]),
            "w_ret_o": f(inp["w_ret_o"][0]), "w_att_o": f(inp["w_att_o"][0]), "w_out": f(inp["w_out"][0]),
            "b_out_rep": f(np.broadcast_to(inp["b_out"][0:1], (128, D))),
            "ln1_g_rep": f(np.broadcast_to(inp["ln1_g"][0:1], (128, D))),
            "ln1_b_rep": f(np.broadcast_to(inp["ln1_b"][0:1], (128, D))),
            "ln2_g_rep": f(np.broadcast_to(inp["ln2_g"][0:1], (128, D))),
            "ln2_b_rep": f(np.broadcast_to(inp["ln2_b"][0:1], (128, D))),
            "peer_wq": f(inp["peer_wq"][0]), "peer_keys": f(inp["peer_keys"][0].reshape(2048, 128)),
            "peer_u": f(inp["peer_u"][0]), "peer_v": f(inp["peer_v"][0]),
        }
        maps.append(m)
    return maps


def kernel(**inputs):
    inp = {k: np.asarray(v) for k, v in inputs.items()}
    nc, es, S = build_program()
    with es:
        maps = _shard_inputs(inp)
        res = run_bass_kernel_spmd(nc, maps, core_ids=list(range(NCORES)))
    R = res.results
    yp = np.stack([R[i]["yp"] for i in range(NCORES)], 0)
    ys = np.concatenate([R[i]["ys"].reshape(NSEQ_S, TS, D) for i in range(NCORES)], 0)
    srp = np.stack([R[i]["srp"] for i in range(NCORES)], 0)[None]
    srs = np.concatenate([R[i]["srs"] for i in range(NCORES)], 0)[None]
    cpo = [np.stack([R[i][f"cpo{g}"] for i in range(NCORES)], 0).reshape(1, NCORES, GROUPS[g][0], 2, 8, 64)
           for g in range(3)]
    cso = [np.concatenate([R[i][f"cso{g}"] for i in range(NCORES)], 0).reshape(
        1, NCORES * NSEQ_S, GROUPS[g][0], 2, 8, 64) for g in range(3)]
    return (yp.astype(np.float32), ys.astype(np.float32), srp, cpo[0], cpo[1], cpo[2], srs, cso[0], cso[1], cso[2])
```

```python
import math
from contextlib import ExitStack

import numpy as np
import concourse.bass as bass
import concourse.mybir as mybir
from concourse.bass_utils import run_bass_kernel_spmd

F32 = mybir.dt.float32
BF16 = mybir.dt.bfloat16
U32 = mybir.dt.uint32
I32 = mybir.dt.int32
AF = mybir.ActivationFunctionType
ALU = mybir.AluOpType
AX = mybir.AxisListType

NCORES = 8
D = 1024
SEQ = 4096
NPT = 32
NT = 33
NTOK = NT * 128
NSEQ_S = 16
TS = 8
PAST = 8192
IN_COLS = 8704
GROUPS = ((128, 1), (512, 4), (2048, 16))
ALPHA = 2.0 ** 0.25
LN_EPS = 1e-5
HN_EPS = 1e-6
NEGB = -30000.0
SEM_LIMIT = 30000


class Sched:
    def __init__(self, nc, es):
        self.nc = nc
        self.es = es
        self.eng = {"pe": nc.tensor, "act": nc.scalar, "dve": nc.vector, "pool": nc.gpsimd, "sp": nc.sync}
        self.sem = {}
        self.cnt = {}
        self.nsem = 0
        for e in ("pe", "act", "dve", "pool"):
            self._new_engine_sem(e)
        self.known = {e: {} for e in self.eng}
        self.bufs = {}
        self.dpool = {}
        self.drr = {}
        for q, n in (("sp", 24), ("pool", 16), ("act", 8)):
            self.dpool[q] = [self._new_dma_slot() for _ in range(n)]
            self.drr[q] = 0
        self.ninstr = 0

    def _mksem(self, name):
        self.nsem += 1
        return self.es.enter_context(self.nc.semaphore(f"{name}_{self.nsem}"))

    def _new_engine_sem(self, e):
        self.sem[e] = self._mksem("c" + e)
        self.cnt[e] = 0

    def _new_dma_slot(self):
        return {"sem": self._mksem("d"), "val": 0}

    def _wait(self, e, ev):
        sem, val = ev
        k = self.known[e]
        if k.get(id(sem), 0) >= val:
            return
        self.eng[e].wait_ge(sem, val)
        self.ninstr += 1
        k[id(sem)] = val

    def _deps(self, r, w):
        deps = []
        for key in r:
            b = self.bufs.get(key)
            if b is not None and b["w"] is not None:
                deps.append(b["w"])
        for key in w:
            b = self.bufs.get(key)
            if b is not None:
                if b["w"] is not None:
                    deps.append(b["w"])
                deps.extend(b["r"].values())
        return deps

    def _record(self, ev, r, w):
        sem, val = ev
        for key in r:
            b = self.bufs.setdefault(key, {"w": None, "r": {}})
            old = b["r"].get(id(sem))
            if old is None or old[1] < val:
                b["r"][id(sem)] = ev
        for key in w:
            self.bufs[key] = {"w": ev, "r": {}}

    def op(self, e, fn, r=(), w=()):
        deps = self._deps(r, w)
        own = self.sem[e]
        for ev in deps:
            if e == "pe" and ev[0] is own:
                continue
            self._wait(e, ev)
        ins = fn(self.eng[e])
        if self.cnt[e] >= SEM_LIMIT:
            self._new_engine_sem(e)
        self.cnt[e] += 1
        ins.then_inc(self.sem[e], 1)
        self.ninstr += 1
        ev = (self.sem[e], self.cnt[e])
        self._record(ev, r, w)
        return ev

    def dma(self, q, fn, r=(), w=()):
        deps = self._deps(r, w)
        pool = self.dpool[q]
        i = self.drr[q]
        self.drr[q] = (i + 1) % len(pool)
        slot = pool[i]
        if slot["val"] >= SEM_LIMIT:
            slot = pool[i] = self._new_dma_slot()
        if slot["val"] > 0:
            self._wait(q, (slot["sem"], slot["val"]))
        for ev in deps:
            self._wait(q, ev)
        ins = fn(self.eng[q])
        slot["val"] += 16
        ins.then_inc(slot["sem"], 16)
        self.ninstr += 1
        ev = (slot["sem"], slot["val"])
        self._record(ev, r, w)
        return ev

    def barrier(self, e, keys):
        for ev in self._deps((), keys):
            self._wait(e, ev)

    def full_barrier(self):
        evs = [(self.sem[x], self.cnt[x]) for x in ("pe", "act", "dve", "pool") if self.cnt[x] > 0]
        for pool in self.dpool.values():
            for slot in pool:
                if slot["val"] > 0:
                    evs.append((slot["sem"], slot["val"]))
        for e in ("pe", "act", "dve", "pool", "sp"):
            for ev in evs:
                if ev[0] is self.sem.get(e):
                    continue
                self._wait(e, ev)

    def finish(self):
        for q, pool in self.dpool.items():
            for slot in pool:
                if slot["val"] > 0:
                    self._wait("sp", (slot["sem"], slot["val"]))


def _t5_bucket(dist):
    d = dist.astype(np.float32)
    large = 16 + (np.log(np.maximum(d, 1.0) / 16) / math.log(2048 / 16) * 16)
    large = np.minimum(large.astype(np.int32), 31)
    return np.where(dist < 16, dist, large)


_CONST_CACHE = {}


def _consts():
    if _CONST_CACHE:
        return _CONST_CACHE
    c = _CONST_CACHE
    c["ident"] = np.eye(128, dtype=np.float32)
    pos = np.concatenate([np.arange(SEQ), PAST + (np.arange(128) % TS)]).astype(np.float32)
    inv = (10000.0 ** (-np.arange(64, dtype=np.float32) / 64)).astype(np.float32)
    ang = (pos[:, None] * inv[None, :]).astype(np.float32)
    cos = np.cos(ang).astype(np.float32); sin = np.sin(ang).astype(np.float32)
    c["rotc"] = np.concatenate([cos, cos], 1)
    c["rots"] = np.concatenate([-sin, sin], 1)
    lg = np.log1p(-np.exp2(-5.0 - np.arange(4, dtype=np.float64)))
    p = np.arange(128)
    for name, C in (("p", 128), ("s", TS)):
        tpos = p % C
        seq = p // C
        rel = tpos[None, :] - tpos[:, None]
        ok = (rel >= 0) & (seq[None, :] == seq[:, None])
        intra = np.where(ok[:, None, :], np.exp(lg[None, :, None] * np.maximum(rel, 0)[:, None, :]), 0.0)
        c["intraT_" + name] = intra.reshape(128, 512).astype(np.float32)
        qd = np.exp(lg[:, None] * (tpos[None, :] + 1.0))
        c["qdec_" + name] = np.broadcast_to(qd.reshape(1, 512), (128, 512)).astype(np.float32).copy()
        c["kdec_" + name] = np.exp(lg[None, :] * (C - 1.0 - tpos[:, None])).astype(np.float32)
        c["cdec_" + name] = [float(v) for v in np.exp(lg * C)]
    c["rowmask"] = (p[:, None] // TS == np.arange(NSEQ_S)[None, :]).astype(np.float32)
    c["iota16"] = np.broadcast_to(np.arange(16, dtype=np.float32)[None, :], (128, 16)).copy()
    for g, (win, dil) in enumerate(GROUPS):
        oh = np.zeros((33, 385), np.float32)
        for m in range(385):
            rel = m - 128
            if 0 <= rel <= 128:
                oh[int(_t5_bucket(np.array([rel * dil]))[0]), m] = 1.0
            else:
                oh[32, m] = NEGB
        c[f"oh{g}"] = oh
    return c


def build_program(stop=99, nocopy=False, cbs=None, nocso=False, tiles=None):
    nc = bass.Bass("TRN2", target_bir_lowering=False)
    es = ExitStack()
    S = Sched(nc, es)
    CST = _consts()

    def din(name, shape, dt=F32):
        return nc.dram_tensor(name, list(shape), dt, kind="ExternalInput").ap()

    def dout(name, shape, dt=F32):
        return nc.dram_tensor(name, list(shape), dt, kind="ExternalOutput").ap()

    def dscr(name, shape, dt):
        return nc.dram_tensor(name, list(shape), dt).ap()

    def sb(name, shape, dt):
        return es.enter_context(nc.sbuf_tensor("s_" + name, list(shape), dt))

    xp = din("xp", [SEQ, D]); xs = din("xs", [128, D])
    cp_rep = din("cp_rep", [128, D]); cs_rep = din("cs_rep", [128, D])
    st_in = din("st_in", [NSEQ_S, 4, 128, 128])
    c_in = [din(f"c_in{g}", [NSEQ_S, GROUPS[g][0], 2, 512]) for g in range(3)]
    w_ada = din("w_ada", [D, 6 * D]); b_ada = din("b_ada", [1, 6 * D])
    w_in = din("w_in", [D, IN_COLS])
    ident_d = din("ident", [128, 128])
    rotc_d = din("rotc", [NTOK, 128]); rots_d = din("rots", [NTOK, 128])
    intra_d = [din("intraT_p", [128, 512]), din("intraT_s", [128, 512])]
    qdec_d = [din("qdec_p", [128, 512]), din("qdec_s", [128, 512])]
    kdec_d = [din("kdec_p", [128, 4]), din("kdec_s", [128, 4])]
    rowmask_d = din("rowmask", [128, NSEQ_S])
    iota16_d = din("iota16", [128, 16])
    oh_d = [din(f"oh{g}", [33, 385]) for g in range(3)]
    rel_bias_d = din("rel_bias", [32, 24])
    w_ret_o = din("w_ret_o", [512, D]); w_att_o = din("w_att_o", [512, D]); w_out = din("w_out", [D, D])
    b_out_rep = din("b_out_rep", [128, D])
    ln_rep = {k: din(k + "_rep", [128, D]) for k in ("ln1_g", "ln1_b", "ln2_g", "ln2_b")}
    peer_wq = din("peer_wq", [D, 2048]); peer_keys = din("peer_keys", [2048, 128])
    peer_u = din("peer_u", [16384, D]); peer_v = din("peer_v", [16384, D])

    yp = dout("yp", [SEQ, D]); ys = dout("ys", [128, D])
    srp = dout("srp", [4, 128, 128]); srs = dout("srs", [NSEQ_S, 4, 128, 128])
    cpo = [dout(f"cpo{g}", [GROUPS[g][0], 2, 512]) for g in range(3)]
    cso = [dout(f"cso{g}", [NSEQ_S, GROUPS[g][0], 2, 512]) for g in range(3)]

    P = dscr("proj", [NTOK, IN_COLS], BF16)
    RG = dscr("retg", [NTOK, 512], BF16)
    ATT = dscr("attacc", [NTOK, 520], F32)
    EXT = dscr("biasext", [24, 385], F32)
    UB = dscr("peer_u_bf", [16384, D], BF16)
    VB = dscr("peer_v_bf", [16384, D], BF16)
    EXT2 = dscr("biasext2", [24, 128 * 385], F32)

    banks = [es.enter_context(nc.psum_tensor(f"bank{i}", [128, 512], F32)) for i in range(8)]

    def bk(i):
        return f"bank{i}"

    ident_f = sb("ident_f", [128, 128], F32)
    ident_b = sb("ident_b", [128, 128], BF16)
    ones1 = sb("ones1", [1, 128], F32)
    modD = sb("modD", [128, 2, 4, D], F32)
    mod_stack = ExitStack()
    modp = mod_stack.enter_context(nc.sbuf_tensor("s_modp", [128, 6 * D], F32))
    mods = mod_stack.enter_context(nc.sbuf_tensor("s_mods", [128, 6 * D], F32))

    S.dma("sp", lambda e: e.dma_start(out=ident_f[:], in_=ident_d), w=["ident_f"])
    S.op("dve", lambda e: e.tensor_copy(out=ident_b[:], in_=ident_f[:]), r=["ident_f"], w=["ident_b"])
    S.op("dve", lambda e: e.memset(ones1[:], 1.0), w=["ones1"])

    tabkeys = []
    for name, src_t, dst_t in (("UB", peer_u, UB), ("VB", peer_v, VB)):
        for c in range(8):
            rs_ = slice(c * 2048, (c + 1) * 2048)
            S.dma("pool", lambda e, src_t=src_t, dst_t=dst_t, rs_=rs_: e.dma_start(out=dst_t[rs_, :], in_=src_t[rs_, :]),
                  w=[(name, c)])
            tabkeys.append((name, c))
    WQB = dscr("peer_wq_bf", [D, 2048], BF16)
    wqbkeys = []
    for c in range(2):
        cs_ = slice(c * 1024, (c + 1) * 1024)
        S.dma("pool", lambda e, cs_=cs_: e.dma_start(out=WQB[:, cs_], in_=peer_wq[:, cs_]), w=[("WQB", c)])
        wqbkeys.append(("WQB", c))
    ubkeys = [k for k in tabkeys if k[0] == "UB"]
    vbkeys = [k for k in tabkeys if k[0] == "VB"]
    for g in range(0 if not nocopy else 3, 3):
        nb = GROUPS[g][0]
        for b in range(NSEQ_S):
            src = c_in[g][b, TS:nb].rearrange("(a r) k c -> a (r k c)", r=8)
            dst = cso[g][b, 0:nb - TS].rearrange("(a r) k c -> a (r k c)", r=8)
            S.dma("act", lambda e, s=src, d=dst: e.dma_start(out=d, in_=s), w=[("cso_copy", g, b)])

    with ExitStack() as ph:
        def psb(name, shape, dt):
            return ph.enter_context(nc.sbuf_tensor("s_" + name, list(shape), dt))
        c_tok = psb("c_tok", [128, D], F32)
        c_act = psb("c_act", [128, D], F32)
        cT = [psb(f"cT{i}", [128, 8, 128], F32) for i in range(2)]
        wada_t = [psb(f"wada{i}", [128, 8, 512], F32) for i in range(2)]
        bada_t = psb("bada", [1, 6 * D], F32)
        S.dma("sp", lambda e: e.dma_start(out=bada_t[:], in_=b_ada), w=["bada"])
        for i, src in enumerate((cp_rep, cs_rep)):
            S.dma("sp", lambda e, s=src: e.dma_start(out=c_tok[:], in_=s), w=["c_tok"])
            S.op("act", lambda e: e.activation(out=c_act[:], in_=c_tok[:], func=AF.Silu), r=["c_tok"], w=["c_act"])
            for half in range(2):
                def tr4(e, half=half):
                    for j in range(4):
                        kc = half * 4 + j
                        ins = e.transpose(out=banks[half][:, j * 128:(j + 1) * 128],
                                          in_=c_act[:, kc * 128:(kc + 1) * 128], identity=ident_f[:])
                    return ins
                S.op("pe", tr4, r=["c_act", "ident_f"], w=[bk(half)])
                S.op("dve", lambda e, half=half, i=i: e.tensor_copy(
                    out=cT[i][:, half * 4:(half + 1) * 4, :],
                    in_=banks[half][:].rearrange("p (a b) -> p a b", a=4)), r=[bk(half)], w=[f"cT{i}"])
        wv = w_ada.rearrange("(kc p) n -> p kc n", p=128)
        for n in range(12):
            wt = wada_t[n % 2]
            wk = f"wada{n % 2}"
            S.dma("sp", lambda e, wt=wt, n=n: e.dma_start(out=wt[:], in_=wv[:, :, n * 512:(n + 1) * 512]), w=[wk])
            for i, mod in enumerate((modp, mods)):
                b_ = 2 + i
                def mm(e, b_=b_, n=n, wt=wt, i=i):
                    e.matmul(banks[b_][:], lhsT=ones1[0:1, :], rhs=bada_t[0:1, n * 512:(n + 1) * 512],
                             start=True, stop=False)
                    for kc in range(8):
                        ins = e.matmul(banks[b_][:], lhsT=cT[i][:, kc, :], rhs=wt[:, kc, :],
                                       start=False, stop=(kc == 7))
                    return ins
                S.op("pe", mm, r=["ones1", "bada", f"cT{i}", wk], w=[bk(b_)])
                S.op("act", lambda e, b_=b_, mod=mod, n=n: e.activation(
                    out=mod[:, n * 512:(n + 1) * 512], in_=banks[b_][:], func=AF.Copy),
                    r=[bk(b_)], w=[("mod", i, n)])
        for i, mod in enumerate((modp, mods)):
            for j in (1, 4):
                S.op("dve", lambda e, mod=mod, j=j: e.tensor_scalar_add(
                    out=mod[:, j * D:(j + 1) * D], in0=mod[:, j * D:(j + 1) * D], scalar1=1.0),
                    r=[], w=[("mod", i, 2 * j), ("mod", i, 2 * j + 1)])

    for i, mod in enumerate((modp, mods)):
        for jj, j in enumerate((2, 3, 4, 5)):
            S.op("dve" if jj % 2 == 0 else "act",
                 (lambda e, i=i, jj=jj, j=j, mod=mod: e.tensor_copy(out=modD[:, i, jj, :], in_=mod[:, j * D:(j + 1) * D]))
                 if jj % 2 == 0 else
                 (lambda e, i=i, jj=jj, j=j, mod=mod: e.activation(out=modD[:, i, jj, :], in_=mod[:, j * D:(j + 1) * D],
                                                                  func=AF.Copy)),
                 r=[("mod", i, 2 * j), ("mod", i, 2 * j + 1)], w=[("modD", i, jj)])
    S.full_barrier()
    if stop <= 0:
        S.finish()
        return nc, es, S

    def modkeys(i, j):
        return [("mod", i, 2 * j), ("mod", i, 2 * j + 1)]

    hT_stack = ExitStack()
    hT = hT_stack.enter_context(nc.sbuf_tensor("hT", [128, 8, NTOK], BF16))
    with ExitStack() as ph:
        def psb(name, shape, dt):
            return ph.enter_context(nc.sbuf_tensor("s_" + name, list(shape), dt))
        xt = [psb(f"xt{i}", [128, D], F32) for i in range(2)]
        ht = [psb(f"ht{i}", [128, D], F32) for i in range(2)]
        for t in range(NT):
            i = 0 if t < NPT else 1
            mod = modp if t < NPT else mods
            src = xp[t * 128:(t + 1) * 128, :] if t < NPT else xs
            x_ = xt[t % 2]; h_ = ht[t % 2]
            xk = f"xt{t % 2}"; hk = f"ht{t % 2}"
            S.dma("sp", lambda e, x_=x_, src=src: e.dma_start(out=x_[:], in_=src), w=[xk])
            S.op("dve", lambda e, x_=x_, h_=h_, mod=mod: e.tensor_tensor(
                out=h_[:], in0=x_[:], in1=mod[:, D:2 * D], op=ALU.mult), r=[xk] + modkeys(i, 1), w=[hk])
            S.op("dve", lambda e, h_=h_, mod=mod: e.tensor_tensor(
                out=h_[:], in0=h_[:], in1=mod[:, 0:D], op=ALU.add), r=[hk] + modkeys(i, 0), w=[hk])
            for half in range(2):
                b_ = (t % 2) * 2 + half
                def tr4(e, b_=b_, half=half, h_=h_):
                    for j in range(4):
                        kc = half * 4 + j
                        ins = e.transpose(out=banks[b_][:, j * 128:(j + 1) * 128],
                                          in_=h_[:, kc * 128:(kc + 1) * 128], identity=ident_f[:])
                    return ins
                S.op("pe", tr4, r=[hk, "ident_f"], w=[bk(b_)])
                eng = "act" if half == 0 else "dve"
                if eng == "act":
                    S.op("act", lambda e, b_=b_, half=half, t=t: e.activation(
                        out=hT[:, half * 4:(half + 1) * 4, t * 128:(t + 1) * 128],
                        in_=banks[b_][:].rearrange("p (a b) -> p a b", a=4), func=AF.Copy),
                        r=[bk(b_)], w=[("hT", t, half)])
                else:
                    S.op("dve", lambda e, b_=b_, half=half, t=t: e.tensor_copy(
                        out=hT[:, half * 4:(half + 1) * 4, t * 128:(t + 1) * 128],
                        in_=banks[b_][:].rearrange("p (a b) -> p a b", a=4)),
                        r=[bk(b_)], w=[("hT", t, half)])

    S.full_barrier()
    if stop <= 1:
        S.finish()
        return nc, es, S
    with ExitStack() as ph:
        def psb(name, shape, dt):
            return ph.enter_context(nc.sbuf_tensor("s_" + name, list(shape), dt))
        wf = [psb(f"wf{i}", [128, 8, 512], F32) for i in range(2)]
        wb = [psb(f"wb{i}", [128, 8, 512], BF16) for i in range(2)]
        stg = [psb(f"stg{i}", [128, 512], BF16) for i in range(4)]
        stg32 = [psb(f"stg32_{i}", [128, 512], F32) for i in range(2)]
        wv = w_in.rearrange("(kc p) n -> p kc n", p=128)
        it = 0
        i32 = 0
        for cb in (range(17) if cbs is None else cbs):
            wf_ = wf[cb % 2]; wb_ = wb[cb % 2]
            wfk = f"wf{cb % 2}"; wbk = f"wb{cb % 2}"
            S.dma("sp", lambda e, wf_=wf_, cb=cb: e.dma_start(out=wf_[:], in_=wv[:, :, cb * 512:(cb + 1) * 512]),
                  w=[wfk])
            S.op("dve", lambda e, wf_=wf_, wb_=wb_: e.tensor_copy(out=wb_[:, 0:4, :], in_=wf_[:, 0:4, :]),
                 r=[wfk], w=[(wbk, 0)])
            S.op("act", lambda e, wf_=wf_, wb_=wb_: e.activation(out=wb_[:, 4:8, :], in_=wf_[:, 4:8, :], func=AF.Copy),
                 r=[wfk], w=[(wbk, 1)])
            scale = 1.0
            if cb == 1:
                scale = 128.0 ** -0.5
            if 4 <= cb <= 6:
                scale = 0.125
            for t in range(NT):
                b_ = it % 4
                sg = stg[it % 4]; sgk = f"stg{it % 4}"
                it += 1
                def mm(e, b_=b_, t=t, wb_=wb_):
                    for kc in range(8):
                        ins = e.matmul(banks[b_][:], lhsT=hT[:, kc, t * 128:(t + 1) * 128], rhs=wb_[:, kc, :],
                                       start=(kc == 0), stop=(kc == 7))
                    return ins
                S.op("pe", mm, r=[("hT", t, 0), ("hT", t, 1), (wbk, 0), (wbk, 1)], w=[bk(b_)])
                S.op("act", lambda e, b_=b_, sg=sg, scale=scale: e.activation(
                    out=sg[:], in_=banks[b_][:], func=AF.Copy, scale=scale), r=[bk(b_)], w=[sgk])
                S.dma("sp", lambda e, sg=sg, t=t, cb=cb: e.dma_start(
                    out=P[t * 128:(t + 1) * 128, cb * 512:(cb + 1) * 512], in_=sg[:]),
                    r=[sgk], w=[("P", t, cb)])
                if 7 <= cb <= 12:
                    g = (cb - 7) % 3
                    kv = (cb - 7) // 3
                    win = GROUPS[g][0]
                    if t < NPT and t * 128 >= SEQ - win:
                        s32 = stg32[i32 % 2]; s32k = f"stg32_{i32 % 2}"; i32 += 1
                        S.op("act", lambda e, b_=b_, s32=s32: e.activation(out=s32[:], in_=banks[b_][:], func=AF.Copy),
                             r=[bk(b_)], w=[s32k])
                        r0 = t * 128 - (SEQ - win)
                        S.dma("sp", lambda e, s32=s32, g=g, kv=kv, r0=r0: e.dma_start(
                            out=cpo[g][r0:r0 + 128, kv, :], in_=s32[:]), r=[s32k], w=[("cpo", g, kv, t)])
                    if t == NPT and not nocso:
                        s32 = stg32[i32 % 2]; s32k = f"stg32_{i32 % 2}"; i32 += 1
                        S.op("act", lambda e, b_=b_, s32=s32: e.activation(out=s32[:], in_=banks[b_][:], func=AF.Copy),
                             r=[bk(b_)], w=[s32k])
                        for b in range(NSEQ_S):
                            S.dma("sp", lambda e, s32=s32, g=g, kv=kv, b=b, win=win: e.dma_start(
                                out=cso[g][b, win - TS:win, kv, :], in_=s32[b * TS:(b + 1) * TS, :]),
                                r=[s32k], w=[("cso_new", g, kv, b)])

    S.full_barrier()
    hT_stack.close()
    mod_stack.close()
    if stop <= 2:
        S.finish()
        return nc, es, S

    def bcast_mid(ap2d, n):
        return ap2d.unsqueeze(1).to_broadcast([128, n, ap2d.shape[1]])

    def bcast_last(ap2d, n):
        return ap2d.unsqueeze(2).to_broadcast([128, ap2d.shape[1], n])

    def v4(ap2d):
        return ap2d.rearrange("p (h d) -> p h d", h=4)

    with ExitStack() as ph:
        def psb(name, shape, dt):
            return ph.enter_context(nc.sbuf_tensor("s_" + name, list(shape), dt))
        intra_t = [psb(f"intra{i}", [128, 512], F32) for i in range(2)]
        qdec_t = [psb(f"qdec{i}", [128, 512], F32) for i in range(2)]
        kdec_t = [psb(f"kdec{i}", [128, 4], F32) for i in range(2)]
        rowmask_t = psb("rowmask", [128, NSEQ_S], F32)
        for i in range(2):
            S.dma("sp", lambda e, i=i: e.dma_start(out=intra_t[i][:], in_=intra_d[i]), w=[f"intra{i}"])
            S.dma("sp", lambda e, i=i: e.dma_start(out=qdec_t[i][:], in_=qdec_d[i]), w=[f"qdec{i}"])
            S.dma("sp", lambda e, i=i: e.dma_start(out=kdec_t[i][:], in_=kdec_d[i]), w=[f"kdec{i}"])
        S.dma("sp", lambda e: e.dma_start(out=rowmask_t[:], in_=rowmask_d), w=["rowmask"])
        qin = [psb(f"qin{i}", [128, 512], BF16) for i in range(2)]
        kin = [psb(f"kin{i}", [128, 512], BF16) for i in range(2)]
        vin = [psb(f"vin{i}", [128, 512], BF16) for i in range(2)]
        gin = [psb(f"gin{i}", [128, 512], BF16) for i in range(2)]
        rc = [psb(f"rc{i}", [128, 128], F32) for i in range(2)]
        rs = [psb(f"rs{i}", [128, 128], F32) for i in range(2)]
        At = psb("rotA", [128, 512], F32)
        Bt = psb("rotB", [128, 512], F32)
        qr = psb("qr", [128, 512], BF16)
        kr = psb("kr", [128, 512], BF16)
        kd = psb("kd", [128, 512], BF16)
        qT = psb("qT", [128, 4, 128], BF16)
        qdT = psb("qdT", [128, 4, 128], BF16)
        kT = psb("kT", [128, 4, 128], BF16)
        PTr = psb("PTr", [128, 4, 128], BF16)
        Sst = psb("Sst", [128, 4, 128], F32)
        Sb = psb("Sb", [128, 4, 128], BF16)
        o_sb = psb("o_sb", [128, 512], F32)
        osq = psb("osq", [128, 512], F32)
        sil = psb("sil", [128, 512], F32)
        retg = [psb(f"retg{i}", [128, 512], BF16) for i in range(2)]
        ssum = psb("ssum", [128, 4], F32); ssq = psb("ssq", [128, 4], F32)
        mean = psb("mean", [128, 4], F32); msq = psb("msq", [128, 4], F32)
        var = psb("var", [128, 4], F32); rstd = psb("rstd", [128, 4], F32)
        S0f = psb("S0f", [128, NSEQ_S, 4, 128], F32)
        S0b = psb("S0b", [128, NSEQ_S, 4, 128], BF16)
        qdTm = psb("qdTm", [128, NSEQ_S, 4, 128], BF16)
        kdm = psb("kdm", [128, NSEQ_S, 512], BF16)
        Snew = [psb(f"Snew{i}", [128, 4, 128], F32) for i in range(2)]
        S.dma("sp", lambda e: e.dma_start(out=S0f[:], in_=st_in.rearrange("b h k v -> k b h v")), w=["S0f"])
        S.op("act", lambda e: e.activation(out=S0b[:], in_=S0f[:], func=AF.Copy), r=["S0f"], w=["S0b"])
        S.op("dve", lambda e: e.memset(qdTm[:], 0.0), w=["qdTm"])
        bq = banks[0][:].bitcast(BF16)[:, 0:512]
        bkk = banks[1][:].bitcast(BF16)[:, 0:512]

        for t in range(NT):
            i = 0 if t < NPT else 1
            par = t % 2
            cdec = CST["cdec_p"] if i == 0 else CST["cdec_s"]
            rows = slice(t * 128, (t + 1) * 128)
            for name, tl, c0 in (("qin", qin, 0), ("kin", kin, 512), ("vin", vin, 1024), ("gin", gin, 1536)):
                S.dma("sp", lambda e, tl=tl, c0=c0: e.dma_start(out=tl[par][:], in_=P[rows, c0:c0 + 512]),
                      r=[("P", t, c0 // 512)], w=[f"{name}{par}"])
            S.dma("sp", lambda e: e.dma_start(out=rc[par][:], in_=rotc_d[rows, :]), w=[f"rc{par}"])
            S.dma("sp", lambda e: e.dma_start(out=rs[par][:], in_=rots_d[rows, :]), w=[f"rs{par}"])

            def rotary(src, srck, dst, dstk):
                s4 = v4(src[:]); a4 = v4(At[:]); b4 = v4(Bt[:])
                S.op("dve", lambda e: e.tensor_tensor(out=a4, in0=s4, in1=bcast_mid(rc[par][:, :], 4), op=ALU.mult),
                     r=[srck, f"rc{par}"], w=["rotA"])
                S.op("dve", lambda e: e.tensor_tensor(out=b4[:, :, 0:64], in0=s4[:, :, 64:128],
                                                      in1=bcast_mid(rs[par][:, 0:64], 4), op=ALU.mult),
                     r=[srck, f"rs{par}"], w=["rotB0"])
                S.op("dve", lambda e: e.tensor_tensor(out=b4[:, :, 64:128], in0=s4[:, :, 0:64],
                                                      in1=bcast_mid(rs[par][:, 64:128], 4), op=ALU.mult),
                     r=[srck, f"rs{par}"], w=["rotB1"])
                S.op("dve", lambda e: e.tensor_tensor(out=dst[:], in0=At[:], in1=Bt[:], op=ALU.add),
                     r=["rotA", "rotB0", "rotB1"], w=[dstk])
            rotary(qin[par], f"qin{par}", qr, "qr")
            rotary(kin[par], f"kin{par}", kr, "kr")
            S.op("dve", lambda e: e.tensor_tensor(out=v4(kd[:]), in0=v4(kr[:]), in1=bcast_last(kdec_t[i][:, :], 128),
                                                  op=ALU.mult), r=["kr", f"kdec{i}"], w=["kd"])

            def tr4(e, src, dstb):
                for h in range(4):
                    ins = e.transpose(out=dstb[:, h * 128:(h + 1) * 128], in_=src[:, h * 128:(h + 1) * 128],
                                      identity=ident_b[:])
                return ins
            S.op("pe", lambda e: tr4(e, qr, bq), r=["qr", "ident_b"], w=[bk(0)])
            S.op("act", lambda e: e.activation(out=qT[:].rearrange("p h d -> p (h d)"), in_=bq, func=AF.Copy),
                 r=[], w=[bk(0), "qT"])
            S.op("dve", lambda e: e.tensor_tensor(out=qdT[:].rearrange("p h d -> p (h d)"), in0=bq,
                                                  in1=qdec_t[i][:], op=ALU.mult),
                 r=[f"qdec{i}"], w=[bk(0), "qdT"])
            S.op("pe", lambda e: tr4(e, kr, bkk), r=["kr", "ident_b"], w=[bk(1)])
            S.op("act", lambda e: e.activation(out=kT[:].rearrange("p h d -> p (h d)"), in_=bkk, func=AF.Copy),
                 r=[], w=[bk(1), "kT"])

            def sc4(e):
                for h in range(4):
                    ins = e.matmul(banks[2][:, h * 128:(h + 1) * 128], lhsT=kT[:, h, :], rhs=qT[:, h, :],
                                   start=True, stop=True)
                return ins
            S.op("pe", sc4, r=["kT", "qT"], w=[bk(2)])
            S.op("dve", lambda e: e.tensor_tensor(out=PTr[:].rearrange("p h d -> p (h d)"), in0=banks[2][:],
                                                  in1=intra_t[i][:], op=ALU.mult),
                 r=[f"intra{i}"], w=[bk(2), "PTr"])

            if i == 1:
                for b in range(NSEQ_S):
                    S.op("dve", lambda e, b=b: e.tensor_copy(out=qdTm[:, b, :, b * TS:(b + 1) * TS],
                                                             in_=qdT[:, :, b * TS:(b + 1) * TS]),
                         r=["qdT"], w=["qdTm"])
                    S.op("dve", lambda e, b=b: e.tensor_scalar(out=kdm[:, b, :], in0=kd[:], scalar1=rowmask_t[:, b:b + 1],
                                                               scalar2=None, op0=ALU.mult),
                         r=["kd", "rowmask"], w=[("kdm", b)])

            def o4(e):
                for h in range(4):
                    hs = slice(h * 128, (h + 1) * 128)
                    first_only = (i == 0 and t == 0)
                    ins = e.matmul(banks[3][:, hs], lhsT=PTr[:, h, :], rhs=vin[par][:, hs], start=True, stop=first_only)
                    if i == 0 and t > 0:
                        ins = e.matmul(banks[3][:, hs], lhsT=qdT[:, h, :], rhs=Sb[:, h, :], start=False, stop=True)
                    if i == 1:
                        for b in range(NSEQ_S):
                            ins = e.matmul(banks[3][:, hs], lhsT=qdTm[:, b, h, :], rhs=S0b[:, b, h, :],
                                           start=False, stop=(b == NSEQ_S - 1))
                return ins
            S.op("pe", o4, r=["PTr", f"vin{par}", "qdT", "Sb", "qdTm", "S0b"], w=[bk(3)])

            if i == 0:
                def ds4(e):
                    for h in range(4):
                        hs = slice(h * 128, (h + 1) * 128)
                        ins = e.matmul(banks[4][:, hs], lhsT=kd[:, hs], rhs=vin[par][:, hs], start=True, stop=True)
                    return ins
                S.op("pe", ds4, r=["kd", f"vin{par}"], w=[bk(4)])
                if t == 0:
                    S.op("dve", lambda e: e.tensor_copy(out=Sst[:].rearrange("p h d -> p (h d)"), in_=banks[4][:]),
                         r=[], w=[bk(4), "Sst"])
                else:
                    def upd(e):
                        for h in range(4):
                            ins = e.scalar_tensor_tensor(out=Sst[:, h, :], in0=Sst[:, h, :], scalar=cdec[h],
                                                         in1=banks[4][:, h * 128:(h + 1) * 128],
                                                         op0=ALU.mult, op1=ALU.add)
                        return ins
                    S.op("dve", upd, r=[], w=[bk(4), "Sst"])
                S.op("act", lambda e: e.activation(out=Sb[:], in_=Sst[:], func=AF.Copy), r=["Sst"], w=["Sb"])
                if t == NPT - 1:
                    S.dma("sp", lambda e: e.dma_start(out=srp.rearrange("h k v -> k h v"), in_=Sst[:]),
                          r=["Sst"], w=["srp"])
            else:
                for b in range(NSEQ_S):
                    bb = 4 + (b % 2)
                    sn = Snew[b % 2]; snk = f"Snew{b % 2}"
                    def ds4(e, b=b, bb=bb):
                        for h in range(4):
                            hs = slice(h * 128, (h + 1) * 128)
                            ins = e.matmul(banks[bb][:, hs], lhsT=kdm[:, b, hs], rhs=vin[par][:, hs],
                                           start=True, stop=True)
                        return ins
                    S.op("pe", ds4, r=[("kdm", b), f"vin{par}"], w=[bk(bb)])
                    def upd(e, b=b, bb=bb, sn=sn):
                        for h in range(4):
                            ins = e.scalar_tensor_tensor(out=sn[:, h, :], in0=S0f[:, b, h, :], scalar=cdec[h],
                                                         in1=banks[bb][:, h * 128:(h + 1) * 128],
                                                         op0=ALU.mult, op1=ALU.add)
                        return ins
                    S.op("dve", upd, r=["S0f"], w=[bk(bb), snk])
                    S.dma("sp", lambda e, b=b, sn=sn: e.dma_start(out=srs[b].rearrange("h k v -> k h v"), in_=sn[:]),
                          r=[snk], w=[("srs", b)])

            S.op("act", lambda e: e.activation(out=o_sb[:], in_=banks[3][:], func=AF.Copy), r=[], w=[bk(3), "o_sb"])
            S.op("dve", lambda e: e.tensor_reduce(out=ssum[:], in_=v4(o_sb[:]), axis=AX.X, op=ALU.add),
                 r=["o_sb"], w=["ssum"])
            S.op("act", lambda e: e.activation(out=osq[:], in_=o_sb[:], func=AF.Square), r=["o_sb"], w=["osq"])
            S.op("dve", lambda e: e.tensor_reduce(out=ssq[:], in_=v4(osq[:]), axis=AX.X, op=ALU.add),
                 r=["osq"], w=["ssq"])
            S.op("dve", lambda e: e.tensor_scalar_mul(out=mean[:], in0=ssum[:], scalar1=1.0 / 128), r=["ssum"], w=["mean"])
            S.op("dve", lambda e: e.tensor_tensor(out=msq[:], in0=mean[:], in1=mean[:], op=ALU.mult), r=["mean"], w=["msq"])
            S.op("dve", lambda e: e.scalar_tensor_tensor(out=var[:], in0=ssq[:], scalar=1.0 / 128, in1=msq[:],
                                                         op0=ALU.mult, op1=ALU.subtract), r=["ssq", "msq"], w=["var"])
            S.op("dve", lambda e: e.tensor_scalar_add(out=var[:], in0=var[:], scalar1=HN_EPS), r=["var"], w=["var"])
            S.op("act", lambda e: e.activation(out=var[:], in_=var[:], func=AF.Sqrt), r=["var"], w=["var"])
            S.op("dve", lambda e: e.reciprocal(out=rstd[:], in_=var[:]), r=["var"], w=["rstd"])
            S.op("dve", lambda e: e.tensor_tensor(out=v4(o_sb[:]), in0=v4(o_sb[:]), in1=bcast_last(mean[:, :], 128),
                                                  op=ALU.subtract), r=["o_sb", "mean", "osq"], w=["o_sb"])
            S.op("dve", lambda e: e.tensor_tensor(out=v4(o_sb[:]), in0=v4(o_sb[:]), in1=bcast_last(rstd[:, :], 128),
                                                  op=ALU.mult), r=["o_sb", "rstd"], w=["o_sb"])
            S.op("act", lambda e: e.activation(out=sil[:], in_=gin[par][:], func=AF.Silu), r=[f"gin{par}"], w=["sil"])
            S.op("dve", lambda e: e.tensor_tensor(out=retg[par][:], in0=o_sb[:], in1=sil[:], op=ALU.mult),
                 r=["o_sb", "sil"], w=[f"retg{par}"])
            S.dma("sp", lambda e: e.dma_start(out=RG[rows, :], in_=retg[par][:]), r=[f"retg{par}"], w=[("RG", t)])

    S.full_barrier()
    if stop <= 3:
        S.finish()
        return nc, es, S

    with ExitStack() as ph:
        def psb(name, shape, dt):
            return ph.enter_context(nc.sbuf_tensor("s_" + name, list(shape), dt))
        rb_aug = psb("rb_aug", [33, 24], F32)
        oh_t = [psb(f"oh{g}", [33, 385], F32) for g in range(3)]
        ext_sb = psb("ext_sb", [8, 385], F32)
        btmp = [psb(f"btmp{i}", [128, 256], F32) for i in range(2)]
        biasT = psb("biasT", [128, 24, 256], BF16)
        S.op("dve", lambda e: e.memset(rb_aug[:], 1.0), w=["rb_aug"])
        S.dma("sp", lambda e: e.dma_start(out=rb_aug[0:32, :], in_=rel_bias_d), w=["rb_aug"])
        for g in range(3):
            S.dma("sp", lambda e, g=g: e.dma_start(out=oh_t[g][:], in_=oh_d[g]), w=[f"oh{g}"])
            S.op("pe", lambda e, g=g: e.matmul(banks[0][0:8, 0:385], lhsT=rb_aug[:, g * 8:(g + 1) * 8], rhs=oh_t[g][:],
                                               start=True, stop=True), r=["rb_aug", f"oh{g}"], w=[bk(0)])
            S.op("act", lambda e: e.activation(out=ext_sb[:], in_=banks[0][0:8, 0:385], func=AF.Copy),
                 r=[], w=[bk(0), "ext_sb"])
            S.dma("sp", lambda e, g=g: e.dma_start(out=EXT[g * 8:(g + 1) * 8, :], in_=ext_sb[:]),
                  r=["ext_sb"], w=[("EXT", g)])
        for gh in range(24):
            bt = btmp[gh % 2]; btk = f"btmp{gh % 2}"
            srcb = bass.AP(tensor=EXT.tensor, offset=gh * 385, ap=[[0, 128], [1, 385]])
            dstb = bass.AP(tensor=EXT2.tensor, offset=gh * 128 * 385, ap=[[385, 128], [1, 385]])
            S.dma("sp", lambda e, srcb=srcb, dstb=dstb: e.dma_start(out=dstb, in_=srcb),
                  r=[("EXT", gh // 8)], w=[("EXT2", gh)])
            for kc, c0 in ((0, 256), (1, 128)):
                src = bass.AP(tensor=EXT2.tensor, offset=gh * 128 * 385 + c0, ap=[[384, 128], [1, 128]])
                S.dma("sp", lambda e, bt=bt, kc=kc, src=src: e.dma_start(out=bt[:, kc * 128:(kc + 1) * 128], in_=src),
                      r=[("EXT2", gh)], w=[(btk, kc)])
            S.op("dve", lambda e, bt=bt, gh=gh: e.tensor_copy(out=biasT[:, gh, :], in_=bt[:]),
                 r=[(btk, 0), (btk, 1)], w=[("biasT", gh)])

        Qt = [psb(f"Qt{i}", [128, 512], BF16) for i in range(2)]
        Kt = [psb(f"Kt{i}", [128, 512], BF16) for i in range(2)]
        QT = [psb(f"QT{i}", [128, 4, 128], BF16) for i in range(2)]
        KT = [psb(f"KT{i}", [128, 4, 128], BF16) for i in range(3)]
        Va = [psb(f"Va{i}", [128, 8, 65], BF16) for i in range(3)]
        PT = [psb(f"PT{i}", [128, 2, 2, 128], BF16) for i in range(2)]
        OLs = [psb(f"OLs{i}", [128, 520], F32) for i in range(2)]
        kvf = [psb(f"kvf{i}", [128, 2, 512], F32) for i in range(2)]
        for i in range(3):
            S.op("dve", lambda e, i=i: e.memset(Va[i][:], 1.0), w=[f"Va{i}"])
        for i in range(2):
            S.op("dve", lambda e, i=i: e.memset(Qt[i][:], 0.0), w=[f"Qt{i}"])
            S.op("dve", lambda e, i=i: e.memset(Kt[i][:], 0.0), w=[f"Kt{i}"])
        bq = banks[0][:].bitcast(BF16)[:, 0:512]
        bkk = banks[1][:].bitcast(BF16)[:, 0:512]
        state = {"blk": 0}

        def tr4(e, src, dstb):
            for j in range(4):
                ins = e.transpose(out=dstb[:, j * 128:(j + 1) * 128], in_=src[:, j * 128:(j + 1) * 128],
                                  identity=ident_b[:])
            return ins

        def attn_core(g, NQ, qT, qTk, kTc, kTck, kTp, kTpk, vc, vck, vp, vpk):
            blk = state["blk"]; state["blk"] += 1
            has_prev = kTp is not None
            for hp in range(4):
                bnk = banks[2 + hp]
                pt = PT[hp % 2]; ptk = f"PT{hp % 2}"
                def sc(e, hp=hp, bnk=bnk):
                    for hh in range(2):
                        h = 2 * hp + hh
                        gh = g * 8 + h
                        ps_ = slice((h % 2) * 64, (h % 2) * 64 + 64)
                        base = hh * 256
                        if has_prev:
                            e.matmul(bnk[:, base:base + NQ], lhsT=ident_b[:], rhs=biasT[:, gh, 0:NQ],
                                     start=True, stop=False)
                            e.matmul(bnk[:, base:base + NQ], lhsT=kTp[ps_, h // 2, :], rhs=qT[ps_, h // 2, 0:NQ],
                                     start=False, stop=True)
                        e.matmul(bnk[:, base + 128:base + 128 + NQ], lhsT=ident_b[:], rhs=biasT[:, gh, 128:128 + NQ],
                                 start=True, stop=False)
                        ins = e.matmul(bnk[:, base + 128:base + 128 + NQ], lhsT=kTc[ps_, h // 2, :],
                                       rhs=qT[ps_, h // 2, 0:NQ], start=False, stop=True)
                    return ins
                S.op("pe", sc, r=[qTk, kTck, "ident_b"] + ([kTpk] if has_prev else []) +
                     [("biasT", g * 8 + 2 * hp), ("biasT", g * 8 + 2 * hp + 1)], w=[bk(2 + hp)])
                bv = bnk[:].rearrange("p (a b c) -> p a b c", a=2, b=2)
                if has_prev:
                    S.op("act", lambda e, pt=pt, bv=bv: e.activation(out=pt[:, :, :, 0:NQ], in_=bv[:, :, :, 0:NQ],
                                                                     func=AF.Exp), r=[], w=[bk(2 + hp), ptk])
                else:
                    S.op("act", lambda e, pt=pt, bv=bv: e.activation(out=pt[:, :, 1, 0:NQ], in_=bv[:, :, 1, 0:NQ],
                                                                     func=AF.Exp), r=[], w=[bk(2 + hp), ptk])
                def pv(e, hp=hp, pt=pt):
                    for hh in range(2):
                        h = 2 * hp + hh
                        ob = banks[6 + h // 4]
                        oreg = ob[0:NQ, (h % 4) * 65:(h % 4) * 65 + 65]
                        if has_prev:
                            e.matmul(oreg, lhsT=pt[:, hh, 0, 0:NQ], rhs=vp[:, h, :], start=True, stop=False)
                        ins = e.matmul(oreg, lhsT=pt[:, hh, 1, 0:NQ], rhs=vc[:, h, :], start=(not has_prev), stop=True)
                    return ins
                S.op("pe", pv, r=[ptk, vck] + ([vpk] if has_prev else []), w=[bk(6 + hp // 2)])
            ol = OLs[blk % 2]; olk = f"OLs{blk % 2}"
            S.op("act", lambda e: e.activation(out=ol[0:NQ, 0:260], in_=banks[6][0:NQ, 0:260], func=AF.Copy),
                 r=[], w=[bk(6), (olk, 0)])
            S.op("dve", lambda e: e.tensor_copy(out=ol[0:NQ, 260:520], in_=banks[7][0:NQ, 0:260]),
                 r=[], w=[bk(7), (olk, 1)])
            return ol, olk

        att_events = []
        for g, (win, dil) in enumerate(GROUPS):
            prev_events = att_events
            att_events = []
            Pv = P[0:SEQ].rearrange("(u d) c -> d u c", d=dil)
            Av = ATT[0:SEQ].rearrange("(u d) c -> d u c", d=dil)
            nb = NPT // dil
            cnt = 0
            for r in range(dil):
                for ub in range(nb):
                    par = cnt % 2; p3 = cnt % 3; pp3 = (cnt - 1) % 3
                    cnt += 1
                    us = slice(ub * 128, (ub + 1) * 128)
                    rkeys = [("P", (r + dil * (ub * 128 + i)) // 128, 0) for i in (0, 127)]
                    S.dma("sp", lambda e: e.dma_start(out=Qt[par][:], in_=Pv[r, us, 2048 + g * 512:2048 + (g + 1) * 512]),
                          w=[f"Qt{par}"])
                    S.dma("sp", lambda e: e.dma_start(out=Kt[par][:], in_=Pv[r, us, 3584 + g * 512:3584 + (g + 1) * 512]),
                          w=[f"Kt{par}"])
                    S.dma("sp", lambda e: e.dma_start(
                        out=Va[p3][:, :, 0:64],
                        in_=Pv[r, us, 5120 + g * 512:5120 + (g + 1) * 512].rearrange("p (h d) -> p h d", h=8)),
                        w=[f"Va{p3}"])
                    S.op("pe", lambda e: tr4(e, Qt[par], bq), r=[f"Qt{par}", "ident_b"], w=[bk(0)])
                    S.op("act", lambda e: e.activation(out=QT[par][:].rearrange("p h d -> p (h d)"), in_=bq, func=AF.Copy),
                         r=[], w=[bk(0), f"QT{par}"])
                    S.op("pe", lambda e: tr4(e, Kt[par], bkk), r=[f"Kt{par}", "ident_b"], w=[bk(1)])
                    S.op("dve", lambda e: e.tensor_copy(out=KT[p3][:].rearrange("p h d -> p (h d)"), in_=bkk),
                         r=[], w=[bk(1), f"KT{p3}"])
                    hp_ = ub > 0
                    ol, olk = attn_core(g, 128, QT[par], f"QT{par}", KT[p3], f"KT{p3}",
                                        KT[pp3] if hp_ else None, f"KT{pp3}", Va[p3], f"Va{p3}",
                                        Va[pp3] if hp_ else None, f"Va{pp3}")
                    aq_ = "sp" if g == 0 else "pool"
                    if g > 0 and r == 0 and ub == 0:
                        for ev in prev_events:
                            S._wait(aq_, ev)
                    kw = {} if g == 0 else {"accum_op": ALU.add}
                    ev = S.dma(aq_, lambda e: e.dma_start(out=Av[r, us, :], in_=ol[:], **kw),
                               r=[(olk, 0), (olk, 1)], w=[("ATT", g, r, ub)])
                    att_events.append(ev)
        if stop > 4:
            for g, (win, dil) in enumerate(GROUPS):
                nq = TS // dil if dil <= TS else 1
                classes = list(range(min(dil, TS)))
                prev_events = att_events
                att_events = []
                cnt = 0
                for b in range(NSEQ_S):
                    cv = c_in[g][b].rearrange("(u d) k c -> d u k c", d=dil)
                    for r in classes:
                        par = cnt % 2; p3 = cnt % 3
                        cnt += 1
                        tok0 = SEQ + b * TS + r
                        rows = slice(tok0, tok0 + dil * (nq - 1) + 1, dil)
                        S.dma("sp", lambda e: e.dma_start(out=kvf[par][:], in_=cv[r]), w=[f"kvf{par}"])
                        S.dma("sp", lambda e: e.dma_start(out=Qt[par][0:nq, :],
                                                          in_=P[rows, 2048 + g * 512:2048 + (g + 1) * 512]),
                              r=[("P", NPT, 4 + g)], w=[f"Qt{par}"])
                        S.dma("sp", lambda e: e.dma_start(out=Kt[par][0:nq, :],
                                                          in_=P[rows, 3584 + g * 512:3584 + (g + 1) * 512]),
                              r=[("P", NPT, 7 + g)], w=[f"Kt{par}"])
                        vcur = Va[(2 * cnt) % 3]; vck = f"Va{(2 * cnt) % 3}"
                        vprv = Va[(2 * cnt + 1) % 3]; vpk = f"Va{(2 * cnt + 1) % 3}"
                        S.dma("sp", lambda e: e.dma_start(
                            out=vcur[0:nq, :, 0:64],
                            in_=P[rows, 5120 + g * 512:5120 + (g + 1) * 512].rearrange("p (h d) -> p h d", h=8)),
                            r=[("P", NPT, 10 + g)], w=[vck])
                        S.op("act", lambda e: e.activation(out=Kt[1 - par][:], in_=kvf[par][:, 0, :], func=AF.Copy),
                             r=[f"kvf{par}"], w=[f"Kt{1 - par}"])
                        S.op("dve", lambda e: e.tensor_copy(out=vprv[:, :, 0:64],
                                                            in_=kvf[par][:, 1, :].rearrange("p (h d) -> p h d", h=8)),
                             r=[f"kvf{par}"], w=[vpk])
                        S.op("pe", lambda e: tr4(e, Qt[par], bq), r=[f"Qt{par}", "ident_b"], w=[bk(0)])
                        S.op("act", lambda e: e.activation(out=QT[par][:].rearrange("p h d -> p (h d)"), in_=bq,
                                                           func=AF.Copy), r=[], w=[bk(0), f"QT{par}"])
                        S.op("pe", lambda e: tr4(e, Kt[par], bkk), r=[f"Kt{par}", "ident_b"], w=[bk(1)])
                        S.op("dve", lambda e: e.tensor_copy(out=KT[0][:].rearrange("p h d -> p (h d)"), in_=bkk),
                             r=[], w=[bk(1), "KT0"])
                        S.op("pe", lambda e: tr4(e, Kt[1 - par], bq), r=[f"Kt{1 - par}", "ident_b"], w=[bk(0)])
                        S.op("act", lambda e: e.activation(out=KT[1][:].rearrange("p h d -> p (h d)"), in_=bq,
                                                           func=AF.Copy), r=[], w=[bk(0), "KT1"])
                        ol, olk = attn_core(g, 8, QT[par], f"QT{par}", KT[0], "KT0", KT[1], "KT1",
                                            vcur, vck, vprv, vpk)
                        aq_ = "sp" if g == 0 else "pool"
                        if g > 0 and cnt == 1:
                            for ev in prev_events:
                                S._wait(aq_, ev)
                        kw = {} if g == 0 else {"accum_op": ALU.add}
                        ev = S.dma(aq_, lambda e: e.dma_start(out=ATT[rows, :], in_=ol[0:nq, :], **kw),
                                   r=[(olk, 0), (olk, 1)], w=[("ATTs", g, b, r)])
                        att_events.append(ev)

    S.full_barrier()
    if stop <= 5:
        S.finish()
        return nc, es, S

    with ExitStack() as ph:
        def psb(name, shape, dt):
            return ph.enter_context(nc.sbuf_tensor("s_" + name, list(shape), dt))
        wro_b = psb("wro_b", [128, 4, D], BF16)
        wao_b = psb("wao_b", [128, 4, D], BF16)
        wout_b = psb("wout_b", [128, 8, D], BF16)
        wqs = [psb(f"wqs{i}", [128, 8, 512], BF16) for i in range(2)]
        WQv = WQB.rearrange("(kc p) n -> p kc n", p=128)
        keysT = psb("keysT", [128, 16, 128], BF16)
        bout_t = psb("bout_t", [128, D], F32)
        lnt = {k: psb(k + "_t", [128, D], F32) for k in ("ln1_g", "ln1_b", "ln2_g", "ln2_b")}
        iota16 = psb("iota16", [128, 16], F32)
        S.dma("sp", lambda e: e.dma_start(out=bout_t[:], in_=b_out_rep), w=["bout_t"])
        for k in lnt:
            S.dma("sp", lambda e, k=k: e.dma_start(out=lnt[k][:], in_=ln_rep[k]), w=[k + "_t"])
        S.dma("sp", lambda e: e.dma_start(out=iota16[:], in_=iota16_d), w=["iota16"])
        with ExitStack() as wl:
            wst = [wl.enter_context(nc.sbuf_tensor(f"s_wst{i}", [128, 4, 1024], F32)) for i in range(2)]
            jobs = []
            for kc0 in (0,):
                jobs.append((w_ret_o.rearrange("(kc p) n -> p kc n", p=128), wro_b[:, :, :], "wro_b"))
                jobs.append((w_att_o.rearrange("(kc p) n -> p kc n", p=128), wao_b[:, :, :], "wao_b"))
            wov = w_out.rearrange("(kc p) n -> p kc n", p=128)
            jobs.append((wov[:, 0:4, :], wout_b[:, 0:4, :], ("wout_b", 0)))
            jobs.append((wov[:, 4:8, :], wout_b[:, 4:8, :], ("wout_b", 1)))
            for j, (src, dst, key) in enumerate(jobs):
                st_ = wst[j % 2]; stk = f"wst{j % 2}"
                S.dma("sp", lambda e, st_=st_, src=src: e.dma_start(out=st_[:], in_=src), w=[stk])
                if j % 2 == 0:
                    S.op("dve", lambda e, st_=st_, dst=dst: e.tensor_copy(out=dst, in_=st_[:]), r=[stk], w=[key])
                else:
                    S.op("act", lambda e, st_=st_, dst=dst: e.activation(out=dst, in_=st_[:], func=AF.Copy),
                         r=[stk], w=[key])
            kst = wst[0]
            for q4 in range(4):
                S.dma("sp", lambda e, q4=q4: e.dma_start(
                    out=kst[:, q4, 0:512].rearrange("p (a c) -> p a c", a=4),
                    in_=peer_keys[q4 * 512:(q4 + 1) * 512, :].rearrange("(a p) c -> p a c", p=128)), w=["wst0"])
            for q4 in range(4):
                def trk(e, q4=q4):
                    for a in range(4):
                        ins = e.transpose(out=banks[q4][:, a * 128:(a + 1) * 128], in_=kst[:, q4, a * 128:(a + 1) * 128],
                                          identity=ident_f[:])
                    return ins
                S.op("pe", trk, r=["wst0", "ident_f"], w=[bk(q4)])
                S.op("act", lambda e, q4=q4: e.activation(
                    out=keysT[:, q4 * 4:(q4 + 1) * 4, :].rearrange("p a k -> p (a k)"), in_=banks[q4][:], func=AF.Copy),
                    r=[], w=[bk(q4), ("keysT", q4)])
            S.full_barrier()
        wkeys = ["wro_b", "wao_b", ("wout_b", 0), ("wout_b", 1)]
        wqkeys = [("wq_b", a, b) for a in range(2) for b in range(2)]

        x_t = psb("x_t", [128, D], F32)
        att_t = psb("att_t", [128, 520], F32)
        rg_t = psb("rg_t", [128, 512], BF16)
        attn_t = psb("attn_t", [128, 512], BF16)
        rl = psb("rl", [128, 8], F32)
        aT = psb("aT", [128, 4, 128], BF16)
        rT = psb("rT", [128, 4, 128], BF16)
        bufA = psb("bufA", [128, 2048], F32)
        bufB = psb("bufB", [128, 2048], F32)
        tmp1 = psb("tmp1", [128, D], F32)
        x1 = psb("x1", [128, D], F32)
        h2 = psb("h2", [128, D], F32)
        h2b = psb("h2b", [128, D], BF16)
        h2T = psb("h2T", [128, 8, 128], BF16)
        junk = psb("junk", [128, D], BF16)
        st1 = psb("st1", [128, 8], F32)
        sv = psb("sv", [128, 16, 16], F32)
        si = psb("si", [128, 16, 16], U32)
        sif = psb("sif", [128, 16, 16], F32)
        cv = psb("cv", [128, 8, 16], F32)
        ci = psb("ci", [128, 8, 16], U32)
        hi_u = psb("hi_u", [128, 8, 16], U32); lo_u = psb("lo_u", [128, 8, 16], U32)
        hi_f = psb("hi_f", [128, 8, 16], F32); lo_f = psb("lo_f", [128, 8, 16], F32)
        i0 = psb("i0", [128, 8, 16], F32); i1 = psb("i1", [128, 8, 16], F32)
        e_f = psb("e_f", [128, 128], F32); e_i = psb("e_i", [128, 128], I32)
        gsm = psb("gsm", [128, 8, 16], F32); ssm = psb("ssm", [128, 8], F32)
        a_t = psb("a_t", [128, 128], F32); ga = psb("ga", [128, 128], BF16)
        NSL = 8
        Ug = [psb(f"Ug{i}", [128, D], BF16) for i in range(NSL)]
        Vg = [psb(f"Vg{i}", [128, D], BF16) for i in range(NSL)]
        Dg = [psb(f"Dg{i}", [128, 16, 128], BF16) for i in range(2)]
        Gb = bufA[:].bitcast(BF16)
        G_t = Gb[:, 0:2048]; sg_t = Gb[:, 2048:4096]
        Bb = bufB[:].bitcast(BF16)
        mixin = Bb[:, 0:1024]; mixT = Bb[:, 1024:2048].rearrange("p (k t) -> p k t", k=8)
        qpe = Bb[:, 0:2048]; qpT = Bb[:, 2048:4096].rearrange("p (k t) -> p k t", k=16)
        s_sc = bufA[:].rearrange("p (a k) -> p a k", a=16)
        oh4 = bufA[:].rearrange("p (h k i) -> p h k i", h=8, k=16)
        cand = bufB[:].rearrange("p (h c) -> p h c", h=8)
        cand4 = bufB[:].rearrange("p (h i j) -> p h i j", h=8, i=16)
        prod4 = bufB[:].rearrange("p (h k i) -> p h k i", h=8, k=16)
        bf_banks = [banks[i][:].bitcast(BF16) for i in range(8)]

        def layer_norm(src, srck, gk, bk_, dst, dstk):
            S.op("dve", lambda e: e.tensor_reduce(out=st1[:, 0:1], in_=src[:], axis=AX.X, op=ALU.add), r=[srck], w=["st_sum"])
            S.op("act", lambda e: e.activation(out=tmp1[:], in_=src[:], func=AF.Square), r=[srck], w=["tmp1"])
            S.op("dve", lambda e: e.tensor_reduce(out=st1[:, 1:2], in_=tmp1[:], axis=AX.X, op=ALU.add), r=["tmp1"], w=["st_sq"])
            S.op("dve", lambda e: e.tensor_scalar_mul(out=st1[:, 2:3], in0=st1[:, 0:1], scalar1=1.0 / D), r=["st_sum"], w=["st_mean"])
            S.op("dve", lambda e: e.tensor_tensor(out=st1[:, 3:4], in0=st1[:, 2:3], in1=st1[:, 2:3], op=ALU.mult),
                 r=["st_mean"], w=["st_msq"])
            S.op("dve", lambda e: e.scalar_tensor_tensor(out=st1[:, 4:5], in0=st1[:, 1:2], scalar=1.0 / D, in1=st1[:, 3:4],
                                                         op0=ALU.mult, op1=ALU.subtract), r=["st_sq", "st_msq"], w=["st_var"])
            S.op("dve", lambda e: e.tensor_scalar_add(out=st1[:, 4:5], in0=st1[:, 4:5], scalar1=LN_EPS), r=["st_var"], w=["st_var"])
            S.op("act", lambda e: e.activation(out=st1[:, 5:6], in_=st1[:, 4:5], func=AF.Sqrt), r=["st_var"], w=["st_std"])
            S.op("dve", lambda e: e.reciprocal(out=st1[:, 6:7], in_=st1[:, 5:6]), r=["st_std"], w=["st_rstd"])
            S.op("dve", lambda e: e.tensor_scalar(out=src[:], in0=src[:], scalar1=st1[:, 2:3], scalar2=st1[:, 6:7],
                                                  op0=ALU.subtract, op1=ALU.mult), r=[srck, "st_mean", "st_rstd"], w=[srck])
            S.op("dve", lambda e: e.tensor_tensor(out=src[:], in0=src[:], in1=lnt[gk][:], op=ALU.mult), r=[srck, gk + "_t"], w=[srck])
            S.op("dve", lambda e: e.tensor_tensor(out=dst[:], in0=src[:], in1=lnt[bk_][:], op=ALU.add), r=[srck, bk_ + "_t"], w=[dstk])

        def tr_n(e, src, dstb, n):
            for j in range(n):
                ins = e.transpose(out=dstb[:, j * 128:(j + 1) * 128], in_=src[:, j * 128:(j + 1) * 128], identity=ident_b[:])
            return ins

        gslot = {"u": 0, "v": 0}
        x1_2 = [x1, psb("x1b", [128, D], F32)]
        e_i2 = [e_i, psb("e_ib", [128, 128], I32)]
        tlist = list(range(NT) if tiles is None else tiles)

        def ctx(n):
            t = tlist[n]
            return dict(t=t, i=0 if t < NPT else 1, rows=slice(t * 128, (t + 1) * 128),
                        xsrc=xp[t * 128:(t + 1) * 128, :] if t < NPT else xs,
                        ydst=yp[t * 128:(t + 1) * 128, :] if t < NPT else ys,
                        x1c=x1_2[n % 2], x1k=f"x1_{n % 2}", e_ic=e_i2[n % 2], e_ik=f"e_i_{n % 2}")

        def stage1(t, i, rows, xsrc, ydst, x1c, x1k, e_ic, e_ik):
                S.dma("sp", lambda e: e.dma_start(out=x_t[:], in_=xsrc), w=["x_t"])
                S.dma("sp", lambda e: e.dma_start(out=att_t[:], in_=ATT[rows, :]), w=["att_t"])
                S.dma("sp", lambda e: e.dma_start(out=rg_t[:], in_=RG[rows, :]), r=[("RG", t)], w=["rg_t"])
                S.dma("sp", lambda e: e.dma_start(out=G_t, in_=P[rows, 6656:8704]), w=["bufA"])
                a3 = att_t[:].rearrange("p (h c) -> p h c", h=8)
                S.op("dve", lambda e: e.reciprocal(out=rl[:], in_=a3[:, :, 64]), r=["att_t"], w=["rl"])
                S.op("dve", lambda e: e.tensor_tensor(out=attn_t[:].rearrange("p (h d) -> p h d", h=8), in0=a3[:, :, 0:64],
                                                      in1=bcast_last(rl[:, :], 64), op=ALU.mult), r=["att_t", "rl"], w=["attn_t"])
                S.op("pe", lambda e: tr_n(e, attn_t, bf_banks[0], 4), r=["attn_t", "ident_b"], w=[bk(0)])
                S.op("act", lambda e: e.activation(out=aT[:].rearrange("p k t -> p (k t)"), in_=bf_banks[0][:, 0:512], func=AF.Copy),
                     r=[], w=[bk(0), "aT"])
                S.op("pe", lambda e: tr_n(e, rg_t, bf_banks[1], 4), r=["rg_t", "ident_b"], w=[bk(1)])
                S.op("dve", lambda e: e.tensor_copy(out=rT[:].rearrange("p k t -> p (k t)"), in_=bf_banks[1][:, 0:512]),
                     r=[], w=[bk(1), "rT"])
                S.op("act", lambda e: e.activation(out=sg_t, in_=G_t, func=AF.Sigmoid), r=["bufA"], w=["bufA"])

                def proj4(e, srcT, w_b, b0):
                    for half in range(2):
                        for kc in range(4):
                            ins = e.matmul(banks[b0 + half][:], lhsT=srcT[:, kc, :], rhs=w_b[:, kc, half * 512:(half + 1) * 512],
                                           start=(kc == 0), stop=(kc == 3))
                    return ins
                S.op("pe", lambda e: proj4(e, rT, wro_b, 2), r=["rT", "wro_b"], w=[bk(2), bk(3)])
                S.op("pe", lambda e: proj4(e, aT, wao_b, 4), r=["aT", "wao_b"], w=[bk(4), bk(5)])
                for half in range(2):
                    hs_ = slice(half * 512, (half + 1) * 512)
                    S.op("dve", lambda e, half=half, hs_=hs_: e.tensor_tensor(out=tmp1[:, hs_], in0=banks[2 + half][:],
                                                                              in1=sg_t[:, hs_], op=ALU.mult),
                         r=["bufA"], w=[bk(2 + half), ("tmp1h", half)])
                    S.op("dve", lambda e, half=half, hs_=hs_: e.tensor_tensor(
                        out=h2[:, hs_], in0=banks[4 + half][:], in1=sg_t[:, 1024 + half * 512:1024 + (half + 1) * 512],
                        op=ALU.mult), r=["bufA"], w=[bk(4 + half), ("h2h", half)])
                S.op("dve", lambda e: e.tensor_tensor(out=mixin, in0=tmp1[:], in1=h2[:], op=ALU.add),
                     r=[("tmp1h", 0), ("tmp1h", 1), ("h2h", 0), ("h2h", 1)], w=["bufB", "tmp1", "h2"])
                S.op("pe", lambda e: tr_n(e, mixin, bf_banks[0], 8), r=["bufB", "ident_b"], w=[bk(0)])
                S.op("act", lambda e: e.activation(out=mixT.rearrange("p k t -> p (k t)"), in_=bf_banks[0][:, 0:1024], func=AF.Copy),
                     r=[], w=[bk(0), "bufB"])

                def proj8(e, srcT, w_b, b0, nb_):
                    for nb in range(nb_):
                        for kc in range(8):
                            ins = e.matmul(banks[b0 + nb][:], lhsT=srcT[:, kc, :], rhs=w_b[:, kc, nb * 512:(nb + 1) * 512],
                                           start=(kc == 0), stop=(kc == 7))
                    return ins
                S.op("pe", lambda e: proj8(e, mixT, wout_b, 2, 2), r=["bufB", ("wout_b", 0), ("wout_b", 1)], w=[bk(2), bk(3)])
                for half in range(2):
                    hs_ = slice(half * 512, (half + 1) * 512)
                    S.op("dve", lambda e, half=half, hs_=hs_: e.tensor_tensor(out=h2[:, hs_], in0=banks[2 + half][:],
                                                                              in1=bout_t[:, hs_], op=ALU.add),
                         r=["bout_t", "h2"], w=[bk(2 + half), ("h2h", half)])
                S.op("dve", lambda e: e.tensor_tensor(out=h2[:], in0=h2[:], in1=modD[:, i, 0, :], op=ALU.mult),
                     r=[("h2h", 0), ("h2h", 1), ("modD", i, 0)], w=["h2"])
                S.op("dve", lambda e: e.scalar_tensor_tensor(out=x_t[:], in0=x_t[:], scalar=ALPHA, in1=h2[:],
                                                             op0=ALU.mult, op1=ALU.add), r=["x_t", "h2"], w=["x_t"])
                layer_norm(x_t, "x_t", "ln1_g", "ln1_b", x1c, x1k)
                S.op("dve", lambda e: e.tensor_tensor(out=h2[:], in0=x1c[:], in1=modD[:, i, 2, :], op=ALU.mult),
                     r=[x1k, ("modD", i, 2)], w=["h2"])
                S.op("dve", lambda e: e.tensor_tensor(out=h2b[:], in0=h2[:], in1=modD[:, i, 1, :], op=ALU.add),
                     r=["h2", ("modD", i, 1)], w=["h2b"])
                S.op("pe", lambda e: tr_n(e, h2b, bf_banks[1], 8), r=["h2b", "ident_b"], w=[bk(1)])
                S.op("act", lambda e: e.activation(out=h2T[:].rearrange("p k t -> p (k t)"), in_=bf_banks[1][:, 0:1024], func=AF.Copy),
                     r=[], w=[bk(1), "h2T"])
                for nb in range(4):
                    wq_ = wqs[nb % 2]; wqk_ = f"wqs{nb % 2}"
                    S.dma("sp", lambda e, wq_=wq_, nb=nb: e.dma_start(out=wq_[:], in_=WQv[:, :, nb * 512:(nb + 1) * 512]),
                          r=wqbkeys, w=[wqk_])
                    def pq(e, wq_=wq_, nb=nb):
                        for kc in range(8):
                            ins = e.matmul(banks[2 + nb][:], lhsT=h2T[:, kc, :], rhs=wq_[:, kc, :], start=(kc == 0), stop=(kc == 7))
                        return ins
                    S.op("pe", pq, r=["h2T", wqk_], w=[bk(2 + nb)])
                for nb in range(4):
                    if nb % 2 == 0:
                        S.op("act", lambda e, nb=nb: e.activation(out=qpe[:, nb * 512:(nb + 1) * 512], in_=banks[2 + nb][:], func=AF.Copy),
                             r=["bufB"], w=[bk(2 + nb), "bufB"])
                    else:
                        S.op("dve", lambda e, nb=nb: e.tensor_copy(out=qpe[:, nb * 512:(nb + 1) * 512], in_=banks[2 + nb][:]),
                             r=["bufB"], w=[bk(2 + nb), "bufB"])
                for half in range(2):
                    def trq(e, half=half):
                        for j in range(8):
                            hs = half * 8 + j
                            ins = e.transpose(out=bf_banks[half][:, j * 128:(j + 1) * 128], in_=qpe[:, hs * 128:(hs + 1) * 128],
                                              identity=ident_b[:])
                        return ins
                    S.op("pe", trq, r=["bufB", "ident_b"], w=[bk(half)])
                    if half == 0:
                        S.op("act", lambda e: e.activation(out=qpT[:, 0:8, :].rearrange("p k t -> p (k t)"), in_=bf_banks[0][:, 0:1024],
                                                           func=AF.Copy), r=["bufB"], w=[bk(0), "bufB"])
                    else:
                        S.op("dve", lambda e: e.tensor_copy(out=qpT[:, 8:16, :].rearrange("p k t -> p (k t)"), in_=bf_banks[1][:, 0:1024]),
                             r=["bufB"], w=[bk(1), "bufB"])
                for q4 in range(4):
                    def scq(e, q4=q4):
                        for a in range(4):
                            hs = q4 * 4 + a
                            ins = e.matmul(banks[2 + q4][:, a * 128:(a + 1) * 128], lhsT=qpT[:, hs, :], rhs=keysT[:, hs, :],
                                           start=True, stop=True)
                        return ins
                    S.op("pe", scq, r=["bufB", ("keysT", q4)], w=[bk(2 + q4)])
                    if q4 % 2 == 0:
                        S.op("act", lambda e, q4=q4: e.activation(out=bufA[:, q4 * 512:(q4 + 1) * 512], in_=banks[2 + q4][:], func=AF.Copy),
                             r=["bufA"], w=[bk(2 + q4), "bufA"])
                    else:
                        S.op("dve", lambda e, q4=q4: e.tensor_copy(out=bufA[:, q4 * 512:(q4 + 1) * 512], in_=banks[2 + q4][:]),
                             r=["bufA"], w=[bk(2 + q4), "bufA"])
                ssk = ["bufA"]

                def topk_rounds(n, vals, vk, outv, outvk, outi, outik):
                    for rnd in range(2):
                        sl = slice(rnd * 8, rnd * 8 + 8)
                        def mx(e, sl=sl):
                            for j in range(n):
                                ins = e.max(out=outv[:, j, sl], in_=vals[:, j, :])
                            return ins
                        S.op("dve", mx, r=vk, w=[(outvk, rnd)])
                        def mi(e, sl=sl):
                            for j in range(n):
                                ins = e.max_index(out=outi[:, j, sl], in_max=outv[:, j, sl], in_values=vals[:, j, :])
                            return ins
                        S.op("dve", mi, r=vk + [(outvk, rnd)], w=[(outik, rnd)])
                        if rnd == 0:
                            def mr(e, sl=sl):
                                for j in range(n):
                                    ins = e.match_replace(out=vals[:, j, :], in_to_replace=outv[:, j, sl], in_values=vals[:, j, :],
                                                          imm_value=-1e30)
                                return ins
                            S.op("dve", mr, r=[(outvk, rnd), (outik, rnd)], w=vk)
                topk_rounds(16, s_sc, ssk, sv, "sv", si, "si")
                sv4 = sv[:].rearrange("p (h s) k -> p h s k", s=2)
                S.op("dve", lambda e: e.tensor_tensor(
                    out=cand4, in0=sv4[:, :, 0, :].unsqueeze(3).to_broadcast([128, 8, 16, 16]),
                    in1=sv4[:, :, 1, :].unsqueeze(2).to_broadcast([128, 8, 16, 16]), op=ALU.add),
                    r=[("sv", 0), ("sv", 1), "bufB"], w=["bufB"])
                topk_rounds(8, cand, ["bufB"], cv, "cv", ci, "ci")
                cvk = [("cv", 0), ("cv", 1)]; cik = [("ci", 0), ("ci", 1)]
                S.op("dve", lambda e: e.tensor_tensor(out=gsm[:], in0=cv[:], in1=bcast_last(cv[:, :, 0], 16), op=ALU.subtract),
                     r=cvk, w=["gsm"])
                S.op("act", lambda e: e.activation(out=gsm[:], in_=gsm[:], func=AF.Exp), r=["gsm"], w=["gsm"])
                S.op("dve", lambda e: e.tensor_reduce(out=ssm[:], in_=gsm[:], axis=AX.X, op=ALU.add), r=["gsm"], w=["ssm"])
                S.op("dve", lambda e: e.reciprocal(out=ssm[:], in_=ssm[:]), r=["ssm"], w=["ssm"])
                S.op("dve", lambda e: e.tensor_tensor(out=gsm[:], in0=gsm[:], in1=bcast_last(ssm[:, :], 16), op=ALU.mult),
                     r=["gsm", "ssm"], w=["gsm"])
                S.op("dve", lambda e: e.tensor_single_scalar(out=hi_u[:], in_=ci[:], scalar=4, op=ALU.logical_shift_right),
                     r=cik, w=["hi_u"])
                S.op("dve", lambda e: e.tensor_single_scalar(out=lo_u[:], in_=ci[:], scalar=15, op=ALU.bitwise_and),
                     r=cik, w=["lo_u"])
                S.op("dve", lambda e: e.tensor_copy(out=hi_f[:], in_=hi_u[:]), r=["hi_u"], w=["hi_f"])
                S.op("dve", lambda e: e.tensor_copy(out=lo_f[:], in_=lo_u[:]), r=["lo_u"], w=["lo_f"])
                S.op("dve", lambda e: e.tensor_copy(out=sif[:], in_=si[:]), r=[("si", 0), ("si", 1)], w=["sif"])
                sif4 = sif[:].rearrange("p (h s) k -> p h s k", s=2)
                iot4 = iota16[:, :].unsqueeze(1).unsqueeze(1).to_broadcast([128, 8, 16, 16])
                for side, (xf, xk, dsti, dstk) in enumerate(((hi_f, "hi_f", i0, "i0"), (lo_f, "lo_f", i1, "i1"))):
                    S.op("dve", lambda e, xf=xf: e.tensor_tensor(
                        out=oh4, in0=iot4, in1=xf[:].unsqueeze(3).to_broadcast([128, 8, 16, 16]), op=ALU.is_equal),
                        r=[xk, "iota16"] + ssk, w=["bufA"])
                    S.op("dve", lambda e, side=side: e.tensor_tensor(
                        out=prod4, in0=oh4, in1=sif4[:, :, side, :].unsqueeze(2).to_broadcast([128, 8, 16, 16]), op=ALU.mult),
                        r=["bufA", "sif", "bufB"], w=["bufB"])
                    S.op("dve", lambda e, dsti=dsti: e.tensor_reduce(out=dsti[:], in_=prod4, axis=AX.X, op=ALU.add),
                         r=["bufB"], w=[dstk])
                S.op("dve", lambda e: e.scalar_tensor_tensor(out=e_f[:].rearrange("p (h k) -> p h k", h=8), in0=i0[:], scalar=128.0,
                                                             in1=i1[:], op0=ALU.mult, op1=ALU.add), r=["i0", "i1"], w=["e_f"])
                S.op("dve", lambda e: e.tensor_copy(out=e_ic[:], in_=e_f[:]), r=["e_f"], w=[e_ik])

        def stage2(t, i, rows, xsrc, ydst, x1c, x1k, e_ic, e_ik):
                for hk in range(128):
                    sl_ = gslot["u"] % NSL; gslot["u"] += 1
                    S.dma("pool", lambda e, sl_=sl_, hk=hk: e.indirect_dma_start(
                        out=Ug[sl_][:], out_offset=None, in_=UB,
                        in_offset=bass.IndirectOffsetOnAxis(ap=e_ic[:, hk:hk + 1], axis=0)), r=[e_ik] + ubkeys, w=[f"Ug{sl_}"])
                    S.op("dve", lambda e, sl_=sl_, hk=hk: e.scalar_tensor_tensor(
                        out=junk[:], in0=Ug[sl_][:], scalar=1.0, in1=h2b[:], op0=ALU.mult, op1=ALU.mult,
                        accum_out=a_t[:, hk:hk + 1]), r=[f"Ug{sl_}", "h2b"], w=["junk", ("a_t", hk)])
                S.op("act", lambda e: e.activation(out=a_t[:], in_=a_t[:], func=AF.Gelu), r=[("a_t", hk) for hk in range(128)], w=["a_g"])
                S.op("dve", lambda e: e.tensor_tensor(out=ga[:], in0=a_t[:], in1=gsm[:].rearrange("p h k -> p (h k)"), op=ALU.mult),
                     r=["a_g", "gsm"], w=["ga"])

        def stage3a(t, i, rows, xsrc, ydst, x1c, x1k, e_ic, e_ik):
                for h in range(8):
                    dg = Dg[h % 2]; dgk = f"Dg{h % 2}"
                    S.op("dve", lambda e, dg=dg, h=h: e.tensor_tensor(
                        out=dg[:], in0=ident_b[:, :].unsqueeze(1).to_broadcast([128, 16, 128]),
                        in1=ga[:, h * 16:(h + 1) * 16].unsqueeze(2).to_broadcast([128, 16, 128]), op=ALU.mult),
                        r=["ga", "ident_b"], w=[dgk])
                    for k in range(16):
                        hk = h * 16 + k
                        sl_ = gslot["v"] % NSL; gslot["v"] += 1
                        S.dma("pool", lambda e, sl_=sl_, hk=hk: e.indirect_dma_start(
                            out=Vg[sl_][:], out_offset=None, in_=VB,
                            in_offset=bass.IndirectOffsetOnAxis(ap=e_ic[:, hk:hk + 1], axis=0)), r=[e_ik] + vbkeys, w=[f"Vg{sl_}"])
                        def vmm(e, sl_=sl_, hk=hk, dg=dg, k=k):
                            for half in range(2):
                                ins = e.matmul(banks[6 + half][:], lhsT=dg[:, k, :], rhs=Vg[sl_][:, half * 512:(half + 1) * 512],
                                               start=(hk == 0), stop=(hk == 127))
                            return ins
                        S.op("pe", vmm, r=[f"Vg{sl_}", dgk], w=[bk(6), bk(7)])

        def stage3b(t, i, rows, xsrc, ydst, x1c, x1k, e_ic, e_ik):
                for half in range(2):
                    hs_ = slice(half * 512, (half + 1) * 512)
                    S.op("dve", lambda e, half=half, hs_=hs_: e.tensor_tensor(out=h2[:, hs_], in0=banks[6 + half][:],
                                                                              in1=modD[:, i, 3, hs_], op=ALU.mult),
                         r=[("modD", i, 3), "h2"], w=[bk(6 + half), ("h2h", half)])
                S.op("dve", lambda e: e.scalar_tensor_tensor(out=x1c[:], in0=x1c[:], scalar=ALPHA, in1=h2[:],
                                                             op0=ALU.mult, op1=ALU.add), r=[x1k, ("h2h", 0), ("h2h", 1)], w=[x1k, "h2"])
                layer_norm(x1c, x1k, "ln2_g", "ln2_b", x_t, "x_t")
                S.dma("sp", lambda e: e.dma_start(out=ydst, in_=x_t[:]), r=["x_t"], w=[("y", t)])


        stage1(**ctx(0))
        for n in range(len(tlist)):
            stage2(**ctx(n))
            stage3a(**ctx(n))
            if n + 1 < len(tlist):
                stage1(**ctx(n + 1))
            stage3b(**ctx(n))

    S.finish()
    return nc, es, S


def _shard_inputs(inp):
    c = _consts()
    maps = []
    f = np.ascontiguousarray
    for i in range(NCORES):
        sl = slice(i * NSEQ_S, (i + 1) * NSEQ_S)
        m = {
            "xp": f(inp["x_prompt"][i]),
            "xs": f(inp["x_sample"][sl].reshape(128, D)),
            "cp_rep": f(np.broadcast_to(inp["c_prompt"][i:i + 1], (128, D))),
            "cs_rep": f(np.repeat(inp["c_sample"][sl], TS, axis=0)),
            "st_in": f(inp["state_ret"][0, sl]),
            "c_in0": f(inp["cache_att_w128"][0, sl].reshape(NSEQ_S, 128, 2, 512)),
            "c_in1": f(inp["cache_att_w512"][0, sl].reshape(NSEQ_S, 512, 2, 512)),
            "c_in2": f(inp["cache_att_w2048"][0, sl].reshape(NSEQ_S, 2048, 2, 512)),
            "w_ada": f(inp["w_ada"][0]),
            "b_ada": f(inp["b_ada"][0:1]),
            "w_in": f(inp["w_in"][0]),
            "ident": c["ident"],
            "rotc": c["rotc"], "rots": c["rots"],
            "intraT_p": c["intraT_p"], "intraT_s": c["intraT_s"],
            "qdec_p": c["qdec_p"], "qdec_s": c["qdec_s"],
            "kdec_p": c["kdec_p"], "kdec_s": c["kdec_s"],
            "rowmask": c["rowmask"],
            "iota16": c["iota16"],
            "oh0": c["oh0"], "oh1": c["oh1"], "oh2": c["oh2"],
            "rel_bias": f(inp["rel_bias"]),
            "w_ret_o": f(inp["w_ret_o"][0]), "w_att_o": f(inp["w_att_o"][0]), "w_out": f(inp["w_out"][0]),
            "b_out_rep": f(np.broadcast_to(inp["b_out"][0:1], (128, D))),
            "ln1_g_rep": f(np.broadcast_to(inp["ln1_g"][0:1], (128, D))),
            "ln1_b_rep": f(np.broadcast_to(inp["ln1_b"][0:1], (128, D))),
            "ln2_g_rep": f(np.broadcast_to(inp["ln2_g"][0:1], (128, D))),
            "ln2_b_rep": f(np.broadcast_to(inp["ln2_b"][0:1], (128, D))),
            "peer_wq": f(inp["peer_wq"][0]), "peer_keys": f(inp["peer_keys"][0].reshape(2048, 128)),
            "peer_u": f(inp["peer_u"][0]), "peer_v": f(inp["peer_v"][0]),
        }
        maps.append(m)
    return maps


def kernel(**inputs):
    inp = {k: np.asarray(v) for k, v in inputs.items()}
    nc, es, S = build_program()
    with es:
        maps = _shard_inputs(inp)
        res = run_bass_kernel_spmd(nc, maps, core_ids=list(range(NCORES)))
    R = res.results
    yp = np.stack([R[i]["yp"] for i in range(NCORES)], 0)
    ys = np.concatenate([R[i]["ys"].reshape(NSEQ_S, TS, D) for i in range(NCORES)], 0)
    srp = np.stack([R[i]["srp"] for i in range(NCORES)], 0)[None]
    srs = np.concatenate([R[i]["srs"] for i in range(NCORES)], 0)[None]
    cpo = [np.stack([R[i][f"cpo{g}"] for i in range(NCORES)], 0).reshape(1, NCORES, GROUPS[g][0], 2, 8, 64)
           for g in range(3)]
    cso = [np.concatenate([R[i][f"cso{g}"] for i in range(NCORES)], 0).reshape(
        1, NCORES * NSEQ_S, GROUPS[g][0], 2, 8, 64) for g in range(3)]
    return (yp.astype(np.float32), ys.astype(np.float32), srp, cpo[0], cpo[1], cpo[2], srs, cso[0], cso[1], cso[2])
```

```python
import math
from contextlib import ExitStack

import numpy as np
import concourse.bass as bass
import concourse.mybir as mybir
from concourse.bass_utils import run_bass_kernel_spmd

F32 = mybir.dt.float32
BF16 = mybir.dt.bfloat16
U32 = mybir.dt.uint32
I32 = mybir.dt.int32
AF = mybir.ActivationFunctionType
ALU = mybir.AluOpType
AX = mybir.AxisListType

NCORES = 8
D = 1024
SEQ = 4096
NPT = 32
NT = 33
NTOK = NT * 128
NSEQ_S = 16
TS = 8
PAST = 8192
IN_COLS = 8704
GROUPS = ((128, 1), (512, 4), (2048, 16))
ALPHA = 2.0 ** 0.25
LN_EPS = 1e-5
HN_EPS = 1e-6
NEGB = -30000.0
SEM_LIMIT = 30000


class Sched:
    def __init__(self, nc, es):
        self.nc = nc
        self.es = es
        self.eng = {"pe": nc.tensor, "act": nc.scalar, "dve": nc.vector, "pool": nc.gpsimd, "sp": nc.sync}
        self.sem = {}
        self.cnt = {}
        self.nsem = 0
        for e in ("pe", "act", "dve", "pool"):
            self._new_engine_sem(e)
        self.known = {e: {} for e in self.eng}
        self.bufs = {}
        self.dpool = {}
        self.drr = {}
        for q, n in (("sp", 24), ("pool", 16), ("act", 8)):
            self.dpool[q] = [self._new_dma_slot() for _ in range(n)]
            self.drr[q] = 0
        self.ninstr = 0

    def _mksem(self, name):
        self.nsem += 1
        return self.es.enter_context(self.nc.semaphore(f"{name}_{self.nsem}"))

    def _new_engine_sem(self, e):
        self.sem[e] = self._mksem("c" + e)
        self.cnt[e] = 0

    def _new_dma_slot(self):
        return {"sem": self._mksem("d"), "val": 0}

    def _wait(self, e, ev):
        sem, val = ev
        k = self.known[e]
        if k.get(id(sem), 0) >= val:
            return
        self.eng[e].wait_ge(sem, val)
        self.ninstr += 1
        k[id(sem)] = val

    def _deps(self, r, w):
        deps = []
        for key in r:
            b = self.bufs.get(key)
            if b is not None and b["w"] is not None:
                deps.append(b["w"])
        for key in w:
            b = self.bufs.get(key)
            if b is not None:
                if b["w"] is not None:
                    deps.append(b["w"])
                deps.extend(b["r"].values())
        return deps

    def _record(self, ev, r, w):
        sem, val = ev
        for key in r:
            b = self.bufs.setdefault(key, {"w": None, "r": {}})
            old = b["r"].get(id(sem))
            if old is None or old[1] < val:
                b["r"][id(sem)] = ev
        for key in w:
            self.bufs[key] = {"w": ev, "r": {}}

    def defer_begin(self):
        self.deferq = []
        self.deferring = True

    def defer_end(self):
        self.deferring = False

    def flush(self, k=None):
        q = getattr(self, "deferq", [])
        n = len(q) if k is None else min(k, len(q))
        for _ in range(n):
            kind, e, fn, r, w = q.pop(0)
            (self.op if kind == "op" else self.dma)(e, fn, r, w)

    def op(self, e, fn, r=(), w=()):
        if getattr(self, "deferring", False):
            self.deferq.append(("op", e, fn, list(r), list(w)))
            return None
        deps = self._deps(r, w)
        own = self.sem[e]
        for ev in deps:
            if e == "pe" and ev[0] is own:
                continue
            self._wait(e, ev)
        ins = fn(self.eng[e])
        if self.cnt[e] >= SEM_LIMIT:
            self._new_engine_sem(e)
        self.cnt[e] += 1
        ins.then_inc(self.sem[e], 1)
        self.ninstr += 1
        ev = (self.sem[e], self.cnt[e])
        self._record(ev, r, w)
        return ev

    def dma(self, q, fn, r=(), w=()):
        if getattr(self, "deferring", False):
            self.deferq.append(("dma", q, fn, list(r), list(w)))
            return None
        deps = self._deps(r, w)
        pool = self.dpool[q]
        i = self.drr[q]
        self.drr[q] = (i + 1) % len(pool)
        slot = pool[i]
        if slot["val"] >= SEM_LIMIT:
            slot = pool[i] = self._new_dma_slot()
        if slot["val"] > 0:
            self._wait(q, (slot["sem"], slot["val"]))
        for ev in deps:
            self._wait(q, ev)
        ins = fn(self.eng[q])
        slot["val"] += 16
        ins.then_inc(slot["sem"], 16)
        self.ninstr += 1
        ev = (slot["sem"], slot["val"])
        self._record(ev, r, w)
        return ev

    def barrier(self, e, keys):
        for ev in self._deps((), keys):
            self._wait(e, ev)

    def full_barrier(self):
        evs = [(self.sem[x], self.cnt[x]) for x in ("pe", "act", "dve", "pool") if self.cnt[x] > 0]
        for pool in self.dpool.values():
            for slot in pool:
                if slot["val"] > 0:
                    evs.append((slot["sem"], slot["val"]))
        for e in ("pe", "act", "dve", "pool", "sp"):
            for ev in evs:
                if ev[0] is self.sem.get(e):
                    continue
                self._wait(e, ev)

    def finish(self):
        for q, pool in self.dpool.items():
            for slot in pool:
                if slot["val"] > 0:
                    self._wait("sp", (slot["sem"], slot["val"]))


def _t5_bucket(dist):
    d = dist.astype(np.float32)
    large = 16 + (np.log(np.maximum(d, 1.0) / 16) / math.log(2048 / 16) * 16)
    large = np.minimum(large.astype(np.int32), 31)
    return np.where(dist < 16, dist, large)


_CONST_CACHE = {}


def _consts():
    if _CONST_CACHE:
        return _CONST_CACHE
    c = _CONST_CACHE
    c["ident"] = np.eye(128, dtype=np.float32)
    pos = np.concatenate([np.arange(SEQ), PAST + (np.arange(128) % TS)]).astype(np.float32)
    inv = (10000.0 ** (-np.arange(64, dtype=np.float32) / 64)).astype(np.float32)
    ang = (pos[:, None] * inv[None, :]).astype(np.float32)
    cos = np.cos(ang).astype(np.float32); sin = np.sin(ang).astype(np.float32)
    c["rotc"] = np.concatenate([cos, cos], 1)
    c["rots"] = np.concatenate([-sin, sin], 1)
    lg = np.log1p(-np.exp2(-5.0 - np.arange(4, dtype=np.float64)))
    p = np.arange(128)
    for name, C in (("p", 128), ("s", TS)):
        tpos = p % C
        seq = p // C
        rel = tpos[None, :] - tpos[:, None]
        ok = (rel >= 0) & (seq[None, :] == seq[:, None])
        intra = np.where(ok[:, None, :], np.exp(lg[None, :, None] * np.maximum(rel, 0)[:, None, :]), 0.0)
        c["intraT_" + name] = intra.reshape(128, 512).astype(np.float32)
        qd = np.exp(lg[:, None] * (tpos[None, :] + 1.0))
        c["qdec_" + name] = np.broadcast_to(qd.reshape(1, 512), (128, 512)).astype(np.float32).copy()
        c["kdec_" + name] = np.exp(lg[None, :] * (C - 1.0 - tpos[:, None])).astype(np.float32)
        c["cdec_" + name] = [float(v) for v in np.exp(lg * C)]
    c["rowmask"] = (p[:, None] // TS == np.arange(NSEQ_S)[None, :]).astype(np.float32)
    c["iota16"] = np.broadcast_to(np.arange(16, dtype=np.float32)[None, :], (128, 16)).copy()
    for g, (win, dil) in enumerate(GROUPS):
        oh = np.zeros((33, 385), np.float32)
        for m in range(385):
            rel = m - 128
            if 0 <= rel <= 128:
                oh[int(_t5_bucket(np.array([rel * dil]))[0]), m] = 1.0
            else:
                oh[32, m] = NEGB
        c[f"oh{g}"] = oh
    return c


def build_program(stop=99, nocopy=False, cbs=None, nocso=False, tiles=None):
    nc = bass.Bass("TRN2", target_bir_lowering=False)
    es = ExitStack()
    S = Sched(nc, es)
    CST = _consts()

    def din(name, shape, dt=F32):
        return nc.dram_tensor(name, list(shape), dt, kind="ExternalInput").ap()

    def dout(name, shape, dt=F32):
        return nc.dram_tensor(name, list(shape), dt, kind="ExternalOutput").ap()

    def dscr(name, shape, dt):
        return nc.dram_tensor(name, list(shape), dt).ap()

    def sb(name, shape, dt):
        return es.enter_context(nc.sbuf_tensor("s_" + name, list(shape), dt))

    xp = din("xp", [SEQ, D]); xs = din("xs", [128, D])
    cp_rep = din("cp_rep", [128, D]); cs_rep = din("cs_rep", [128, D])
    st_in = din("st_in", [NSEQ_S, 4, 128, 128])
    c_in = [din(f"c_in{g}", [NSEQ_S, GROUPS[g][0], 2, 512]) for g in range(3)]
    w_ada = din("w_ada", [D, 6 * D]); b_ada = din("b_ada", [1, 6 * D])
    w_in = din("w_in", [D, IN_COLS])
    ident_d = din("ident", [128, 128])
    rotc_d = din("rotc", [NTOK, 128]); rots_d = din("rots", [NTOK, 128])
    intra_d = [din("intraT_p", [128, 512]), din("intraT_s", [128, 512])]
    qdec_d = [din("qdec_p", [128, 512]), din("qdec_s", [128, 512])]
    kdec_d = [din("kdec_p", [128, 4]), din("kdec_s", [128, 4])]
    rowmask_d = din("rowmask", [128, NSEQ_S])
    iota16_d = din("iota16", [128, 16])
    oh_d = [din(f"oh{g}", [33, 385]) for g in range(3)]
    rel_bias_d = din("rel_bias", [32, 24])
    w_ret_o = din("w_ret_o", [512, D]); w_att_o = din("w_att_o", [512, D]); w_out = din("w_out", [D, D])
    b_out_rep = din("b_out_rep", [128, D])
    ln_rep = {k: din(k + "_rep", [128, D]) for k in ("ln1_g", "ln1_b", "ln2_g", "ln2_b")}
    peer_wq = din("peer_wq", [D, 2048]); peer_keys = din("peer_keys", [2048, 128])
    peer_u = din("peer_u", [16384, D]); peer_v = din("peer_v", [16384, D])

    yp = dout("yp", [SEQ, D]); ys = dout("ys", [128, D])
    srp = dout("srp", [4, 128, 128]); srs = dout("srs", [NSEQ_S, 4, 128, 128])
    cpo = [dout(f"cpo{g}", [GROUPS[g][0], 2, 512]) for g in range(3)]
    cso = [dout(f"cso{g}", [NSEQ_S, GROUPS[g][0], 2, 512]) for g in range(3)]

    P = dscr("proj", [NTOK, IN_COLS], BF16)
    RG = dscr("retg", [NTOK, 512], BF16)
    ATT = dscr("attacc", [NTOK, 520], F32)
    EXT = dscr("biasext", [24, 385], F32)
    UB = dscr("peer_u_bf", [16384, D], BF16)
    VB = dscr("peer_v_bf", [16384, D], BF16)
    EXT2 = dscr("biasext2", [24, 128 * 385], F32)

    banks = [es.enter_context(nc.psum_tensor(f"bank{i}", [128, 512], F32)) for i in range(8)]

    def bk(i):
        return f"bank{i}"

    ident_f = sb("ident_f", [128, 128], F32)
    ident_b = sb("ident_b", [128, 128], BF16)
    ones1 = sb("ones1", [1, 128], F32)
    modD = sb("modD", [128, 2, 4, D], F32)
    mod_stack = ExitStack()
    modp = mod_stack.enter_context(nc.sbuf_tensor("s_modp", [128, 6 * D], F32))
    mods = mod_stack.enter_context(nc.sbuf_tensor("s_mods", [128, 6 * D], F32))

    S.dma("sp", lambda e: e.dma_start(out=ident_f[:], in_=ident_d), w=["ident_f"])
    S.op("dve", lambda e: e.tensor_copy(out=ident_b[:], in_=ident_f[:]), r=["ident_f"], w=["ident_b"])
    S.op("dve", lambda e: e.memset(ones1[:], 1.0), w=["ones1"])

    tabkeys = []
    for name, src_t, dst_t in (("UB", peer_u, UB), ("VB", peer_v, VB)):
        for c in range(8):
            rs_ = slice(c * 2048, (c + 1) * 2048)
            S.dma("pool", lambda e, src_t=src_t, dst_t=dst_t, rs_=rs_: e.dma_start(out=dst_t[rs_, :], in_=src_t[rs_, :]),
                  w=[(name, c)])
            tabkeys.append((name, c))
    WQB = dscr("peer_wq_bf", [D, 2048], BF16)
    wqbkeys = []
    for c in range(2):
        cs_ = slice(c * 1024, (c + 1) * 1024)
        S.dma("pool", lambda e, cs_=cs_: e.dma_start(out=WQB[:, cs_], in_=peer_wq[:, cs_]), w=[("WQB", c)])
        wqbkeys.append(("WQB", c))
    ubkeys = [k for k in tabkeys if k[0] == "UB"]
    vbkeys = [k for k in tabkeys if k[0] == "VB"]
    for g in range(0 if not nocopy else 3, 3):
        nb = GROUPS[g][0]
        for b in range(NSEQ_S):
            src = c_in[g][b, TS:nb].rearrange("(a r) k c -> a (r k c)", r=8)
            dst = cso[g][b, 0:nb - TS].rearrange("(a r) k c -> a (r k c)", r=8)
            S.dma("act", lambda e, s=src, d=dst: e.dma_start(out=d, in_=s), w=[("cso_copy", g, b)])

    with ExitStack() as ph:
        def psb(name, shape, dt):
            return ph.enter_context(nc.sbuf_tensor("s_" + name, list(shape), dt))
        c_tok = psb("c_tok", [128, D], F32)
        c_act = psb("c_act", [128, D], F32)
        cT = [psb(f"cT{i}", [128, 8, 128], F32) for i in range(2)]
        wada_t = [psb(f"wada{i}", [128, 8, 512], F32) for i in range(2)]
        bada_t = psb("bada", [1, 6 * D], F32)
        S.dma("sp", lambda e: e.dma_start(out=bada_t[:], in_=b_ada), w=["bada"])
        for i, src in enumerate((cp_rep, cs_rep)):
            S.dma("sp", lambda e, s=src: e.dma_start(out=c_tok[:], in_=s), w=["c_tok"])
            S.op("act", lambda e: e.activation(out=c_act[:], in_=c_tok[:], func=AF.Silu), r=["c_tok"], w=["c_act"])
            for half in range(2):
                def tr4(e, half=half):
                    for j in range(4):
                        kc = half * 4 + j
                        ins = e.transpose(out=banks[half][:, j * 128:(j + 1) * 128],
                                          in_=c_act[:, kc * 128:(kc + 1) * 128], identity=ident_f[:])
                    return ins
                S.op("pe", tr4, r=["c_act", "ident_f"], w=[bk(half)])
                S.op("dve", lambda e, half=half, i=i: e.tensor_copy(
                    out=cT[i][:, half * 4:(half + 1) * 4, :],
                    in_=banks[half][:].rearrange("p (a b) -> p a b", a=4)), r=[bk(half)], w=[f"cT{i}"])
        wv = w_ada.rearrange("(kc p) n -> p kc n", p=128)
        for n in range(12):
            wt = wada_t[n % 2]
            wk = f"wada{n % 2}"
            S.dma("sp", lambda e, wt=wt, n=n: e.dma_start(out=wt[:], in_=wv[:, :, n * 512:(n + 1) * 512]), w=[wk])
            for i, mod in enumerate((modp, mods)):
                b_ = 2 + i
                def mm(e, b_=b_, n=n, wt=wt, i=i):
                    e.matmul(banks[b_][:], lhsT=ones1[0:1, :], rhs=bada_t[0:1, n * 512:(n + 1) * 512],
                             start=True, stop=False)
                    for kc in range(8):
                        ins = e.matmul(banks[b_][:], lhsT=cT[i][:, kc, :], rhs=wt[:, kc, :],
                                       start=False, stop=(kc == 7))
                    return ins
                S.op("pe", mm, r=["ones1", "bada", f"cT{i}", wk], w=[bk(b_)])
                S.op("act", lambda e, b_=b_, mod=mod, n=n: e.activation(
                    out=mod[:, n * 512:(n + 1) * 512], in_=banks[b_][:], func=AF.Copy),
                    r=[bk(b_)], w=[("mod", i, n)])
        for i, mod in enumerate((modp, mods)):
            for j in (1, 4):
                S.op("dve", lambda e, mod=mod, j=j: e.tensor_scalar_add(
                    out=mod[:, j * D:(j + 1) * D], in0=mod[:, j * D:(j + 1) * D], scalar1=1.0),
                    r=[], w=[("mod", i, 2 * j), ("mod", i, 2 * j + 1)])

    for i, mod in enumerate((modp, mods)):
        for jj, j in enumerate((2, 3, 4, 5)):
            S.op("dve" if jj % 2 == 0 else "act",
                 (lambda e, i=i, jj=jj, j=j, mod=mod: e.tensor_copy(out=modD[:, i, jj, :], in_=mod[:, j * D:(j + 1) * D]))
                 if jj % 2 == 0 else
                 (lambda e, i=i, jj=jj, j=j, mod=mod: e.activation(out=modD[:, i, jj, :], in_=mod[:, j * D:(j + 1) * D],
                                                                  func=AF.Copy)),
                 r=[("mod", i, 2 * j), ("mod", i, 2 * j + 1)], w=[("modD", i, jj)])
    S.full_barrier()
    if stop <= 0:
        S.finish()
        return nc, es, S

    def modkeys(i, j):
        return [("mod", i, 2 * j), ("mod", i, 2 * j + 1)]

    hT_stack = ExitStack()
    hT = hT_stack.enter_context(nc.sbuf_tensor("hT", [128, 8, NTOK], BF16))
    with ExitStack() as ph:
        def psb(name, shape, dt):
            return ph.enter_context(nc.sbuf_tensor("s_" + name, list(shape), dt))
        xt = [psb(f"xt{i}", [128, D], F32) for i in range(2)]
        ht = [psb(f"ht{i}", [128, D], F32) for i in range(2)]
        for t in range(NT):
            i = 0 if t < NPT else 1
            mod = modp if t < NPT else mods
            src = xp[t * 128:(t + 1) * 128, :] if t < NPT else xs
            x_ = xt[t % 2]; h_ = ht[t % 2]
            xk = f"xt{t % 2}"; hk = f"ht{t % 2}"
            S.dma("sp", lambda e, x_=x_, src=src: e.dma_start(out=x_[:], in_=src), w=[xk])
            S.op("dve", lambda e, x_=x_, h_=h_, mod=mod: e.tensor_tensor(
                out=h_[:], in0=x_[:], in1=mod[:, D:2 * D], op=ALU.mult), r=[xk] + modkeys(i, 1), w=[hk])
            S.op("dve", lambda e, h_=h_, mod=mod: e.tensor_tensor(
                out=h_[:], in0=h_[:], in1=mod[:, 0:D], op=ALU.add), r=[hk] + modkeys(i, 0), w=[hk])
            for half in range(2):
                b_ = (t % 2) * 2 + half
                def tr4(e, b_=b_, half=half, h_=h_):
                    for j in range(4):
                        kc = half * 4 + j
                        ins = e.transpose(out=banks[b_][:, j * 128:(j + 1) * 128],
                                          in_=h_[:, kc * 128:(kc + 1) * 128], identity=ident_f[:])
                    return ins
                S.op("pe", tr4, r=[hk, "ident_f"], w=[bk(b_)])
                eng = "act" if half == 0 else "dve"
                if eng == "act":
                    S.op("act", lambda e, b_=b_, half=half, t=t: e.activation(
                        out=hT[:, half * 4:(half + 1) * 4, t * 128:(t + 1) * 128],
                        in_=banks[b_][:].rearrange("p (a b) -> p a b", a=4), func=AF.Copy),
                        r=[bk(b_)], w=[("hT", t, half)])
                else:
                    S.op("dve", lambda e, b_=b_, half=half, t=t: e.tensor_copy(
                        out=hT[:, half * 4:(half + 1) * 4, t * 128:(t + 1) * 128],
                        in_=banks[b_][:].rearrange("p (a b) -> p a b", a=4)),
                        r=[bk(b_)], w=[("hT", t, half)])

    S.full_barrier()
    if stop <= 1:
        S.finish()
        return nc, es, S
    with ExitStack() as ph:
        def psb(name, shape, dt):
            return ph.enter_context(nc.sbuf_tensor("s_" + name, list(shape), dt))
        wf = [psb(f"wf{i}", [128, 8, 512], F32) for i in range(2)]
        wb = [psb(f"wb{i}", [128, 8, 512], BF16) for i in range(2)]
        stg = [psb(f"stg{i}", [128, 512], BF16) for i in range(4)]
        stg32 = [psb(f"stg32_{i}", [128, 512], F32) for i in range(2)]
        wv = w_in.rearrange("(kc p) n -> p kc n", p=128)
        it = 0
        i32 = 0
        for cb in (range(17) if cbs is None else cbs):
            wf_ = wf[cb % 2]; wb_ = wb[cb % 2]
            wfk = f"wf{cb % 2}"; wbk = f"wb{cb % 2}"
            S.dma("sp", lambda e, wf_=wf_, cb=cb: e.dma_start(out=wf_[:], in_=wv[:, :, cb * 512:(cb + 1) * 512]),
                  w=[wfk])
            S.op("dve", lambda e, wf_=wf_, wb_=wb_: e.tensor_copy(out=wb_[:, 0:4, :], in_=wf_[:, 0:4, :]),
                 r=[wfk], w=[(wbk, 0)])
            S.op("act", lambda e, wf_=wf_, wb_=wb_: e.activation(out=wb_[:, 4:8, :], in_=wf_[:, 4:8, :], func=AF.Copy),
                 r=[wfk], w=[(wbk, 1)])
            scale = 1.0
            if cb == 1:
                scale = 128.0 ** -0.5
            if 4 <= cb <= 6:
                scale = 0.125
            for t in range(NT):
                b_ = it % 4
                sg = stg[it % 4]; sgk = f"stg{it % 4}"
                it += 1
                def mm(e, b_=b_, t=t, wb_=wb_):
                    for kc in range(8):
                        ins = e.matmul(banks[b_][:], lhsT=hT[:, kc, t * 128:(t + 1) * 128], rhs=wb_[:, kc, :],
                                       start=(kc == 0), stop=(kc == 7))
                    return ins
                S.op("pe", mm, r=[("hT", t, 0), ("hT", t, 1), (wbk, 0), (wbk, 1)], w=[bk(b_)])
                S.op("act", lambda e, b_=b_, sg=sg, scale=scale: e.activation(
                    out=sg[:], in_=banks[b_][:], func=AF.Copy, scale=scale), r=[bk(b_)], w=[sgk])
                S.dma("sp", lambda e, sg=sg, t=t, cb=cb: e.dma_start(
                    out=P[t * 128:(t + 1) * 128, cb * 512:(cb + 1) * 512], in_=sg[:]),
                    r=[sgk], w=[("P", t, cb)])
                if 7 <= cb <= 12:
                    g = (cb - 7) % 3
                    kv = (cb - 7) // 3
                    win = GROUPS[g][0]
                    if t < NPT and t * 128 >= SEQ - win:
                        s32 = stg32[i32 % 2]; s32k = f"stg32_{i32 % 2}"; i32 += 1
                        S.op("act", lambda e, b_=b_, s32=s32: e.activation(out=s32[:], in_=banks[b_][:], func=AF.Copy),
                             r=[bk(b_)], w=[s32k])
                        r0 = t * 128 - (SEQ - win)
                        S.dma("sp", lambda e, s32=s32, g=g, kv=kv, r0=r0: e.dma_start(
                            out=cpo[g][r0:r0 + 128, kv, :], in_=s32[:]), r=[s32k], w=[("cpo", g, kv, t)])
                    if t == NPT and not nocso:
                        s32 = stg32[i32 % 2]; s32k = f"stg32_{i32 % 2}"; i32 += 1
                        S.op("act", lambda e, b_=b_, s32=s32: e.activation(out=s32[:], in_=banks[b_][:], func=AF.Copy),
                             r=[bk(b_)], w=[s32k])
                        for b in range(NSEQ_S):
                            S.dma("sp", lambda e, s32=s32, g=g, kv=kv, b=b, win=win: e.dma_start(
                                out=cso[g][b, win - TS:win, kv, :], in_=s32[b * TS:(b + 1) * TS, :]),
                                r=[s32k], w=[("cso_new", g, kv, b)])

    S.full_barrier()
    hT_stack.close()
    mod_stack.close()
    if stop <= 2:
        S.finish()
        return nc, es, S

    def bcast_mid(ap2d, n):
        return ap2d.unsqueeze(1).to_broadcast([128, n, ap2d.shape[1]])

    def bcast_last(ap2d, n):
        return ap2d.unsqueeze(2).to_broadcast([128, ap2d.shape[1], n])

    def v4(ap2d):
        return ap2d.rearrange("p (h d) -> p h d", h=4)

    with ExitStack() as ph:
        def psb(name, shape, dt):
            return ph.enter_context(nc.sbuf_tensor("s_" + name, list(shape), dt))
        intra_t = [psb(f"intra{i}", [128, 512], F32) for i in range(2)]
        qdec_t = [psb(f"qdec{i}", [128, 512], F32) for i in range(2)]
        kdec_t = [psb(f"kdec{i}", [128, 4], F32) for i in range(2)]
        rowmask_t = psb("rowmask", [128, NSEQ_S], F32)
        for i in range(2):
            S.dma("sp", lambda e, i=i: e.dma_start(out=intra_t[i][:], in_=intra_d[i]), w=[f"intra{i}"])
            S.dma("sp", lambda e, i=i: e.dma_start(out=qdec_t[i][:], in_=qdec_d[i]), w=[f"qdec{i}"])
            S.dma("sp", lambda e, i=i: e.dma_start(out=kdec_t[i][:], in_=kdec_d[i]), w=[f"kdec{i}"])
        S.dma("sp", lambda e: e.dma_start(out=rowmask_t[:], in_=rowmask_d), w=["rowmask"])
        qin = [psb(f"qin{i}", [128, 512], BF16) for i in range(2)]
        kin = [psb(f"kin{i}", [128, 512], BF16) for i in range(2)]
        vin = [psb(f"vin{i}", [128, 512], BF16) for i in range(2)]
        gin = [psb(f"gin{i}", [128, 512], BF16) for i in range(2)]
        rc = [psb(f"rc{i}", [128, 128], F32) for i in range(2)]
        rs = [psb(f"rs{i}", [128, 128], F32) for i in range(2)]
        At = psb("rotA", [128, 512], F32)
        Bt = psb("rotB", [128, 512], F32)
        qr = psb("qr", [128, 512], BF16)
        kr = psb("kr", [128, 512], BF16)
        kd = psb("kd", [128, 512], BF16)
        qT = psb("qT", [128, 4, 128], BF16)
        qdT = psb("qdT", [128, 4, 128], BF16)
        kT = psb("kT", [128, 4, 128], BF16)
        PTr = psb("PTr", [128, 4, 128], BF16)
        Sst = psb("Sst", [128, 4, 128], F32)
        Sb = psb("Sb", [128, 4, 128], BF16)
        o_sb = psb("o_sb", [128, 512], F32)
        osq = psb("osq", [128, 512], F32)
        sil = psb("sil", [128, 512], F32)
        retg = [psb(f"retg{i}", [128, 512], BF16) for i in range(2)]
        ssum = psb("ssum", [128, 4], F32); ssq = psb("ssq", [128, 4], F32)
        mean = psb("mean", [128, 4], F32); msq = psb("msq", [128, 4], F32)
        var = psb("var", [128, 4], F32); rstd = psb("rstd", [128, 4], F32)
        S0f = psb("S0f", [128, NSEQ_S, 4, 128], F32)
        S0b = psb("S0b", [128, NSEQ_S, 4, 128], BF16)
        qdTm = psb("qdTm", [128, NSEQ_S, 4, 128], BF16)
        kdm = psb("kdm", [128, NSEQ_S, 512], BF16)
        Snew = [psb(f"Snew{i}", [128, 4, 128], F32) for i in range(2)]
        S.dma("sp", lambda e: e.dma_start(out=S0f[:], in_=st_in.rearrange("b h k v -> k b h v")), w=["S0f"])
        S.op("act", lambda e: e.activation(out=S0b[:], in_=S0f[:], func=AF.Copy), r=["S0f"], w=["S0b"])
        S.op("dve", lambda e: e.memset(qdTm[:], 0.0), w=["qdTm"])
        bq = banks[0][:].bitcast(BF16)[:, 0:512]
        bkk = banks[1][:].bitcast(BF16)[:, 0:512]

        for t in range(NT):
            i = 0 if t < NPT else 1
            par = t % 2
            cdec = CST["cdec_p"] if i == 0 else CST["cdec_s"]
            rows = slice(t * 128, (t + 1) * 128)
            for name, tl, c0 in (("qin", qin, 0), ("kin", kin, 512), ("vin", vin, 1024), ("gin", gin, 1536)):
                S.dma("sp", lambda e, tl=tl, c0=c0: e.dma_start(out=tl[par][:], in_=P[rows, c0:c0 + 512]),
                      r=[("P", t, c0 // 512)], w=[f"{name}{par}"])
            S.dma("sp", lambda e: e.dma_start(out=rc[par][:], in_=rotc_d[rows, :]), w=[f"rc{par}"])
            S.dma("sp", lambda e: e.dma_start(out=rs[par][:], in_=rots_d[rows, :]), w=[f"rs{par}"])

            def rotary(src, srck, dst, dstk):
                s4 = v4(src[:]); a4 = v4(At[:]); b4 = v4(Bt[:])
                S.op("dve", lambda e: e.tensor_tensor(out=a4, in0=s4, in1=bcast_mid(rc[par][:, :], 4), op=ALU.mult),
                     r=[srck, f"rc{par}"], w=["rotA"])
                S.op("dve", lambda e: e.tensor_tensor(out=b4[:, :, 0:64], in0=s4[:, :, 64:128],
                                                      in1=bcast_mid(rs[par][:, 0:64], 4), op=ALU.mult),
                     r=[srck, f"rs{par}"], w=["rotB0"])
                S.op("dve", lambda e: e.tensor_tensor(out=b4[:, :, 64:128], in0=s4[:, :, 0:64],
                                                      in1=bcast_mid(rs[par][:, 64:128], 4), op=ALU.mult),
                     r=[srck, f"rs{par}"], w=["rotB1"])
                S.op("dve", lambda e: e.tensor_tensor(out=dst[:], in0=At[:], in1=Bt[:], op=ALU.add),
                     r=["rotA", "rotB0", "rotB1"], w=[dstk])
            rotary(qin[par], f"qin{par}", qr, "qr")
            rotary(kin[par], f"kin{par}", kr, "kr")
            S.op("dve", lambda e: e.tensor_tensor(out=v4(kd[:]), in0=v4(kr[:]), in1=bcast_last(kdec_t[i][:, :], 128),
                                                  op=ALU.mult), r=["kr", f"kdec{i}"], w=["kd"])

            def tr4(e, src, dstb):
                for h in range(4):
                    ins = e.transpose(out=dstb[:, h * 128:(h + 1) * 128], in_=src[:, h * 128:(h + 1) * 128],
                                      identity=ident_b[:])
                return ins
            S.op("pe", lambda e: tr4(e, qr, bq), r=["qr", "ident_b"], w=[bk(0)])
            S.op("act", lambda e: e.activation(out=qT[:].rearrange("p h d -> p (h d)"), in_=bq, func=AF.Copy),
                 r=[], w=[bk(0), "qT"])
            S.op("dve", lambda e: e.tensor_tensor(out=qdT[:].rearrange("p h d -> p (h d)"), in0=bq,
                                                  in1=qdec_t[i][:], op=ALU.mult),
                 r=[f"qdec{i}"], w=[bk(0), "qdT"])
            S.op("pe", lambda e: tr4(e, kr, bkk), r=["kr", "ident_b"], w=[bk(1)])
            S.op("act", lambda e: e.activation(out=kT[:].rearrange("p h d -> p (h d)"), in_=bkk, func=AF.Copy),
                 r=[], w=[bk(1), "kT"])

            def sc4(e):
                for h in range(4):
                    ins = e.matmul(banks[2][:, h * 128:(h + 1) * 128], lhsT=kT[:, h, :], rhs=qT[:, h, :],
                                   start=True, stop=True)
                return ins
            S.op("pe", sc4, r=["kT", "qT"], w=[bk(2)])
            S.op("dve", lambda e: e.tensor_tensor(out=PTr[:].rearrange("p h d -> p (h d)"), in0=banks[2][:],
                                                  in1=intra_t[i][:], op=ALU.mult),
                 r=[f"intra{i}"], w=[bk(2), "PTr"])

            if i == 1:
                for b in range(NSEQ_S):
                    S.op("dve", lambda e, b=b: e.tensor_copy(out=qdTm[:, b, :, b * TS:(b + 1) * TS],
                                                             in_=qdT[:, :, b * TS:(b + 1) * TS]),
                         r=["qdT"], w=["qdTm"])
                    S.op("dve", lambda e, b=b: e.tensor_scalar(out=kdm[:, b, :], in0=kd[:], scalar1=rowmask_t[:, b:b + 1],
                                                               scalar2=None, op0=ALU.mult),
                         r=["kd", "rowmask"], w=[("kdm", b)])

            def o4(e):
                for h in range(4):
                    hs = slice(h * 128, (h + 1) * 128)
                    first_only = (i == 0 and t == 0)
                    ins = e.matmul(banks[3][:, hs], lhsT=PTr[:, h, :], rhs=vin[par][:, hs], start=True, stop=first_only)
                    if i == 0 and t > 0:
                        ins = e.matmul(banks[3][:, hs], lhsT=qdT[:, h, :], rhs=Sb[:, h, :], start=False, stop=True)
                    if i == 1:
                        for b in range(NSEQ_S):
                            ins = e.matmul(banks[3][:, hs], lhsT=qdTm[:, b, h, :], rhs=S0b[:, b, h, :],
                                           start=False, stop=(b == NSEQ_S - 1))
                return ins
            S.op("pe", o4, r=["PTr", f"vin{par}", "qdT", "Sb", "qdTm", "S0b"], w=[bk(3)])

            if i == 0:
                def ds4(e):
                    for h in range(4):
                        hs = slice(h * 128, (h + 1) * 128)
                        ins = e.matmul(banks[4][:, hs], lhsT=kd[:, hs], rhs=vin[par][:, hs], start=True, stop=True)
                    return ins
                S.op("pe", ds4, r=["kd", f"vin{par}"], w=[bk(4)])
                if t == 0:
                    S.op("dve", lambda e: e.tensor_copy(out=Sst[:].rearrange("p h d -> p (h d)"), in_=banks[4][:]),
                         r=[], w=[bk(4), "Sst"])
                else:
                    def upd(e):
                        for h in range(4):
                            ins = e.scalar_tensor_tensor(out=Sst[:, h, :], in0=Sst[:, h, :], scalar=cdec[h],
                                                         in1=banks[4][:, h * 128:(h + 1) * 128],
                                                         op0=ALU.mult, op1=ALU.add)
                        return ins
                    S.op("dve", upd, r=[], w=[bk(4), "Sst"])
                S.op("act", lambda e: e.activation(out=Sb[:], in_=Sst[:], func=AF.Copy), r=["Sst"], w=["Sb"])
                if t == NPT - 1:
                    S.dma("sp", lambda e: e.dma_start(out=srp.rearrange("h k v -> k h v"), in_=Sst[:]),
                          r=["Sst"], w=["srp"])
            else:
                for b in range(NSEQ_S):
                    bb = 4 + (b % 2)
                    sn = Snew[b % 2]; snk = f"Snew{b % 2}"
                    def ds4(e, b=b, bb=bb):
                        for h in range(4):
                            hs = slice(h * 128, (h + 1) * 128)
                            ins = e.matmul(banks[bb][:, hs], lhsT=kdm[:, b, hs], rhs=vin[par][:, hs],
                                           start=True, stop=True)
                        return ins
                    S.op("pe", ds4, r=[("kdm", b), f"vin{par}"], w=[bk(bb)])
                    def upd(e, b=b, bb=bb, sn=sn):
                        for h in range(4):
                            ins = e.scalar_tensor_tensor(out=sn[:, h, :], in0=S0f[:, b, h, :], scalar=cdec[h],
                                                         in1=banks[bb][:, h * 128:(h + 1) * 128],
                                                         op0=ALU.mult, op1=ALU.add)
                        return ins
                    S.op("dve", upd, r=["S0f"], w=[bk(bb), snk])
                    S.dma("sp", lambda e, b=b, sn=sn: e.dma_start(out=srs[b].rearrange("h k v -> k h v"), in_=sn[:]),
                          r=[snk], w=[("srs", b)])

            S.op("act", lambda e: e.activation(out=o_sb[:], in_=banks[3][:], func=AF.Copy), r=[], w=[bk(3), "o_sb"])
            S.op("dve", lambda e: e.tensor_reduce(out=ssum[:], in_=v4(o_sb[:]), axis=AX.X, op=ALU.add),
                 r=["o_sb"], w=["ssum"])
            S.op("act", lambda e: e.activation(out=osq[:], in_=o_sb[:], func=AF.Square), r=["o_sb"], w=["osq"])
            S.op("dve", lambda e: e.tensor_reduce(out=ssq[:], in_=v4(osq[:]), axis=AX.X, op=ALU.add),
                 r=["osq"], w=["ssq"])
            S.op("dve", lambda e: e.tensor_scalar_mul(out=mean[:], in0=ssum[:], scalar1=1.0 / 128), r=["ssum"], w=["mean"])
            S.op("dve", lambda e: e.tensor_tensor(out=msq[:], in0=mean[:], in1=mean[:], op=ALU.mult), r=["mean"], w=["msq"])
            S.op("dve", lambda e: e.scalar_tensor_tensor(out=var[:], in0=ssq[:], scalar=1.0 / 128, in1=msq[:],
                                                         op0=ALU.mult, op1=ALU.subtract), r=["ssq", "msq"], w=["var"])
            S.op("dve", lambda e: e.tensor_scalar_add(out=var[:], in0=var[:], scalar1=HN_EPS), r=["var"], w=["var"])
            S.op("act", lambda e: e.activation(out=var[:], in_=var[:], func=AF.Sqrt), r=["var"], w=["var"])
            S.op("dve", lambda e: e.reciprocal(out=rstd[:], in_=var[:]), r=["var"], w=["rstd"])
            S.op("dve", lambda e: e.tensor_tensor(out=v4(o_sb[:]), in0=v4(o_sb[:]), in1=bcast_last(mean[:, :], 128),
                                                  op=ALU.subtract), r=["o_sb", "mean", "osq"], w=["o_sb"])
            S.op("dve", lambda e: e.tensor_tensor(out=v4(o_sb[:]), in0=v4(o_sb[:]), in1=bcast_last(rstd[:, :], 128),
                                                  op=ALU.mult), r=["o_sb", "rstd"], w=["o_sb"])
            S.op("act", lambda e: e.activation(out=sil[:], in_=gin[par][:], func=AF.Silu), r=[f"gin{par}"], w=["sil"])
            S.op("dve", lambda e: e.tensor_tensor(out=retg[par][:], in0=o_sb[:], in1=sil[:], op=ALU.mult),
                 r=["o_sb", "sil"], w=[f"retg{par}"])
            S.dma("sp", lambda e: e.dma_start(out=RG[rows, :], in_=retg[par][:]), r=[f"retg{par}"], w=[("RG", t)])

    S.full_barrier()
    if stop <= 3:
        S.finish()
        return nc, es, S

    with ExitStack() as ph:
        def psb(name, shape, dt):
            return ph.enter_context(nc.sbuf_tensor("s_" + name, list(shape), dt))
        rb_aug = psb("rb_aug", [33, 24], F32)
        oh_t = [psb(f"oh{g}", [33, 385], F32) for g in range(3)]
        ext_sb = psb("ext_sb", [8, 385], F32)
        btmp = [psb(f"btmp{i}", [128, 256], F32) for i in range(2)]
        biasT = psb("biasT", [128, 24, 256], BF16)
        S.op("dve", lambda e: e.memset(rb_aug[:], 1.0), w=["rb_aug"])
        S.dma("sp", lambda e: e.dma_start(out=rb_aug[0:32, :], in_=rel_bias_d), w=["rb_aug"])
        for g in range(3):
            S.dma("sp", lambda e, g=g: e.dma_start(out=oh_t[g][:], in_=oh_d[g]), w=[f"oh{g}"])
            S.op("pe", lambda e, g=g: e.matmul(banks[0][0:8, 0:385], lhsT=rb_aug[:, g * 8:(g + 1) * 8], rhs=oh_t[g][:],
                                               start=True, stop=True), r=["rb_aug", f"oh{g}"], w=[bk(0)])
            S.op("act", lambda e: e.activation(out=ext_sb[:], in_=banks[0][0:8, 0:385], func=AF.Copy),
                 r=[], w=[bk(0), "ext_sb"])
            S.dma("sp", lambda e, g=g: e.dma_start(out=EXT[g * 8:(g + 1) * 8, :], in_=ext_sb[:]),
                  r=["ext_sb"], w=[("EXT", g)])
        for gh in range(24):
            bt = btmp[gh % 2]; btk = f"btmp{gh % 2}"
            srcb = bass.AP(tensor=EXT.tensor, offset=gh * 385, ap=[[0, 128], [1, 385]])
            dstb = bass.AP(tensor=EXT2.tensor, offset=gh * 128 * 385, ap=[[385, 128], [1, 385]])
            S.dma("sp", lambda e, srcb=srcb, dstb=dstb: e.dma_start(out=dstb, in_=srcb),
                  r=[("EXT", gh // 8)], w=[("EXT2", gh)])
            for kc, c0 in ((0, 256), (1, 128)):
                src = bass.AP(tensor=EXT2.tensor, offset=gh * 128 * 385 + c0, ap=[[384, 128], [1, 128]])
                S.dma("sp", lambda e, bt=bt, kc=kc, src=src: e.dma_start(out=bt[:, kc * 128:(kc + 1) * 128], in_=src),
                      r=[("EXT2", gh)], w=[(btk, kc)])
            S.op("dve", lambda e, bt=bt, gh=gh: e.tensor_copy(out=biasT[:, gh, :], in_=bt[:]),
                 r=[(btk, 0), (btk, 1)], w=[("biasT", gh)])

        Qt = [psb(f"Qt{i}", [128, 512], BF16) for i in range(2)]
        Kt = [psb(f"Kt{i}", [128, 512], BF16) for i in range(2)]
        QT = [psb(f"QT{i}", [128, 4, 128], BF16) for i in range(2)]
        KT = [psb(f"KT{i}", [128, 4, 128], BF16) for i in range(3)]
        Va = [psb(f"Va{i}", [128, 8, 65], BF16) for i in range(3)]
        PT = [psb(f"PT{i}", [128, 2, 2, 128], BF16) for i in range(2)]
        OLs = [psb(f"OLs{i}", [128, 520], F32) for i in range(2)]
        kvf = [psb(f"kvf{i}", [128, 2, 512], F32) for i in range(2)]
        for i in range(3):
            S.op("dve", lambda e, i=i: e.memset(Va[i][:], 1.0), w=[f"Va{i}"])
        for i in range(2):
            S.op("dve", lambda e, i=i: e.memset(Qt[i][:], 0.0), w=[f"Qt{i}"])
            S.op("dve", lambda e, i=i: e.memset(Kt[i][:], 0.0), w=[f"Kt{i}"])
        bq = banks[0][:].bitcast(BF16)[:, 0:512]
        bkk = banks[1][:].bitcast(BF16)[:, 0:512]
        state = {"blk": 0}

        def tr4(e, src, dstb):
            for j in range(4):
                ins = e.transpose(out=dstb[:, j * 128:(j + 1) * 128], in_=src[:, j * 128:(j + 1) * 128],
                                  identity=ident_b[:])
            return ins

        def attn_core(g, NQ, qT, qTk, kTc, kTck, kTp, kTpk, vc, vck, vp, vpk):
            blk = state["blk"]; state["blk"] += 1
            has_prev = kTp is not None
            for hp in range(4):
                bnk = banks[2 + hp]
                pt = PT[hp % 2]; ptk = f"PT{hp % 2}"
                def sc(e, hp=hp, bnk=bnk):
                    for hh in range(2):
                        h = 2 * hp + hh
                        gh = g * 8 + h
                        ps_ = slice((h % 2) * 64, (h % 2) * 64 + 64)
                        base = hh * 256
                        if has_prev:
                            e.matmul(bnk[:, base:base + NQ], lhsT=ident_b[:], rhs=biasT[:, gh, 0:NQ],
                                     start=True, stop=False)
                            e.matmul(bnk[:, base:base + NQ], lhsT=kTp[ps_, h // 2, :], rhs=qT[ps_, h // 2, 0:NQ],
                                     start=False, stop=True)
                        e.matmul(bnk[:, base + 128:base + 128 + NQ], lhsT=ident_b[:], rhs=biasT[:, gh, 128:128 + NQ],
                                 start=True, stop=False)
                        ins = e.matmul(bnk[:, base + 128:base + 128 + NQ], lhsT=kTc[ps_, h // 2, :],
                                       rhs=qT[ps_, h // 2, 0:NQ], start=False, stop=True)
                    return ins
                S.op("pe", sc, r=[qTk, kTck, "ident_b"] + ([kTpk] if has_prev else []) +
                     [("biasT", g * 8 + 2 * hp), ("biasT", g * 8 + 2 * hp + 1)], w=[bk(2 + hp)])
                bv = bnk[:].rearrange("p (a b c) -> p a b c", a=2, b=2)
                if has_prev:
                    S.op("act", lambda e, pt=pt, bv=bv: e.activation(out=pt[:, :, :, 0:NQ], in_=bv[:, :, :, 0:NQ],
                                                                     func=AF.Exp), r=[], w=[bk(2 + hp), ptk])
                else:
                    S.op("act", lambda e, pt=pt, bv=bv: e.activation(out=pt[:, :, 1, 0:NQ], in_=bv[:, :, 1, 0:NQ],
                                                                     func=AF.Exp), r=[], w=[bk(2 + hp), ptk])
                def pv(e, hp=hp, pt=pt):
                    for hh in range(2):
                        h = 2 * hp + hh
                        ob = banks[6 + h // 4]
                        oreg = ob[0:NQ, (h % 4) * 65:(h % 4) * 65 + 65]
                        if has_prev:
                            e.matmul(oreg, lhsT=pt[:, hh, 0, 0:NQ], rhs=vp[:, h, :], start=True, stop=False)
                        ins = e.matmul(oreg, lhsT=pt[:, hh, 1, 0:NQ], rhs=vc[:, h, :], start=(not has_prev), stop=True)
                    return ins
                S.op("pe", pv, r=[ptk, vck] + ([vpk] if has_prev else []), w=[bk(6 + hp // 2)])
            ol = OLs[blk % 2]; olk = f"OLs{blk % 2}"
            S.op("act", lambda e: e.activation(out=ol[0:NQ, 0:260], in_=banks[6][0:NQ, 0:260], func=AF.Copy),
                 r=[], w=[bk(6), (olk, 0)])
            S.op("dve", lambda e: e.tensor_copy(out=ol[0:NQ, 260:520], in_=banks[7][0:NQ, 0:260]),
                 r=[], w=[bk(7), (olk, 1)])
            return ol, olk

        att_events = []
        for g, (win, dil) in enumerate(GROUPS):
            prev_events = att_events
            att_events = []
            Pv = P[0:SEQ].rearrange("(u d) c -> d u c", d=dil)
            Av = ATT[0:SEQ].rearrange("(u d) c -> d u c", d=dil)
            nb = NPT // dil
            cnt = 0
            for r in range(dil):
                for ub in range(nb):
                    par = cnt % 2; p3 = cnt % 3; pp3 = (cnt - 1) % 3
                    cnt += 1
                    us = slice(ub * 128, (ub + 1) * 128)
                    rkeys = [("P", (r + dil * (ub * 128 + i)) // 128, 0) for i in (0, 127)]
                    S.dma("sp", lambda e: e.dma_start(out=Qt[par][:], in_=Pv[r, us, 2048 + g * 512:2048 + (g + 1) * 512]),
                          w=[f"Qt{par}"])
                    S.dma("sp", lambda e: e.dma_start(out=Kt[par][:], in_=Pv[r, us, 3584 + g * 512:3584 + (g + 1) * 512]),
                          w=[f"Kt{par}"])
                    S.dma("sp", lambda e: e.dma_start(
                        out=Va[p3][:, :, 0:64],
                        in_=Pv[r, us, 5120 + g * 512:5120 + (g + 1) * 512].rearrange("p (h d) -> p h d", h=8)),
                        w=[f"Va{p3}"])
                    S.op("pe", lambda e: tr4(e, Qt[par], bq), r=[f"Qt{par}", "ident_b"], w=[bk(0)])
                    S.op("act", lambda e: e.activation(out=QT[par][:].rearrange("p h d -> p (h d)"), in_=bq, func=AF.Copy),
                         r=[], w=[bk(0), f"QT{par}"])
                    S.op("pe", lambda e: tr4(e, Kt[par], bkk), r=[f"Kt{par}", "ident_b"], w=[bk(1)])
                    S.op("dve", lambda e: e.tensor_copy(out=KT[p3][:].rearrange("p h d -> p (h d)"), in_=bkk),
                         r=[], w=[bk(1), f"KT{p3}"])
                    hp_ = ub > 0
                    ol, olk = attn_core(g, 128, QT[par], f"QT{par}", KT[p3], f"KT{p3}",
                                        KT[pp3] if hp_ else None, f"KT{pp3}", Va[p3], f"Va{p3}",
                                        Va[pp3] if hp_ else None, f"Va{pp3}")
                    aq_ = "sp" if g == 0 else "pool"
                    if g > 0 and r == 0 and ub == 0:
                        for ev in prev_events:
                            S._wait(aq_, ev)
                    kw = {} if g == 0 else {"accum_op": ALU.add}
                    ev = S.dma(aq_, lambda e: e.dma_start(out=Av[r, us, :], in_=ol[:], **kw),
                               r=[(olk, 0), (olk, 1)], w=[("ATT", g, r, ub)])
                    att_events.append(ev)
        if stop > 4:
            for g, (win, dil) in enumerate(GROUPS):
                nq = TS // dil if dil <= TS else 1
                classes = list(range(min(dil, TS)))
                prev_events = att_events
                att_events = []
                cnt = 0
                for b in range(NSEQ_S):
                    cv = c_in[g][b].rearrange("(u d) k c -> d u k c", d=dil)
                    for r in classes:
                        par = cnt % 2; p3 = cnt % 3
                        cnt += 1
                        tok0 = SEQ + b * TS + r
                        rows = slice(tok0, tok0 + dil * (nq - 1) + 1, dil)
                        S.dma("sp", lambda e: e.dma_start(out=kvf[par][:], in_=cv[r]), w=[f"kvf{par}"])
                        S.dma("sp", lambda e: e.dma_start(out=Qt[par][0:nq, :],
                                                          in_=P[rows, 2048 + g * 512:2048 + (g + 1) * 512]),
                              r=[("P", NPT, 4 + g)], w=[f"Qt{par}"])
                        S.dma("sp", lambda e: e.dma_start(out=Kt[par][0:nq, :],
                                                          in_=P[rows, 3584 + g * 512:3584 + (g + 1) * 512]),
                              r=[("P", NPT, 7 + g)], w=[f"Kt{par}"])
                        vcur = Va[(2 * cnt) % 3]; vck = f"Va{(2 * cnt) % 3}"
                        vprv = Va[(2 * cnt + 1) % 3]; vpk = f"Va{(2 * cnt + 1) % 3}"
                        S.dma("sp", lambda e: e.dma_start(
                            out=vcur[0:nq, :, 0:64],
                            in_=P[rows, 5120 + g * 512:5120 + (g + 1) * 512].rearrange("p (h d) -> p h d", h=8)),
                            r=[("P", NPT, 10 + g)], w=[vck])
                        S.op("act", lambda e: e.activation(out=Kt[1 - par][:], in_=kvf[par][:, 0, :], func=AF.Copy),
                             r=[f"kvf{par}"], w=[f"Kt{1 - par}"])
                        S.op("dve", lambda e: e.tensor_copy(out=vprv[:, :, 0:64],
                                                            in_=kvf[par][:, 1, :].rearrange("p (h d) -> p h d", h=8)),
                             r=[f"kvf{par}"], w=[vpk])
                        S.op("pe", lambda e: tr4(e, Qt[par], bq), r=[f"Qt{par}", "ident_b"], w=[bk(0)])
                        S.op("act", lambda e: e.activation(out=QT[par][:].rearrange("p h d -> p (h d)"), in_=bq,
                                                           func=AF.Copy), r=[], w=[bk(0), f"QT{par}"])
                        S.op("pe", lambda e: tr4(e, Kt[par], bkk), r=[f"Kt{par}", "ident_b"], w=[bk(1)])
                        S.op("dve", lambda e: e.tensor_copy(out=KT[0][:].rearrange("p h d -> p (h d)"), in_=bkk),
                             r=[], w=[bk(1), "KT0"])
                        S.op("pe", lambda e: tr4(e, Kt[1 - par], bq), r=[f"Kt{1 - par}", "ident_b"], w=[bk(0)])
                        S.op("act", lambda e: e.activation(out=KT[1][:].rearrange("p h d -> p (h d)"), in_=bq,
                                                           func=AF.Copy), r=[], w=[bk(0), "KT1"])
                        ol, olk = attn_core(g, 8, QT[par], f"QT{par}", KT[0], "KT0", KT[1], "KT1",
                                            vcur, vck, vprv, vpk)
                        aq_ = "sp" if g == 0 else "pool"
                        if g > 0 and cnt == 1:
                            for ev in prev_events:
                                S._wait(aq_, ev)
                        kw = {} if g == 0 else {"accum_op": ALU.add}
                        ev = S.dma(aq_, lambda e: e.dma_start(out=ATT[rows, :], in_=ol[0:nq, :], **kw),
                                   r=[(olk, 0), (olk, 1)], w=[("ATTs", g, b, r)])
                        att_events.append(ev)

    S.full_barrier()
    if stop <= 5:
        S.finish()
        return nc, es, S

    with ExitStack() as ph:
        def psb(name, shape, dt):
            return ph.enter_context(nc.sbuf_tensor("s_" + name, list(shape), dt))
        wro_b = psb("wro_b", [128, 4, D], BF16)
        wao_b = psb("wao_b", [128, 4, D], BF16)
        wout_b = psb("wout_b", [128, 8, D], BF16)
        wqs = [psb(f"wqs{i}", [128, 8, 512], BF16) for i in range(2)]
        WQv = WQB.rearrange("(kc p) n -> p kc n", p=128)
        keysT = psb("keysT", [128, 16, 128], BF16)
        bout_t = psb("bout_t", [128, D], F32)
        lnt = {k: psb(k + "_t", [128, D], F32) for k in ("ln1_g", "ln1_b", "ln2_g", "ln2_b")}
        iota16 = psb("iota16", [128, 16], F32)
        S.dma("sp", lambda e: e.dma_start(out=bout_t[:], in_=b_out_rep), w=["bout_t"])
        for k in lnt:
            S.dma("sp", lambda e, k=k: e.dma_start(out=lnt[k][:], in_=ln_rep[k]), w=[k + "_t"])
        S.dma("sp", lambda e: e.dma_start(out=iota16[:], in_=iota16_d), w=["iota16"])
        with ExitStack() as wl:
            wst = [wl.enter_context(nc.sbuf_tensor(f"s_wst{i}", [128, 4, 1024], F32)) for i in range(2)]
            jobs = []
            for kc0 in (0,):
                jobs.append((w_ret_o.rearrange("(kc p) n -> p kc n", p=128), wro_b[:, :, :], "wro_b"))
                jobs.append((w_att_o.rearrange("(kc p) n -> p kc n", p=128), wao_b[:, :, :], "wao_b"))
            wov = w_out.rearrange("(kc p) n -> p kc n", p=128)
            jobs.append((wov[:, 0:4, :], wout_b[:, 0:4, :], ("wout_b", 0)))
            jobs.append((wov[:, 4:8, :], wout_b[:, 4:8, :], ("wout_b", 1)))
            for j, (src, dst, key) in enumerate(jobs):
                st_ = wst[j % 2]; stk = f"wst{j % 2}"
                S.dma("sp", lambda e, st_=st_, src=src: e.dma_start(out=st_[:], in_=src), w=[stk])
                if j % 2 == 0:
                    S.op("dve", lambda e, st_=st_, dst=dst: e.tensor_copy(out=dst, in_=st_[:]), r=[stk], w=[key])
                else:
                    S.op("act", lambda e, st_=st_, dst=dst: e.activation(out=dst, in_=st_[:], func=AF.Copy),
                         r=[stk], w=[key])
            kst = wst[0]
            for q4 in range(4):
                S.dma("sp", lambda e, q4=q4: e.dma_start(
                    out=kst[:, q4, 0:512].rearrange("p (a c) -> p a c", a=4),
                    in_=peer_keys[q4 * 512:(q4 + 1) * 512, :].rearrange("(a p) c -> p a c", p=128)), w=["wst0"])
            for q4 in range(4):
                def trk(e, q4=q4):
                    for a in range(4):
                        ins = e.transpose(out=banks[q4][:, a * 128:(a + 1) * 128], in_=kst[:, q4, a * 128:(a + 1) * 128],
                                          identity=ident_f[:])
                    return ins
                S.op("pe", trk, r=["wst0", "ident_f"], w=[bk(q4)])
                S.op("act", lambda e, q4=q4: e.activation(
                    out=keysT[:, q4 * 4:(q4 + 1) * 4, :].rearrange("p a k -> p (a k)"), in_=banks[q4][:], func=AF.Copy),
                    r=[], w=[bk(q4), ("keysT", q4)])
            S.full_barrier()
        wkeys = ["wro_b", "wao_b", ("wout_b", 0), ("wout_b", 1)]
        wqkeys = [("wq_b", a, b) for a in range(2) for b in range(2)]

        x_t = psb("x_t", [128, D], F32)
        att_t = psb("att_t", [128, 520], F32)
        rg_t = psb("rg_t", [128, 512], BF16)
        attn_t = psb("attn_t", [128, 512], BF16)
        rl = psb("rl", [128, 8], F32)
        aT = psb("aT", [128, 4, 128], BF16)
        rT = psb("rT", [128, 4, 128], BF16)
        bufA = psb("bufA", [128, 2048], F32)
        bufB = psb("bufB", [128, 2048], F32)
        tmp1 = psb("tmp1", [128, D], F32)
        x1 = psb("x1", [128, D], F32)
        h2 = psb("h2", [128, D], F32)
        h2b = psb("h2b", [128, D], BF16)
        h2T = psb("h2T", [128, 8, 128], BF16)
        junk = psb("junk", [128, D], BF16)
        st1 = psb("st1", [128, 8], F32)
        sv = psb("sv", [128, 16, 16], F32)
        si = psb("si", [128, 16, 16], U32)
        sif = psb("sif", [128, 16, 16], F32)
        cv = psb("cv", [128, 8, 16], F32)
        ci = psb("ci", [128, 8, 16], U32)
        hi_u = psb("hi_u", [128, 8, 16], U32); lo_u = psb("lo_u", [128, 8, 16], U32)
        hi_f = psb("hi_f", [128, 8, 16], F32); lo_f = psb("lo_f", [128, 8, 16], F32)
        i0 = psb("i0", [128, 8, 16], F32); i1 = psb("i1", [128, 8, 16], F32)
        e_f = psb("e_f", [128, 128], F32); e_i = psb("e_i", [128, 128], I32)
        gsm = psb("gsm", [128, 8, 16], F32); ssm = psb("ssm", [128, 8], F32)
        a_t = psb("a_t", [128, 128], F32); ga = psb("ga", [128, 128], BF16)
        NSL = 8
        Ug = [psb(f"Ug{i}", [128, D], BF16) for i in range(NSL)]
        Vg = [psb(f"Vg{i}", [128, D], BF16) for i in range(NSL)]
        Dg = [psb(f"Dg{i}", [128, 16, 128], BF16) for i in range(2)]
        Gb = bufA[:].bitcast(BF16)
        G_t = Gb[:, 0:2048]; sg_t = Gb[:, 2048:4096]
        Bb = bufB[:].bitcast(BF16)
        mixin = Bb[:, 0:1024]; mixT = Bb[:, 1024:2048].rearrange("p (k t) -> p k t", k=8)
        qpe = Bb[:, 0:2048]; qpT = Bb[:, 2048:4096].rearrange("p (k t) -> p k t", k=16)
        s_sc = bufA[:].rearrange("p (a k) -> p a k", a=16)
        oh4 = bufA[:].rearrange("p (h k i) -> p h k i", h=8, k=16)
        cand = bufB[:].rearrange("p (h c) -> p h c", h=8)
        cand4 = bufB[:].rearrange("p (h i j) -> p h i j", h=8, i=16)
        prod4 = bufB[:].rearrange("p (h k i) -> p h k i", h=8, k=16)
        bf_banks = [banks[i][:].bitcast(BF16) for i in range(8)]

        def layer_norm(src, srck, gk, bk_, dst, dstk):
            S.op("dve", lambda e: e.tensor_reduce(out=st1[:, 0:1], in_=src[:], axis=AX.X, op=ALU.add), r=[srck], w=["st_sum"])
            S.op("act", lambda e: e.activation(out=tmp1[:], in_=src[:], func=AF.Square), r=[srck], w=["tmp1"])
            S.op("dve", lambda e: e.tensor_reduce(out=st1[:, 1:2], in_=tmp1[:], axis=AX.X, op=ALU.add), r=["tmp1"], w=["st_sq"])
            S.op("dve", lambda e: e.tensor_scalar_mul(out=st1[:, 2:3], in0=st1[:, 0:1], scalar1=1.0 / D), r=["st_sum"], w=["st_mean"])
            S.op("dve", lambda e: e.tensor_tensor(out=st1[:, 3:4], in0=st1[:, 2:3], in1=st1[:, 2:3], op=ALU.mult),
                 r=["st_mean"], w=["st_msq"])
            S.op("dve", lambda e: e.scalar_tensor_tensor(out=st1[:, 4:5], in0=st1[:, 1:2], scalar=1.0 / D, in1=st1[:, 3:4],
                                                         op0=ALU.mult, op1=ALU.subtract), r=["st_sq", "st_msq"], w=["st_var"])
            S.op("dve", lambda e: e.tensor_scalar_add(out=st1[:, 4:5], in0=st1[:, 4:5], scalar1=LN_EPS), r=["st_var"], w=["st_var"])
            S.op("act", lambda e: e.activation(out=st1[:, 5:6], in_=st1[:, 4:5], func=AF.Sqrt), r=["st_var"], w=["st_std"])
            S.op("dve", lambda e: e.reciprocal(out=st1[:, 6:7], in_=st1[:, 5:6]), r=["st_std"], w=["st_rstd"])
            S.op("dve", lambda e: e.tensor_scalar(out=src[:], in0=src[:], scalar1=st1[:, 2:3], scalar2=st1[:, 6:7],
                                                  op0=ALU.subtract, op1=ALU.mult), r=[srck, "st_mean", "st_rstd"], w=[srck])
            S.op("dve", lambda e: e.tensor_tensor(out=src[:], in0=src[:], in1=lnt[gk][:], op=ALU.mult), r=[srck, gk + "_t"], w=[srck])
            S.op("dve", lambda e: e.tensor_tensor(out=dst[:], in0=src[:], in1=lnt[bk_][:], op=ALU.add), r=[srck, bk_ + "_t"], w=[dstk])

        def tr_n(e, src, dstb, n):
            for j in range(n):
                ins = e.transpose(out=dstb[:, j * 128:(j + 1) * 128], in_=src[:, j * 128:(j + 1) * 128], identity=ident_b[:])
            return ins

        gslot = {"u": 0, "v": 0}
        x1_2 = [x1, psb("x1b", [128, D], F32)]
        e_i2 = [e_i, psb("e_ib", [128, 128], I32)]
        h2b_2 = [h2b, psb("h2bb", [128, D], BF16)]
        gsm_2 = [gsm, psb("gsmb", [128, 8, 16], F32)]
        tlist = list(range(NT) if tiles is None else tiles)

        def ctx(n):
            t = tlist[n]
            return dict(t=t, i=0 if t < NPT else 1, rows=slice(t * 128, (t + 1) * 128),
                        xsrc=xp[t * 128:(t + 1) * 128, :] if t < NPT else xs,
                        ydst=yp[t * 128:(t + 1) * 128, :] if t < NPT else ys,
                        x1c=x1_2[n % 2], x1k=f"x1_{n % 2}", e_ic=e_i2[n % 2], e_ik=f"e_i_{n % 2}",
                        h2bc=h2b_2[n % 2], h2bk=f"h2b_{n % 2}", gsmc=gsm_2[n % 2], gsmk=f"gsm_{n % 2}")

        def stage1(t, i, rows, xsrc, ydst, x1c, x1k, e_ic, e_ik, h2bc, h2bk, gsmc, gsmk):
                S.dma("sp", lambda e: e.dma_start(out=x_t[:], in_=xsrc), w=["x_t"])
                S.dma("sp", lambda e: e.dma_start(out=att_t[:], in_=ATT[rows, :]), w=["att_t"])
                S.dma("sp", lambda e: e.dma_start(out=rg_t[:], in_=RG[rows, :]), r=[("RG", t)], w=["rg_t"])
                S.dma("sp", lambda e: e.dma_start(out=G_t, in_=P[rows, 6656:8704]), w=["bufA"])
                a3 = att_t[:].rearrange("p (h c) -> p h c", h=8)
                S.op("dve", lambda e: e.reciprocal(out=rl[:], in_=a3[:, :, 64]), r=["att_t"], w=["rl"])
                S.op("dve", lambda e: e.tensor_tensor(out=attn_t[:].rearrange("p (h d) -> p h d", h=8), in0=a3[:, :, 0:64],
                                                      in1=bcast_last(rl[:, :], 64), op=ALU.mult), r=["att_t", "rl"], w=["attn_t"])
                S.op("pe", lambda e: tr_n(e, attn_t, bf_banks[0], 4), r=["attn_t", "ident_b"], w=[bk(0)])
                S.op("act", lambda e: e.activation(out=aT[:].rearrange("p k t -> p (k t)"), in_=bf_banks[0][:, 0:512], func=AF.Copy),
                     r=[], w=[bk(0), "aT"])
                S.op("pe", lambda e: tr_n(e, rg_t, bf_banks[1], 4), r=["rg_t", "ident_b"], w=[bk(1)])
                S.op("dve", lambda e: e.tensor_copy(out=rT[:].rearrange("p k t -> p (k t)"), in_=bf_banks[1][:, 0:512]),
                     r=[], w=[bk(1), "rT"])
                S.op("act", lambda e: e.activation(out=sg_t, in_=G_t, func=AF.Sigmoid), r=["bufA"], w=["bufA"])

                def proj4(e, srcT, w_b, b0):
                    for half in range(2):
                        for kc in range(4):
                            ins = e.matmul(banks[b0 + half][:], lhsT=srcT[:, kc, :], rhs=w_b[:, kc, half * 512:(half + 1) * 512],
                                           start=(kc == 0), stop=(kc == 3))
                    return ins
                S.op("pe", lambda e: proj4(e, rT, wro_b, 2), r=["rT", "wro_b"], w=[bk(2), bk(3)])
                S.op("pe", lambda e: proj4(e, aT, wao_b, 4), r=["aT", "wao_b"], w=[bk(4), bk(5)])
                for half in range(2):
                    hs_ = slice(half * 512, (half + 1) * 512)
                    S.op("dve", lambda e, half=half, hs_=hs_: e.tensor_tensor(out=tmp1[:, hs_], in0=banks[2 + half][:],
                                                                              in1=sg_t[:, hs_], op=ALU.mult),
                         r=["bufA"], w=[bk(2 + half), ("tmp1h", half)])
                    S.op("dve", lambda e, half=half, hs_=hs_: e.tensor_tensor(
                        out=h2[:, hs_], in0=banks[4 + half][:], in1=sg_t[:, 1024 + half * 512:1024 + (half + 1) * 512],
                        op=ALU.mult), r=["bufA"], w=[bk(4 + half), ("h2h", half)])
                S.op("dve", lambda e: e.tensor_tensor(out=mixin, in0=tmp1[:], in1=h2[:], op=ALU.add),
                     r=[("tmp1h", 0), ("tmp1h", 1), ("h2h", 0), ("h2h", 1)], w=["bufB", "tmp1", "h2"])
                S.op("pe", lambda e: tr_n(e, mixin, bf_banks[0], 8), r=["bufB", "ident_b"], w=[bk(0)])
                S.op("act", lambda e: e.activation(out=mixT.rearrange("p k t -> p (k t)"), in_=bf_banks[0][:, 0:1024], func=AF.Copy),
                     r=[], w=[bk(0), "bufB"])

                def proj8(e, srcT, w_b, b0, nb_):
                    for nb in range(nb_):
                        for kc in range(8):
                            ins = e.matmul(banks[b0 + nb][:], lhsT=srcT[:, kc, :], rhs=w_b[:, kc, nb * 512:(nb + 1) * 512],
                                           start=(kc == 0), stop=(kc == 7))
                    return ins
                S.op("pe", lambda e: proj8(e, mixT, wout_b, 2, 2), r=["bufB", ("wout_b", 0), ("wout_b", 1)], w=[bk(2), bk(3)])
                for half in range(2):
                    hs_ = slice(half * 512, (half + 1) * 512)
                    S.op("dve", lambda e, half=half, hs_=hs_: e.tensor_tensor(out=h2[:, hs_], in0=banks[2 + half][:],
                                                                              in1=bout_t[:, hs_], op=ALU.add),
                         r=["bout_t", "h2"], w=[bk(2 + half), ("h2h", half)])
                S.op("dve", lambda e: e.tensor_tensor(out=h2[:], in0=h2[:], in1=modD[:, i, 0, :], op=ALU.mult),
                     r=[("h2h", 0), ("h2h", 1), ("modD", i, 0)], w=["h2"])
                S.op("dve", lambda e: e.scalar_tensor_tensor(out=x_t[:], in0=x_t[:], scalar=ALPHA, in1=h2[:],
                                                             op0=ALU.mult, op1=ALU.add), r=["x_t", "h2"], w=["x_t"])
                layer_norm(x_t, "x_t", "ln1_g", "ln1_b", x1c, x1k)
                S.op("dve", lambda e: e.tensor_tensor(out=h2[:], in0=x1c[:], in1=modD[:, i, 2, :], op=ALU.mult),
                     r=[x1k, ("modD", i, 2)], w=["h2"])
                S.op("dve", lambda e: e.tensor_tensor(out=h2bc[:], in0=h2[:], in1=modD[:, i, 1, :], op=ALU.add),
                     r=["h2", ("modD", i, 1)], w=[h2bk])
                S.op("pe", lambda e: tr_n(e, h2bc, bf_banks[1], 8), r=[h2bk, "ident_b"], w=[bk(1)])
                S.op("act", lambda e: e.activation(out=h2T[:].rearrange("p k t -> p (k t)"), in_=bf_banks[1][:, 0:1024], func=AF.Copy),
                     r=[], w=[bk(1), "h2T"])
                for nb in range(4):
                    wq_ = wqs[nb % 2]; wqk_ = f"wqs{nb % 2}"
                    S.dma("sp", lambda e, wq_=wq_, nb=nb: e.dma_start(out=wq_[:], in_=WQv[:, :, nb * 512:(nb + 1) * 512]),
                          r=wqbkeys, w=[wqk_])
                    def pq(e, wq_=wq_, nb=nb):
                        for kc in range(8):
                            ins = e.matmul(banks[2 + nb][:], lhsT=h2T[:, kc, :], rhs=wq_[:, kc, :], start=(kc == 0), stop=(kc == 7))
                        return ins
                    S.op("pe", pq, r=["h2T", wqk_], w=[bk(2 + nb)])
                for nb in range(4):
                    if nb % 2 == 0:
                        S.op("act", lambda e, nb=nb: e.activation(out=qpe[:, nb * 512:(nb + 1) * 512], in_=banks[2 + nb][:], func=AF.Copy),
                             r=["bufB"], w=[bk(2 + nb), "bufB"])
                    else:
                        S.op("dve", lambda e, nb=nb: e.tensor_copy(out=qpe[:, nb * 512:(nb + 1) * 512], in_=banks[2 + nb][:]),
                             r=["bufB"], w=[bk(2 + nb), "bufB"])
                for half in range(2):
                    def trq(e, half=half):
                        for j in range(8):
                            hs = half * 8 + j
                            ins = e.transpose(out=bf_banks[half][:, j * 128:(j + 1) * 128], in_=qpe[:, hs * 128:(hs + 1) * 128],
                                              identity=ident_b[:])
                        return ins
                    S.op("pe", trq, r=["bufB", "ident_b"], w=[bk(half)])
                    if half == 0:
                        S.op("act", lambda e: e.activation(out=qpT[:, 0:8, :].rearrange("p k t -> p (k t)"), in_=bf_banks[0][:, 0:1024],
                                                           func=AF.Copy), r=["bufB"], w=[bk(0), "bufB"])
                    else:
                        S.op("dve", lambda e: e.tensor_copy(out=qpT[:, 8:16, :].rearrange("p k t -> p (k t)"), in_=bf_banks[1][:, 0:1024]),
                             r=["bufB"], w=[bk(1), "bufB"])
                for q4 in range(4):
                    def scq(e, q4=q4):
                        for a in range(4):
                            hs = q4 * 4 + a
                            ins = e.matmul(banks[2 + q4][:, a * 128:(a + 1) * 128], lhsT=qpT[:, hs, :], rhs=keysT[:, hs, :],
                                           start=True, stop=True)
                        return ins
                    S.op("pe", scq, r=["bufB", ("keysT", q4)], w=[bk(2 + q4)])
                    if q4 % 2 == 0:
                        S.op("act", lambda e, q4=q4: e.activation(out=bufA[:, q4 * 512:(q4 + 1) * 512], in_=banks[2 + q4][:], func=AF.Copy),
                             r=["bufA"], w=[bk(2 + q4), "bufA"])
                    else:
                        S.op("dve", lambda e, q4=q4: e.tensor_copy(out=bufA[:, q4 * 512:(q4 + 1) * 512], in_=banks[2 + q4][:]),
                             r=["bufA"], w=[bk(2 + q4), "bufA"])
                ssk = ["bufA"]

                def topk_rounds(n, vals, vk, outv, outvk, outi, outik):
                    for rnd in range(2):
                        sl = slice(rnd * 8, rnd * 8 + 8)
                        def mx(e, sl=sl):
                            for j in range(n):
                                ins = e.max(out=outv[:, j, sl], in_=vals[:, j, :])
                            return ins
                        S.op("dve", mx, r=vk, w=[(outvk, rnd)])
                        def mi(e, sl=sl):
                            for j in range(n):
                                ins = e.max_index(out=outi[:, j, sl], in_max=outv[:, j, sl], in_values=vals[:, j, :])
                            return ins
                        S.op("dve", mi, r=vk + [(outvk, rnd)], w=[(outik, rnd)])
                        if rnd == 0:
                            def mr(e, sl=sl):
                                for j in range(n):
                                    ins = e.match_replace(out=vals[:, j, :], in_to_replace=outv[:, j, sl], in_values=vals[:, j, :],
                                                          imm_value=-1e30)
                                return ins
                            S.op("dve", mr, r=[(outvk, rnd), (outik, rnd)], w=vk)
                topk_rounds(16, s_sc, ssk, sv, "sv", si, "si")
                sv4 = sv[:].rearrange("p (h s) k -> p h s k", s=2)
                S.op("dve", lambda e: e.tensor_tensor(
                    out=cand4, in0=sv4[:, :, 0, :].unsqueeze(3).to_broadcast([128, 8, 16, 16]),
                    in1=sv4[:, :, 1, :].unsqueeze(2).to_broadcast([128, 8, 16, 16]), op=ALU.add),
                    r=[("sv", 0), ("sv", 1), "bufB"], w=["bufB"])
                topk_rounds(8, cand, ["bufB"], cv, "cv", ci, "ci")
                cvk = [("cv", 0), ("cv", 1)]; cik = [("ci", 0), ("ci", 1)]
                S.op("dve", lambda e: e.tensor_tensor(out=gsmc[:], in0=cv[:], in1=bcast_last(cv[:, :, 0], 16), op=ALU.subtract),
                     r=cvk, w=[gsmk])
                S.op("act", lambda e: e.activation(out=gsmc[:], in_=gsmc[:], func=AF.Exp), r=[gsmk], w=[gsmk])
                S.op("dve", lambda e: e.tensor_reduce(out=ssm[:], in_=gsmc[:], axis=AX.X, op=ALU.add), r=[gsmk], w=["ssm"])
                S.op("dve", lambda e: e.reciprocal(out=ssm[:], in_=ssm[:]), r=["ssm"], w=["ssm"])
                S.op("dve", lambda e: e.tensor_tensor(out=gsmc[:], in0=gsmc[:], in1=bcast_last(ssm[:, :], 16), op=ALU.mult),
                     r=[gsmk, "ssm"], w=[gsmk])
                S.op("dve", lambda e: e.tensor_single_scalar(out=hi_u[:], in_=ci[:], scalar=4, op=ALU.logical_shift_right),
                     r=cik, w=["hi_u"])
                S.op("dve", lambda e: e.tensor_single_scalar(out=lo_u[:], in_=ci[:], scalar=15, op=ALU.bitwise_and),
                     r=cik, w=["lo_u"])
                S.op("dve", lambda e: e.tensor_copy(out=hi_f[:], in_=hi_u[:]), r=["hi_u"], w=["hi_f"])
                S.op("dve", lambda e: e.tensor_copy(out=lo_f[:], in_=lo_u[:]), r=["lo_u"], w=["lo_f"])
                S.op("dve", lambda e: e.tensor_copy(out=sif[:], in_=si[:]), r=[("si", 0), ("si", 1)], w=["sif"])
                sif4 = sif[:].rearrange("p (h s) k -> p h s k", s=2)
                iot4 = iota16[:, :].unsqueeze(1).unsqueeze(1).to_broadcast([128, 8, 16, 16])
                for side, (xf, xk, dsti, dstk) in enumerate(((hi_f, "hi_f", i0, "i0"), (lo_f, "lo_f", i1, "i1"))):
                    S.op("dve", lambda e, xf=xf: e.tensor_tensor(
                        out=oh4, in0=iot4, in1=xf[:].unsqueeze(3).to_broadcast([128, 8, 16, 16]), op=ALU.is_equal),
                        r=[xk, "iota16"] + ssk, w=["bufA"])
                    S.op("dve", lambda e, side=side: e.tensor_tensor(
                        out=prod4, in0=oh4, in1=sif4[:, :, side, :].unsqueeze(2).to_broadcast([128, 8, 16, 16]), op=ALU.mult),
                        r=["bufA", "sif", "bufB"], w=["bufB"])
                    S.op("dve", lambda e, dsti=dsti: e.tensor_reduce(out=dsti[:], in_=prod4, axis=AX.X, op=ALU.add),
                         r=["bufB"], w=[dstk])
                S.op("dve", lambda e: e.scalar_tensor_tensor(out=e_f[:].rearrange("p (h k) -> p h k", h=8), in0=i0[:], scalar=128.0,
                                                             in1=i1[:], op0=ALU.mult, op1=ALU.add), r=["i0", "i1"], w=["e_f"])
                S.op("dve", lambda e: e.tensor_copy(out=e_ic[:], in_=e_f[:]), r=["e_f"], w=[e_ik])

        def stage2(t, i, rows, xsrc, ydst, x1c, x1k, e_ic, e_ik, h2bc, h2bk, gsmc, gsmk):
                for hk in range(128):
                    sl_ = gslot["u"] % NSL; gslot["u"] += 1
                    S.dma("pool", lambda e, sl_=sl_, hk=hk: e.indirect_dma_start(
                        out=Ug[sl_][:], out_offset=None, in_=UB,
                        in_offset=bass.IndirectOffsetOnAxis(ap=e_ic[:, hk:hk + 1], axis=0)), r=[e_ik] + ubkeys, w=[f"Ug{sl_}"])
                    S.op("dve", lambda e, sl_=sl_, hk=hk: e.scalar_tensor_tensor(
                        out=junk[:], in0=Ug[sl_][:], scalar=1.0, in1=h2bc[:], op0=ALU.mult, op1=ALU.mult,
                        accum_out=a_t[:, hk:hk + 1]), r=[f"Ug{sl_}", h2bk], w=["junk", ("a_t", hk)])
                    S.flush(1)
                S.op("act", lambda e: e.activation(out=a_t[:], in_=a_t[:], func=AF.Gelu), r=[("a_t", hk) for hk in range(128)], w=["a_g"])
                S.op("dve", lambda e: e.tensor_tensor(out=ga[:], in0=a_t[:], in1=gsmc[:].rearrange("p h k -> p (h k)"), op=ALU.mult),
                     r=["a_g", gsmk], w=["ga"])

        def stage3a(t, i, rows, xsrc, ydst, x1c, x1k, e_ic, e_ik, h2bc, h2bk, gsmc, gsmk):
                for h in range(8):
                    dg = Dg[h % 2]; dgk = f"Dg{h % 2}"
                    S.op("dve", lambda e, dg=dg, h=h: e.tensor_tensor(
                        out=dg[:], in0=ident_b[:, :].unsqueeze(1).to_broadcast([128, 16, 128]),
                        in1=ga[:, h * 16:(h + 1) * 16].unsqueeze(2).to_broadcast([128, 16, 128]), op=ALU.mult),
                        r=["ga", "ident_b"], w=[dgk])
                    for k in range(16):
                        hk = h * 16 + k
                        sl_ = gslot["v"] % NSL; gslot["v"] += 1
                        S.dma("pool", lambda e, sl_=sl_, hk=hk: e.indirect_dma_start(
                            out=Vg[sl_][:], out_offset=None, in_=VB,
                            in_offset=bass.IndirectOffsetOnAxis(ap=e_ic[:, hk:hk + 1], axis=0)), r=[e_ik] + vbkeys, w=[f"Vg{sl_}"])
                        def vmm(e, sl_=sl_, hk=hk, dg=dg, k=k):
                            for half in range(2):
                                ins = e.matmul(banks[6 + half][:], lhsT=dg[:, k, :], rhs=Vg[sl_][:, half * 512:(half + 1) * 512],
                                               start=(hk == 0), stop=(hk == 127))
                            return ins
                        S.op("pe", vmm, r=[f"Vg{sl_}", dgk], w=[bk(6), bk(7)])
                        S.flush(1)

        def stage3b(t, i, rows, xsrc, ydst, x1c, x1k, e_ic, e_ik, h2bc, h2bk, gsmc, gsmk):
                for half in range(2):
                    hs_ = slice(half * 512, (half + 1) * 512)
                    S.op("dve", lambda e, half=half, hs_=hs_: e.tensor_tensor(out=h2[:, hs_], in0=banks[6 + half][:],
                                                                              in1=modD[:, i, 3, hs_], op=ALU.mult),
                         r=[("modD", i, 3), "h2"], w=[bk(6 + half), ("h2h", half)])
                S.op("dve", lambda e: e.scalar_tensor_tensor(out=x1c[:], in0=x1c[:], scalar=ALPHA, in1=h2[:],
                                                             op0=ALU.mult, op1=ALU.add), r=[x1k, ("h2h", 0), ("h2h", 1)], w=[x1k, "h2"])
                layer_norm(x1c, x1k, "ln2_g", "ln2_b", x_t, "x_t")
                S.dma("sp", lambda e: e.dma_start(out=ydst, in_=x_t[:]), r=["x_t"], w=[("y", t)])


        stage1(**ctx(0))
        for n in range(len(tlist)):
            if n + 1 < len(tlist):
                S.defer_begin()
                stage1(**ctx(n + 1))
                S.defer_end()
            stage2(**ctx(n))
            stage3a(**ctx(n))
            S.flush()
            stage3b(**ctx(n))

    S.finish()
    return nc, es, S


def _shard_inputs(inp):
    c = _consts()
    maps = []
    f = np.ascontiguousarray
    for i in range(NCORES):
        sl = slice(i * NSEQ_S, (i + 1) * NSEQ_S)
        m = {
            "xp": f(inp["x_prompt"][i]),
            "xs": f(inp["x_sample"][sl].reshape(128, D)),
            "cp_rep": f(np.broadcast_to(inp["c_prompt"][i:i + 1], (128, D))),
            "cs_rep": f(np.repeat(inp["c_sample"][sl], TS, axis=0)),
            "st_in": f(inp["state_ret"][0, sl]),
            "c_in0": f(inp["cache_att_w128"][0, sl].reshape(NSEQ_S, 128, 2, 512)),
            "c_in1": f(inp["cache_att_w512"][0, sl].reshape(NSEQ_S, 512, 2, 512)),
            "c_in2": f(inp["cache_att_w2048"][0, sl].reshape(NSEQ_S, 2048, 2, 512)),
            "w_ada": f(inp["w_ada"][0]),
            "b_ada": f(inp["b_ada"][0:1]),
            "w_in": f(inp["w_in"][0]),
            "ident": c["ident"],
            "rotc": c["rotc"], "rots": c["rots"],
            "intraT_p": c["intraT_p"], "intraT_s": c["intraT_s"],
            "qdec_p": c["qdec_p"], "qdec_s": c["qdec_s"],
            "kdec_p": c["kdec_p"], "kdec_s": c["kdec_s"],
            "rowmask": c["rowmask"],
            "iota16": c["iota16"],
            "oh0": c["oh0"], "oh1": c["oh1"], "oh2": c["oh2"],
            "rel_bias": f(inp["rel_bias"]),
            "w_ret_o": f(inp["w_ret_o"][0]), "w_att_o": f(inp["w_att_o"][0]), "w_out": f(inp["w_out"][0]),
            "b_out_rep": f(np.broadcast_to(inp["b_out"][0:1], (128, D))),
            "ln1_g_rep": f(np.broadcast_to(inp["ln1_g"][0:1], (128, D))),
            "ln1_b_rep": f(np.broadcast_to(inp["ln1_b"][0:1], (128, D))),
            "ln2_g_rep": f(np.broadcast_to(inp["ln2_g"][0:1], (128, D))),
            "ln2_b_rep": f(np.broadcast_to(inp["ln2_b"][0:1], (128, D))),
            "peer_wq": f(inp["peer_wq"][0]), "peer_keys": f(inp["peer_keys"][0].reshape(2048, 128)),
            "peer_u": f(inp["peer_u"][0]), "peer_v": f(inp["peer_v"][0]),
        }
        maps.append(m)
    return maps


def kernel(**inputs):
    inp = {k: np.asarray(v) for k, v in inputs.items()}
    nc, es, S = build_program()
    with es:
        maps = _shard_inputs(inp)
        res = run_bass_kernel_spmd(nc, maps, core_ids=list(range(NCORES)))
    R = res.results
    yp = np.stack([R[i]["yp"] for i in range(NCORES)], 0)
    ys = np.concatenate([R[i]["ys"].reshape(NSEQ_S, TS, D) for i in range(NCORES)], 0)
    srp = np.stack([R[i]["srp"] for i in range(NCORES)], 0)[None]
    srs = np.concatenate([R[i]["srs"] for i in range(NCORES)], 0)[None]
    cpo = [np.stack([R[i][f"cpo{g}"] for i in range(NCORES)], 0).reshape(1, NCORES, GROUPS[g][0], 2, 8, 64)
           for g in range(3)]
    cso = [np.concatenate([R[i][f"cso{g}"] for i in range(NCORES)], 0).reshape(
        1, NCORES * NSEQ_S, GROUPS[g][0], 2, 8, 64) for g in range(3)]
    return (yp.astype(np.float32), ys.astype(np.float32), srp, cpo[0], cpo[1], cpo[2], srs, cso[0], cso[1], cso[2])
```

```python
import math
from contextlib import ExitStack

import numpy as np
import concourse.bass as bass
import concourse.mybir as mybir
from concourse.bass_utils import run_bass_kernel_spmd

F32 = mybir.dt.float32
BF16 = mybir.dt.bfloat16
U32 = mybir.dt.uint32
I32 = mybir.dt.int32
AF = mybir.ActivationFunctionType
ALU = mybir.AluOpType
AX = mybir.AxisListType

NCORES = 8
D = 1024
SEQ = 4096
NPT = 32
NT = 33
NTOK = NT * 128
NSEQ_S = 16
TS = 8
PAST = 8192
IN_COLS = 8704
GROUPS = ((128, 1), (512, 4), (2048, 16))
ALPHA = 2.0 ** 0.25
LN_EPS = 1e-5
HN_EPS = 1e-6
NEGB = -30000.0
SEM_LIMIT = 30000


class Sched:
    def __init__(self, nc, es):
        self.nc = nc
        self.es = es
        self.eng = {"pe": nc.tensor, "act": nc.scalar, "dve": nc.vector, "pool": nc.gpsimd, "sp": nc.sync}
        self.sem = {}
        self.cnt = {}
        self.nsem = 0
        for e in ("pe", "act", "dve", "pool"):
            self._new_engine_sem(e)
        self.known = {e: {} for e in self.eng}
        self.bufs = {}
        self.dpool = {}
        self.drr = {}
        for q, n in (("sp", 24), ("pool", 16), ("act", 8)):
            self.dpool[q] = [self._new_dma_slot() for _ in range(n)]
            self.drr[q] = 0
        self.ninstr = 0

    def _mksem(self, name):
        self.nsem += 1
        return self.es.enter_context(self.nc.semaphore(f"{name}_{self.nsem}"))

    def _new_engine_sem(self, e):
        self.sem[e] = self._mksem("c" + e)
        self.cnt[e] = 0

    def _new_dma_slot(self):
        return {"sem": self._mksem("d"), "val": 0}

    def _wait(self, e, ev):
        sem, val = ev
        k = self.known[e]
        if k.get(id(sem), 0) >= val:
            return
        self.eng[e].wait_ge(sem, val)
        self.ninstr += 1
        k[id(sem)] = val

    def _deps(self, r, w):
        deps = []
        for key in r:
            b = self.bufs.get(key)
            if b is not None and b["w"] is not None:
                deps.append(b["w"])
        for key in w:
            b = self.bufs.get(key)
            if b is not None:
                if b["w"] is not None:
                    deps.append(b["w"])
                deps.extend(b["r"].values())
        return deps

    def _record(self, ev, r, w):
        sem, val = ev
        for key in r:
            b = self.bufs.setdefault(key, {"w": None, "r": {}})
            old = b["r"].get(id(sem))
            if old is None or old[1] < val:
                b["r"][id(sem)] = ev
        for key in w:
            self.bufs[key] = {"w": ev, "r": {}}

    def defer_begin(self):
        self.deferq = []
        self.deferring = True

    def defer_end(self):
        self.deferring = False

    def flush(self, k=None):
        q = getattr(self, "deferq", [])
        n = len(q) if k is None else min(k, len(q))
        for _ in range(n):
            kind, e, fn, r, w = q.pop(0)
            (self.op if kind == "op" else self.dma)(e, fn, r, w)

    def op(self, e, fn, r=(), w=()):
        if getattr(self, "deferring", False):
            self.deferq.append(("op", e, fn, list(r), list(w)))
            return None
        deps = self._deps(r, w)
        own = self.sem[e]
        for ev in deps:
            if e == "pe" and ev[0] is own:
                continue
            self._wait(e, ev)
        ins = fn(self.eng[e])
        if self.cnt[e] >= SEM_LIMIT:
            self._new_engine_sem(e)
        self.cnt[e] += 1
        ins.then_inc(self.sem[e], 1)
        self.ninstr += 1
        ev = (self.sem[e], self.cnt[e])
        self._record(ev, r, w)
        return ev

    def dma(self, q, fn, r=(), w=()):
        if getattr(self, "deferring", False):
            self.deferq.append(("dma", q, fn, list(r), list(w)))
            return None
        deps = self._deps(r, w)
        pool = self.dpool[q]
        i = self.drr[q]
        self.drr[q] = (i + 1) % len(pool)
        slot = pool[i]
        if slot["val"] >= SEM_LIMIT:
            slot = pool[i] = self._new_dma_slot()
        if slot["val"] > 0:
            self._wait(q, (slot["sem"], slot["val"]))
        for ev in deps:
            self._wait(q, ev)
        ins = fn(self.eng[q])
        slot["val"] += 16
        ins.then_inc(slot["sem"], 16)
        self.ninstr += 1
        ev = (slot["sem"], slot["val"])
        self._record(ev, r, w)
        return ev

    def barrier(self, e, keys):
        for ev in self._deps((), keys):
            self._wait(e, ev)

    def full_barrier(self):
        evs = [(self.sem[x], self.cnt[x]) for x in ("pe", "act", "dve", "pool") if self.cnt[x] > 0]
        for pool in self.dpool.values():
            for slot in pool:
                if slot["val"] > 0:
                    evs.append((slot["sem"], slot["val"]))
        for e in ("pe", "act", "dve", "pool", "sp"):
            for ev in evs:
                if ev[0] is self.sem.get(e):
                    continue
                self._wait(e, ev)

    def finish(self):
        for q, pool in self.dpool.items():
            for slot in pool:
                if slot["val"] > 0:
                    self._wait("sp", (slot["sem"], slot["val"]))


def _t5_bucket(dist):
    d = dist.astype(np.float32)
    large = 16 + (np.log(np.maximum(d, 1.0) / 16) / math.log(2048 / 16) * 16)
    large = np.minimum(large.astype(np.int32), 31)
    return np.where(dist < 16, dist, large)


_CONST_CACHE = {}


def _consts():
    if _CONST_CACHE:
        return _CONST_CACHE
    c = _CONST_CACHE
    c["ident"] = np.eye(128, dtype=np.float32)
    pos = np.concatenate([np.arange(SEQ), PAST + (np.arange(128) % TS)]).astype(np.float32)
    inv = (10000.0 ** (-np.arange(64, dtype=np.float32) / 64)).astype(np.float32)
    ang = (pos[:, None] * inv[None, :]).astype(np.float32)
    cos = np.cos(ang).astype(np.float32); sin = np.sin(ang).astype(np.float32)
    c["rotc"] = np.concatenate([cos, cos], 1)
    c["rots"] = np.concatenate([-sin, sin], 1)
    lg = np.log1p(-np.exp2(-5.0 - np.arange(4, dtype=np.float64)))
    p = np.arange(128)
    for name, C in (("p", 128), ("s", TS)):
        tpos = p % C
        seq = p // C
        rel = tpos[None, :] - tpos[:, None]
        ok = (rel >= 0) & (seq[None, :] == seq[:, None])
        intra = np.where(ok[:, None, :], np.exp(lg[None, :, None] * np.maximum(rel, 0)[:, None, :]), 0.0)
        c["intraT_" + name] = intra.reshape(128, 512).astype(np.float32)
        qd = np.exp(lg[:, None] * (tpos[None, :] + 1.0))
        c["qdec_" + name] = np.broadcast_to(qd.reshape(1, 512), (128, 512)).astype(np.float32).copy()
        c["kdec_" + name] = np.exp(lg[None, :] * (C - 1.0 - tpos[:, None])).astype(np.float32)
        c["cdec_" + name] = [float(v) for v in np.exp(lg * C)]
    c["rowmask"] = (p[:, None] // TS == np.arange(NSEQ_S)[None, :]).astype(np.float32)
    c["iota16"] = np.broadcast_to(np.arange(16, dtype=np.float32)[None, :], (128, 16)).copy()
    for g, (win, dil) in enumerate(GROUPS):
        oh = np.zeros((33, 385), np.float32)
        for m in range(385):
            rel = m - 128
            if 0 <= rel <= 128:
                oh[int(_t5_bucket(np.array([rel * dil]))[0]), m] = 1.0
            else:
                oh[32, m] = NEGB
        c[f"oh{g}"] = oh
    return c


def build_program(stop=99, nocopy=False, cbs=None, nocso=False, tiles=None):
    nc = bass.Bass("TRN2", target_bir_lowering=False)
    es = ExitStack()
    S = Sched(nc, es)
    CST = _consts()

    def din(name, shape, dt=F32):
        return nc.dram_tensor(name, list(shape), dt, kind="ExternalInput").ap()

    def dout(name, shape, dt=F32):
        return nc.dram_tensor(name, list(shape), dt, kind="ExternalOutput").ap()

    def dscr(name, shape, dt):
        return nc.dram_tensor(name, list(shape), dt).ap()

    def sb(name, shape, dt):
        return es.enter_context(nc.sbuf_tensor("s_" + name, list(shape), dt))

    xp = din("xp", [SEQ, D]); xs = din("xs", [128, D])
    cp_rep = din("cp_rep", [128, D]); cs_rep = din("cs_rep", [128, D])
    st_in = din("st_in", [NSEQ_S, 4, 128, 128])
    c_in = [din(f"c_in{g}", [NSEQ_S, GROUPS[g][0], 2, 512]) for g in range(3)]
    w_ada = din("w_ada", [D, 6 * D]); b_ada = din("b_ada", [1, 6 * D])
    w_in = din("w_in", [D, IN_COLS])
    ident_d = din("ident", [128, 128])
    rotc_d = din("rotc", [NTOK, 128]); rots_d = din("rots", [NTOK, 128])
    intra_d = [din("intraT_p", [128, 512]), din("intraT_s", [128, 512])]
    qdec_d = [din("qdec_p", [128, 512]), din("qdec_s", [128, 512])]
    kdec_d = [din("kdec_p", [128, 4]), din("kdec_s", [128, 4])]
    rowmask_d = din("rowmask", [128, NSEQ_S])
    iota16_d = din("iota16", [128, 16])
    oh_d = [din(f"oh{g}", [33, 385]) for g in range(3)]
    rel_bias_d = din("rel_bias", [32, 24])
    w_ret_o = din("w_ret_o", [512, D]); w_att_o = din("w_att_o", [512, D]); w_out = din("w_out", [D, D])
    b_out_rep = din("b_out_rep", [128, D])
    ln_rep = {k: din(k + "_rep", [128, D]) for k in ("ln1_g", "ln1_b", "ln2_g", "ln2_b")}
    peer_wq = din("peer_wq", [D, 2048]); peer_keys = din("peer_keys", [2048, 128])
    peer_u = din("peer_u", [16384, D]); peer_v = din("peer_v", [16384, D])

    yp = dout("yp", [SEQ, D]); ys = dout("ys", [128, D])
    srp = dout("srp", [4, 128, 128]); srs = dout("srs", [NSEQ_S, 4, 128, 128])
    cpo = [dout(f"cpo{g}", [GROUPS[g][0], 2, 512]) for g in range(3)]
    cso = [dout(f"cso{g}", [NSEQ_S, GROUPS[g][0], 2, 512]) for g in range(3)]

    P = dscr("proj", [NTOK, IN_COLS], BF16)
    RG = dscr("retg", [NTOK, 512], BF16)
    ATT = dscr("attacc", [NTOK, 520], F32)
    EXT = dscr("biasext", [24, 385], F32)
    UB = dscr("peer_u_bf", [16384, D], BF16)
    VB = dscr("peer_v_bf", [16384, D], BF16)
    EXT2 = dscr("biasext2", [24, 128 * 385], F32)

    banks = [es.enter_context(nc.psum_tensor(f"bank{i}", [128, 512], F32)) for i in range(8)]

    def bk(i):
        return f"bank{i}"

    ident_f = sb("ident_f", [128, 128], F32)
    ident_b = sb("ident_b", [128, 128], BF16)
    ones1 = sb("ones1", [1, 128], F32)
    modD = sb("modD", [128, 2, 4, D], F32)
    mod_stack = ExitStack()
    modp = mod_stack.enter_context(nc.sbuf_tensor("s_modp", [128, 6 * D], F32))
    mods = mod_stack.enter_context(nc.sbuf_tensor("s_mods", [128, 6 * D], F32))

    S.dma("sp", lambda e: e.dma_start(out=ident_f[:], in_=ident_d), w=["ident_f"])
    S.op("dve", lambda e: e.tensor_copy(out=ident_b[:], in_=ident_f[:]), r=["ident_f"], w=["ident_b"])
    S.op("dve", lambda e: e.memset(ones1[:], 1.0), w=["ones1"])

    tabkeys = []
    for name, src_t, dst_t in (("UB", peer_u, UB), ("VB", peer_v, VB)):
        for c in range(8):
            rs_ = slice(c * 2048, (c + 1) * 2048)
            S.dma("pool", lambda e, src_t=src_t, dst_t=dst_t, rs_=rs_: e.dma_start(out=dst_t[rs_, :], in_=src_t[rs_, :]),
                  w=[(name, c)])
            tabkeys.append((name, c))
    WQB = dscr("peer_wq_bf", [D, 2048], BF16)
    wqbkeys = []
    for c in range(2):
        cs_ = slice(c * 1024, (c + 1) * 1024)
        S.dma("pool", lambda e, cs_=cs_: e.dma_start(out=WQB[:, cs_], in_=peer_wq[:, cs_]), w=[("WQB", c)])
        wqbkeys.append(("WQB", c))
    ubkeys = [k for k in tabkeys if k[0] == "UB"]
    vbkeys = [k for k in tabkeys if k[0] == "VB"]
    for g in range(0 if not nocopy else 3, 3):
        nb = GROUPS[g][0]
        for b in range(NSEQ_S):
            src = c_in[g][b, TS:nb].rearrange("(a r) k c -> a (r k c)", r=8)
            dst = cso[g][b, 0:nb - TS].rearrange("(a r) k c -> a (r k c)", r=8)
            S.dma("act", lambda e, s=src, d=dst: e.dma_start(out=d, in_=s), w=[("cso_copy", g, b)])

    with ExitStack() as ph:
        def psb(name, shape, dt):
            return ph.enter_context(nc.sbuf_tensor("s_" + name, list(shape), dt))
        c_tok = psb("c_tok", [128, D], F32)
        c_act = psb("c_act", [128, D], F32)
        cT = [psb(f"cT{i}", [128, 8, 128], F32) for i in range(2)]
        wada_t = [psb(f"wada{i}", [128, 8, 512], F32) for i in range(2)]
        bada_t = psb("bada", [1, 6 * D], F32)
        S.dma("sp", lambda e: e.dma_start(out=bada_t[:], in_=b_ada), w=["bada"])
        for i, src in enumerate((cp_rep, cs_rep)):
            S.dma("sp", lambda e, s=src: e.dma_start(out=c_tok[:], in_=s), w=["c_tok"])
            S.op("act", lambda e: e.activation(out=c_act[:], in_=c_tok[:], func=AF.Silu), r=["c_tok"], w=["c_act"])
            for half in range(2):
                def tr4(e, half=half):
                    for j in range(4):
                        kc = half * 4 + j
                        ins = e.transpose(out=banks[half][:, j * 128:(j + 1) * 128],
                                          in_=c_act[:, kc * 128:(kc + 1) * 128], identity=ident_f[:])
                    return ins
                S.op("pe", tr4, r=["c_act", "ident_f"], w=[bk(half)])
                S.op("dve", lambda e, half=half, i=i: e.tensor_copy(
                    out=cT[i][:, half * 4:(half + 1) * 4, :],
                    in_=banks[half][:].rearrange("p (a b) -> p a b", a=4)), r=[bk(half)], w=[f"cT{i}"])
        wv = w_ada.rearrange("(kc p) n -> p kc n", p=128)
        for n in range(12):
            wt = wada_t[n % 2]
            wk = f"wada{n % 2}"
            S.dma("sp", lambda e, wt=wt, n=n: e.dma_start(out=wt[:], in_=wv[:, :, n * 512:(n + 1) * 512]), w=[wk])
            for i, mod in enumerate((modp, mods)):
                b_ = 2 + i
                def mm(e, b_=b_, n=n, wt=wt, i=i):
                    e.matmul(banks[b_][:], lhsT=ones1[0:1, :], rhs=bada_t[0:1, n * 512:(n + 1) * 512],
                             start=True, stop=False)
                    for kc in range(8):
                        ins = e.matmul(banks[b_][:], lhsT=cT[i][:, kc, :], rhs=wt[:, kc, :],
                                       start=False, stop=(kc == 7))
                    return ins
                S.op("pe", mm, r=["ones1", "bada", f"cT{i}", wk], w=[bk(b_)])
                S.op("act", lambda e, b_=b_, mod=mod, n=n: e.activation(
                    out=mod[:, n * 512:(n + 1) * 512], in_=banks[b_][:], func=AF.Copy),
                    r=[bk(b_)], w=[("mod", i, n)])
        for i, mod in enumerate((modp, mods)):
            for j in (1, 4):
                S.op("dve", lambda e, mod=mod, j=j: e.tensor_scalar_add(
                    out=mod[:, j * D:(j + 1) * D], in0=mod[:, j * D:(j + 1) * D], scalar1=1.0),
                    r=[], w=[("mod", i, 2 * j), ("mod", i, 2 * j + 1)])

    for i, mod in enumerate((modp, mods)):
        for jj, j in enumerate((2, 3, 4, 5)):
            S.op("dve" if jj % 2 == 0 else "act",
                 (lambda e, i=i, jj=jj, j=j, mod=mod: e.tensor_copy(out=modD[:, i, jj, :], in_=mod[:, j * D:(j + 1) * D]))
                 if jj % 2 == 0 else
                 (lambda e, i=i, jj=jj, j=j, mod=mod: e.activation(out=modD[:, i, jj, :], in_=mod[:, j * D:(j + 1) * D],
                                                                  func=AF.Copy)),
                 r=[("mod", i, 2 * j), ("mod", i, 2 * j + 1)], w=[("modD", i, jj)])
    S.full_barrier()
    if stop <= 0:
        S.finish()
        return nc, es, S

    def modkeys(i, j):
        return [("mod", i, 2 * j), ("mod", i, 2 * j + 1)]

    hT_stack = ExitStack()
    hT = hT_stack.enter_context(nc.sbuf_tensor("hT", [128, 8, NTOK], BF16))
    with ExitStack() as ph:
        def psb(name, shape, dt):
            return ph.enter_context(nc.sbuf_tensor("s_" + name, list(shape), dt))
        xt = [psb(f"xt{i}", [128, D], F32) for i in range(2)]
        ht = [psb(f"ht{i}", [128, D], F32) for i in range(2)]
        for t in range(NT):
            i = 0 if t < NPT else 1
            mod = modp if t < NPT else mods
            src = xp[t * 128:(t + 1) * 128, :] if t < NPT else xs
            x_ = xt[t % 2]; h_ = ht[t % 2]
            xk = f"xt{t % 2}"; hk = f"ht{t % 2}"
            S.dma("sp", lambda e, x_=x_, src=src: e.dma_start(out=x_[:], in_=src), w=[xk])
            S.op("dve", lambda e, x_=x_, h_=h_, mod=mod: e.tensor_tensor(
                out=h_[:], in0=x_[:], in1=mod[:, D:2 * D], op=ALU.mult), r=[xk] + modkeys(i, 1), w=[hk])
            S.op("dve", lambda e, h_=h_, mod=mod: e.tensor_tensor(
                out=h_[:], in0=h_[:], in1=mod[:, 0:D], op=ALU.add), r=[hk] + modkeys(i, 0), w=[hk])
            for half in range(2):
                b_ = (t % 2) * 2 + half
                def tr4(e, b_=b_, half=half, h_=h_):
                    for j in range(4):
                        kc = half * 4 + j
                        ins = e.transpose(out=banks[b_][:, j * 128:(j + 1) * 128],
                                          in_=h_[:, kc * 128:(kc + 1) * 128], identity=ident_f[:])
                    return ins
                S.op("pe", tr4, r=[hk, "ident_f"], w=[bk(b_)])
                eng = "act" if half == 0 else "dve"
                if eng == "act":
                    S.op("act", lambda e, b_=b_, half=half, t=t: e.activation(
                        out=hT[:, half * 4:(half + 1) * 4, t * 128:(t + 1) * 128],
                        in_=banks[b_][:].rearrange("p (a b) -> p a b", a=4), func=AF.Copy),
                        r=[bk(b_)], w=[("hT", t, half)])
                else:
                    S.op("dve", lambda e, b_=b_, half=half, t=t: e.tensor_copy(
                        out=hT[:, half * 4:(half + 1) * 4, t * 128:(t + 1) * 128],
                        in_=banks[b_][:].rearrange("p (a b) -> p a b", a=4)),
                        r=[bk(b_)], w=[("hT", t, half)])

    S.full_barrier()
    if stop <= 1:
        S.finish()
        return nc, es, S
    with ExitStack() as ph:
        def psb(name, shape, dt):
            return ph.enter_context(nc.sbuf_tensor("s_" + name, list(shape), dt))
        wf = [psb(f"wf{i}", [128, 8, 512], F32) for i in range(2)]
        wb = [psb(f"wb{i}", [128, 8, 512], BF16) for i in range(2)]
        stg = [psb(f"stg{i}", [128, 512], BF16) for i in range(4)]
        stg32 = [psb(f"stg32_{i}", [128, 512], F32) for i in range(2)]
        wv = w_in.rearrange("(kc p) n -> p kc n", p=128)
        it = 0
        i32 = 0
        for cb in (range(17) if cbs is None else cbs):
            wf_ = wf[cb % 2]; wb_ = wb[cb % 2]
            wfk = f"wf{cb % 2}"; wbk = f"wb{cb % 2}"
            S.dma("sp", lambda e, wf_=wf_, cb=cb: e.dma_start(out=wf_[:], in_=wv[:, :, cb * 512:(cb + 1) * 512]),
                  w=[wfk])
            S.op("dve", lambda e, wf_=wf_, wb_=wb_: e.tensor_copy(out=wb_[:, 0:4, :], in_=wf_[:, 0:4, :]),
                 r=[wfk], w=[(wbk, 0)])
            S.op("act", lambda e, wf_=wf_, wb_=wb_: e.activation(out=wb_[:, 4:8, :], in_=wf_[:, 4:8, :], func=AF.Copy),
                 r=[wfk], w=[(wbk, 1)])
            scale = 1.0
            if cb == 1:
                scale = 128.0 ** -0.5
            if 4 <= cb <= 6:
                scale = 0.125
            for t in range(NT):
                b_ = it % 4
                sg = stg[it % 4]; sgk = f"stg{it % 4}"
                it += 1
                def mm(e, b_=b_, t=t, wb_=wb_):
                    for kc in range(8):
                        ins = e.matmul(banks[b_][:], lhsT=hT[:, kc, t * 128:(t + 1) * 128], rhs=wb_[:, kc, :],
                                       start=(kc == 0), stop=(kc == 7))
                    return ins
                S.op("pe", mm, r=[("hT", t, 0), ("hT", t, 1), (wbk, 0), (wbk, 1)], w=[bk(b_)])
                S.op("act", lambda e, b_=b_, sg=sg, scale=scale: e.activation(
                    out=sg[:], in_=banks[b_][:], func=AF.Copy, scale=scale), r=[bk(b_)], w=[sgk])
                S.dma("sp", lambda e, sg=sg, t=t, cb=cb: e.dma_start(
                    out=P[t * 128:(t + 1) * 128, cb * 512:(cb + 1) * 512], in_=sg[:]),
                    r=[sgk], w=[("P", t, cb)])
                if 7 <= cb <= 12:
                    g = (cb - 7) % 3
                    kv = (cb - 7) // 3
                    win = GROUPS[g][0]
                    if t < NPT and t * 128 >= SEQ - win:
                        s32 = stg32[i32 % 2]; s32k = f"stg32_{i32 % 2}"; i32 += 1
                        S.op("act", lambda e, b_=b_, s32=s32: e.activation(out=s32[:], in_=banks[b_][:], func=AF.Copy),
                             r=[bk(b_)], w=[s32k])
                        r0 = t * 128 - (SEQ - win)
                        S.dma("sp", lambda e, s32=s32, g=g, kv=kv, r0=r0: e.dma_start(
                            out=cpo[g][r0:r0 + 128, kv, :], in_=s32[:]), r=[s32k], w=[("cpo", g, kv, t)])
                    if t == NPT and not nocso:
                        s32 = stg32[i32 % 2]; s32k = f"stg32_{i32 % 2}"; i32 += 1
                        S.op("act", lambda e, b_=b_, s32=s32: e.activation(out=s32[:], in_=banks[b_][:], func=AF.Copy),
                             r=[bk(b_)], w=[s32k])
                        for b in range(NSEQ_S):
                            S.dma("sp", lambda e, s32=s32, g=g, kv=kv, b=b, win=win: e.dma_start(
                                out=cso[g][b, win - TS:win, kv, :], in_=s32[b * TS:(b + 1) * TS, :]),
                                r=[s32k], w=[("cso_new", g, kv, b)])

    S.full_barrier()
    hT_stack.close()
    mod_stack.close()
    if stop <= 2:
        S.finish()
        return nc, es, S

    def bcast_mid(ap2d, n):
        return ap2d.unsqueeze(1).to_broadcast([128, n, ap2d.shape[1]])

    def bcast_last(ap2d, n):
        return ap2d.unsqueeze(2).to_broadcast([128, ap2d.shape[1], n])

    def v4(ap2d):
        return ap2d.rearrange("p (h d) -> p h d", h=4)

    with ExitStack() as ph:
        def psb(name, shape, dt):
            return ph.enter_context(nc.sbuf_tensor("s_" + name, list(shape), dt))
        intra_t = [psb(f"intra{i}", [128, 512], F32) for i in range(2)]
        qdec_t = [psb(f"qdec{i}", [128, 512], F32) for i in range(2)]
        kdec_t = [psb(f"kdec{i}", [128, 4], F32) for i in range(2)]
        rowmask_t = psb("rowmask", [128, NSEQ_S], F32)
        for i in range(2):
            S.dma("sp", lambda e, i=i: e.dma_start(out=intra_t[i][:], in_=intra_d[i]), w=[f"intra{i}"])
            S.dma("sp", lambda e, i=i: e.dma_start(out=qdec_t[i][:], in_=qdec_d[i]), w=[f"qdec{i}"])
            S.dma("sp", lambda e, i=i: e.dma_start(out=kdec_t[i][:], in_=kdec_d[i]), w=[f"kdec{i}"])
        S.dma("sp", lambda e: e.dma_start(out=rowmask_t[:], in_=rowmask_d), w=["rowmask"])
        qin = [psb(f"qin{i}", [128, 512], BF16) for i in range(2)]
        kin = [psb(f"kin{i}", [128, 512], BF16) for i in range(2)]
        vin = [psb(f"vin{i}", [128, 512], BF16) for i in range(2)]
        gin = [psb(f"gin{i}", [128, 512], BF16) for i in range(2)]
        rc = [psb(f"rc{i}", [128, 128], F32) for i in range(2)]
        rs = [psb(f"rs{i}", [128, 128], F32) for i in range(2)]
        At = psb("rotA", [128, 512], F32)
        Bt = psb("rotB", [128, 512], F32)
        qr = psb("qr", [128, 512], BF16)
        kr = psb("kr", [128, 512], BF16)
        kd = psb("kd", [128, 512], BF16)
        qT = psb("qT", [128, 4, 128], BF16)
        qdT = psb("qdT", [128, 4, 128], BF16)
        kT = psb("kT", [128, 4, 128], BF16)
        PTr = psb("PTr", [128, 4, 128], BF16)
        Sst = psb("Sst", [128, 4, 128], F32)
        Sb = psb("Sb", [128, 4, 128], BF16)
        o_sb = psb("o_sb", [128, 512], F32)
        osq = psb("osq", [128, 512], F32)
        sil = psb("sil", [128, 512], F32)
        retg = [psb(f"retg{i}", [128, 512], BF16) for i in range(2)]
        ssum = psb("ssum", [128, 4], F32); ssq = psb("ssq", [128, 4], F32)
        mean = psb("mean", [128, 4], F32); msq = psb("msq", [128, 4], F32)
        var = psb("var", [128, 4], F32); rstd = psb("rstd", [128, 4], F32)
        S0f = psb("S0f", [128, NSEQ_S, 4, 128], F32)
        S0b = psb("S0b", [128, NSEQ_S, 4, 128], BF16)
        qdTm = psb("qdTm", [128, NSEQ_S, 4, 128], BF16)
        kdm = psb("kdm", [128, NSEQ_S, 512], BF16)
        Snew = [psb(f"Snew{i}", [128, 4, 128], F32) for i in range(2)]
        S.dma("sp", lambda e: e.dma_start(out=S0f[:], in_=st_in.rearrange("b h k v -> k b h v")), w=["S0f"])
        S.op("act", lambda e: e.activation(out=S0b[:], in_=S0f[:], func=AF.Copy), r=["S0f"], w=["S0b"])
        S.op("dve", lambda e: e.memset(qdTm[:], 0.0), w=["qdTm"])
        bq = banks[0][:].bitcast(BF16)[:, 0:512]
        bkk = banks[1][:].bitcast(BF16)[:, 0:512]

        for t in range(NT):
            i = 0 if t < NPT else 1
            par = t % 2
            cdec = CST["cdec_p"] if i == 0 else CST["cdec_s"]
            rows = slice(t * 128, (t + 1) * 128)
            for name, tl, c0 in (("qin", qin, 0), ("kin", kin, 512), ("vin", vin, 1024), ("gin", gin, 1536)):
                S.dma("sp", lambda e, tl=tl, c0=c0: e.dma_start(out=tl[par][:], in_=P[rows, c0:c0 + 512]),
                      r=[("P", t, c0 // 512)], w=[f"{name}{par}"])
            S.dma("sp", lambda e: e.dma_start(out=rc[par][:], in_=rotc_d[rows, :]), w=[f"rc{par}"])
            S.dma("sp", lambda e: e.dma_start(out=rs[par][:], in_=rots_d[rows, :]), w=[f"rs{par}"])

            def rotary(src, srck, dst, dstk):
                s4 = v4(src[:]); a4 = v4(At[:]); b4 = v4(Bt[:])
                S.op("dve", lambda e: e.tensor_tensor(out=a4, in0=s4, in1=bcast_mid(rc[par][:, :], 4), op=ALU.mult),
                     r=[srck, f"rc{par}"], w=["rotA"])
                S.op("dve", lambda e: e.tensor_tensor(out=b4[:, :, 0:64], in0=s4[:, :, 64:128],
                                                      in1=bcast_mid(rs[par][:, 0:64], 4), op=ALU.mult),
                     r=[srck, f"rs{par}"], w=["rotB0"])
                S.op("dve", lambda e: e.tensor_tensor(out=b4[:, :, 64:128], in0=s4[:, :, 0:64],
                                                      in1=bcast_mid(rs[par][:, 64:128], 4), op=ALU.mult),
                     r=[srck, f"rs{par}"], w=["rotB1"])
                S.op("dve", lambda e: e.tensor_tensor(out=dst[:], in0=At[:], in1=Bt[:], op=ALU.add),
                     r=["rotA", "rotB0", "rotB1"], w=[dstk])
            rotary(qin[par], f"qin{par}", qr, "qr")
            rotary(kin[par], f"kin{par}", kr, "kr")
            S.op("dve", lambda e: e.tensor_tensor(out=v4(kd[:]), in0=v4(kr[:]), in1=bcast_last(kdec_t[i][:, :], 128),
                                                  op=ALU.mult), r=["kr", f"kdec{i}"], w=["kd"])

            def tr4(e, src, dstb):
                for h in range(4):
                    ins = e.transpose(out=dstb[:, h * 128:(h + 1) * 128], in_=src[:, h * 128:(h + 1) * 128],
                                      identity=ident_b[:])
                return ins
            S.op("pe", lambda e: tr4(e, qr, bq), r=["qr", "ident_b"], w=[bk(0)])
            S.op("act", lambda e: e.activation(out=qT[:].rearrange("p h d -> p (h d)"), in_=bq, func=AF.Copy),
                 r=[], w=[bk(0), "qT"])
            S.op("dve", lambda e: e.tensor_tensor(out=qdT[:].rearrange("p h d -> p (h d)"), in0=bq,
                                                  in1=qdec_t[i][:], op=ALU.mult),
                 r=[f"qdec{i}"], w=[bk(0), "qdT"])
            S.op("pe", lambda e: tr4(e, kr, bkk), r=["kr", "ident_b"], w=[bk(1)])
            S.op("act", lambda e: e.activation(out=kT[:].rearrange("p h d -> p (h d)"), in_=bkk, func=AF.Copy),
                 r=[], w=[bk(1), "kT"])

            def sc4(e):
                for h in range(4):
                    ins = e.matmul(banks[2][:, h * 128:(h + 1) * 128], lhsT=kT[:, h, :], rhs=qT[:, h, :],
                                   start=True, stop=True)
                return ins
            S.op("pe", sc4, r=["kT", "qT"], w=[bk(2)])
            S.op("dve", lambda e: e.tensor_tensor(out=PTr[:].rearrange("p h d -> p (h d)"), in0=banks[2][:],
                                                  in1=intra_t[i][:], op=ALU.mult),
                 r=[f"intra{i}"], w=[bk(2), "PTr"])

            if i == 1:
                for b in range(NSEQ_S):
                    S.op("dve", lambda e, b=b: e.tensor_copy(out=qdTm[:, b, :, b * TS:(b + 1) * TS],
                                                             in_=qdT[:, :, b * TS:(b + 1) * TS]),
                         r=["qdT"], w=["qdTm"])
                    S.op("dve", lambda e, b=b: e.tensor_scalar(out=kdm[:, b, :], in0=kd[:], scalar1=rowmask_t[:, b:b + 1],
                                                               scalar2=None, op0=ALU.mult),
                         r=["kd", "rowmask"], w=[("kdm", b)])

            def o4(e):
                for h in range(4):
                    hs = slice(h * 128, (h + 1) * 128)
                    first_only = (i == 0 and t == 0)
                    ins = e.matmul(banks[3][:, hs], lhsT=PTr[:, h, :], rhs=vin[par][:, hs], start=True, stop=first_only)
                    if i == 0 and t > 0:
                        ins = e.matmul(banks[3][:, hs], lhsT=qdT[:, h, :], rhs=Sb[:, h, :], start=False, stop=True)
                    if i == 1:
                        for b in range(NSEQ_S):
                            ins = e.matmul(banks[3][:, hs], lhsT=qdTm[:, b, h, :], rhs=S0b[:, b, h, :],
                                           start=False, stop=(b == NSEQ_S - 1))
                return ins
            S.op("pe", o4, r=["PTr", f"vin{par}", "qdT", "Sb", "qdTm", "S0b"], w=[bk(3)])

            if i == 0:
                def ds4(e):
                    for h in range(4):
                        hs = slice(h * 128, (h + 1) * 128)
                        ins = e.matmul(banks[4][:, hs], lhsT=kd[:, hs], rhs=vin[par][:, hs], start=True, stop=True)
                    return ins
                S.op("pe", ds4, r=["kd", f"vin{par}"], w=[bk(4)])
                if t == 0:
                    S.op("dve", lambda e: e.tensor_copy(out=Sst[:].rearrange("p h d -> p (h d)"), in_=banks[4][:]),
                         r=[], w=[bk(4), "Sst"])
                else:
                    def upd(e):
                        for h in range(4):
                            ins = e.scalar_tensor_tensor(out=Sst[:, h, :], in0=Sst[:, h, :], scalar=cdec[h],
                                                         in1=banks[4][:, h * 128:(h + 1) * 128],
                                                         op0=ALU.mult, op1=ALU.add)
                        return ins
                    S.op("dve", upd, r=[], w=[bk(4), "Sst"])
                S.op("act", lambda e: e.activation(out=Sb[:], in_=Sst[:], func=AF.Copy), r=["Sst"], w=["Sb"])
                if t == NPT - 1:
                    S.dma("sp", lambda e: e.dma_start(out=srp.rearrange("h k v -> k h v"), in_=Sst[:]),
                          r=["Sst"], w=["srp"])
            else:
                for b in range(NSEQ_S):
                    bb = 4 + (b % 2)
                    sn = Snew[b % 2]; snk = f"Snew{b % 2}"
                    def ds4(e, b=b, bb=bb):
                        for h in range(4):
                            hs = slice(h * 128, (h + 1) * 128)
                            ins = e.matmul(banks[bb][:, hs], lhsT=kdm[:, b, hs], rhs=vin[par][:, hs],
                                           start=True, stop=True)
                        return ins
                    S.op("pe", ds4, r=[("kdm", b), f"vin{par}"], w=[bk(bb)])
                    def upd(e, b=b, bb=bb, sn=sn):
                        for h in range(4):
                            ins = e.scalar_tensor_tensor(out=sn[:, h, :], in0=S0f[:, b, h, :], scalar=cdec[h],
                                                         in1=banks[bb][:, h * 128:(h + 1) * 128],
                                                         op0=ALU.mult, op1=ALU.add)
                        return ins
                    S.op("dve", upd, r=["S0f"], w=[bk(bb), snk])
                    S.dma("sp", lambda e, b=b, sn=sn: e.dma_start(out=srs[b].rearrange("h k v -> k h v"), in_=sn[:]),
                          r=[snk], w=[("srs", b)])

            S.op("act", lambda e: e.activation(out=o_sb[:], in_=banks[3][:], func=AF.Copy), r=[], w=[bk(3), "o_sb"])
            S.op("dve", lambda e: e.tensor_reduce(out=ssum[:], in_=v4(o_sb[:]), axis=AX.X, op=ALU.add),
                 r=["o_sb"], w=["ssum"])
            S.op("act", lambda e: e.activation(out=osq[:], in_=o_sb[:], func=AF.Square), r=["o_sb"], w=["osq"])
            S.op("dve", lambda e: e.tensor_reduce(out=ssq[:], in_=v4(osq[:]), axis=AX.X, op=ALU.add),
                 r=["osq"], w=["ssq"])
            S.op("dve", lambda e: e.tensor_scalar_mul(out=mean[:], in0=ssum[:], scalar1=1.0 / 128), r=["ssum"], w=["mean"])
            S.op("dve", lambda e: e.tensor_tensor(out=msq[:], in0=mean[:], in1=mean[:], op=ALU.mult), r=["mean"], w=["msq"])
            S.op("dve", lambda e: e.scalar_tensor_tensor(out=var[:], in0=ssq[:], scalar=1.0 / 128, in1=msq[:],
                                                         op0=ALU.mult, op1=ALU.subtract), r=["ssq", "msq"], w=["var"])
            S.op("dve", lambda e: e.tensor_scalar_add(out=var[:], in0=var[:], scalar1=HN_EPS), r=["var"], w=["var"])
            S.op("act", lambda e: e.activation(out=var[:], in_=var[:], func=AF.Sqrt), r=["var"], w=["var"])
            S.op("dve", lambda e: e.reciprocal(out=rstd[:], in_=var[:]), r=["var"], w=["rstd"])
            S.op("dve", lambda e: e.tensor_tensor(out=v4(o_sb[:]), in0=v4(o_sb[:]), in1=bcast_last(mean[:, :], 128),
                                                  op=ALU.subtract), r=["o_sb", "mean", "osq"], w=["o_sb"])
            S.op("dve", lambda e: e.tensor_tensor(out=v4(o_sb[:]), in0=v4(o_sb[:]), in1=bcast_last(rstd[:, :], 128),
                                                  op=ALU.mult), r=["o_sb", "rstd"], w=["o_sb"])
            S.op("act", lambda e: e.activation(out=sil[:], in_=gin[par][:], func=AF.Silu), r=[f"gin{par}"], w=["sil"])
            S.op("dve", lambda e: e.tensor_tensor(out=retg[par][:], in0=o_sb[:], in1=sil[:], op=ALU.mult),
                 r=["o_sb", "sil"], w=[f"retg{par}"])
            S.dma("sp", lambda e: e.dma_start(out=RG[rows, :], in_=retg[par][:]), r=[f"retg{par}"], w=[("RG", t)])

    S.full_barrier()
    if stop <= 3:
        S.finish()
        return nc, es, S

    with ExitStack() as ph:
        def psb(name, shape, dt):
            return ph.enter_context(nc.sbuf_tensor("s_" + name, list(shape), dt))
        rb_aug = psb("rb_aug", [33, 24], F32)
        oh_t = [psb(f"oh{g}", [33, 385], F32) for g in range(3)]
        ext_sb = psb("ext_sb", [8, 385], F32)
        btmp = [psb(f"btmp{i}", [128, 256], F32) for i in range(2)]
        biasT = psb("biasT", [128, 24, 256], BF16)
        S.op("dve", lambda e: e.memset(rb_aug[:], 1.0), w=["rb_aug"])
        S.dma("sp", lambda e: e.dma_start(out=rb_aug[0:32, :], in_=rel_bias_d), w=["rb_aug"])
        for g in range(3):
            S.dma("sp", lambda e, g=g: e.dma_start(out=oh_t[g][:], in_=oh_d[g]), w=[f"oh{g}"])
            S.op("pe", lambda e, g=g: e.matmul(banks[0][0:8, 0:385], lhsT=rb_aug[:, g * 8:(g + 1) * 8], rhs=oh_t[g][:],
                                               start=True, stop=True), r=["rb_aug", f"oh{g}"], w=[bk(0)])
            S.op("act", lambda e: e.activation(out=ext_sb[:], in_=banks[0][0:8, 0:385], func=AF.Copy),
                 r=[], w=[bk(0), "ext_sb"])
            S.dma("sp", lambda e, g=g: e.dma_start(out=EXT[g * 8:(g + 1) * 8, :], in_=ext_sb[:]),
                  r=["ext_sb"], w=[("EXT", g)])
        for gh in range(24):
            bt = btmp[gh % 2]; btk = f"btmp{gh % 2}"
            srcb = bass.AP(tensor=EXT.tensor, offset=gh * 385, ap=[[0, 128], [1, 385]])
            dstb = bass.AP(tensor=EXT2.tensor, offset=gh * 128 * 385, ap=[[385, 128], [1, 385]])
            S.dma("sp", lambda e, srcb=srcb, dstb=dstb: e.dma_start(out=dstb, in_=srcb),
                  r=[("EXT", gh // 8)], w=[("EXT2", gh)])
            for kc, c0 in ((0, 256), (1, 128)):
                src = bass.AP(tensor=EXT2.tensor, offset=gh * 128 * 385 + c0, ap=[[384, 128], [1, 128]])
                S.dma("sp", lambda e, bt=bt, kc=kc, src=src: e.dma_start(out=bt[:, kc * 128:(kc + 1) * 128], in_=src),
                      r=[("EXT2", gh)], w=[(btk, kc)])
            S.op("dve", lambda e, bt=bt, gh=gh: e.tensor_copy(out=biasT[:, gh, :], in_=bt[:]),
                 r=[(btk, 0), (btk, 1)], w=[("biasT", gh)])

        Qt = [psb(f"Qt{i}", [128, 512], BF16) for i in range(2)]
        Kt = [psb(f"Kt{i}", [128, 512], BF16) for i in range(2)]
        QT = [psb(f"QT{i}", [128, 4, 128], BF16) for i in range(2)]
        KT = [psb(f"KT{i}", [128, 4, 128], BF16) for i in range(3)]
        Va = [psb(f"Va{i}", [128, 8, 65], BF16) for i in range(3)]
        PT = [psb(f"PT{i}", [128, 2, 2, 128], BF16) for i in range(2)]
        OLs = [psb(f"OLs{i}", [128, 520], F32) for i in range(2)]
        kvf = [psb(f"kvf{i}", [128, 2, 512], F32) for i in range(2)]
        for i in range(3):
            S.op("dve", lambda e, i=i: e.memset(Va[i][:], 1.0), w=[f"Va{i}"])
        for i in range(2):
            S.op("dve", lambda e, i=i: e.memset(Qt[i][:], 0.0), w=[f"Qt{i}"])
            S.op("dve", lambda e, i=i: e.memset(Kt[i][:], 0.0), w=[f"Kt{i}"])
        bq = banks[0][:].bitcast(BF16)[:, 0:512]
        bkk = banks[1][:].bitcast(BF16)[:, 0:512]
        state = {"blk": 0}

        def tr4(e, src, dstb):
            for j in range(4):
                ins = e.transpose(out=dstb[:, j * 128:(j + 1) * 128], in_=src[:, j * 128:(j + 1) * 128],
                                  identity=ident_b[:])
            return ins

        def attn_core(g, NQ, qT, qTk, kTc, kTck, kTp, kTpk, vc, vck, vp, vpk):
            blk = state["blk"]; state["blk"] += 1
            has_prev = kTp is not None
            for hp in range(4):
                bnk = banks[2 + hp]
                pt = PT[hp % 2]; ptk = f"PT{hp % 2}"
                def sc(e, hp=hp, bnk=bnk):
                    for hh in range(2):
                        h = 2 * hp + hh
                        gh = g * 8 + h
                        ps_ = slice((h % 2) * 64, (h % 2) * 64 + 64)
                        base = hh * 256
                        if has_prev:
                            e.matmul(bnk[:, base:base + NQ], lhsT=ident_b[:], rhs=biasT[:, gh, 0:NQ],
                                     start=True, stop=False)
                            e.matmul(bnk[:, base:base + NQ], lhsT=kTp[ps_, h // 2, :], rhs=qT[ps_, h // 2, 0:NQ],
                                     start=False, stop=True)
                        e.matmul(bnk[:, base + 128:base + 128 + NQ], lhsT=ident_b[:], rhs=biasT[:, gh, 128:128 + NQ],
                                 start=True, stop=False)
                        ins = e.matmul(bnk[:, base + 128:base + 128 + NQ], lhsT=kTc[ps_, h // 2, :],
                                       rhs=qT[ps_, h // 2, 0:NQ], start=False, stop=True)
                    return ins
                S.op("pe", sc, r=[qTk, kTck, "ident_b"] + ([kTpk] if has_prev else []) +
                     [("biasT", g * 8 + 2 * hp), ("biasT", g * 8 + 2 * hp + 1)], w=[bk(2 + hp)])
                bv = bnk[:].rearrange("p (a b c) -> p a b c", a=2, b=2)
                if has_prev:
                    S.op("act", lambda e, pt=pt, bv=bv: e.activation(out=pt[:, :, :, 0:NQ], in_=bv[:, :, :, 0:NQ],
                                                                     func=AF.Exp), r=[], w=[bk(2 + hp), ptk])
                else:
                    S.op("act", lambda e, pt=pt, bv=bv: e.activation(out=pt[:, :, 1, 0:NQ], in_=bv[:, :, 1, 0:NQ],
                                                                     func=AF.Exp), r=[], w=[bk(2 + hp), ptk])
                def pv(e, hp=hp, pt=pt):
                    for hh in range(2):
                        h = 2 * hp + hh
                        ob = banks[6 + h // 4]
                        oreg = ob[0:NQ, (h % 4) * 65:(h % 4) * 65 + 65]
                        if has_prev:
                            e.matmul(oreg, lhsT=pt[:, hh, 0, 0:NQ], rhs=vp[:, h, :], start=True, stop=False)
                        ins = e.matmul(oreg, lhsT=pt[:, hh, 1, 0:NQ], rhs=vc[:, h, :], start=(not has_prev), stop=True)
                    return ins
                S.op("pe", pv, r=[ptk, vck] + ([vpk] if has_prev else []), w=[bk(6 + hp // 2)])
            ol = OLs[blk % 2]; olk = f"OLs{blk % 2}"
            S.op("act", lambda e: e.activation(out=ol[0:NQ, 0:260], in_=banks[6][0:NQ, 0:260], func=AF.Copy),
                 r=[], w=[bk(6), (olk, 0)])
            S.op("dve", lambda e: e.tensor_copy(out=ol[0:NQ, 260:520], in_=banks[7][0:NQ, 0:260]),
                 r=[], w=[bk(7), (olk, 1)])
            return ol, olk

        att_events = []
        for g, (win, dil) in enumerate(GROUPS):
            prev_events = att_events
            att_events = []
            Pv = P[0:SEQ].rearrange("(u d) c -> d u c", d=dil)
            Av = ATT[0:SEQ].rearrange("(u d) c -> d u c", d=dil)
            nb = NPT // dil
            cnt = 0
            for r in range(dil):
                for ub in range(nb):
                    par = cnt % 2; p3 = cnt % 3; pp3 = (cnt - 1) % 3
                    cnt += 1
                    us = slice(ub * 128, (ub + 1) * 128)
                    rkeys = [("P", (r + dil * (ub * 128 + i)) // 128, 0) for i in (0, 127)]
                    S.dma("sp", lambda e: e.dma_start(out=Qt[par][:], in_=Pv[r, us, 2048 + g * 512:2048 + (g + 1) * 512]),
                          w=[f"Qt{par}"])
                    S.dma("sp", lambda e: e.dma_start(out=Kt[par][:], in_=Pv[r, us, 3584 + g * 512:3584 + (g + 1) * 512]),
                          w=[f"Kt{par}"])
                    S.dma("sp", lambda e: e.dma_start(
                        out=Va[p3][:, :, 0:64],
                        in_=Pv[r, us, 5120 + g * 512:5120 + (g + 1) * 512].rearrange("p (h d) -> p h d", h=8)),
                        w=[f"Va{p3}"])
                    S.op("pe", lambda e: tr4(e, Qt[par], bq), r=[f"Qt{par}", "ident_b"], w=[bk(0)])
                    S.op("act", lambda e: e.activation(out=QT[par][:].rearrange("p h d -> p (h d)"), in_=bq, func=AF.Copy),
                         r=[], w=[bk(0), f"QT{par}"])
                    S.op("pe", lambda e: tr4(e, Kt[par], bkk), r=[f"Kt{par}", "ident_b"], w=[bk(1)])
                    S.op("dve", lambda e: e.tensor_copy(out=KT[p3][:].rearrange("p h d -> p (h d)"), in_=bkk),
                         r=[], w=[bk(1), f"KT{p3}"])
                    hp_ = ub > 0
                    ol, olk = attn_core(g, 128, QT[par], f"QT{par}", KT[p3], f"KT{p3}",
                                        KT[pp3] if hp_ else None, f"KT{pp3}", Va[p3], f"Va{p3}",
                                        Va[pp3] if hp_ else None, f"Va{pp3}")
                    aq_ = "sp" if g == 0 else "pool"
                    if g > 0 and r == 0 and ub == 0:
                        for ev in prev_events:
                            S._wait(aq_, ev)
                    kw = {} if g == 0 else {"accum_op": ALU.add}
                    ev = S.dma(aq_, lambda e: e.dma_start(out=Av[r, us, :], in_=ol[:], **kw),
                               r=[(olk, 0), (olk, 1)], w=[("ATT", g, r, ub)])
                    att_events.append(ev)
        if stop > 4:
            for g, (win, dil) in enumerate(GROUPS):
                nq = TS // dil if dil <= TS else 1
                classes = list(range(min(dil, TS)))
                prev_events = att_events
                att_events = []
                cnt = 0
                for b in range(NSEQ_S):
                    cv = c_in[g][b].rearrange("(u d) k c -> d u k c", d=dil)
                    for r in classes:
                        par = cnt % 2; p3 = cnt % 3
                        cnt += 1
                        tok0 = SEQ + b * TS + r
                        rows = slice(tok0, tok0 + dil * (nq - 1) + 1, dil)
                        S.dma("sp", lambda e: e.dma_start(out=kvf[par][:], in_=cv[r]), w=[f"kvf{par}"])
                        S.dma("sp", lambda e: e.dma_start(out=Qt[par][0:nq, :],
                                                          in_=P[rows, 2048 + g * 512:2048 + (g + 1) * 512]),
                              r=[("P", NPT, 4 + g)], w=[f"Qt{par}"])
                        S.dma("sp", lambda e: e.dma_start(out=Kt[par][0:nq, :],
                                                          in_=P[rows, 3584 + g * 512:3584 + (g + 1) * 512]),
                              r=[("P", NPT, 7 + g)], w=[f"Kt{par}"])
                        vcur = Va[(2 * cnt) % 3]; vck = f"Va{(2 * cnt) % 3}"
                        vprv = Va[(2 * cnt + 1) % 3]; vpk = f"Va{(2 * cnt + 1) % 3}"
                        S.dma("sp", lambda e: e.dma_start(
                            out=vcur[0:nq, :, 0:64],
                            in_=P[rows, 5120 + g * 512:5120 + (g + 1) * 512].rearrange("p (h d) -> p h d", h=8)),
                            r=[("P", NPT, 10 + g)], w=[vck])
                        S.op("act", lambda e: e.activation(out=Kt[1 - par][:], in_=kvf[par][:, 0, :], func=AF.Copy),
                             r=[f"kvf{par}"], w=[f"Kt{1 - par}"])
                        S.op("dve", lambda e: e.tensor_copy(out=vprv[:, :, 0:64],
                                                            in_=kvf[par][:, 1, :].rearrange("p (h d) -> p h d", h=8)),
                             r=[f"kvf{par}"], w=[vpk])
                        S.op("pe", lambda e: tr4(e, Qt[par], bq), r=[f"Qt{par}", "ident_b"], w=[bk(0)])
                        S.op("act", lambda e: e.activation(out=QT[par][:].rearrange("p h d -> p (h d)"), in_=bq,
                                                           func=AF.Copy), r=[], w=[bk(0), f"QT{par}"])
                        S.op("pe", lambda e: tr4(e, Kt[par], bkk), r=[f"Kt{par}", "ident_b"], w=[bk(1)])
                        S.op("dve", lambda e: e.tensor_copy(out=KT[0][:].rearrange("p h d -> p (h d)"), in_=bkk),
                             r=[], w=[bk(1), "KT0"])
                        S.op("pe", lambda e: tr4(e, Kt[1 - par], bq), r=[f"Kt{1 - par}", "ident_b"], w=[bk(0)])
                        S.op("act", lambda e: e.activation(out=KT[1][:].rearrange("p h d -> p (h d)"), in_=bq,
                                                           func=AF.Copy), r=[], w=[bk(0), "KT1"])
                        ol, olk = attn_core(g, 8, QT[par], f"QT{par}", KT[0], "KT0", KT[1], "KT1",
                                            vcur, vck, vprv, vpk)
                        aq_ = "sp" if g == 0 else "pool"
                        if g > 0 and cnt == 1:
                            for ev in prev_events:
                                S._wait(aq_, ev)
                        kw = {} if g == 0 else {"accum_op": ALU.add}
                        ev = S.dma(aq_, lambda e: e.dma_start(out=ATT[rows, :], in_=ol[0:nq, :], **kw),
                                   r=[(olk, 0), (olk, 1)], w=[("ATTs", g, b, r)])
                        att_events.append(ev)

    S.full_barrier()
    if stop <= 5:
        S.finish()
        return nc, es, S

    with ExitStack() as ph:
        def psb(name, shape, dt):
            return ph.enter_context(nc.sbuf_tensor("s_" + name, list(shape), dt))
        wro_b = psb("wro_b", [128, 4, D], BF16)
        wao_b = psb("wao_b", [128, 4, D], BF16)
        wout_b = psb("wout_b", [128, 8, D], BF16)
        wqs = [psb(f"wqs{i}", [128, 8, 512], BF16) for i in range(2)]
        WQv = WQB.rearrange("(kc p) n -> p kc n", p=128)
        keysT = psb("keysT", [128, 16, 128], BF16)
        bout_t = psb("bout_t", [128, D], F32)
        lnt = {k: psb(k + "_t", [128, D], F32) for k in ("ln1_g", "ln1_b", "ln2_g", "ln2_b")}
        iota16 = psb("iota16", [128, 16], F32)
        S.dma("sp", lambda e: e.dma_start(out=bout_t[:], in_=b_out_rep), w=["bout_t"])
        for k in lnt:
            S.dma("sp", lambda e, k=k: e.dma_start(out=lnt[k][:], in_=ln_rep[k]), w=[k + "_t"])
        S.dma("sp", lambda e: e.dma_start(out=iota16[:], in_=iota16_d), w=["iota16"])
        with ExitStack() as wl:
            wst = [wl.enter_context(nc.sbuf_tensor(f"s_wst{i}", [128, 4, 1024], F32)) for i in range(2)]
            jobs = []
            for kc0 in (0,):
                jobs.append((w_ret_o.rearrange("(kc p) n -> p kc n", p=128), wro_b[:, :, :], "wro_b"))
                jobs.append((w_att_o.rearrange("(kc p) n -> p kc n", p=128), wao_b[:, :, :], "wao_b"))
            wov = w_out.rearrange("(kc p) n -> p kc n", p=128)
            jobs.append((wov[:, 0:4, :], wout_b[:, 0:4, :], ("wout_b", 0)))
            jobs.append((wov[:, 4:8, :], wout_b[:, 4:8, :], ("wout_b", 1)))
            for j, (src, dst, key) in enumerate(jobs):
                st_ = wst[j % 2]; stk = f"wst{j % 2}"
                S.dma("sp", lambda e, st_=st_, src=src: e.dma_start(out=st_[:], in_=src), w=[stk])
                if j % 2 == 0:
                    S.op("dve", lambda e, st_=st_, dst=dst: e.tensor_copy(out=dst, in_=st_[:]), r=[stk], w=[key])
                else:
                    S.op("act", lambda e, st_=st_, dst=dst: e.activation(out=dst, in_=st_[:], func=AF.Copy),
                         r=[stk], w=[key])
            kst = wst[0]
            for q4 in range(4):
                S.dma("sp", lambda e, q4=q4: e.dma_start(
                    out=kst[:, q4, 0:512].rearrange("p (a c) -> p a c", a=4),
                    in_=peer_keys[q4 * 512:(q4 + 1) * 512, :].rearrange("(a p) c -> p a c", p=128)), w=["wst0"])
            for q4 in range(4):
                def trk(e, q4=q4):
                    for a in range(4):
                        ins = e.transpose(out=banks[q4][:, a * 128:(a + 1) * 128], in_=kst[:, q4, a * 128:(a + 1) * 128],
                                          identity=ident_f[:])
                    return ins
                S.op("pe", trk, r=["wst0", "ident_f"], w=[bk(q4)])
                S.op("act", lambda e, q4=q4: e.activation(
                    out=keysT[:, q4 * 4:(q4 + 1) * 4, :].rearrange("p a k -> p (a k)"), in_=banks[q4][:], func=AF.Copy),
                    r=[], w=[bk(q4), ("keysT", q4)])
            S.full_barrier()
        wkeys = ["wro_b", "wao_b", ("wout_b", 0), ("wout_b", 1)]
        wqkeys = [("wq_b", a, b) for a in range(2) for b in range(2)]

        x_t = psb("x_t", [128, D], F32)
        att_t = psb("att_t", [128, 520], F32)
        rg_t = psb("rg_t", [128, 512], BF16)
        attn_t = psb("attn_t", [128, 512], BF16)
        rl = psb("rl", [128, 8], F32)
        aT = psb("aT", [128, 4, 128], BF16)
        rT = psb("rT", [128, 4, 128], BF16)
        bufA = psb("bufA", [128, 2048], F32)
        bufB = psb("bufB", [128, 2048], F32)
        tmp1 = psb("tmp1", [128, D], F32)
        x1 = psb("x1", [128, D], F32)
        h2 = psb("h2", [128, D], F32)
        h2b = psb("h2b", [128, D], BF16)
        h2T = psb("h2T", [128, 8, 128], BF16)
        junk = psb("junk", [128, D], BF16)
        st1 = psb("st1", [128, 8], F32)
        sv = psb("sv", [128, 16, 16], F32)
        si = psb("si", [128, 16, 16], U32)
        sif = psb("sif", [128, 16, 16], F32)
        cv = psb("cv", [128, 8, 16], F32)
        ci = psb("ci", [128, 8, 16], U32)
        hi_u = psb("hi_u", [128, 8, 16], U32); lo_u = psb("lo_u", [128, 8, 16], U32)
        hi_f = psb("hi_f", [128, 8, 16], F32); lo_f = psb("lo_f", [128, 8, 16], F32)
        i0 = psb("i0", [128, 8, 16], F32); i1 = psb("i1", [128, 8, 16], F32)
        e_f = psb("e_f", [128, 128], F32); e_i = psb("e_i", [128, 128], I32)
        gsm = psb("gsm", [128, 8, 16], F32); ssm = psb("ssm", [128, 8], F32)
        a_t = psb("a_t", [128, 128], F32); ga = psb("ga", [128, 128], BF16)
        NSL = 8
        Ug = [psb(f"Ug{i}", [128, D], BF16) for i in range(NSL)]
        Vg = [psb(f"Vg{i}", [128, D], BF16) for i in range(NSL)]
        Dg = [psb(f"Dg{i}", [128, 16, 128], BF16) for i in range(2)]
        Gb = bufA[:].bitcast(BF16)
        G_t = Gb[:, 0:2048]; sg_t = Gb[:, 2048:4096]
        Bb = bufB[:].bitcast(BF16)
        mixin = Bb[:, 0:1024]; mixT = Bb[:, 1024:2048].rearrange("p (k t) -> p k t", k=8)
        qpe = Bb[:, 0:2048]; qpT = Bb[:, 2048:4096].rearrange("p (k t) -> p k t", k=16)
        s_sc = bufA[:].rearrange("p (a k) -> p a k", a=16)
        oh4 = bufA[:].rearrange("p (h k i) -> p h k i", h=8, k=16)
        cand = bufB[:].rearrange("p (h c) -> p h c", h=8)
        cand4 = bufB[:].rearrange("p (h i j) -> p h i j", h=8, i=16)
        prod4 = bufB[:].rearrange("p (h k i) -> p h k i", h=8, k=16)
        bf_banks = [banks[i][:].bitcast(BF16) for i in range(8)]

        def layer_norm(src, srck, gk, bk_, dst, dstk):
            S.op("dve", lambda e: e.tensor_reduce(out=st1[:, 0:1], in_=src[:], axis=AX.X, op=ALU.add), r=[srck], w=["st_sum"])
            S.op("act", lambda e: e.activation(out=tmp1[:], in_=src[:], func=AF.Square), r=[srck], w=["tmp1"])
            S.op("dve", lambda e: e.tensor_reduce(out=st1[:, 1:2], in_=tmp1[:], axis=AX.X, op=ALU.add), r=["tmp1"], w=["st_sq"])
            S.op("dve", lambda e: e.tensor_scalar_mul(out=st1[:, 2:3], in0=st1[:, 0:1], scalar1=1.0 / D), r=["st_sum"], w=["st_mean"])
            S.op("dve", lambda e: e.tensor_tensor(out=st1[:, 3:4], in0=st1[:, 2:3], in1=st1[:, 2:3], op=ALU.mult),
                 r=["st_mean"], w=["st_msq"])
            S.op("dve", lambda e: e.scalar_tensor_tensor(out=st1[:, 4:5], in0=st1[:, 1:2], scalar=1.0 / D, in1=st1[:, 3:4],
                                                         op0=ALU.mult, op1=ALU.subtract), r=["st_sq", "st_msq"], w=["st_var"])
            S.op("dve", lambda e: e.tensor_scalar_add(out=st1[:, 4:5], in0=st1[:, 4:5], scalar1=LN_EPS), r=["st_var"], w=["st_var"])
            S.op("act", lambda e: e.activation(out=st1[:, 5:6], in_=st1[:, 4:5], func=AF.Sqrt), r=["st_var"], w=["st_std"])
            S.op("dve", lambda e: e.reciprocal(out=st1[:, 6:7], in_=st1[:, 5:6]), r=["st_std"], w=["st_rstd"])
            S.op("dve", lambda e: e.tensor_scalar(out=src[:], in0=src[:], scalar1=st1[:, 2:3], scalar2=st1[:, 6:7],
                                                  op0=ALU.subtract, op1=ALU.mult), r=[srck, "st_mean", "st_rstd"], w=[srck])
            S.op("dve", lambda e: e.tensor_tensor(out=src[:], in0=src[:], in1=lnt[gk][:], op=ALU.mult), r=[srck, gk + "_t"], w=[srck])
            S.op("dve", lambda e: e.tensor_tensor(out=dst[:], in0=src[:], in1=lnt[bk_][:], op=ALU.add), r=[srck, bk_ + "_t"], w=[dstk])

        def tr_n(e, src, dstb, n):
            for j in range(n):
                ins = e.transpose(out=dstb[:, j * 128:(j + 1) * 128], in_=src[:, j * 128:(j + 1) * 128], identity=ident_b[:])
            return ins

        gslot = {"u": 0, "v": 0}
        x1_2 = [x1, psb("x1b", [128, D], F32)]
        e_i2 = [e_i, psb("e_ib", [128, 128], I32)]
        h2b_2 = [h2b, psb("h2bb", [128, D], BF16)]
        gsm_2 = [gsm, psb("gsmb", [128, 8, 16], F32)]
        tlist = list(range(NT) if tiles is None else tiles)

        def ctx(n):
            t = tlist[n]
            return dict(t=t, i=0 if t < NPT else 1, rows=slice(t * 128, (t + 1) * 128),
                        xsrc=xp[t * 128:(t + 1) * 128, :] if t < NPT else xs,
                        ydst=yp[t * 128:(t + 1) * 128, :] if t < NPT else ys,
                        x1c=x1_2[n % 2], x1k=f"x1_{n % 2}", e_ic=e_i2[n % 2], e_ik=f"e_i_{n % 2}",
                        h2bc=h2b_2[n % 2], h2bk=f"h2b_{n % 2}", gsmc=gsm_2[n % 2], gsmk=f"gsm_{n % 2}")

        def stage1(t, i, rows, xsrc, ydst, x1c, x1k, e_ic, e_ik, h2bc, h2bk, gsmc, gsmk):
                S.dma("sp", lambda e: e.dma_start(out=x_t[:], in_=xsrc), w=["x_t"])
                S.dma("sp", lambda e: e.dma_start(out=att_t[:], in_=ATT[rows, :]), w=["att_t"])
                S.dma("sp", lambda e: e.dma_start(out=rg_t[:], in_=RG[rows, :]), r=[("RG", t)], w=["rg_t"])
                S.dma("sp", lambda e: e.dma_start(out=G_t, in_=P[rows, 6656:8704]), w=["bufA"])
                a3 = att_t[:].rearrange("p (h c) -> p h c", h=8)
                S.op("dve", lambda e: e.reciprocal(out=rl[:], in_=a3[:, :, 64]), r=["att_t"], w=["rl"])
                S.op("dve", lambda e: e.tensor_tensor(out=attn_t[:].rearrange("p (h d) -> p h d", h=8), in0=a3[:, :, 0:64],
                                                      in1=bcast_last(rl[:, :], 64), op=ALU.mult), r=["att_t", "rl"], w=["attn_t"])
                S.op("pe", lambda e: tr_n(e, attn_t, bf_banks[0], 4), r=["attn_t", "ident_b"], w=[bk(0)])
                S.op("act", lambda e: e.activation(out=aT[:].rearrange("p k t -> p (k t)"), in_=bf_banks[0][:, 0:512], func=AF.Copy),
                     r=[], w=[bk(0), "aT"])
                S.op("pe", lambda e: tr_n(e, rg_t, bf_banks[1], 4), r=["rg_t", "ident_b"], w=[bk(1)])
                S.op("dve", lambda e: e.tensor_copy(out=rT[:].rearrange("p k t -> p (k t)"), in_=bf_banks[1][:, 0:512]),
                     r=[], w=[bk(1), "rT"])
                S.op("act", lambda e: e.activation(out=sg_t, in_=G_t, func=AF.Sigmoid), r=["bufA"], w=["bufA"])

                def proj4(e, srcT, w_b, b0):
                    for half in range(2):
                        for kc in range(4):
                            ins = e.matmul(banks[b0 + half][:], lhsT=srcT[:, kc, :], rhs=w_b[:, kc, half * 512:(half + 1) * 512],
                                           start=(kc == 0), stop=(kc == 3))
                    return ins
                S.op("pe", lambda e: proj4(e, rT, wro_b, 2), r=["rT", "wro_b"], w=[bk(2), bk(3)])
                S.op("pe", lambda e: proj4(e, aT, wao_b, 4), r=["aT", "wao_b"], w=[bk(4), bk(5)])
                for half in range(2):
                    hs_ = slice(half * 512, (half + 1) * 512)
                    S.op("dve", lambda e, half=half, hs_=hs_: e.tensor_tensor(out=tmp1[:, hs_], in0=banks[2 + half][:],
                                                                              in1=sg_t[:, hs_], op=ALU.mult),
                         r=["bufA"], w=[bk(2 + half), ("tmp1h", half)])
                    S.op("dve", lambda e, half=half, hs_=hs_: e.tensor_tensor(
                        out=h2[:, hs_], in0=banks[4 + half][:], in1=sg_t[:, 1024 + half * 512:1024 + (half + 1) * 512],
                        op=ALU.mult), r=["bufA"], w=[bk(4 + half), ("h2h", half)])
                S.op("dve", lambda e: e.tensor_tensor(out=mixin, in0=tmp1[:], in1=h2[:], op=ALU.add),
                     r=[("tmp1h", 0), ("tmp1h", 1), ("h2h", 0), ("h2h", 1)], w=["bufB", "tmp1", "h2"])
                S.op("pe", lambda e: tr_n(e, mixin, bf_banks[0], 8), r=["bufB", "ident_b"], w=[bk(0)])
                S.op("act", lambda e: e.activation(out=mixT.rearrange("p k t -> p (k t)"), in_=bf_banks[0][:, 0:1024], func=AF.Copy),
                     r=[], w=[bk(0), "bufB"])

                def proj8(e, srcT, w_b, b0, nb_):
                    for nb in range(nb_):
                        for kc in range(8):
                            ins = e.matmul(banks[b0 + nb][:], lhsT=srcT[:, kc, :], rhs=w_b[:, kc, nb * 512:(nb + 1) * 512],
                                           start=(kc == 0), stop=(kc == 7))
                    return ins
                S.op("pe", lambda e: proj8(e, mixT, wout_b, 2, 2), r=["bufB", ("wout_b", 0), ("wout_b", 1)], w=[bk(2), bk(3)])
                for half in range(2):
                    hs_ = slice(half * 512, (half + 1) * 512)
                    S.op("dve", lambda e, half=half, hs_=hs_: e.tensor_tensor(out=h2[:, hs_], in0=banks[2 + half][:],
                                                                              in1=bout_t[:, hs_], op=ALU.add),
                         r=["bout_t", "h2"], w=[bk(2 + half), ("h2h", half)])
                S.op("dve", lambda e: e.tensor_tensor(out=h2[:], in0=h2[:], in1=modD[:, i, 0, :], op=ALU.mult),
                     r=[("h2h", 0), ("h2h", 1), ("modD", i, 0)], w=["h2"])
                S.op("dve", lambda e: e.scalar_tensor_tensor(out=x_t[:], in0=x_t[:], scalar=ALPHA, in1=h2[:],
                                                             op0=ALU.mult, op1=ALU.add), r=["x_t", "h2"], w=["x_t"])
                layer_norm(x_t, "x_t", "ln1_g", "ln1_b", x1c, x1k)
                S.op("dve", lambda e: e.tensor_tensor(out=h2[:], in0=x1c[:], in1=modD[:, i, 2, :], op=ALU.mult),
                     r=[x1k, ("modD", i, 2)], w=["h2"])
                S.op("dve", lambda e: e.tensor_tensor(out=h2bc[:], in0=h2[:], in1=modD[:, i, 1, :], op=ALU.add),
                     r=["h2", ("modD", i, 1)], w=[h2bk])
                S.op("pe", lambda e: tr_n(e, h2bc, bf_banks[1], 8), r=[h2bk, "ident_b"], w=[bk(1)])
                S.op("act", lambda e: e.activation(out=h2T[:].rearrange("p k t -> p (k t)"), in_=bf_banks[1][:, 0:1024], func=AF.Copy),
                     r=[], w=[bk(1), "h2T"])
                for nb in range(4):
                    wq_ = wqs[nb % 2]; wqk_ = f"wqs{nb % 2}"
                    S.dma("sp", lambda e, wq_=wq_, nb=nb: e.dma_start(out=wq_[:], in_=WQv[:, :, nb * 512:(nb + 1) * 512]),
                          r=wqbkeys, w=[wqk_])
                    def pq(e, wq_=wq_, nb=nb):
                        for kc in range(8):
                            ins = e.matmul(banks[2 + nb][:], lhsT=h2T[:, kc, :], rhs=wq_[:, kc, :], start=(kc == 0), stop=(kc == 7))
                        return ins
                    S.op("pe", pq, r=["h2T", wqk_], w=[bk(2 + nb)])
                for nb in range(4):
                    if nb % 2 == 0:
                        S.op("act", lambda e, nb=nb: e.activation(out=qpe[:, nb * 512:(nb + 1) * 512], in_=banks[2 + nb][:], func=AF.Copy),
                             r=["bufB"], w=[bk(2 + nb), "bufB"])
                    else:
                        S.op("dve", lambda e, nb=nb: e.tensor_copy(out=qpe[:, nb * 512:(nb + 1) * 512], in_=banks[2 + nb][:]),
                             r=["bufB"], w=[bk(2 + nb), "bufB"])
                for half in range(2):
                    def trq(e, half=half):
                        for j in range(8):
                            hs = half * 8 + j
                            ins = e.transpose(out=bf_banks[half][:, j * 128:(j + 1) * 128], in_=qpe[:, hs * 128:(hs + 1) * 128],
                                              identity=ident_b[:])
                        return ins
                    S.op("pe", trq, r=["bufB", "ident_b"], w=[bk(half)])
                    if half == 0:
                        S.op("act", lambda e: e.activation(out=qpT[:, 0:8, :].rearrange("p k t -> p (k t)"), in_=bf_banks[0][:, 0:1024],
                                                           func=AF.Copy), r=["bufB"], w=[bk(0), "bufB"])
                    else:
                        S.op("dve", lambda e: e.tensor_copy(out=qpT[:, 8:16, :].rearrange("p k t -> p (k t)"), in_=bf_banks[1][:, 0:1024]),
                             r=["bufB"], w=[bk(1), "bufB"])
                for q4 in range(4):
                    def scq(e, q4=q4):
                        for a in range(4):
                            hs = q4 * 4 + a
                            ins = e.matmul(banks[2 + q4][:, a * 128:(a + 1) * 128], lhsT=qpT[:, hs, :], rhs=keysT[:, hs, :],
                                           start=True, stop=True)
                        return ins
                    S.op("pe", scq, r=["bufB", ("keysT", q4)], w=[bk(2 + q4)])
                    if q4 % 2 == 0:
                        S.op("act", lambda e, q4=q4: e.activation(out=bufA[:, q4 * 512:(q4 + 1) * 512], in_=banks[2 + q4][:], func=AF.Copy),
                             r=["bufA"], w=[bk(2 + q4), "bufA"])
                    else:
                        S.op("dve", lambda e, q4=q4: e.tensor_copy(out=bufA[:, q4 * 512:(q4 + 1) * 512], in_=banks[2 + q4][:]),
                             r=["bufA"], w=[bk(2 + q4), "bufA"])
                ssk = ["bufA"]

                def topk_rounds(n, vals, vk, outv, outvk, outi, outik):
                    for rnd in range(2):
                        sl = slice(rnd * 8, rnd * 8 + 8)
                        def mx(e, sl=sl):
                            for j in range(n):
                                ins = e.max(out=outv[:, j, sl], in_=vals[:, j, :])
                            return ins
                        S.op("dve", mx, r=vk, w=[(outvk, rnd)])
                        def mi(e, sl=sl):
                            for j in range(n):
                                ins = e.max_index(out=outi[:, j, sl], in_max=outv[:, j, sl], in_values=vals[:, j, :])
                            return ins
                        S.op("dve", mi, r=vk + [(outvk, rnd)], w=[(outik, rnd)])
                        if rnd == 0:
                            def mr(e, sl=sl):
                                for j in range(n):
                                    ins = e.match_replace(out=vals[:, j, :], in_to_replace=outv[:, j, sl], in_values=vals[:, j, :],
                                                          imm_value=-1e30)
                                return ins
                            S.op("dve", mr, r=[(outvk, rnd), (outik, rnd)], w=vk)
                topk_rounds(16, s_sc, ssk, sv, "sv", si, "si")
                sv4 = sv[:].rearrange("p (h s) k -> p h s k", s=2)
                S.op("dve", lambda e: e.tensor_tensor(
                    out=cand4, in0=sv4[:, :, 0, :].unsqueeze(3).to_broadcast([128, 8, 16, 16]),
                    in1=sv4[:, :, 1, :].unsqueeze(2).to_broadcast([128, 8, 16, 16]), op=ALU.add),
                    r=[("sv", 0), ("sv", 1), "bufB"], w=["bufB"])
                topk_rounds(8, cand, ["bufB"], cv, "cv", ci, "ci")
                cvk = [("cv", 0), ("cv", 1)]; cik = [("ci", 0), ("ci", 1)]
                S.op("dve", lambda e: e.tensor_tensor(out=gsmc[:], in0=cv[:], in1=bcast_last(cv[:, :, 0], 16), op=ALU.subtract),
                     r=cvk, w=[gsmk])
                S.op("act", lambda e: e.activation(out=gsmc[:], in_=gsmc[:], func=AF.Exp), r=[gsmk], w=[gsmk])
                S.op("dve", lambda e: e.tensor_reduce(out=ssm[:], in_=gsmc[:], axis=AX.X, op=ALU.add), r=[gsmk], w=["ssm"])
                S.op("dve", lambda e: e.reciprocal(out=ssm[:], in_=ssm[:]), r=["ssm"], w=["ssm"])
                S.op("dve", lambda e: e.tensor_tensor(out=gsmc[:], in0=gsmc[:], in1=bcast_last(ssm[:, :], 16), op=ALU.mult),
                     r=[gsmk, "ssm"], w=[gsmk])
                S.op("dve", lambda e: e.tensor_single_scalar(out=hi_u[:], in_=ci[:], scalar=4, op=ALU.logical_shift_right),
                     r=cik, w=["hi_u"])
                S.op("dve", lambda e: e.tensor_single_scalar(out=lo_u[:], in_=ci[:], scalar=15, op=ALU.bitwise_and),
                     r=cik, w=["lo_u"])
                S.op("dve", lambda e: e.tensor_copy(out=hi_f[:], in_=hi_u[:]), r=["hi_u"], w=["hi_f"])
                S.op("dve", lambda e: e.tensor_copy(out=lo_f[:], in_=lo_u[:]), r=["lo_u"], w=["lo_f"])
                S.op("dve", lambda e: e.tensor_copy(out=sif[:], in_=si[:]), r=[("si", 0), ("si", 1)], w=["sif"])
                sif4 = sif[:].rearrange("p (h s) k -> p h s k", s=2)
                iot4 = iota16[:, :].unsqueeze(1).unsqueeze(1).to_broadcast([128, 8, 16, 16])
                for side, (xf, xk, dsti, dstk) in enumerate(((hi_f, "hi_f", i0, "i0"), (lo_f, "lo_f", i1, "i1"))):
                    S.op("dve", lambda e, xf=xf: e.tensor_tensor(
                        out=oh4, in0=iot4, in1=xf[:].unsqueeze(3).to_broadcast([128, 8, 16, 16]), op=ALU.is_equal),
                        r=[xk, "iota16"] + ssk, w=["bufA"])
                    S.op("dve", lambda e, side=side: e.tensor_tensor(
                        out=prod4, in0=oh4, in1=sif4[:, :, side, :].unsqueeze(2).to_broadcast([128, 8, 16, 16]), op=ALU.mult),
                        r=["bufA", "sif", "bufB"], w=["bufB"])
                    S.op("dve", lambda e, dsti=dsti: e.tensor_reduce(out=dsti[:], in_=prod4, axis=AX.X, op=ALU.add),
                         r=["bufB"], w=[dstk])
                S.op("dve", lambda e: e.scalar_tensor_tensor(out=e_f[:].rearrange("p (h k) -> p h k", h=8), in0=i0[:], scalar=128.0,
                                                             in1=i1[:], op0=ALU.mult, op1=ALU.add), r=["i0", "i1"], w=["e_f"])
                S.op("dve", lambda e: e.tensor_copy(out=e_ic[:], in_=e_f[:]), r=["e_f"], w=[e_ik])

        def stage2(t, i, rows, xsrc, ydst, x1c, x1k, e_ic, e_ik, h2bc, h2bk, gsmc, gsmk):
                for hk in range(128):
                    sl_ = gslot["u"] % NSL; gslot["u"] += 1
                    S.dma("pool", lambda e, sl_=sl_, hk=hk: e.indirect_dma_start(
                        out=Ug[sl_][:], out_offset=None, in_=UB,
                        in_offset=bass.IndirectOffsetOnAxis(ap=e_ic[:, hk:hk + 1], axis=0)), r=[e_ik] + ubkeys, w=[f"Ug{sl_}"])
                    S.op("dve", lambda e, sl_=sl_, hk=hk: e.scalar_tensor_tensor(
                        out=junk[:], in0=Ug[sl_][:], scalar=1.0, in1=h2bc[:], op0=ALU.mult, op1=ALU.mult,
                        accum_out=a_t[:, hk:hk + 1]), r=[f"Ug{sl_}", h2bk], w=["junk", ("a_t", hk)])
                S.op("act", lambda e: e.activation(out=a_t[:], in_=a_t[:], func=AF.Gelu), r=[("a_t", hk) for hk in range(128)], w=["a_g"])
                S.op("dve", lambda e: e.tensor_tensor(out=ga[:], in0=a_t[:], in1=gsmc[:].rearrange("p h k -> p (h k)"), op=ALU.mult),
                     r=["a_g", gsmk], w=["ga"])

        def stage3a(t, i, rows, xsrc, ydst, x1c, x1k, e_ic, e_ik, h2bc, h2bk, gsmc, gsmk):
                for h in range(8):
                    dg = Dg[h % 2]; dgk = f"Dg{h % 2}"
                    S.op("dve", lambda e, dg=dg, h=h: e.tensor_tensor(
                        out=dg[:], in0=ident_b[:, :].unsqueeze(1).to_broadcast([128, 16, 128]),
                        in1=ga[:, h * 16:(h + 1) * 16].unsqueeze(2).to_broadcast([128, 16, 128]), op=ALU.mult),
                        r=["ga", "ident_b"], w=[dgk])
                    for k in range(16):
                        hk = h * 16 + k
                        sl_ = gslot["v"] % NSL; gslot["v"] += 1
                        S.dma("pool", lambda e, sl_=sl_, hk=hk: e.indirect_dma_start(
                            out=Vg[sl_][:], out_offset=None, in_=VB,
                            in_offset=bass.IndirectOffsetOnAxis(ap=e_ic[:, hk:hk + 1], axis=0)), r=[e_ik] + vbkeys, w=[f"Vg{sl_}"])
                        def vmm(e, sl_=sl_, hk=hk, dg=dg, k=k):
                            for half in range(2):
                                ins = e.matmul(banks[6 + half][:], lhsT=dg[:, k, :], rhs=Vg[sl_][:, half * 512:(half + 1) * 512],
                                               start=(hk == 0), stop=(hk == 127))
                            return ins
                        S.op("pe", vmm, r=[f"Vg{sl_}", dgk], w=[bk(6), bk(7)])
                        S.flush(2)

        def stage3b(t, i, rows, xsrc, ydst, x1c, x1k, e_ic, e_ik, h2bc, h2bk, gsmc, gsmk):
                for half in range(2):
                    hs_ = slice(half * 512, (half + 1) * 512)
                    S.op("dve", lambda e, half=half, hs_=hs_: e.tensor_tensor(out=h2[:, hs_], in0=banks[6 + half][:],
                                                                              in1=modD[:, i, 3, hs_], op=ALU.mult),
                         r=[("modD", i, 3), "h2"], w=[bk(6 + half), ("h2h", half)])
                S.op("dve", lambda e: e.scalar_tensor_tensor(out=x1c[:], in0=x1c[:], scalar=ALPHA, in1=h2[:],
                                                             op0=ALU.mult, op1=ALU.add), r=[x1k, ("h2h", 0), ("h2h", 1)], w=[x1k, "h2"])
                layer_norm(x1c, x1k, "ln2_g", "ln2_b", x_t, "x_t")
                S.dma("sp", lambda e: e.dma_start(out=ydst, in_=x_t[:]), r=["x_t"], w=[("y", t)])


        stage1(**ctx(0))
        for n in range(len(tlist)):
            if n + 1 < len(tlist):
                S.defer_begin()
                stage1(**ctx(n + 1))
                S.defer_end()
            stage2(**ctx(n))
            stage3a(**ctx(n))
            S.flush()
            stage3b(**ctx(n))

    S.finish()
    return nc, es, S


def _shard_inputs(inp):
    c = _consts()
    maps = []
    f = np.ascontiguousarray
    for i in range(NCORES):
        sl = slice(i * NSEQ_S, (i + 1) * NSEQ_S)
        m = {
            "xp": f(inp["x_prompt"][i]),
            "xs": f(inp["x_sample"][sl].reshape(128, D)),
            "cp_rep": f(np.broadcast_to(inp["c_prompt"][i:i + 1], (128, D))),
            "cs_rep": f(np.repeat(inp["c_sample"][sl], TS, axis=0)),
            "st_in": f(inp["state_ret"][0, sl]),
            "c_in0": f(inp["cache_att_w128"][0, sl].reshape(NSEQ_S, 128, 2, 512)),
            "c_in1": f(inp["cache_att_w512"][0, sl].reshape(NSEQ_S, 512, 2, 512)),
            "c_in2": f(inp["cache_att_w2048"][0, sl].reshape(NSEQ_S, 2048, 2, 512)),
            "w_ada": f(inp["w_ada"][0]),
            "b_ada": f(inp["b_ada"][0:1]),
            "w_in": f(inp["w_in"][0]),
            "ident": c["ident"],
            "rotc": c["rotc"], "rots": c["rots"],
            "intraT_p": c["intraT_p"], "intraT_s": c["intraT_s"],
            "qdec_p": c["qdec_p"], "qdec_s": c["qdec_s"],
            "kdec_p": c["kdec_p"], "kdec_s": c["kdec_s"],
            "rowmask": c["rowmask"],
            "iota16": c["iota16"],
            "oh0": c["oh0"], "oh1": c["oh1"], "oh2": c["oh2"],
            "rel_bias": f(inp["rel_bias"]),
            "w_ret_o": f(inp["w_ret_o"][0]), "w_att_o": f(inp["w_att_o"][0]), "w_out": f(inp["w_out"][0]),
            "b_out_rep": f(np.broadcast_to(inp["b_out"][0:1], (128, D))),
            "ln1_g_rep": f(np.broadcast_to(inp["ln1_g"][0:1], (128, D))),
            "ln1_b_rep": f(np.broadcast_to(inp["ln1_b"][0:1], (128, D))),
            "ln2_g_rep": f(np.broadcast_to(inp["ln2_g"][0:1], (128, D))),
            "ln2_b_rep": f(np.broadcast_to(inp["ln2_b"][0:1], (128, D))),
            "peer_wq": f(inp["peer_wq"][0]), "peer_keys": f(inp["peer_keys"][0].reshape(2048, 128)),
            "peer_u": f(inp["peer_u"][0]), "peer_v": f(inp["peer_v"][0]),
        }
        maps.append(m)
    return maps


def kernel(**inputs):
    inp = {k: np.asarray(v) for k, v in inputs.items()}
    nc, es, S = build_program()
    with es:
        maps = _shard_inputs(inp)
        res = run_bass_kernel_spmd(nc, maps, core_ids=list(range(NCORES)))
    R = res.results
    yp = np.stack([R[i]["yp"] for i in range(NCORES)], 0)
    ys = np.concatenate([R[i]["ys"].reshape(NSEQ_S, TS, D) for i in range(NCORES)], 0)
    srp = np.stack([R[i]["srp"] for i in range(NCORES)], 0)[None]
    srs = np.concatenate([R[i]["srs"] for i in range(NCORES)], 0)[None]
    cpo = [np.stack([R[i][f"cpo{g}"] for i in range(NCORES)], 0).reshape(1, NCORES, GROUPS[g][0], 2, 8, 64)
           for g in range(3)]
    cso = [np.concatenate([R[i][f"cso{g}"] for i in range(NCORES)], 0).reshape(
        1, NCORES * NSEQ_S, GROUPS[g][0], 2, 8, 64) for g in range(3)]
    return (yp.astype(np.float32), ys.astype(np.float32), srp, cpo[0], cpo[1], cpo[2], srs, cso[0], cso[1], cso[2])
```
